# Optimizing a Trainium2 kernel written in Bass

```python
import math
import jax, jax.numpy as jnp
from jax import lax
import numpy as np

D_MODEL = 1024
BATCH = 4
SEQ = 4096
DEPTH = 4

CHUNK = 64
N_EVEN = (DEPTH + 1) // 2
N_ODD = DEPTH // 2
EPS = 1e-6
GDN_HEAD_DIM = 128
GDN_WIDTH = D_MODEL // 2
GDN_HEADS = GDN_WIDTH // GDN_HEAD_DIM
CONV_K = 4
S5_WIDTH = D_MODEL - GDN_WIDTH
S5_GROUP = 16
S5_GROUPS = S5_WIDTH // S5_GROUP
S5_STATE = 64
EVEN_IN = 4 * GDN_WIDTH + 2 * GDN_HEADS + S5_WIDTH
DIFF_HEAD_DIM = 64
DIFF_HEADS = D_MODEL // (2 * DIFF_HEAD_DIM)
DIFF_WIDTH = DIFF_HEADS * 2 * DIFF_HEAD_DIM
Q_BLOCK = 128
D_FF = ((8 * D_MODEL // 3 + 127) // 128) * 128
N_EXPERTS = 8
TOP_K = 2

kernel_name = 'hybrid_gdn_s5_diffattn_moe_adaln'


def rmsnorm(x, w):
    x32 = x.astype(jnp.float32)
    y = x32 * lax.rsqrt(jnp.mean(x32 * x32, axis=-1, keepdims=True) + EPS)
    return (y * w.astype(jnp.float32)).astype(x.dtype)


def l2norm(x):
    return x * lax.rsqrt(jnp.sum(x * x, axis=-1, keepdims=True) + EPS)


def causal_conv(x, w):
    k = w.shape[0]
    return lax.conv_general_dilated(x, w[:, None, :].astype(x.dtype), window_strides=(1,), padding=[(k - 1, 0)], dimension_numbers=('NWC', 'WIO', 'NWC'), feature_group_count=w.shape[1])


def to_chunks(t):
    bn, seq, nh, d = t.shape
    return t.reshape(bn, seq // CHUNK, CHUNK, nh, d).transpose(1, 0, 3, 2, 4)


def gated_delta_rule(q, k, v, g, beta):
    bn, seq, nh, dk = q.shape
    dv = v.shape[-1]
    q, k, v = to_chunks(q), to_chunks(k), to_chunks(v)
    g = jnp.cumsum(to_chunks(g[..., None])[..., 0], axis=-1)
    beta = to_chunks(beta[..., None])[..., 0]
    idx = jnp.arange(CHUNK)
    causal = idx[:, None] >= idx[None, :]
    strict = idx[:, None] > idx[None, :]
    decay = jnp.exp(jnp.where(causal, g[..., :, None] - g[..., None, :], -jnp.inf))
    k_beta = k * beta[..., None]
    m = jnp.where(strict, jnp.einsum('nbhid,nbhjd->nbhij', k_beta, k) * decay, 0.0)
    rhs = jnp.concatenate([v * beta[..., None], k_beta * jnp.exp(g)[..., None]], axis=-1)
    sol = lax.linalg.triangular_solve(jnp.eye(CHUNK, dtype=m.dtype) + m, rhs, left_side=True, lower=True)
    u, w = sol[..., :dv], sol[..., dv:]
    qk = jnp.where(causal, jnp.einsum('nbhid,nbhjd->nbhij', q, k) * decay, 0.0)

    def step(state, inp):
        q_c, k_c, u_c, w_c, g_c, qk_c = inp
        v_new = u_c - jnp.einsum('bhcd,bhde->bhce', w_c, state)
        o_c = jnp.einsum('bhcd,bhde->bhce', q_c * jnp.exp(g_c)[..., None], state) + jnp.einsum('bhij,bhje->bhie', qk_c, v_new)
        g_last = g_c[..., -1:]
        state = state * jnp.exp(g_last)[..., None] + jnp.einsum('bhcd,bhce->bhde', k_c * jnp.exp(g_last - g_c)[..., None], v_new)
        return state, o_c

    state0 = jnp.zeros((bn, nh, dk, dv), jnp.float32)
    _, o = lax.scan(step, state0, (q, k, u, w, g, qk))
    return o.transpose(1, 0, 3, 2, 4).reshape(bn, seq, nh, dv)


def s5_glu(u, lam_re, lam_im, log_step, b_re, b_im, c_re, c_im, d_skip, glu_w, glu_b):
    bn, seq, _ = u.shape
    f32 = jnp.float32
    u32 = u.astype(f32).reshape(bn, seq, S5_GROUPS, S5_GROUP)
    lr = jnp.minimum(lam_re.astype(f32), -1e-4)
    li = lam_im.astype(f32)
    dt = jnp.exp(log_step.astype(f32))[:, None]
    mag = jnp.exp(lr * dt)
    ar, ai = mag * jnp.cos(li * dt), mag * jnp.sin(li * dt)
    nr, ni = ar - 1.0, ai
    den = lr * lr + li * li
    cr, ci = (nr * lr + ni * li) / den, (ni * lr - nr * li) / den
    b_re, b_im = b_re.astype(f32), b_im.astype(f32)
    bbr = cr[..., None] * b_re - ci[..., None] * b_im
    bbi = cr[..., None] * b_im + ci[..., None] * b_re
    bu_r = jnp.einsum('gnc,blgc->blgn', bbr, u32)
    bu_i = jnp.einsum('gnc,blgc->blgn', bbi, u32)
    a_r = jnp.broadcast_to(ar, bu_r.shape)
    a_i = jnp.broadcast_to(ai, bu_r.shape)

    def combine(e1, e2):
        a1r, a1i, b1r, b1i = e1
        a2r, a2i, b2r, b2i = e2
        return (a2r * a1r - a2i * a1i, a2r * a1i + a2i * a1r, a2r * b1r - a2i * b1i + b2r, a2r * b1i + a2i * b1r + b2i)

    _, _, xr, xi = lax.associative_scan(combine, (a_r, a_i, bu_r, bu_i), axis=1)
    y = (jnp.einsum('gcn,blgn->blgc', c_re.astype(f32), xr) - jnp.einsum('gcn,blgn->blgc', c_im.astype(f32), xi)
         + d_skip.astype(f32).reshape(S5_GROUPS, S5_GROUP) * u32)
    y = jax.nn.gelu(y.reshape(bn, seq, S5_WIDTH), approximate=False)
    ga, gb = jnp.split(y @ glu_w.astype(f32) + glu_b.astype(f32), 2, axis=-1)
    return (ga * jax.nn.sigmoid(gb)).astype(u.dtype)


def hybrid_mixer(h, w_in, conv_w, a_log, dt_bias, gdn_norm_w, lam_re, lam_im, log_step, b_re, b_im, c_re, c_im, d_skip, glu_w, glu_b, w_out):
    bn, seq, _ = h.shape
    f32 = jnp.float32
    proj = h @ w_in
    qkv, z, b, a, u = jnp.split(proj, [3 * GDN_WIDTH, 4 * GDN_WIDTH, 4 * GDN_WIDTH + GDN_HEADS, 4 * GDN_WIDTH + 2 * GDN_HEADS], axis=-1)
    qkv = jax.nn.silu(causal_conv(qkv, conv_w)).astype(f32)
    q, k, v = (t.reshape(bn, seq, GDN_HEADS, GDN_HEAD_DIM) for t in jnp.split(qkv, 3, axis=-1))
    beta = jax.nn.sigmoid(b.astype(f32))
    g = -jnp.exp(a_log.astype(f32)) * jax.nn.softplus(a.astype(f32) + dt_bias.astype(f32))
    o = gated_delta_rule(l2norm(q) * GDN_HEAD_DIM ** -0.5, l2norm(k), v, g, beta)
    o = rmsnorm(o, gdn_norm_w) * jax.nn.silu(z.astype(f32).reshape(bn, seq, GDN_HEADS, GDN_HEAD_DIM))
    y_a = o.reshape(bn, seq, GDN_WIDTH).astype(h.dtype)
    y_b = s5_glu(u, lam_re, lam_im, log_step, b_re, b_im, c_re, c_im, d_skip, glu_w, glu_b)
    return jnp.concatenate([y_a, y_b], axis=-1) @ w_out


def diff_attention(h, w_qkv, q_norm_w, k_norm_w, lq1, lk1, lq2, lk2, subln_w, w_out, lambda_init):
    bn, seq, _ = h.shape
    f32 = jnp.float32
    q, k, v = jnp.split(h @ w_qkv, 3, axis=-1)
    q = rmsnorm(q.reshape(bn, seq, DIFF_HEADS, 2, DIFF_HEAD_DIM), q_norm_w).astype(f32) * DIFF_HEAD_DIM ** -0.5
    k = rmsnorm(k.reshape(bn, seq, DIFF_HEADS, 2, DIFF_HEAD_DIM), k_norm_w).astype(f32)
    v = v.reshape(bn, seq, DIFF_HEADS, 2 * DIFF_HEAD_DIM).astype(f32)
    lam = (jnp.exp(jnp.sum(lq1.astype(f32) * lk1.astype(f32))) - jnp.exp(jnp.sum(lq2.astype(f32) * lk2.astype(f32))) + lambda_init)
    n_blk = seq // Q_BLOCK
    qb = q.reshape(bn, n_blk, Q_BLOCK, DIFF_HEADS, 2, DIFF_HEAD_DIM).transpose(1, 0, 2, 3, 4, 5)
    key_chunk = jnp.arange(seq) // CHUNK

    def block(args):
        q_blk, blk = args
        q_chunk = (blk * Q_BLOCK + jnp.arange(Q_BLOCK)) // CHUNK
        allowed = key_chunk[None, :] <= q_chunk[:, None]
        s = jnp.einsum('bqhtd,bkhtd->bhtqk', q_blk, k)
        p = jax.nn.softmax(jnp.where(allowed, s, -jnp.inf), axis=-1)
        p = p[:, :, 0] - lam * p[:, :, 1]
        return jnp.einsum('bhqk,bkhe->bqhe', p, v)

    o = lax.map(block, (qb, jnp.arange(n_blk)))
    o = o.transpose(1, 0, 2, 3, 4).reshape(bn, seq, DIFF_HEADS, 2 * DIFF_HEAD_DIM)
    o = rmsnorm(o, subln_w) * (1.0 - lambda_init)
    return o.reshape(bn, seq, DIFF_WIDTH).astype(h.dtype) @ w_out


def swiglu(h, w13, w2):
    a, b = jnp.split(h @ w13, 2, axis=-1)
    return (jax.nn.silu(a) * b) @ w2


def moe_swiglu(h, router_w, w13, w2):
    logits = (h @ router_w).astype(jnp.float32)
    top_v, top_i = lax.top_k(logits, TOP_K)
    gates = jax.nn.softmax(top_v, axis=-1)
    combine = jnp.sum(jax.nn.one_hot(top_i, N_EXPERTS, dtype=jnp.float32) * gates[..., None], axis=-2)
    out = jnp.zeros(h.shape, h.dtype)
    for e in range(N_EXPERTS):
        out = out + combine[..., e:e + 1].astype(h.dtype) * swiglu(h, w13[e], w2[e])
    return out


def setup_inputs(seed: int = 0) -> dict:
    key = jax.random.key(seed)
    keys = iter(jax.random.split(key, 64))
    f32 = jnp.float32
    D, F, NE, NO = D_MODEL, D_FF, N_EVEN, N_ODD

    def nrm(shape, scale):
        return jax.random.normal(next(keys), shape, f32) * scale

    def gain(shape):
        return 1.0 + nrm(shape, 0.01)

    def log_uniform(shape, lo, hi):
        return jax.random.uniform(next(keys), shape, f32, minval=math.log(lo), maxval=math.log(hi))

    dt = jnp.exp(log_uniform((NE, GDN_HEADS), 1e-3, 1e-1))
    return {
        'x': nrm((BATCH, SEQ, D), 1.0),
        'c': nrm((BATCH, D), 1.0),
        'ada_w': nrm((DEPTH, D, 6 * D), 0.5 * D ** -0.5),
        'ada_b': nrm((DEPTH, 6 * D), 0.01),
        'norm_mix_w': gain((DEPTH, D)),
        'norm_ffn_w': gain((DEPTH, D)),
        'even_w_in': nrm((NE, D, EVEN_IN), D ** -0.5),
        'even_conv_w': nrm((NE, CONV_K, 3 * GDN_WIDTH), CONV_K ** -0.5),
        'even_a_log': jnp.log(jax.random.uniform(next(keys), (NE, GDN_HEADS), f32, minval=1.0, maxval=16.0)),
        'even_dt_bias': dt + jnp.log(-jnp.expm1(-dt)),
        'even_gdn_norm_w': gain((NE, GDN_HEAD_DIM)),
        'even_lam_re': -0.5 + nrm((NE, S5_GROUPS, S5_STATE), 0.01),
        'even_lam_im': math.pi * jnp.arange(S5_STATE, dtype=f32) + nrm((NE, S5_GROUPS, S5_STATE), 0.01),
        'even_log_step': log_uniform((NE, S5_GROUPS), 1e-3, 1e-1),
        'even_b_re': nrm((NE, S5_GROUPS, S5_STATE, S5_GROUP), (2 * S5_GROUP) ** -0.5),
        'even_b_im': nrm((NE, S5_GROUPS, S5_STATE, S5_GROUP), (2 * S5_GROUP) ** -0.5),
        'even_c_re': nrm((NE, S5_GROUPS, S5_GROUP, S5_STATE), S5_STATE ** -0.5),
        'even_c_im': nrm((NE, S5_GROUPS, S5_GROUP, S5_STATE), S5_STATE ** -0.5),
        'even_d_skip': nrm((NE, S5_WIDTH), 1.0),
        'even_glu_w': nrm((NE, S5_WIDTH, 2 * S5_WIDTH), S5_WIDTH ** -0.5),
        'even_glu_b': nrm((NE, 2 * S5_WIDTH), 0.01),
        'even_w_out': nrm((NE, GDN_WIDTH + S5_WIDTH, D), (GDN_WIDTH + S5_WIDTH) ** -0.5),
        'even_ffn_w13': nrm((NE, D, 2 * F), D ** -0.5),
        'even_ffn_w2': nrm((NE, F, D), F ** -0.5),
        'odd_w_qkv': nrm((NO, D, 3 * DIFF_WIDTH), D ** -0.5),
        'odd_q_norm_w': gain((NO, DIFF_HEAD_DIM)),
        'odd_k_norm_w': gain((NO, DIFF_HEAD_DIM)),
        'odd_lambda_q1': nrm((NO, DIFF_HEAD_DIM), 0.1),
        'odd_lambda_k1': nrm((NO, DIFF_HEAD_DIM), 0.1),
        'odd_lambda_q2': nrm((NO, DIFF_HEAD_DIM), 0.1),
        'odd_lambda_k2': nrm((NO, DIFF_HEAD_DIM), 0.1),
        'odd_subln_w': gain((NO, 2 * DIFF_HEAD_DIM)),
        'odd_w_out': nrm((NO, DIFF_WIDTH, D), DIFF_WIDTH ** -0.5),
        'odd_router_w': nrm((NO, D, N_EXPERTS), D ** -0.5),
        'odd_expert_w13': nrm((NO, N_EXPERTS, D, 2 * F), D ** -0.5),
        'odd_expert_w2': nrm((NO, N_EXPERTS, F, D), F ** -0.5),
    }


def reference(x, c, ada_w, ada_b, norm_mix_w, norm_ffn_w,
              even_w_in, even_conv_w, even_a_log, even_dt_bias, even_gdn_norm_w,
              even_lam_re, even_lam_im, even_log_step, even_b_re, even_b_im, even_c_re, even_c_im,
              even_d_skip, even_glu_w, even_glu_b, even_w_out, even_ffn_w13, even_ffn_w2,
              odd_w_qkv, odd_q_norm_w, odd_k_norm_w, odd_lambda_q1, odd_lambda_k1, odd_lambda_q2, odd_lambda_k2,
              odd_subln_w, odd_w_out, odd_router_w, odd_expert_w13, odd_expert_w2):
    cond = jax.nn.silu(c)
    for layer in range(DEPTH):
        i = layer // 2
        mod = (cond @ ada_w[layer] + ada_b[layer])[:, None, :].astype(x.dtype)
        sh1, sc1, g1, sh2, sc2, g2 = jnp.split(mod, 6, axis=-1)
        h = rmsnorm(x, norm_mix_w[layer]) * (1.0 + sc1) + sh1
        if layer % 2 == 0:
            y = hybrid_mixer(h, even_w_in[i], even_conv_w[i], even_a_log[i], even_dt_bias[i], even_gdn_norm_w[i],
                             even_lam_re[i], even_lam_im[i], even_log_step[i], even_b_re[i], even_b_im[i],
                             even_c_re[i], even_c_im[i], even_d_skip[i], even_glu_w[i], even_glu_b[i], even_w_out[i])
        else:
            lambda_init = 0.8 - 0.6 * math.exp(-0.3 * layer)
            y = diff_attention(h, odd_w_qkv[i], odd_q_norm_w[i], odd_k_norm_w[i], odd_lambda_q1[i], odd_lambda_k1[i],
                               odd_lambda_q2[i], odd_lambda_k2[i], odd_subln_w[i], odd_w_out[i], lambda_init)
        x = x + g1 * y
        h = rmsnorm(x, norm_ffn_w[layer]) * (1.0 + sc2) + sh2
        if layer % 2 == 0:
            f = swiglu(h, even_ffn_w13[i], even_ffn_w2[i])
        else:
            f = moe_swiglu(h, odd_router_w[i], odd_expert_w13[i], odd_expert_w2[i])
        x = x + g2 * f
    return x
```

```python
import contextlib
import math
import numpy as np
import concourse.bass as bass
import concourse.mybir as mybir
from concourse.bass_utils import run_bass_kernel_spmd

F32 = mybir.dt.float32
BF16 = mybir.dt.bfloat16
AF = mybir.ActivationFunctionType
ALU = mybir.AluOpType
AX = mybir.AxisListType

D = 1024
DEPTH = 4
EPS = 1e-6
DFF = 2816
NEXP = 8
ENGS = ['pe', 'act', 'dve', 'pool', 'sp']


class V:
    __slots__ = ('res', 'ap')

    def __init__(self, res, ap):
        self.res = res
        self.ap = ap

    def __getitem__(self, idx):
        return V(self.res, self.ap[idx])

    def bc(self, shape):
        return V(self.res, self.ap.to_broadcast(list(shape)))

    def re(self, s, **kw):
        return V(self.res, self.ap.rearrange(s, **kw))


class Res:
    __slots__ = ('name', 'w', 'r', 'ap')

    def __init__(self, name, ap=None):
        self.name = name
        self.w = {}
        self.r = {}
        self.ap = ap

    def __getitem__(self, idx):
        return V(self, self.ap[idx])

    @property
    def v(self):
        return V(self, self.ap)


def _rv(x):
    return x.v if isinstance(x, Res) else x


class Prog:
    def __init__(self, nc, stack, n_dma_sems=32):
        self.nc = nc
        self.stack = stack
        self.streams = {e: [] for e in ENGS}
        self.cnt = {e: 0 for e in ENGS}
        self.semh = {}
        for e in ['pe', 'act', 'dve', 'pool']:
            self.semh['c_' + e] = stack.enter_context(nc.semaphore('c_' + e))
        self.ndma = n_dma_sems
        self.dma_tot = [0] * n_dma_sems
        self.dma_next = 0
        for k in range(n_dma_sems):
            self.semh['d%d' % k] = stack.enter_context(nc.semaphore('d%d' % k))
        self.known = {e: {} for e in ENGS}
        self.nt = 0
        self.nins = 0

    def sb(self, shape, dtype=F32, name=None):
        self.nt += 1
        name = name or ('t%d' % self.nt)
        t = self.stack.enter_context(self.nc.sbuf_tensor(name, list(shape), dtype))
        return Res(name, t[:])

    def ps(self, shape, dtype=F32, name=None):
        self.nt += 1
        name = name or ('p%d' % self.nt)
        t = self.stack.enter_context(self.nc.psum_tensor(name, list(shape), dtype))
        return Res(name, t[:])

    def dram(self, name, shape, dtype, kind="Internal"):
        t = self.nc.dram_tensor(name, list(shape), dtype, kind=kind)
        return Res(name, t.ap())

    def _deps(self, eng, reads, writes):
        deps = {}

        def add(k, v):
            if deps.get(k, -1) < v:
                deps[k] = v
        for r in reads:
            for k, v in r.w.items():
                add(k, v)
        for w in writes:
            for k, v in w.w.items():
                add(k, v)
            for k, v in w.r.items():
                add(k, v)
        out = []
        kn = self.known[eng]
        for k, v in deps.items():
            if eng == 'pe' and k == 'c_pe':
                continue
            if kn.get(k, -1) < v:
                kn[k] = v
                out.append((k, v))
        return out

    def _record(self, ev, reads, writes, merge=False):
        k, v = ev
        for w in writes:
            if merge:
                w.w[k] = v
            else:
                w.w = {k: v}
            w.r = {}
        for r in reads:
            if r.r.get(k, -1) < v:
                r.r[k] = v

    def op(self, eng, fn, reads=(), writes=(), inc=True):
        reads = [x for x in reads if x is not None]
        waits = self._deps(eng, reads, writes)
        key = 'c_' + eng
        if inc:
            self.cnt[eng] += 1
            ev = (key, self.cnt[eng])
        else:
            ev = (key, self.cnt[eng] + 1)
        self.streams[eng].append((waits, fn, key if inc else None, 1))
        self._record(ev, reads, writes)
        self.nins += 1

    def dma(self, out, in_, eng='sp'):
        out = _rv(out)
        in_ = _rv(in_)
        k = self.dma_next
        self.dma_next = (k + 1) % self.ndma
        key = 'd%d' % k
        waits = self._deps(eng, [in_.res], [out.res])
        kn = self.known[eng]
        if kn.get(key, -1) < self.dma_tot[k]:
            kn[key] = self.dma_tot[k]
            waits.append((key, self.dma_tot[k]))
        self.dma_tot[k] += 16
        ev = (key, self.dma_tot[k])
        oa, ia = out.ap, in_.ap

        def fn(e):
            return e.dma_start(out=oa, in_=ia)
        self.streams[eng].append((waits, fn, key, 16))
        self._record(ev, [in_.res], [out.res], merge=True)
        self.nins += 1

    def barrier(self):
        allw = [('c_' + e, self.cnt[e]) for e in ['pe', 'act', 'dve', 'pool']]
        allw += [('d%d' % k, self.dma_tot[k]) for k in range(self.ndma)]
        for e in ENGS:
            kn = self.known[e]
            waits = []
            for k, v in allw:
                if e == 'pe' and k == 'c_pe':
                    continue
                if kn.get(k, -1) < v:
                    kn[k] = v
                    waits.append((k, v))
            self.streams[e].append((waits, None, None, 0))

    def flush(self):
        nc = self.nc
        engobj = {'pe': 'tensor', 'act': 'scalar', 'dve': 'vector', 'pool': 'gpsimd', 'sp': 'sync'}
        semh = self.semh
        with nc.allow_non_contiguous_dma(reason="small strided param loads"), nc.Block() as block:
            for e in ENGS:
                stream = self.streams[e]

                def body(eng, stream=stream):
                    for waits, fn, key, n in stream:
                        for k, v in waits:
                            eng.wait_ge(semh[k], v)
                        if fn is not None:
                            ins = fn(eng)
                            if key is not None:
                                ins.then_inc(semh[key], n)
                getattr(block, engobj[e])(body)
        self.streams = {e: [] for e in ENGS}

    def mm(self, out, lhsT, rhs, start=True, stop=True):
        out, lhsT, rhs = _rv(out), _rv(lhsT), _rv(rhs)
        o, l, r = out.ap, lhsT.ap, rhs.ap
        self.op('pe', lambda e: e.matmul(o, l, r, start=start, stop=stop),
                [lhsT.res, rhs.res], [out.res], inc=stop)

    def tr(self, out, in_, ident):
        out, in_, ident = _rv(out), _rv(in_), _rv(ident)
        o, i, d = out.ap, in_.ap, ident.ap
        self.op('pe', lambda e: e.transpose(o, i, d), [in_.res, ident.res], [out.res])

    def act(self, out, in_, func, bias=None, scale=None, eng='act'):
        out, in_ = _rv(out), _rv(in_)
        reads = [in_.res]
        kw = {}
        if bias is not None:
            if isinstance(bias, (V, Res)):
                bias = _rv(bias)
                reads.append(bias.res)
                kw['bias'] = bias.ap
            else:
                kw['bias'] = float(bias)
        if scale is not None:
            if isinstance(scale, (V, Res)):
                scale = _rv(scale)
                reads.append(scale.res)
                kw['scale'] = scale.ap
            else:
                kw['scale'] = float(scale)
        o, i = out.ap, in_.ap
        self.op(eng, lambda e: e.activation(o, i, func, **kw), reads, [out.res])

    def tt(self, out, a, b, op, eng='dve'):
        out, a, b = _rv(out), _rv(a), _rv(b)
        o, x, y = out.ap, a.ap, b.ap
        self.op(eng, lambda e: e.tensor_tensor(o, x, y, op), [a.res, b.res], [out.res])

    def ts(self, out, a, s1, op0, s2=None, op1=None, eng='dve'):
        out, a = _rv(out), _rv(a)
        reads = [a.res]

        def cv(s):
            if isinstance(s, (V, Res)):
                s = _rv(s)
                reads.append(s.res)
                return s.ap
            return None if s is None else float(s)
        c1, c2 = cv(s1), cv(s2)
        o, x = out.ap, a.ap
        if op1 is None:
            self.op(eng, lambda e: e.tensor_single_scalar(o, x, c1, op0), reads, [out.res])
        else:
            self.op(eng, lambda e: e.tensor_scalar(o, x, c1, c2, op0, op1), reads, [out.res])

    def stt(self, out, a, s, b, op0, op1):
        out, a, b = _rv(out), _rv(a), _rv(b)
        reads = [a.res, b.res]
        if isinstance(s, (V, Res)):
            s = _rv(s)
            reads.append(s.res)
            c = s.ap
        else:
            c = float(s)
        o, x, y = out.ap, a.ap, b.ap
        self.op('dve', lambda e: e.scalar_tensor_tensor(o, x, c, y, op0, op1), reads, [out.res])

    def copy(self, out, in_, eng='dve'):
        out, in_ = _rv(out), _rv(in_)
        o, i = out.ap, in_.ap
        if eng == 'act':
            self.op('act', lambda e: e.activation(o, i, AF.Copy), [in_.res], [out.res])
        else:
            self.op(eng, lambda e: e.tensor_copy(o, i), [in_.res], [out.res])

    def recip(self, out, in_):
        out, in_ = _rv(out), _rv(in_)
        o, i = out.ap, in_.ap
        self.op('dve', lambda e: e.reciprocal(o, i), [in_.res], [out.res])

    def memset(self, out, val, eng='dve'):
        out = _rv(out)
        o = out.ap
        self.op(eng, lambda e: e.memset(o, float(val)), [], [out.res])

    def scan(self, out, d0, d1, init, op0=ALU.mult, op1=ALU.add):
        out, d0, d1 = _rv(out), _rv(d0), _rv(d1)
        reads = [d0.res, d1.res]
        if isinstance(init, (V, Res)):
            init = _rv(init)
            reads.append(init.res)
            c = init.ap
        else:
            c = float(init)
        o, x, y = out.ap, d0.ap, d1.ap
        self.op('dve', lambda e: e.tensor_tensor_scan(o, x, y, c, op0, op1), reads, [out.res])

    def reduce(self, out, in_, op, axis=AX.X):
        out, in_ = _rv(out), _rv(in_)
        o, i = out.ap, in_.ap
        self.op('dve', lambda e: e.tensor_reduce(o, i, axis, op), [in_.res], [out.res])


class Ctx:
    pass


def rstd_from_ss(P, out_sb, ss_ps, scale, bias):
    P.act(out_sb, ss_ps, AF.Ln, bias=bias, scale=scale)
    P.act(out_sb, out_sb, AF.Exp, scale=-0.5)


def stage_prep(P, C, L):
    nc = P.nc
    I = C.inp
    with contextlib.ExitStack() as st:
        P.stack = st
        cT = P.sb([128, 8])
        with nc.allow_non_contiguous_dma(reason="tiny"):
            P.dma(cT, I['c'].v.re("(k p) -> p k", p=128))
        cond = P.sb([128, 8])
        P.act(cond, cT, AF.Silu)
        wbuf = [P.sb([128, 8, 768]) for _ in range(2)]
        pm = C.PS[0]
        for l in range(C.depth):
            bt = P.sb([128, 48])
            with nc.allow_non_contiguous_dma(reason="tiny"):
                P.dma(bt, I['ada_b'][l].re("(j p) -> p j", p=128))
            for cb in range(8):
                wb = wbuf[cb % 2]
                P.dma(wb, I['ada_w'][l][:, cb * 768:(cb + 1) * 768].re("(k p) m -> p k m", p=128))
                for j in range(6):
                    col = cb * 6 + j
                    for k in range(8):
                        P.mm(pm[:, col:col + 1], wb[:, k, j * 128:(j + 1) * 128], cond[:, k:k + 1],
                             start=(k == 0), stop=(k == 7))
            mod = C.mod[l]
            P.tt(mod, pm[:, 0:48], bt, ALU.add)
            nw = P.sb([128, 16])
            with nc.allow_non_contiguous_dma(reason="tiny"):
                P.dma(nw[:, 0:8], I['norm_mix_w'][l].re("(k p) -> p k", p=128))
                P.dma(nw[:, 8:16], I['norm_ffn_w'][l].re("(k p) -> p k", p=128))
            A = C.modA[l]
            P.stt(A[:, 0:8], mod[:, 8:16], 1.0, nw[:, 0:8], ALU.add, ALU.mult)
            P.stt(A[:, 8:16], mod[:, 32:40], 1.0, nw[:, 8:16], ALU.add, ALU.mult)
        xin = [P.sb([128, 1024]) for _ in range(2)]
        xo = [P.sb([128, 8, 512]) for _ in range(2)]
        for tt in range(L // 512):
            o = xo[tt % 2]
            for s in range(4):
                xi = xin[s % 2]
                t0 = tt * 512 + s * 128
                P.dma(xi, I['x'][t0:t0 + 128, :])
                for k in range(8):
                    pt = C.PS[1 + (k % 4)]
                    P.tr(pt[:, 0:128], xi[:, k * 128:(k + 1) * 128], C.ident)
                    P.copy(o[:, k, s * 128:(s + 1) * 128], pt[:, 0:128], eng=('act' if k % 2 else 'dve'))
            P.dma(C.xA.v.re("(k p) t -> p k t", p=128)[:, :, tt * 512:(tt + 1) * 512], o)
        P.barrier()
        P.flush()


def stage_out(P, C, L, xsrc):
    I = C.inp
    with contextlib.ExitStack() as st:
        P.stack = st
        xi = [P.sb([128, 8, 512]) for _ in range(2)]
        xo = [P.sb([128, 1024]) for _ in range(2)]
        n = 0
        for tt in range(L // 512):
            t = xi[tt % 2]
            P.dma(t, xsrc.v.re("(k p) t -> p k t", p=128)[:, :, tt * 512:(tt + 1) * 512])
            for s in range(4):
                o = xo[s % 2]
                for k in range(8):
                    pt = C.PS[1 + (k % 4)]
                    P.tr(pt[:, 0:128], t[:, k, s * 128:(s + 1) * 128], C.ident)
                    P.copy(o[:, k * 128:(k + 1) * 128], pt[:, 0:128], eng=('act' if k % 2 else 'dve'))
                t0 = tt * 512 + s * 128
                P.dma(C.out[t0:t0 + 128, :], o)
        P.barrier()
        P.flush()


def load_norm_h(P, C, S, xt, l, which, want32=False):
    A = C.modA[l]
    mod = C.mod[l]
    aoff = 0 if which == 1 else 8
    shoff = 0 if which == 1 else 24
    sq = S.sq
    P.act(sq, xt, AF.Square)
    ss = C.PS[0]
    for k in range(8):
        P.mm(ss, C.ones, sq[:, k, :], start=(k == 0), stop=(k == 7))
    rstd = S.rstd
    rstd_from_ss(P, rstd, ss, 1.0 / D, EPS)
    for k in range(8):
        tmp = S.tmp[k % 2]
        P.stt(tmp, xt[:, k, :], A[:, aoff + k:aoff + k + 1], rstd, ALU.mult, ALU.mult)
        if want32:
            P.act(S.h32[:, k, :], tmp, AF.Identity, bias=mod[:, shoff + k:shoff + k + 1])
            P.copy(S.hb[:, k, :], S.h32[:, k, :], eng='pool')
        else:
            P.act(S.hb[:, k, :], tmp, AF.Identity, bias=mod[:, shoff + k:shoff + k + 1])


def wslab_iter(P, C, S, Wv, ncols, KT, slab=512):
    c0 = 0
    i = 0
    while c0 < ncols:
        w = min(slab, ncols - c0)
        buf = S.wb[S.wbi % len(S.wb)]
        S.wbi += 1
        P.dma(buf[:, 0:KT, 0:w], Wv[:, c0:c0 + w].re("(k p) m -> p k m", p=128), eng='pool')
        yield buf, c0, w
        c0 += w
        i += 1


def stage_even_proj(P, C, L, l, xsrc):
    nc = P.nc
    I = C.inp
    i = l // 2
    Win = I['even_w_in'][i]
    with contextlib.ExitStack() as st:
        P.stack = st
        S = Ctx()
        S.sq = P.sb([128, 8, 512])
        S.rstd = P.sb([128, 512])
        S.tmp = [P.sb([128, 512]) for _ in range(2)]
        S.hb = P.sb([128, 8, 512], BF16)
        S.wb = [P.sb([128, 8, 512], BF16) for _ in range(3)]
        S.wbi = 0
        xts = [P.sb([128, 8, 512]) for _ in range(2)]
        pre = [P.sb([128, 515]) for _ in range(12)]
        for m in range(12):
            P.memset(pre[m][:, 0:3], 0.0)
        cw = P.sb([128, 12, 4])
        with nc.allow_non_contiguous_dma(reason="tiny"):
            for j in range(4):
                P.dma(cw[:, :, j], I['even_conv_w'][i][j].re("(t p) -> p t", p=128))
        wba = P.sb([128, 8, 8])
        with nc.allow_non_contiguous_dma(reason="small"):
            P.dma(wba, Win[:, 2048:2056].re("(k p) m -> p k m", p=128))
        hc = P.sb([4, 2])
        with nc.allow_non_contiguous_dma(reason="tiny"):
            P.dma(hc[:, 0:1], I['even_a_log'][i].re("(h o) -> h o", o=1))
            P.dma(hc[:, 1:2], I['even_dt_bias'][i].re("(h o) -> h o", o=1))
        nA = P.sb([4, 1])
        P.act(nA, hc[:, 0:1], AF.Exp)
        P.ts(nA, nA, -1.0, ALU.mult)
        h32 = P.sb([128, 8, 512])
        S.h32 = h32
        acc = [P.sb([128, 512]) for _ in range(2)]
        ob = [P.sb([128, 512]) for _ in range(3)]
        sm = [P.sb([4, 512]) for _ in range(6)]
        NT = L // 512
        xv = xsrc.v.re("(k p) t -> p k t", p=128)
        P.dma(xts[0], xv[:, :, 0:512])
        nob = 0
        for tt in range(NT):
            xt = xts[tt % 2]
            if tt + 1 < NT:
                P.dma(xts[(tt + 1) % 2], xv[:, :, (tt + 1) * 512:(tt + 2) * 512])
            load_norm_h(P, C, S, xt, l, 1, want32=True)
            tsl = slice(tt * 512, (tt + 1) * 512)
            pb, pa = C.PS[1], C.PS[2]
            for k in range(8):
                P.mm(pb[0:4, :], wba[:, k, 0:4], h32[:, k, :], start=(k == 0), stop=(k == 7))
            for k in range(8):
                P.mm(pa[0:4, :], wba[:, k, 4:8], h32[:, k, :], start=(k == 0), stop=(k == 7))
            beta = sm[0]
            P.act(beta[:], pb[0:4, :], AF.Exp, scale=-1.0)
            P.ts(beta, beta, 1.0, ALU.add)
            P.recip(beta, beta)
            P.dma(C.betaT[:, tsl], beta)
            xa = sm[1]
            P.act(xa[:], pa[0:4, :], AF.Identity, bias=hc[:, 1:2])
            ax = sm[2]
            P.stt(ax, xa, -1.0, xa, ALU.mult, ALU.max)
            P.act(ax, ax, AF.Exp, scale=-1.0)
            P.act(ax, ax, AF.Ln, bias=1.0)
            sp = sm[3]
            P.stt(sp, xa, 0.0, ax, ALU.max, ALU.add)
            g = sm[4]
            P.ts(g, sp, nA[:, 0:1], ALU.mult)
            gc = sm[5]
            P.scan(gc, C.cmask, g, 0.0)
            P.dma(C.gT[:, tsl], gc)
            mt = 0
            for wsl, c0, w in wslab_iter(P, C, S, Win, 2048, 8):
                for j in range(w // 128):
                    m = (c0 // 128) + j
                    pp = C.PS[3 + (m % 4)]
                    for k in range(8):
                        P.mm(pp, wsl[:, k, j * 128:(j + 1) * 128], S.hb[:, k, :], start=(k == 0), stop=(k == 7))
                    if m < 12:
                        pr = pre[m]
                        P.copy(pr[:, 3:515], pp, eng='act')
                        a = acc[m % 2]
                        P.ts(a, pr[:, 0:512], cw[:, m, 0:1], ALU.mult)
                        for jj in range(1, 4):
                            P.stt(a, pr[:, jj:jj + 512], cw[:, m, jj:jj + 1], a, ALU.mult, ALU.add)
                        P.copy(pr[:, 0:3], pr[:, 512:515], eng='pool')
                        o = ob[nob % 3]
                        nob += 1
                        P.act(o, a, AF.Silu)
                        if m < 8:
                            sq = S.tmp[m % 2]
                            P.act(sq, o, AF.Square)
                            ss = C.PS[7]
                            P.mm(ss, C.ones, sq)
                            rs = S.rstd
                            if m < 4:
                                rstd_from_ss(P, rs, ss, 128.0, 128.0 * EPS)
                            else:
                                rstd_from_ss(P, rs, ss, 1.0, EPS)
                            P.tt(o, o, rs, ALU.mult)
                        P.dma(C.qkvT[m * 128:(m + 1) * 128, tsl], o)
                    else:
                        o = ob[nob % 3]
                        nob += 1
                        P.act(o, pp, AF.Silu)
                        P.dma(C.zT[(m - 12) * 128:(m - 11) * 128, tsl], o)
            for wsl, c0, w in wslab_iter(P, C, S, Win[:, 2056:2568], 512, 8):
                for j in range(4):
                    pp = C.PS[3 + (j % 4)]
                    for k in range(8):
                        P.mm(pp, wsl[:, k, j * 128:(j + 1) * 128], S.hb[:, k, :], start=(k == 0), stop=(k == 7))
                    o = ob[nob % 3]
                    nob += 1
                    P.copy(o, pp, eng='act')
                    P.dma(C.uT[j * 128:(j + 1) * 128, tsl], o)
        P.barrier()
        P.flush()


def stage_gdn(P, C, L, l):
    nc = P.nc
    I = C.inp
    i = l // 2
    NT = L // 512
    with contextlib.ExitStack() as st:
        P.stack = st
        Sst = [P.sb([128, 128]) for _ in range(4)]
        for h in range(4):
            P.memset(Sst[h], 0.0)
        gw = P.sb([128, 1])
        with nc.allow_non_contiguous_dma(reason="tiny"):
            P.dma(gw, I['even_gdn_norm_w'][i].re("(p o) -> p o", o=1))
        mk = lambda n, shape, dt=F32: [P.sb(shape, dt) for _ in range(n)]
        qT, kT, vT = mk(2, [128, 512]), mk(2, [128, 512]), mk(2, [128, 512])
        rows = mk(2, [1, 2, 512])
        r_eg, r_ekl, r_ng = mk(2, [1, 512]), mk(2, [1, 512]), mk(2, [1, 512])
        bcs = mk(2, [128, 3, 512])
        kb, kbe, vb, qe, kel = (mk(2, [128, 512]) for _ in range(5))
        tokm = mk(2, [64, 3, 8, 128])
        EL, EU, t1 = mk(2, [64, 512]), mk(2, [64, 512]), mk(2, [64, 512])
        Am, ATm, PTm, Rm = mk(2, [64, 512]), mk(2, [64, 512]), mk(2, [64, 512]), mk(2, [64, 512])
        WTn = mk(2, [128, 512])
        vnew = mk(3, [64, 128])
        zt = mk(2, [128, 512])
        osb = mk(2, [128, 512])
        sq = mk(2, [128, 512])
        rs = mk(2, [128, 512])
        it = 0
        for tt in range(NT):
            tsl = slice(tt * 512, (tt + 1) * 512)
            for h in range(4):
                b = it % 2
                it += 1
                P.dma(qT[b], C.qkvT[h * 128:(h + 1) * 128, tsl])
                P.dma(kT[b], C.qkvT[512 + h * 128:512 + (h + 1) * 128, tsl])
                P.dma(vT[b], C.qkvT[1024 + h * 128:1024 + (h + 1) * 128, tsl])
                P.dma(rows[b][:, 0, :], C.betaT[h:h + 1, tsl])
                P.dma(rows[b][:, 1, :], C.gT[h:h + 1, tsl])
                P.dma(zt[b], C.zT[h * 128:(h + 1) * 128, tsl])
                gc = rows[b][:, 1, :]
                P.act(r_eg[b], gc, AF.Exp)
                g3 = rows[b][:, 1, :].re("o (c j) -> o c j", j=64)
                P.tt(r_ekl[b].v.re("o (c j) -> o c j", j=64), g3[:, :, 63:64].bc([1, 8, 64]), g3, ALU.subtract)
                P.act(r_ekl[b], r_ekl[b], AF.Exp)
                P.ts(r_ng[b], gc, -1.0, ALU.mult)
                pbc = [C.PS[0], C.PS[1], C.PS[2]]
                P.mm(pbc[0], C.ones[0:1, :], rows[b][:, 0, :])
                P.mm(pbc[1], C.ones[0:1, :], r_eg[b])
                P.mm(pbc[2], C.ones[0:1, :], r_ekl[b])
                for j in range(3):
                    P.copy(bcs[b][:, j, :], pbc[j], eng='act')
                P.tt(kb[b], kT[b], bcs[b][:, 0, :], ALU.mult)
                P.tt(kbe[b], kb[b], bcs[b][:, 1, :], ALU.mult, eng='pool')
                P.tt(vb[b], vT[b], bcs[b][:, 0, :], ALU.mult)
                P.tt(qe[b], qT[b], bcs[b][:, 1, :], ALU.mult, eng='pool')
                P.tt(kel[b], kT[b], bcs[b][:, 2, :], ALU.mult)
                for c in range(8):
                    csl = slice(c * 64, (c + 1) * 64)
                    for j, src in enumerate((vb[b], kbe[b], kel[b])):
                        pt = C.PS[3 + ((c * 3 + j) % 2)]
                        P.tr(pt[0:64, 0:128], src[:, csl], C.ident)
                        P.copy(tokm[b][:, j, c, :], pt[0:64, 0:128], eng=('act' if j != 1 else 'dve'))
                pg = C.PS[5]
                for c in range(8):
                    csl = slice(c * 64, (c + 1) * 64)
                    P.mm(pg[0:64, csl], rows[b][:, 1, csl], C.ones[0:1, 0:64], start=True, stop=False)
                    P.mm(pg[0:64, csl], C.ones[0:1, 0:64], r_ng[b][:, csl], start=False, stop=True)
                P.ts(t1[b], pg[0:64, :], 0.0, ALU.min)
                P.act(EL[b], t1[b], AF.Exp)
                P.ts(t1[b], pg[0:64, :], 0.0, ALU.max)
                P.act(EU[b], t1[b], AF.Exp, scale=-1.0)
                pA, pAT, pPT = C.PS[0], C.PS[1], C.PS[2]
                for c in range(8):
                    csl = slice(c * 64, (c + 1) * 64)
                    P.mm(pA[0:64, csl], kb[b][:, csl], kT[b][:, csl])
                    P.mm(pAT[0:64, csl], kT[b][:, csl], kb[b][:, csl])
                    P.mm(pPT[0:64, csl], kT[b][:, csl], qT[b][:, csl])
                P.tt(t1[b], EL[b], C.gmask[:, 0, :], ALU.mult, eng='pool')
                P.tt(Am[b], pA[0:64, :], t1[b], ALU.mult)
                P.tt(EL[b], EU[b], C.gmask[:, 1, :], ALU.mult, eng='pool')
                P.tt(ATm[b], pAT[0:64, :], EL[b], ALU.mult)
                P.tt(EU[b], EU[b], C.gmask[:, 2, :], ALU.mult, eng='pool')
                P.tt(PTm[b], pPT[0:64, :], EU[b], ALU.mult)
                P.tt(Rm[b], ATm[b], C.gmask[:, 3, :], ALU.add)
                X, XT = Am[b], ATm[b]
                X2, XT2 = t1[b], EL[b]
                for step in range(5):
                    p1, p2, p3 = C.PS[3], C.PS[4], C.PS[5]
                    for c in range(8):
                        csl = slice(c * 64, (c + 1) * 64)
                        P.mm(p1[0:64, csl], XT[:, csl], X[:, csl])
                        P.mm(p2[0:64, csl], X[:, csl], XT[:, csl])
                    P.copy(X2, p1[0:64, :], eng='act')
                    P.copy(XT2, p2[0:64, :], eng='dve')
                    X, X2 = X2, X
                    XT, XT2 = XT2, XT
                    for c in range(8):
                        csl = slice(c * 64, (c + 1) * 64)
                        P.mm(p3[0:64, csl], X[:, csl], Rm[b][:, csl])
                    P.tt(Rm[b], Rm[b], p3[0:64, :], ALU.add)
                pW = C.PS[6]
                for c in range(8):
                    csl = slice(c * 64, (c + 1) * 64)
                    P.mm(pW[:, csl], tokm[b][:, 1, c, :], Rm[b][:, csl])
                P.act(WTn[b], pW, AF.Copy, scale=-1.0)
                po = C.PS[7]
                S = Sst[h]
                for c in range(8):
                    csl = slice(c * 64, (c + 1) * 64)
                    pv = C.PS[3 + (c % 2)]
                    P.mm(pv[0:64, 0:128], Rm[b][:, csl], tokm[b][:, 0, c, :], start=True, stop=False)
                    P.mm(pv[0:64, 0:128], WTn[b][:, csl], S, start=False, stop=True)
                    vn = vnew[c % 3]
                    P.copy(vn, pv[0:64, 0:128], eng='act')
                    P.mm(po[:, csl], S, qe[b][:, csl], start=True, stop=False)
                    P.mm(po[:, csl], vn, PTm[b][:, csl], start=False, stop=True)
                    ps_ = C.PS[5]
                    P.mm(ps_[:, 0:128], tokm[b][:, 2, c, :], vn)
                    P.stt(S, S, bcs[b][:, 1, c * 64 + 63:c * 64 + 64], ps_[:, 0:128], ALU.mult, ALU.add)
                P.copy(osb[b], po, eng='act')
                P.act(sq[b], osb[b], AF.Square)
                pss = C.PS[6]
                P.mm(pss, C.ones, sq[b])
                rstd_from_ss(P, rs[b], pss, 1.0 / 128, EPS)
                P.stt(osb[b], osb[b], gw[:, 0:1], rs[b], ALU.mult, ALU.mult)
                P.tt(osb[b], osb[b], zt[b], ALU.mult)
                P.dma(C.yT[h * 128:(h + 1) * 128, tsl], osb[b])
        P.barrier()
        P.flush()


def stage_s5(P, C, L, l):
    nc = P.nc
    I = C.inp
    i = l // 2
    NT = L // 512
    TWO_PI = 2.0 * math.pi
    with contextlib.ExitStack() as st:
        P.stack = st
        lr = P.sb([128, 16])
        li = P.sb([128, 16])
        ls = P.sb([128, 16])
        with nc.allow_non_contiguous_dma(reason="small"):
            P.dma(lr, I['even_lam_re'][i].re("(j g) n -> (g n) j", g=2))
            P.dma(li, I['even_lam_im'][i].re("(j g) n -> (g n) j", g=2))
            lsv = I['even_log_step'][i].re("(j g) -> g j", g=2)
            for g2 in range(2):
                P.dma(ls[g2 * 64:(g2 + 1) * 64, :], lsv[g2:g2 + 1, :].bc([64, 16]))
        dt = P.sb([128, 16])
        P.act(dt, ls, AF.Exp)
        P.ts(lr, lr, -1e-4, ALU.min)
        rho = P.sb([128, 16])
        P.tt(rho, lr, dt, ALU.mult)
        P.act(rho, rho, AF.Exp)
        th = P.sb([128, 16])
        P.tt(th, li, dt, ALU.mult)
        tmpa = P.sb([128, 16])
        sn = P.sb([128, 16])
        cs = P.sb([128, 16])
        P.act(sn, th, AF.Sin, scale=1.0 / 16)
        P.act(cs, th, AF.Sin, scale=1.0 / 16, bias=C.halfpi[:, 0:1])
        t_c2, t_s2 = P.sb([128, 16]), P.sb([128, 16])
        for _ in range(4):
            P.tt(t_c2, cs, cs, ALU.mult)
            P.tt(t_s2, sn, sn, ALU.mult)
            P.tt(sn, cs, sn, ALU.mult)
            P.ts(sn, sn, 2.0, ALU.mult)
            P.tt(cs, t_c2, t_s2, ALU.subtract)
        ar, ai = P.sb([128, 16]), P.sb([128, 16])
        P.tt(ar, rho, cs, ALU.mult)
        P.tt(ai, rho, sn, ALU.mult)
        nr = P.sb([128, 16])
        P.ts(nr, ar, -1.0, ALU.add)
        den = P.sb([128, 16])
        t2 = P.sb([128, 16])
        P.tt(den, lr, lr, ALU.mult)
        P.tt(t2, li, li, ALU.mult)
        P.tt(den, den, t2, ALU.add)
        P.recip(den, den)
        cr, ci = P.sb([128, 16]), P.sb([128, 16])
        P.tt(cr, nr, lr, ALU.mult)
        P.tt(t2, ai, li, ALU.mult)
        P.tt(cr, cr, t2, ALU.add)
        P.tt(cr, cr, den, ALU.mult)
        P.tt(ci, ai, lr, ALU.mult)
        P.tt(t2, nr, li, ALU.mult)
        P.tt(ci, ci, t2, ALU.subtract)
        P.tt(ci, ci, den, ALU.mult)
        nsn = P.sb([128, 16])
        P.ts(nsn, sn, -1.0, ALU.mult)
        Tc = P.sb([128, 16, 512])
        Ts = P.sb([128, 16, 512])
        P.memset(Tc[:, :, 0:1], 1.0)
        P.memset(Ts[:, :, 0:1], 0.0)
        cc, s_ = P.sb([128, 16]), P.sb([128, 16])
        P.copy(cc, cs)
        P.copy(s_, sn)
        ta, tb = P.sb([128, 256]), P.sb([128, 256])
        c2, s2 = P.sb([128, 16]), P.sb([128, 16])
        span = 1
        while span < 512:
            for j in range(16):
                P.ts(ta[:, 0:span], Ts[:, j, 0:span], s_[:, j:j + 1], ALU.mult)
                P.stt(Tc[:, j, span:2 * span], Tc[:, j, 0:span], cc[:, j:j + 1], ta[:, 0:span], ALU.mult, ALU.subtract)
                P.ts(tb[:, 0:span], Tc[:, j, 0:span], s_[:, j:j + 1], ALU.mult)
                P.stt(Ts[:, j, span:2 * span], Ts[:, j, 0:span], cc[:, j:j + 1], tb[:, 0:span], ALU.mult, ALU.add)
            P.tt(c2, cc, cc, ALU.mult)
            P.tt(s2, s_, s_, ALU.mult)
            P.tt(s_, cc, s_, ALU.mult)
            P.ts(s_, s_, 2.0, ALU.mult)
            P.tt(cc, c2, s2, ALU.subtract)
            span *= 2
        BreT = [P.sb([128, 128]) for _ in range(16)]
        BimT = [P.sb([128, 128]) for _ in range(16)]
        CrT = [P.sb([128, 128]) for _ in range(16)]
        CiT = [P.sb([128, 128]) for _ in range(16)]
        pad = [P.sb([128, 128]) for _ in range(4)]
        craw = [P.sb([128, 128]) for _ in range(2)]
        for j in range(16):
            kt = j // 4
            for which, (nm, dst) in enumerate((('even_b_re', BreT), ('even_b_im', BimT))):
                pd = pad[which]
                P.memset(pd, 0.0)
                for g2 in range(2):
                    g = 2 * j + g2
                    off = (g - 8 * kt) * 16
                    P.dma(pd[g2 * 64:(g2 + 1) * 64, off:off + 16], I[nm][i][g])
                pt = C.PS[which]
                P.tr(pt[:, 0:128], pd, C.ident)
                P.copy(dst[j], pt[:, 0:128], eng='act')
            for which, nm in enumerate(('even_c_re', 'even_c_im')):
                pd = pad[2 + which]
                P.memset(pd, 0.0)
                for g2 in range(2):
                    g = 2 * j + g2
                    off = (g - 8 * kt) * 16
                    P.dma(pd[off:off + 16, g2 * 64:(g2 + 1) * 64], I[nm][i][g])
                pt = C.PS[2 + which]
                P.tr(pt[:, 0:128], pd, C.ident)
                P.copy(craw[which], pt[:, 0:128], eng='act')
            P.ts(pad[0], craw[1], ci[:, j:j + 1], ALU.mult)
            P.stt(CrT[j], craw[0], cr[:, j:j + 1], pad[0], ALU.mult, ALU.subtract)
            P.ts(pad[1], craw[1], cr[:, j:j + 1], ALU.mult)
            P.stt(CiT[j], craw[0], ci[:, j:j + 1], pad[1], ALU.mult, ALU.add)
            P.ts(CiT[j], CiT[j], -1.0, ALU.mult)
        dsk = P.sb([128, 4])
        glb = P.sb([128, 8])
        with nc.allow_non_contiguous_dma(reason="tiny"):
            P.dma(dsk, I['even_d_skip'][i].re("(k p) -> p k", p=128))
            P.dma(glb, I['even_glu_b'][i].re("(k p) -> p k", p=128))
        nglb = P.sb([128, 8])
        P.ts(nglb, glb, -1.0, ALU.mult)
        gluw = P.sb([128, 4, 1024], BF16)
        P.dma(gluw, I['even_glu_w'][i].re("(k p) m -> p k m", p=128), eng='pool')
        ini = [P.sb([128, 2]) for _ in range(16)]
        for j in range(16):
            P.memset(ini[j], 0.0)
        uts = [P.sb([128, 4, 512]) for _ in range(2)]
        bu = [P.sb([128, 2, 512])] * 2
        zin = [P.sb([128, 2, 512])] * 2
        zz = [P.sb([128, 2, 512])] * 2
        xx = [P.sb([128, 2, 512]) for _ in range(2)]
        w1 = [P.sb([128, 512]) for _ in range(4)]
        sml = [P.sb([128, 2]) for _ in range(2)]
        yg = P.sb([128, 4, 512], BF16)
        ysb = [P.sb([128, 512]) for _ in range(2)]
        ga = [P.sb([128, 512]) for _ in range(2)]
        gb = [P.sb([128, 512]) for _ in range(2)]
        uv = C.uT.v.re("(k p) t -> p k t", p=128)
        P.dma(uts[0], uv[:, :, 0:512])
        it = 0
        for tt in range(NT):
            tsl = slice(tt * 512, (tt + 1) * 512)
            ut = uts[tt % 2]
            if tt + 1 < NT:
                P.dma(uts[(tt + 1) % 2], uv[:, :, (tt + 1) * 512:(tt + 2) * 512])
            for kt in range(4):
                py = C.PS[4 + (kt % 2)]
                for jj in range(4):
                    j = kt * 4 + jj
                    b = it % 2
                    it += 1
                    pr, pi_ = C.PS[0 + 2 * b], C.PS[1 + 2 * b]
                    P.mm(pr, BreT[j], ut[:, kt, :])
                    P.mm(pi_, BimT[j], ut[:, kt, :])
                    P.copy(bu[b][:, 0, :], pr, eng='act')
                    P.copy(bu[b][:, 1, :], pi_, eng='act')
                    P.tt(w1[0], bu[b][:, 0, :], Tc[:, j, :], ALU.mult)
                    P.tt(w1[1], bu[b][:, 1, :], Ts[:, j, :], ALU.mult, eng='pool')
                    P.tt(zin[b][:, 0, :], w1[0], w1[1], ALU.add)
                    P.tt(w1[2], bu[b][:, 1, :], Tc[:, j, :], ALU.mult, eng='pool')
                    P.tt(w1[3], bu[b][:, 0, :], Ts[:, j, :], ALU.mult)
                    P.tt(zin[b][:, 1, :], w1[2], w1[3], ALU.subtract, eng='pool')
                    P.scan(zz[b][:, 0, :], rho[:, j:j + 1].bc([128, 512]), zin[b][:, 0, :], ini[j][:, 0:1])
                    P.scan(zz[b][:, 1, :], rho[:, j:j + 1].bc([128, 512]), zin[b][:, 1, :], ini[j][:, 1:2])
                    P.tt(w1[0], zz[b][:, 0, :], Tc[:, j, :], ALU.mult)
                    P.tt(w1[1], zz[b][:, 1, :], Ts[:, j, :], ALU.mult, eng='pool')
                    P.tt(xx[b][:, 0, :], w1[0], w1[1], ALU.subtract)
                    P.tt(w1[2], zz[b][:, 1, :], Tc[:, j, :], ALU.mult, eng='pool')
                    P.tt(w1[3], zz[b][:, 0, :], Ts[:, j, :], ALU.mult)
                    P.tt(xx[b][:, 1, :], w1[2], w1[3], ALU.add, eng='pool')
                    sm_ = sml[b]
                    P.ts(sm_[:, 0:1], xx[b][:, 1, 511:512], nsn[:, j:j + 1], ALU.mult)
                    P.ts(sm_[:, 1:2], xx[b][:, 0, 511:512], sn[:, j:j + 1], ALU.mult)
                    P.stt(ini[j][:, 0:1], xx[b][:, 0, 511:512], cs[:, j:j + 1], sm_[:, 0:1], ALU.mult, ALU.add)
                    P.stt(ini[j][:, 1:2], xx[b][:, 1, 511:512], cs[:, j:j + 1], sm_[:, 1:2], ALU.mult, ALU.add)
                    P.mm(py, CrT[j], xx[b][:, 0, :], start=(jj == 0), stop=False)
                    P.mm(py, CiT[j], xx[b][:, 1, :], start=False, stop=(jj == 3))
                y = ysb[kt % 2]
                P.stt(y, ut[:, kt, :], dsk[:, kt:kt + 1], py, ALU.mult, ALU.add)
                P.act(yg[:, kt, :], y, AF.Gelu)
            for m in range(4):
                pa, pb = C.PS[6], C.PS[7]
                for k in range(4):
                    P.mm(pa, gluw[:, k, m * 128:(m + 1) * 128], yg[:, k, :], start=(k == 0), stop=(k == 3))
                for k in range(4):
                    P.mm(pb, gluw[:, k, 512 + m * 128:512 + (m + 1) * 128], yg[:, k, :], start=(k == 0), stop=(k == 3))
                a_, b_ = ga[m % 2], gb[m % 2]
                P.act(a_, pa, AF.Identity, bias=glb[:, m:m + 1])
                P.act(b_, pb, AF.Exp, bias=nglb[:, 4 + m:5 + m], scale=-1.0)
                P.ts(b_, b_, 1.0, ALU.add)
                P.recip(b_, b_)
                P.tt(a_, a_, b_, ALU.mult)
                P.dma(C.yT[512 + m * 128:512 + (m + 1) * 128, tsl], a_)
        P.barrier()
        P.flush()


def stage_ffn(P, C, L, l, xsrc, xdst, ysrc, wout, moe):
    nc = P.nc
    I = C.inp
    i = l // 2
    NT = L // 512
    mod = C.mod[l]
    with contextlib.ExitStack() as st:
        P.stack = st
        S = Ctx()
        facc = P.sb([128, 8, 512])
        S.sq = facc
        S.rstd = P.sb([128, 512])
        S.tmp = [P.sb([128, 512]) for _ in range(2)]
        S.hb = P.sb([128, 8, 512], BF16)
        S.h32 = P.sb([128, 8, 512]) if moe else None
        S.wb = [P.sb([128, 8, 512], BF16) for _ in range(2)]
        S.wbi = 0
        w2b = [P.sb([128, 22, 128], BF16) for _ in range(2)]
        nw2 = 0
        xts = [P.sb([128, 8, 512])]
        ybfs = [P.sb([128, 8, 512], BF16) for _ in range(2)]
        hid = P.sb([128, 22, 512], BF16)
        sa = [P.sb([128, 512]) for _ in range(3)]
        if moe:
            rw = P.sb([128, 8, 8])
            with nc.allow_non_contiguous_dma(reason="small"):
                P.dma(rw, I['odd_router_w'][i].re("(k p) e -> p k e", p=128))
            lg = P.sb([8, 512])
            lt = P.sb([128, 4, 8])
            m1 = P.sb([128, 4])
            m2 = P.sb([128, 4])
            eq1 = P.sb([128, 4, 8])
            eq2 = P.sb([128, 4, 8])
            msk = P.sb([128, 4, 8])
            gg = P.sb([128, 4])
            g2_ = P.sb([128, 4])
            cmb = P.sb([128, 4, 8])
            cmbT = P.sb([8, 512])
            comb = P.sb([128, 8, 512], BF16)
        xv = xsrc.v.re("(k p) t -> p k t", p=128)
        yv = ysrc.v.re("(k p) t -> p k t", p=128)
        ov = xdst.v.re("(k p) t -> p k t", p=128)
        P.dma(ybfs[0], yv[:, :, 0:512], eng='pool')
        nsa = 0
        for tt in range(NT):
            tsl = slice(tt * 512, (tt + 1) * 512)
            xt, ybf = xts[0], ybfs[tt % 2]
            P.dma(xt, xv[:, :, tsl])
            if tt + 1 < NT:
                P.dma(ybfs[(tt + 1) % 2], yv[:, :, (tt + 1) * 512:(tt + 2) * 512], eng='pool')
            for wsl, c0, w in wslab_iter(P, C, S, wout, 1024, 8):
                for j in range(4):
                    m = c0 // 128 + j
                    pp = C.PS[1 + (m % 4)]
                    for k in range(8):
                        P.mm(pp, wsl[:, k, j * 128:(j + 1) * 128], ybf[:, k, :], start=(k == 0), stop=(k == 7))
                    P.stt(xt[:, m, :], pp, mod[:, 16 + m:17 + m], xt[:, m, :], ALU.mult, ALU.add)
            load_norm_h(P, C, S, xt, l, 2, want32=moe)
            if moe:
                pl = C.PS[5]
                for k in range(8):
                    P.mm(pl[0:8, :], rw[:, k, :], S.h32[:, k, :], start=(k == 0), stop=(k == 7))
                P.copy(lg, pl[0:8, :], eng='act')
                pt = C.PS[6]
                for s in range(4):
                    P.tr(pt[:, s * 8:(s + 1) * 8], lg[:, s * 128:(s + 1) * 128], C.ident[0:8, 0:8])
                P.copy(lt, pt[:, 0:32].re("p (s e) -> p s e", e=8), eng='act')
                P.reduce(m1, lt, ALU.max)
                P.tt(eq1, lt, m1.v.re("p (s o) -> p s o", o=1).bc([128, 4, 8]), ALU.is_equal)
                P.stt(msk, eq1, -1e30, lt, ALU.mult, ALU.add)
                P.reduce(m2, msk, ALU.max)
                P.tt(eq2, msk, m2.v.re("p (s o) -> p s o", o=1).bc([128, 4, 8]), ALU.is_equal)
                P.tt(gg, m2, m1, ALU.subtract)
                P.act(gg, gg, AF.Exp)
                P.ts(gg, gg, 1.0, ALU.add)
                P.recip(gg, gg)
                P.ts(g2_, gg, -1.0, ALU.mult, 1.0, ALU.add)
                P.tt(cmb, eq1, gg.v.re("p (s o) -> p s o", o=1).bc([128, 4, 8]), ALU.mult)
                P.tt(eq2, eq2, g2_.v.re("p (s o) -> p s o", o=1).bc([128, 4, 8]), ALU.mult)
                P.tt(cmb, cmb, eq2, ALU.add)
                pc = C.PS[7]
                for s in range(4):
                    P.tr(pc[0:8, s * 128:(s + 1) * 128], cmb[:, s, :], C.ident)
                P.copy(cmbT, pc[0:8, :], eng='act')
                for e in range(NEXP):
                    pb_ = C.PS[5 + (e % 2)]
                    P.mm(pb_, C.sel[:, e, :], cmbT)
                    P.copy(comb[:, e, :], pb_, eng='act')
            nexp = NEXP if moe else 1
            for e in range(nexp):
                if moe:
                    W13 = I['odd_expert_w13'][i][e]
                    W2 = I['odd_expert_w2'][i][e]
                else:
                    W13 = I['even_ffn_w13'][i]
                    W2 = I['even_ffn_w2'][i]
                fi = 0
                for c0 in range(0, DFF, 256):
                    w = min(256, DFF - c0)
                    buf = S.wb[S.wbi % 2]
                    S.wbi += 1
                    P.dma(buf[:, :, 0:w], W13[:, c0:c0 + w].re("(k p) m -> p k m", p=128), eng='pool')
                    P.dma(buf[:, :, 256:256 + w], W13[:, DFF + c0:DFF + c0 + w].re("(k p) m -> p k m", p=128), eng='pool')
                    for j in range(w // 128):
                        f = c0 // 128 + j
                        pa, pb = C.PS[1 + 2 * (f % 2)], C.PS[2 + 2 * (f % 2)]
                        for k in range(8):
                            P.mm(pa, buf[:, k, j * 128:(j + 1) * 128], S.hb[:, k, :], start=(k == 0), stop=(k == 7))
                        for k in range(8):
                            P.mm(pb, buf[:, k, 256 + j * 128:256 + (j + 1) * 128], S.hb[:, k, :], start=(k == 0), stop=(k == 7))
                        s_ = sa[nsa % 3]
                        nsa += 1
                        P.act(s_, pa, AF.Silu)
                        if moe:
                            P.tt(s_, s_, comb[:, e, :], ALU.mult, eng='pool')
                        P.tt(hid[:, f, :], pb, s_, ALU.mult)
                for m in range(8):
                    wb2 = w2b[nw2 % 2]
                    nw2 += 1
                    P.dma(wb2, W2[:, m * 128:(m + 1) * 128].re("(f p) m -> p f m", p=128), eng='pool')
                    pp = C.PS[5 + (m % 2)]
                    for f in range(22):
                        P.mm(pp, wb2[:, f, :], hid[:, f, :], start=(f == 0), stop=(f == 21))
                    if moe:
                        if e == 0:
                            P.copy(facc[:, m, :], pp, eng='act')
                        elif e < nexp - 1:
                            P.tt(facc[:, m, :], pp, facc[:, m, :], ALU.add)
                        else:
                            P.tt(facc[:, m, :], pp, facc[:, m, :], ALU.add)
                            P.stt(facc[:, m, :], facc[:, m, :], mod[:, 40 + m:41 + m], xt[:, m, :], ALU.mult, ALU.add)
                    else:
                        P.stt(facc[:, m, :], pp, mod[:, 40 + m:41 + m], xt[:, m, :], ALU.mult, ALU.add)
            P.dma(ov[:, :, tsl], facc)
        P.barrier()
        P.flush()


def stage_attn_proj(P, C, L, l, xsrc):
    nc = P.nc
    I = C.inp
    i = l // 2
    NT = L // 512
    Wq = I['odd_w_qkv'][i]
    with contextlib.ExitStack() as st:
        P.stack = st
        S = Ctx()
        S.sq = P.sb([128, 8, 512])
        S.rstd = P.sb([128, 512])
        S.tmp = [P.sb([128, 512]) for _ in range(2)]
        S.hb = P.sb([128, 8, 512], BF16)
        S.h32 = None
        S.wb = [P.sb([128, 8, 512], BF16) for _ in range(3)]
        S.wbi = 0
        wv = P.sb([128, 8, 1024], BF16)
        P.dma(wv, Wq[:, 2048:3072].re("(k p) m -> p k m", p=128), eng='pool')
        nw = P.sb([128, 2])
        with nc.allow_non_contiguous_dma(reason="tiny"):
            for t in range(2):
                P.dma(nw[t * 64:(t + 1) * 64, 0:1], I['odd_q_norm_w'][i].re("(p o) -> p o", o=1))
                P.dma(nw[t * 64:(t + 1) * 64, 1:2], I['odd_k_norm_w'][i].re("(p o) -> p o", o=1))
        xts = [P.sb([128, 8, 512]) for _ in range(2)]
        raw = [P.sb([128, 512]) for _ in range(2)]
        sq = [P.sb([128, 512]) for _ in range(2)]
        rs = [P.sb([128, 512]) for _ in range(2)]
        ob = [P.sb([128, 512], BF16) for _ in range(3)]
        vb = [P.sb([128, 1024], BF16) for _ in range(2)]
        xv = xsrc.v.re("(k p) t -> p k t", p=128)
        P.dma(xts[0], xv[:, :, 0:512])
        n = 0
        for tt in range(NT):
            tsl = slice(tt * 512, (tt + 1) * 512)
            xt = xts[tt % 2]
            if tt + 1 < NT:
                P.dma(xts[(tt + 1) % 2], xv[:, :, (tt + 1) * 512:(tt + 2) * 512])
            load_norm_h(P, C, S, xt, l, 1)
            for wsl, c0, w in wslab_iter(P, C, S, Wq, 2048, 8):
                for j in range(4):
                    m = c0 // 128 + j
                    isq = m < 8
                    pp = C.PS[1 + (m % 4)]
                    for k in range(8):
                        P.mm(pp, wsl[:, k, j * 128:(j + 1) * 128], S.hb[:, k, :], start=(k == 0), stop=(k == 7))
                    r = raw[n % 2]
                    P.copy(r, pp, eng='act')
                    P.act(sq[n % 2], r, AF.Square)
                    pss = C.PS[5 + (n % 2)]
                    P.mm(pss, C.blk, sq[n % 2])
                    if isq:
                        rstd_from_ss(P, rs[n % 2], pss, 1.0, 64.0 * EPS)
                    else:
                        rstd_from_ss(P, rs[n % 2], pss, 1.0 / 64, EPS)
                    o = ob[n % 3]
                    P.stt(o, r, nw[:, (0 if isq else 1):(1 if isq else 2)], rs[n % 2], ALU.mult, ALU.mult)
                    P.dma(C.qkT[m * 128:(m + 1) * 128, tsl], o)
                    n += 1
            for s in range(4):
                v_ = vb[s % 2]
                for half in range(2):
                    pp = C.PS[1 + ((s * 2 + half) % 4)]
                    for k in range(8):
                        P.mm(pp, S.hb[:, k, s * 128:(s + 1) * 128], wv[:, k, half * 512:(half + 1) * 512],
                             start=(k == 0), stop=(k == 7))
                    P.copy(v_[:, half * 512:(half + 1) * 512], pp, eng=('act' if half else 'dve'))
                P.dma(C.vtok[tt * 512 + s * 128:tt * 512 + (s + 1) * 128, :], v_)
        P.barrier()
        P.flush()


def stage_attn(P, C, L, l):
    nc = P.nc
    I = C.inp
    i = l // 2
    NT = L // 512
    NK = L // 128
    lambda_init = 0.8 - 0.6 * math.exp(-0.3 * l)
    with contextlib.ExitStack() as st:
        P.stack = st
        lq = P.sb([128, 4, 64])
        for j, nm in enumerate(('odd_lambda_q1', 'odd_lambda_k1', 'odd_lambda_q2', 'odd_lambda_k2')):
            a = I[nm][i].re("(o d) -> o d", o=1)
            P.dma(lq[:, j, :], V(a.res, a.ap.to_broadcast([128, 64])))
        pr = P.sb([128, 2, 64])
        P.tt(pr[:, 0, :], lq[:, 0, :], lq[:, 1, :], ALU.mult)
        P.tt(pr[:, 1, :], lq[:, 2, :], lq[:, 3, :], ALU.mult)
        sm = P.sb([128, 2])
        P.reduce(sm, pr, ALU.add)
        P.act(sm, sm, AF.Exp)
        nlam = P.sb([128, 1])
        P.tt(nlam, sm[:, 1:2], sm[:, 0:1], ALU.subtract)
        P.ts(nlam, nlam, -lambda_init, ALU.add)
        sw = P.sb([128, 1])
        with nc.allow_non_contiguous_dma(reason="tiny"):
            P.dma(sw, I['odd_subln_w'][i].re("(p o) -> p o", o=1))
        P.ts(sw, sw, 1.0 - lambda_init, ALU.mult)
        kT = [P.sb([128, L], BF16) for _ in range(2)]
        vt = [P.sb([128, NK, 128], BF16) for _ in range(2)]
        qt_ = [P.sb([128, 512], BF16) for _ in range(2)]
        E = [P.sb([128, 512], BF16) for _ in range(4)]
        r1 = [P.sb([128, 512]) for _ in range(2)]
        r2 = [P.sb([128, 512]) for _ in range(2)]
        o1 = [P.sb([128, 512]) for _ in range(2)]
        sq = [P.sb([128, 512]) for _ in range(2)]
        ob = [P.sb([128, 512]) for _ in range(2)]
        ne = 0
        nq = 0
        for h in range(8):
            kk, vv = kT[h % 2], vt[h % 2]
            P.dma(kk, C.qkT[1024 + h * 128:1024 + (h + 1) * 128, :])
            P.dma(vv, C.vtok[:, h * 128:(h + 1) * 128].re("(n p) e -> p n e", p=128))
            for qt in range(NT):
                tsl = slice(qt * 512, (qt + 1) * 512)
                q = qt_[nq % 2]
                P.dma(q, C.qkT[h * 128:(h + 1) * 128, tsl])
                pn = [C.PS[0], C.PS[1]]
                pd = [C.PS[2], C.PS[3]]
                nkt = 4 * (qt + 1)
                for kt in range(nkt):
                    for t in range(2):
                        psc = C.PS[4 + (ne % 4)]
                        P.mm(psc, kk[t * 64:(t + 1) * 64, kt * 128:(kt + 1) * 128], q[t * 64:(t + 1) * 64, :])
                        e_ = E[ne % 4]
                        ne += 1
                        P.act(e_, psc, AF.Exp, bias=C.neg8[:, 0:1])
                        r = kt - 4 * qt
                        if r >= 0:
                            P.tt(e_, e_, C.amask[:, r, :], ALU.mult, eng='pool')
                        P.mm(pn[t], vv[:, kt, :], e_, start=(kt == 0), stop=(kt == nkt - 1))
                        P.mm(pd[t], C.onesb, e_, start=(kt == 0), stop=(kt == nkt - 1))
                b = nq % 2
                nq += 1
                P.recip(r1[b], pd[0])
                P.recip(r2[b], pd[1])
                P.tt(o1[b], pn[0], r1[b], ALU.mult)
                P.tt(r2[b], pn[1], r2[b], ALU.mult)
                P.stt(o1[b], r2[b], nlam[:, 0:1], o1[b], ALU.mult, ALU.add)
                P.act(sq[b], o1[b], AF.Square)
                pss = C.PS[2]
                P.mm(pss, C.ones, sq[b])
                rstd_from_ss(P, r1[b], pss, 1.0 / 128, EPS)
                P.stt(ob[b], o1[b], sw[:, 0:1], r1[b], ALU.mult, ALU.mult)
                P.dma(C.oT[h * 128:(h + 1) * 128, tsl], ob[b])
        P.barrier()
        P.flush()


def make_consts():
    c = {}
    c['c_ident'] = np.eye(128, dtype=np.float32)
    am = np.zeros((128, 4, 512), np.float32)
    kk = np.arange(128)[:, None]
    qq = np.arange(512)[None, :]
    for r in range(4):
        am[:, r, :] = ((r * 128 + kk) // 64 <= qq // 64)
    c['c_amask'] = am
    gm = np.zeros((64, 4, 512), np.float32)
    p = np.arange(64)[:, None]
    f = np.arange(64)[None, :]
    for cidx in range(8):
        sl = slice(cidx * 64, (cidx + 1) * 64)
        gm[:, 0, sl] = -1.0 * (p > f)
        gm[:, 1, sl] = -1.0 * (p < f)
        gm[:, 2, sl] = (p <= f)
        gm[:, 3, sl] = (p == f)
    c['c_gmask'] = gm
    sel = np.zeros((8, 8, 128), np.float32)
    for e in range(8):
        sel[e, e, :] = 1.0
    c['c_sel'] = sel
    blk = np.zeros((128, 128), np.float32)
    blk[:64, :64] = 1
    blk[64:, 64:] = 1
    c['c_blk'] = blk
    cm = np.ones((4, 512), np.float32)
    cm[:, ::64] = 0
    c['c_cmask'] = cm
    return c


INPUT_NAMES = ['ada_w', 'ada_b', 'norm_mix_w', 'norm_ffn_w',
               'even_w_in', 'even_conv_w', 'even_a_log', 'even_dt_bias', 'even_gdn_norm_w',
               'even_lam_re', 'even_lam_im', 'even_log_step', 'even_b_re', 'even_b_im', 'even_c_re', 'even_c_im',
               'even_d_skip', 'even_glu_w', 'even_glu_b', 'even_w_out', 'even_ffn_w13', 'even_ffn_w2',
               'odd_w_qkv', 'odd_q_norm_w', 'odd_k_norm_w', 'odd_lambda_q1', 'odd_lambda_k1', 'odd_lambda_q2',
               'odd_lambda_k2', 'odd_subln_w', 'odd_w_out', 'odd_router_w', 'odd_expert_w13', 'odd_expert_w2']


def build(shapes, L, depth=DEPTH, dbg=False):
    nc = bass.Bass("TRN2", target_bir_lowering=False)
    C = Ctx()
    C.depth = depth
    C.inp = {}
    consts = make_consts()
    for nm, shp in shapes.items():
        C.inp[nm] = Res(nm, nc.dram_tensor(nm, list(shp), F32, kind="ExternalInput").ap())
    for nm, arr in consts.items():
        C.inp[nm] = Res(nm, nc.dram_tensor(nm, list(arr.shape), F32, kind="ExternalInput").ap())
    C.out = Res('out', nc.dram_tensor('out', [L, D], F32, kind="ExternalOutput").ap())
    kind = "ExternalOutput" if dbg else "Internal"

    def scr(nm, shape, dt=F32):
        return Res(nm, nc.dram_tensor(nm, list(shape), dt, kind=kind).ap())
    C.xA = scr('xA', [D, L])
    C.xB = scr('xB', [D, L])
    C.qkvT = scr('qkvT', [1536, L])
    C.zT = scr('zT', [512, L])
    C.uT = scr('uT', [512, L])
    C.betaT = scr('betaT', [4, L])
    C.gT = scr('gT', [4, L])
    C.yT = scr('yT', [D, L])
    C.qkT = scr('qkT', [2048, L], BF16)
    C.vtok = scr('vtok', [L, D], BF16)
    C.oT = scr('oT', [D, L])
    with contextlib.ExitStack() as st0:
        P = Prog(nc, st0)
        C.PS = [P.ps([128, 512]) for _ in range(8)]
        C.ident = P.sb([128, 128])
        C.ones = P.sb([128, 128])
        C.onesb = P.sb([128, 128], BF16)
        C.blk = P.sb([128, 128])
        C.amask = P.sb([128, 4, 512], BF16)
        C.gmask = P.sb([64, 4, 512])
        C.sel = P.sb([8, 8, 128])
        C.cmask = P.sb([4, 512])
        C.halfpi = P.sb([128, 1])
        C.neg8 = P.sb([128, 1])
        C.mod = [P.sb([128, 48]) for _ in range(depth)]
        C.modA = [P.sb([128, 16]) for _ in range(depth)]
        P.dma(C.ident, C.inp['c_ident'])
        P.dma(C.blk, C.inp['c_blk'])
        P.dma(C.amask, C.inp['c_amask'], eng='pool')
        P.dma(C.gmask, C.inp['c_gmask'])
        P.dma(C.sel, C.inp['c_sel'])
        P.dma(C.cmask, C.inp['c_cmask'])
        P.memset(C.ones, 1.0)
        P.memset(C.onesb, 1.0)
        P.memset(C.halfpi, math.pi / 2)
        P.memset(C.neg8, -8.0)
        stage_prep(P, C, L)
        cur, nxt = C.xA, C.xB
        for l in range(depth):
            i = l // 2
            if l % 2 == 0:
                stage_even_proj(P, C, L, l, cur)
                stage_gdn(P, C, L, l)
                stage_s5(P, C, L, l)
                stage_ffn(P, C, L, l, cur, nxt, C.yT, C.inp['even_w_out'][i], moe=False)
            else:
                stage_attn_proj(P, C, L, l, cur)
                stage_attn(P, C, L, l)
                stage_ffn(P, C, L, l, cur, nxt, C.oT, C.inp['odd_w_out'][i], moe=True)
            cur, nxt = nxt, cur
        stage_out(P, C, L, cur)
        P.stack = st0
        C.nins = P.nins
    return nc, consts, C


def kernel(**inputs):
    x = np.asarray(inputs['x'], dtype=np.float32)
    B, L, _ = x.shape
    shapes = {'x': (L, D), 'c': (D,)}
    for nm in INPUT_NAMES:
        shapes[nm] = tuple(np.asarray(inputs[nm]).shape)
    nc, consts, C = build(shapes, L)
    in_maps = []
    shared = {nm: np.ascontiguousarray(np.asarray(inputs[nm], dtype=np.float32)) for nm in INPUT_NAMES}
    for core in range(8):
        b = core % B
        m = dict(shared)
        m.update(consts)
        m['x'] = np.ascontiguousarray(x[b])
        m['c'] = np.ascontiguousarray(np.asarray(inputs['c'], dtype=np.float32)[b])
        in_maps.append(m)
    res = run_bass_kernel_spmd(nc, in_maps, core_ids=list(range(8)))
    out = np.stack([res.results[b]['out'] for b in range(B)], axis=0)
    return out.astype(np.float32)
```

```python
import contextlib
import math
import numpy as np
import concourse.bass as bass
import concourse.mybir as mybir
from concourse.bass_utils import run_bass_kernel_spmd

F32 = mybir.dt.float32
BF16 = mybir.dt.bfloat16
AF = mybir.ActivationFunctionType
ALU = mybir.AluOpType
AX = mybir.AxisListType

D = 1024
DEPTH = 4
EPS = 1e-6
DFF = 2816
NEXP = 8
ENGS = ['pe', 'act', 'dve', 'pool', 'sp']


class V:
    __slots__ = ('res', 'ap')

    def __init__(self, res, ap):
        self.res = res
        self.ap = ap

    def __getitem__(self, idx):
        return V(self.res, self.ap[idx])

    def bc(self, shape):
        return V(self.res, self.ap.to_broadcast(list(shape)))

    def re(self, s, **kw):
        return V(self.res, self.ap.rearrange(s, **kw))


class Res:
    __slots__ = ('name', 'w', 'r', 'ap')

    def __init__(self, name, ap=None):
        self.name = name
        self.w = {}
        self.r = {}
        self.ap = ap

    def __getitem__(self, idx):
        return V(self, self.ap[idx])

    @property
    def v(self):
        return V(self, self.ap)


def _rv(x):
    return x.v if isinstance(x, Res) else x


class Prog:
    def __init__(self, nc, stack, n_dma_sems=32):
        self.nc = nc
        self.stack = stack
        self.streams = {e: [] for e in ENGS}
        self.cnt = {e: 0 for e in ENGS}
        self.semh = {}
        for e in ['pe', 'act', 'dve', 'pool']:
            self.semh['c_' + e] = stack.enter_context(nc.semaphore('c_' + e))
        self.ndma = n_dma_sems
        self.dma_tot = [0] * n_dma_sems
        self.dma_next = 0
        for k in range(n_dma_sems):
            self.semh['d%d' % k] = stack.enter_context(nc.semaphore('d%d' % k))
        self.known = {e: {} for e in ENGS}
        self.nt = 0
        self.nins = 0

    def sb(self, shape, dtype=F32, name=None):
        self.nt += 1
        name = name or ('t%d' % self.nt)
        t = self.stack.enter_context(self.nc.sbuf_tensor(name, list(shape), dtype))
        return Res(name, t[:])

    def ps(self, shape, dtype=F32, name=None):
        self.nt += 1
        name = name or ('p%d' % self.nt)
        t = self.stack.enter_context(self.nc.psum_tensor(name, list(shape), dtype))
        return Res(name, t[:])

    def dram(self, name, shape, dtype, kind="Internal"):
        t = self.nc.dram_tensor(name, list(shape), dtype, kind=kind)
        return Res(name, t.ap())

    def _deps(self, eng, reads, writes):
        deps = {}

        def add(k, v):
            if deps.get(k, -1) < v:
                deps[k] = v
        for r in reads:
            for k, v in r.w.items():
                add(k, v)
        for w in writes:
            for k, v in w.w.items():
                add(k, v)
            for k, v in w.r.items():
                add(k, v)
        out = []
        kn = self.known[eng]
        for k, v in deps.items():
            if eng == 'pe' and k == 'c_pe':
                continue
            if kn.get(k, -1) < v:
                kn[k] = v
                out.append((k, v))
        return out

    def _record(self, ev, reads, writes, merge=False):
        k, v = ev
        for w in writes:
            if merge:
                w.w[k] = v
            else:
                w.w = {k: v}
            w.r = {}
        for r in reads:
            if r.r.get(k, -1) < v:
                r.r[k] = v

    def op(self, eng, fn, reads=(), writes=(), inc=True):
        reads = [x for x in reads if x is not None]
        waits = self._deps(eng, reads, writes)
        key = 'c_' + eng
        if inc:
            self.cnt[eng] += 1
            ev = (key, self.cnt[eng])
        else:
            ev = (key, self.cnt[eng] + 1)
        self.streams[eng].append((waits, fn, key if inc else None, 1))
        self._record(ev, reads, writes)
        self.nins += 1

    def dma(self, out, in_, eng='sp'):
        out = _rv(out)
        in_ = _rv(in_)
        k = self.dma_next
        self.dma_next = (k + 1) % self.ndma
        key = 'd%d' % k
        waits = self._deps(eng, [in_.res], [out.res])
        kn = self.known[eng]
        if kn.get(key, -1) < self.dma_tot[k]:
            kn[key] = self.dma_tot[k]
            waits.append((key, self.dma_tot[k]))
        self.dma_tot[k] += 16
        ev = (key, self.dma_tot[k])
        oa, ia = out.ap, in_.ap

        def fn(e):
            return e.dma_start(out=oa, in_=ia)
        self.streams[eng].append((waits, fn, key, 16))
        self._record(ev, [in_.res], [out.res], merge=True)
        self.nins += 1

    def barrier(self):
        allw = [('c_' + e, self.cnt[e]) for e in ['pe', 'act', 'dve', 'pool']]
        allw += [('d%d' % k, self.dma_tot[k]) for k in range(self.ndma)]
        for e in ENGS:
            kn = self.known[e]
            waits = []
            for k, v in allw:
                if e == 'pe' and k == 'c_pe':
                    continue
                if kn.get(k, -1) < v:
                    kn[k] = v
                    waits.append((k, v))
            self.streams[e].append((waits, None, None, 0))

    def flush(self):
        nc = self.nc
        engobj = {'pe': 'tensor', 'act': 'scalar', 'dve': 'vector', 'pool': 'gpsimd', 'sp': 'sync'}
        semh = self.semh
        with nc.allow_non_contiguous_dma(reason="small strided param loads"), nc.Block() as block:
            for e in ENGS:
                stream = self.streams[e]

                def body(eng, stream=stream):
                    for waits, fn, key, n in stream:
                        for k, v in waits:
                            eng.wait_ge(semh[k], v)
                        if fn is not None:
                            ins = fn(eng)
                            if key is not None:
                                ins.then_inc(semh[key], n)
                getattr(block, engobj[e])(body)
        self.streams = {e: [] for e in ENGS}

    def mm(self, out, lhsT, rhs, start=True, stop=True):
        out, lhsT, rhs = _rv(out), _rv(lhsT), _rv(rhs)
        o, l, r = out.ap, lhsT.ap, rhs.ap
        self.op('pe', lambda e: e.matmul(o, l, r, start=start, stop=stop),
                [lhsT.res, rhs.res], [out.res], inc=stop)

    def tr(self, out, in_, ident):
        out, in_, ident = _rv(out), _rv(in_), _rv(ident)
        o, i, d = out.ap, in_.ap, ident.ap
        self.op('pe', lambda e: e.transpose(o, i, d), [in_.res, ident.res], [out.res])

    def act(self, out, in_, func, bias=None, scale=None, eng='act'):
        out, in_ = _rv(out), _rv(in_)
        reads = [in_.res]
        kw = {}
        if bias is not None:
            if isinstance(bias, (V, Res)):
                bias = _rv(bias)
                reads.append(bias.res)
                kw['bias'] = bias.ap
            else:
                kw['bias'] = float(bias)
        if scale is not None:
            if isinstance(scale, (V, Res)):
                scale = _rv(scale)
                reads.append(scale.res)
                kw['scale'] = scale.ap
            else:
                kw['scale'] = float(scale)
        o, i = out.ap, in_.ap
        self.op(eng, lambda e: e.activation(o, i, func, **kw), reads, [out.res])

    def tt(self, out, a, b, op, eng='dve'):
        out, a, b = _rv(out), _rv(a), _rv(b)
        o, x, y = out.ap, a.ap, b.ap
        self.op(eng, lambda e: e.tensor_tensor(o, x, y, op), [a.res, b.res], [out.res])

    def ts(self, out, a, s1, op0, s2=None, op1=None, eng='dve'):
        out, a = _rv(out), _rv(a)
        reads = [a.res]

        def cv(s):
            if isinstance(s, (V, Res)):
                s = _rv(s)
                reads.append(s.res)
                return s.ap
            return None if s is None else float(s)
        c1, c2 = cv(s1), cv(s2)
        o, x = out.ap, a.ap
        if op1 is None:
            self.op(eng, lambda e: e.tensor_single_scalar(o, x, c1, op0), reads, [out.res])
        else:
            self.op(eng, lambda e: e.tensor_scalar(o, x, c1, c2, op0, op1), reads, [out.res])

    def stt(self, out, a, s, b, op0, op1):
        out, a, b = _rv(out), _rv(a), _rv(b)
        reads = [a.res, b.res]
        if isinstance(s, (V, Res)):
            s = _rv(s)
            reads.append(s.res)
            c = s.ap
        else:
            c = float(s)
        o, x, y = out.ap, a.ap, b.ap
        self.op('dve', lambda e: e.scalar_tensor_tensor(o, x, c, y, op0, op1), reads, [out.res])

    def copy(self, out, in_, eng='dve'):
        out, in_ = _rv(out), _rv(in_)
        o, i = out.ap, in_.ap
        if eng == 'act':
            self.op('act', lambda e: e.activation(o, i, AF.Copy), [in_.res], [out.res])
        else:
            self.op(eng, lambda e: e.tensor_copy(o, i), [in_.res], [out.res])

    def recip(self, out, in_):
        out, in_ = _rv(out), _rv(in_)
        o, i = out.ap, in_.ap
        self.op('dve', lambda e: e.reciprocal(o, i), [in_.res], [out.res])

    def memset(self, out, val, eng='dve'):
        out = _rv(out)
        o = out.ap
        self.op(eng, lambda e: e.memset(o, float(val)), [], [out.res])

    def scan(self, out, d0, d1, init, op0=ALU.mult, op1=ALU.add):
        out, d0, d1 = _rv(out), _rv(d0), _rv(d1)
        reads = [d0.res, d1.res]
        if isinstance(init, (V, Res)):
            init = _rv(init)
            reads.append(init.res)
            c = init.ap
        else:
            c = float(init)
        o, x, y = out.ap, d0.ap, d1.ap
        self.op('dve', lambda e: e.tensor_tensor_scan(o, x, y, c, op0, op1), reads, [out.res])

    def reduce(self, out, in_, op, axis=AX.X):
        out, in_ = _rv(out), _rv(in_)
        o, i = out.ap, in_.ap
        self.op('dve', lambda e: e.tensor_reduce(o, i, axis, op), [in_.res], [out.res])


class Ctx:
    pass


def rstd_from_ss(P, out_sb, ss_ps, scale, bias):
    P.act(out_sb, ss_ps, AF.Ln, bias=bias, scale=scale)
    P.act(out_sb, out_sb, AF.Exp, scale=-0.5)


def stage_prep(P, C, L):
    nc = P.nc
    I = C.inp
    with contextlib.ExitStack() as st:
        P.stack = st
        cT = P.sb([128, 8])
        with nc.allow_non_contiguous_dma(reason="tiny"):
            P.dma(cT, I['c'].v.re("(k p) -> p k", p=128))
        cond = P.sb([128, 8])
        P.act(cond, cT, AF.Silu)
        wbuf = [P.sb([128, 8, 768]) for _ in range(2)]
        pm = C.PS[0]
        for l in range(C.depth):
            bt = P.sb([128, 48])
            with nc.allow_non_contiguous_dma(reason="tiny"):
                P.dma(bt, I['ada_b'][l].re("(j p) -> p j", p=128))
            for cb in range(8):
                wb = wbuf[cb % 2]
                P.dma(wb, I['ada_w'][l][:, cb * 768:(cb + 1) * 768].re("(k p) m -> p k m", p=128))
                for j in range(6):
                    col = cb * 6 + j
                    for k in range(8):
                        P.mm(pm[:, col:col + 1], wb[:, k, j * 128:(j + 1) * 128], cond[:, k:k + 1],
                             start=(k == 0), stop=(k == 7))
            mod = C.mod[l]
            P.tt(mod, pm[:, 0:48], bt, ALU.add)
            nw = P.sb([128, 16])
            with nc.allow_non_contiguous_dma(reason="tiny"):
                P.dma(nw[:, 0:8], I['norm_mix_w'][l].re("(k p) -> p k", p=128))
                P.dma(nw[:, 8:16], I['norm_ffn_w'][l].re("(k p) -> p k", p=128))
            A = C.modA[l]
            P.stt(A[:, 0:8], mod[:, 8:16], 1.0, nw[:, 0:8], ALU.add, ALU.mult)
            P.stt(A[:, 8:16], mod[:, 32:40], 1.0, nw[:, 8:16], ALU.add, ALU.mult)
        xin = [P.sb([128, 1024]) for _ in range(2)]
        xo = [P.sb([128, 8, 512]) for _ in range(2)]
        for tt in range(L // 512):
            o = xo[tt % 2]
            for s in range(4):
                xi = xin[s % 2]
                t0 = tt * 512 + s * 128
                P.dma(xi, I['x'][t0:t0 + 128, :])
                for k in range(8):
                    pt = C.PS[1 + (k % 4)]
                    P.tr(pt[:, 0:128], xi[:, k * 128:(k + 1) * 128], C.ident)
                    P.copy(o[:, k, s * 128:(s + 1) * 128], pt[:, 0:128], eng=('act' if k % 2 else 'dve'))
            P.dma(C.xA.v.re("(k p) t -> p k t", p=128)[:, :, tt * 512:(tt + 1) * 512], o)
        P.barrier()
        P.flush()


def stage_out(P, C, L, xsrc):
    I = C.inp
    with contextlib.ExitStack() as st:
        P.stack = st
        xi = [P.sb([128, 8, 512]) for _ in range(2)]
        xo = [P.sb([128, 1024]) for _ in range(2)]
        n = 0
        for tt in range(L // 512):
            t = xi[tt % 2]
            P.dma(t, xsrc.v.re("(k p) t -> p k t", p=128)[:, :, tt * 512:(tt + 1) * 512])
            for s in range(4):
                o = xo[s % 2]
                for k in range(8):
                    pt = C.PS[1 + (k % 4)]
                    P.tr(pt[:, 0:128], t[:, k, s * 128:(s + 1) * 128], C.ident)
                    P.copy(o[:, k * 128:(k + 1) * 128], pt[:, 0:128], eng=('act' if k % 2 else 'dve'))
                t0 = tt * 512 + s * 128
                P.dma(C.out[t0:t0 + 128, :], o)
        P.barrier()
        P.flush()


def load_norm_h(P, C, S, xt, l, which, want32=False):
    A = C.modA[l]
    mod = C.mod[l]
    aoff = 0 if which == 1 else 8
    shoff = 0 if which == 1 else 24
    sq = S.sq
    P.act(sq, xt, AF.Square)
    ss = C.PS[0]
    for k in range(8):
        P.mm(ss, C.ones, sq[:, k, :], start=(k == 0), stop=(k == 7))
    rstd = S.rstd
    rstd_from_ss(P, rstd, ss, 1.0 / D, EPS)
    for k in range(8):
        tmp = S.tmp[k % 2]
        P.stt(tmp, xt[:, k, :], A[:, aoff + k:aoff + k + 1], rstd, ALU.mult, ALU.mult)
        if want32:
            P.act(S.h32[:, k, :], tmp, AF.Identity, bias=mod[:, shoff + k:shoff + k + 1])
            P.copy(S.hb[:, k, :], S.h32[:, k, :], eng='pool')
        else:
            P.act(S.hb[:, k, :], tmp, AF.Identity, bias=mod[:, shoff + k:shoff + k + 1])


def wslab_iter(P, C, S, Wv, ncols, KT, slab=512):
    c0 = 0
    i = 0
    while c0 < ncols:
        w = min(slab, ncols - c0)
        buf = S.wb[S.wbi % len(S.wb)]
        S.wbi += 1
        P.dma(buf[:, 0:KT, 0:w], Wv[:, c0:c0 + w].re("(k p) m -> p k m", p=128), eng='pool')
        yield buf, c0, w
        c0 += w
        i += 1


def stage_even_proj(P, C, L, l, xsrc):
    nc = P.nc
    I = C.inp
    i = l // 2
    Win = I['even_w_in'][i]
    with contextlib.ExitStack() as st:
        P.stack = st
        S = Ctx()
        S.sq = P.sb([128, 8, 512])
        S.rstd = P.sb([128, 512])
        S.tmp = [P.sb([128, 512]) for _ in range(2)]
        S.hb = P.sb([128, 8, 512], BF16)
        S.wb = [P.sb([128, 8, 512], BF16) for _ in range(3)]
        S.wbi = 0
        C.cmask = P.sb([4, 512])
        P.dma(C.cmask, C.inp['c_cmask'])
        xts = [P.sb([128, 8, 512]) for _ in range(2)]
        pre = [P.sb([128, 515]) for _ in range(12)]
        for m in range(12):
            P.memset(pre[m][:, 0:3], 0.0)
        cw = P.sb([128, 12, 4])
        with nc.allow_non_contiguous_dma(reason="tiny"):
            for j in range(4):
                P.dma(cw[:, :, j], I['even_conv_w'][i][j].re("(t p) -> p t", p=128))
        wba = P.sb([128, 8, 8])
        with nc.allow_non_contiguous_dma(reason="small"):
            P.dma(wba, Win[:, 2048:2056].re("(k p) m -> p k m", p=128))
        hc = P.sb([4, 2])
        with nc.allow_non_contiguous_dma(reason="tiny"):
            P.dma(hc[:, 0:1], I['even_a_log'][i].re("(h o) -> h o", o=1))
            P.dma(hc[:, 1:2], I['even_dt_bias'][i].re("(h o) -> h o", o=1))
        nA = P.sb([4, 1])
        P.act(nA, hc[:, 0:1], AF.Exp)
        P.ts(nA, nA, -1.0, ALU.mult)
        h32 = P.sb([128, 8, 512])
        S.h32 = h32
        acc = [P.sb([128, 512]) for _ in range(2)]
        ob = [P.sb([128, 512]) for _ in range(3)]
        sm = [P.sb([4, 512]) for _ in range(6)]
        NT = L // 512
        xv = xsrc.v.re("(k p) t -> p k t", p=128)
        P.dma(xts[0], xv[:, :, 0:512])
        nob = 0
        for tt in range(NT):
            xt = xts[tt % 2]
            if tt + 1 < NT:
                P.dma(xts[(tt + 1) % 2], xv[:, :, (tt + 1) * 512:(tt + 2) * 512])
            load_norm_h(P, C, S, xt, l, 1, want32=True)
            tsl = slice(tt * 512, (tt + 1) * 512)
            pb, pa = C.PS[1], C.PS[2]
            for k in range(8):
                P.mm(pb[0:4, :], wba[:, k, 0:4], h32[:, k, :], start=(k == 0), stop=(k == 7))
            for k in range(8):
                P.mm(pa[0:4, :], wba[:, k, 4:8], h32[:, k, :], start=(k == 0), stop=(k == 7))
            beta = sm[0]
            P.act(beta[:], pb[0:4, :], AF.Exp, scale=-1.0)
            P.ts(beta, beta, 1.0, ALU.add)
            P.recip(beta, beta)
            P.dma(C.betaT[:, tsl], beta)
            xa = sm[1]
            P.act(xa[:], pa[0:4, :], AF.Identity, bias=hc[:, 1:2])
            ax = sm[2]
            P.stt(ax, xa, -1.0, xa, ALU.mult, ALU.max)
            P.act(ax, ax, AF.Exp, scale=-1.0)
            P.act(ax, ax, AF.Ln, bias=1.0)
            sp = sm[3]
            P.stt(sp, xa, 0.0, ax, ALU.max, ALU.add)
            g = sm[4]
            P.ts(g, sp, nA[:, 0:1], ALU.mult)
            gc = sm[5]
            P.scan(gc, C.cmask, g, 0.0)
            P.dma(C.gT[:, tsl], gc)
            mt = 0
            for wsl, c0, w in wslab_iter(P, C, S, Win, 2048, 8):
                for j in range(w // 128):
                    m = (c0 // 128) + j
                    pp = C.PS[3 + (m % 4)]
                    for k in range(8):
                        P.mm(pp, wsl[:, k, j * 128:(j + 1) * 128], S.hb[:, k, :], start=(k == 0), stop=(k == 7))
                    if m < 12:
                        pr = pre[m]
                        P.copy(pr[:, 3:515], pp, eng='act')
                        a = acc[m % 2]
                        P.ts(a, pr[:, 0:512], cw[:, m, 0:1], ALU.mult)
                        for jj in range(1, 4):
                            P.stt(a, pr[:, jj:jj + 512], cw[:, m, jj:jj + 1], a, ALU.mult, ALU.add)
                        P.copy(pr[:, 0:3], pr[:, 512:515], eng='pool')
                        o = ob[nob % 3]
                        nob += 1
                        P.act(o, a, AF.Silu)
                        if m < 8:
                            sq = S.tmp[m % 2]
                            P.act(sq, o, AF.Square)
                            ss = C.PS[7]
                            P.mm(ss, C.ones, sq)
                            rs = S.rstd
                            if m < 4:
                                rstd_from_ss(P, rs, ss, 128.0, 128.0 * EPS)
                            else:
                                rstd_from_ss(P, rs, ss, 1.0, EPS)
                            P.tt(o, o, rs, ALU.mult)
                        P.dma(C.qkvT[m * 128:(m + 1) * 128, tsl], o)
                    else:
                        o = ob[nob % 3]
                        nob += 1
                        P.act(o, pp, AF.Silu)
                        P.dma(C.zT[(m - 12) * 128:(m - 11) * 128, tsl], o)
            for wsl, c0, w in wslab_iter(P, C, S, Win[:, 2056:2568], 512, 8):
                for j in range(4):
                    pp = C.PS[3 + (j % 4)]
                    for k in range(8):
                        P.mm(pp, wsl[:, k, j * 128:(j + 1) * 128], S.hb[:, k, :], start=(k == 0), stop=(k == 7))
                    o = ob[nob % 3]
                    nob += 1
                    P.copy(o, pp, eng='act')
                    P.dma(C.uT[j * 128:(j + 1) * 128, tsl], o)
        P.barrier()
        P.flush()


def stage_gdn(P, C, L, l):
    nc = P.nc
    I = C.inp
    i = l // 2
    NT = L // 512
    with contextlib.ExitStack() as st:
        P.stack = st
        C.gmask = P.sb([64, 4, 512])
        P.dma(C.gmask, C.inp['c_gmask'])
        Sst = [P.sb([128, 128]) for _ in range(4)]
        for h in range(4):
            P.memset(Sst[h], 0.0)
        gw = P.sb([128, 1])
        with nc.allow_non_contiguous_dma(reason="tiny"):
            P.dma(gw, I['even_gdn_norm_w'][i].re("(p o) -> p o", o=1))
        mk = lambda n, shape, dt=F32: [P.sb(shape, dt) for _ in range(n)]
        qT, kT, vT = mk(2, [128, 512]), mk(2, [128, 512]), mk(2, [128, 512])
        rows = mk(2, [1, 2, 512])
        r_eg, r_ekl, r_ng = mk(2, [1, 512]), mk(2, [1, 512]), mk(2, [1, 512])
        bcs = mk(2, [128, 3, 512])
        kb, kbe, vb, qe, kel = (mk(2, [128, 512]) for _ in range(5))
        tokm = mk(2, [64, 3, 8, 128])
        EL, EU, t1 = mk(2, [64, 512]), mk(2, [64, 512]), mk(2, [64, 512])
        Am, ATm, PTm, Rm = mk(2, [64, 512]), mk(2, [64, 512]), mk(2, [64, 512]), mk(2, [64, 512])
        WTn = mk(2, [128, 512])
        vnew = mk(3, [64, 128])
        zt = mk(2, [128, 512])
        osb = mk(2, [128, 512])
        sq = mk(2, [128, 512])
        rs = mk(2, [128, 512])
        it = 0
        for tt in range(NT):
            tsl = slice(tt * 512, (tt + 1) * 512)
            for h in range(4):
                b = it % 2
                it += 1
                P.dma(qT[b], C.qkvT[h * 128:(h + 1) * 128, tsl])
                P.dma(kT[b], C.qkvT[512 + h * 128:512 + (h + 1) * 128, tsl])
                P.dma(vT[b], C.qkvT[1024 + h * 128:1024 + (h + 1) * 128, tsl])
                P.dma(rows[b][:, 0, :], C.betaT[h:h + 1, tsl])
                P.dma(rows[b][:, 1, :], C.gT[h:h + 1, tsl])
                P.dma(zt[b], C.zT[h * 128:(h + 1) * 128, tsl])
                gc = rows[b][:, 1, :]
                P.act(r_eg[b], gc, AF.Exp)
                g3 = rows[b][:, 1, :].re("o (c j) -> o c j", j=64)
                P.tt(r_ekl[b].v.re("o (c j) -> o c j", j=64), g3[:, :, 63:64].bc([1, 8, 64]), g3, ALU.subtract)
                P.act(r_ekl[b], r_ekl[b], AF.Exp)
                P.ts(r_ng[b], gc, -1.0, ALU.mult)
                pbc = [C.PS[0], C.PS[1], C.PS[2]]
                P.mm(pbc[0], C.ones[0:1, :], rows[b][:, 0, :])
                P.mm(pbc[1], C.ones[0:1, :], r_eg[b])
                P.mm(pbc[2], C.ones[0:1, :], r_ekl[b])
                for j in range(3):
                    P.copy(bcs[b][:, j, :], pbc[j], eng='act')
                P.tt(kb[b], kT[b], bcs[b][:, 0, :], ALU.mult)
                P.tt(kbe[b], kb[b], bcs[b][:, 1, :], ALU.mult, eng='pool')
                P.tt(vb[b], vT[b], bcs[b][:, 0, :], ALU.mult)
                P.tt(qe[b], qT[b], bcs[b][:, 1, :], ALU.mult, eng='pool')
                P.tt(kel[b], kT[b], bcs[b][:, 2, :], ALU.mult)
                for c in range(8):
                    csl = slice(c * 64, (c + 1) * 64)
                    for j, src in enumerate((vb[b], kbe[b], kel[b])):
                        pt = C.PS[3 + ((c * 3 + j) % 2)]
                        P.tr(pt[0:64, 0:128], src[:, csl], C.ident)
                        P.copy(tokm[b][:, j, c, :], pt[0:64, 0:128], eng=('act' if j != 1 else 'dve'))
                pg = C.PS[5]
                for c in range(8):
                    csl = slice(c * 64, (c + 1) * 64)
                    P.mm(pg[0:64, csl], rows[b][:, 1, csl], C.ones[0:1, 0:64], start=True, stop=False)
                    P.mm(pg[0:64, csl], C.ones[0:1, 0:64], r_ng[b][:, csl], start=False, stop=True)
                P.ts(t1[b], pg[0:64, :], 0.0, ALU.min)
                P.act(EL[b], t1[b], AF.Exp)
                P.ts(t1[b], pg[0:64, :], 0.0, ALU.max)
                P.act(EU[b], t1[b], AF.Exp, scale=-1.0)
                pA, pAT, pPT = C.PS[0], C.PS[1], C.PS[2]
                for c in range(8):
                    csl = slice(c * 64, (c + 1) * 64)
                    P.mm(pA[0:64, csl], kb[b][:, csl], kT[b][:, csl])
                    P.mm(pAT[0:64, csl], kT[b][:, csl], kb[b][:, csl])
                    P.mm(pPT[0:64, csl], kT[b][:, csl], qT[b][:, csl])
                P.tt(t1[b], EL[b], C.gmask[:, 0, :], ALU.mult, eng='pool')
                P.tt(Am[b], pA[0:64, :], t1[b], ALU.mult)
                P.tt(EL[b], EU[b], C.gmask[:, 1, :], ALU.mult, eng='pool')
                P.tt(ATm[b], pAT[0:64, :], EL[b], ALU.mult)
                P.tt(EU[b], EU[b], C.gmask[:, 2, :], ALU.mult, eng='pool')
                P.tt(PTm[b], pPT[0:64, :], EU[b], ALU.mult)
                P.tt(Rm[b], ATm[b], C.gmask[:, 3, :], ALU.add)
                X, XT = Am[b], ATm[b]
                X2, XT2 = t1[b], EL[b]
                for step in range(5):
                    p1, p2, p3 = C.PS[3], C.PS[4], C.PS[5]
                    for c in range(8):
                        csl = slice(c * 64, (c + 1) * 64)
                        P.mm(p1[0:64, csl], XT[:, csl], X[:, csl])
                        P.mm(p2[0:64, csl], X[:, csl], XT[:, csl])
                    P.copy(X2, p1[0:64, :], eng='act')
                    P.copy(XT2, p2[0:64, :], eng='dve')
                    X, X2 = X2, X
                    XT, XT2 = XT2, XT
                    for c in range(8):
                        csl = slice(c * 64, (c + 1) * 64)
                        P.mm(p3[0:64, csl], X[:, csl], Rm[b][:, csl])
                    P.tt(Rm[b], Rm[b], p3[0:64, :], ALU.add)
                pW = C.PS[6]
                for c in range(8):
                    csl = slice(c * 64, (c + 1) * 64)
                    P.mm(pW[:, csl], tokm[b][:, 1, c, :], Rm[b][:, csl])
                P.act(WTn[b], pW, AF.Copy, scale=-1.0)
                po = C.PS[7]
                S = Sst[h]
                for c in range(8):
                    csl = slice(c * 64, (c + 1) * 64)
                    pv = C.PS[3 + (c % 2)]
                    P.mm(pv[0:64, 0:128], Rm[b][:, csl], tokm[b][:, 0, c, :], start=True, stop=False)
                    P.mm(pv[0:64, 0:128], WTn[b][:, csl], S, start=False, stop=True)
                    vn = vnew[c % 3]
                    P.copy(vn, pv[0:64, 0:128], eng='act')
                    P.mm(po[:, csl], S, qe[b][:, csl], start=True, stop=False)
                    P.mm(po[:, csl], vn, PTm[b][:, csl], start=False, stop=True)
                    ps_ = C.PS[5]
                    P.mm(ps_[:, 0:128], tokm[b][:, 2, c, :], vn)
                    P.stt(S, S, bcs[b][:, 1, c * 64 + 63:c * 64 + 64], ps_[:, 0:128], ALU.mult, ALU.add)
                P.copy(osb[b], po, eng='act')
                P.act(sq[b], osb[b], AF.Square)
                pss = C.PS[6]
                P.mm(pss, C.ones, sq[b])
                rstd_from_ss(P, rs[b], pss, 1.0 / 128, EPS)
                P.stt(osb[b], osb[b], gw[:, 0:1], rs[b], ALU.mult, ALU.mult)
                P.tt(osb[b], osb[b], zt[b], ALU.mult)
                P.dma(C.yT[h * 128:(h + 1) * 128, tsl], osb[b])
        P.barrier()
        P.flush()


def stage_s5(P, C, L, l):
    nc = P.nc
    I = C.inp
    i = l // 2
    NT = L // 512
    TWO_PI = 2.0 * math.pi
    with contextlib.ExitStack() as st:
        P.stack = st
        lr = P.sb([128, 16])
        li = P.sb([128, 16])
        ls = P.sb([128, 16])
        with nc.allow_non_contiguous_dma(reason="small"):
            P.dma(lr, I['even_lam_re'][i].re("(j g) n -> (g n) j", g=2))
            P.dma(li, I['even_lam_im'][i].re("(j g) n -> (g n) j", g=2))
            lsv = I['even_log_step'][i].re("(j g) -> g j", g=2)
            for g2 in range(2):
                P.dma(ls[g2 * 64:(g2 + 1) * 64, :], lsv[g2:g2 + 1, :].bc([64, 16]))
        dt = P.sb([128, 16])
        P.act(dt, ls, AF.Exp)
        P.ts(lr, lr, -1e-4, ALU.min)
        rho = P.sb([128, 16])
        P.tt(rho, lr, dt, ALU.mult)
        P.act(rho, rho, AF.Exp)
        th = P.sb([128, 16])
        P.tt(th, li, dt, ALU.mult)
        tmpa = P.sb([128, 16])
        sn = P.sb([128, 16])
        cs = P.sb([128, 16])
        P.act(sn, th, AF.Sin, scale=1.0 / 16)
        P.act(cs, th, AF.Sin, scale=1.0 / 16, bias=C.halfpi[:, 0:1])
        t_c2, t_s2 = P.sb([128, 16]), P.sb([128, 16])
        for _ in range(4):
            P.tt(t_c2, cs, cs, ALU.mult)
            P.tt(t_s2, sn, sn, ALU.mult)
            P.tt(sn, cs, sn, ALU.mult)
            P.ts(sn, sn, 2.0, ALU.mult)
            P.tt(cs, t_c2, t_s2, ALU.subtract)
        ar, ai = P.sb([128, 16]), P.sb([128, 16])
        P.tt(ar, rho, cs, ALU.mult)
        P.tt(ai, rho, sn, ALU.mult)
        nr = P.sb([128, 16])
        P.ts(nr, ar, -1.0, ALU.add)
        den = P.sb([128, 16])
        t2 = P.sb([128, 16])
        P.tt(den, lr, lr, ALU.mult)
        P.tt(t2, li, li, ALU.mult)
        P.tt(den, den, t2, ALU.add)
        P.recip(den, den)
        cr, ci = P.sb([128, 16]), P.sb([128, 16])
        P.tt(cr, nr, lr, ALU.mult)
        P.tt(t2, ai, li, ALU.mult)
        P.tt(cr, cr, t2, ALU.add)
        P.tt(cr, cr, den, ALU.mult)
        P.tt(ci, ai, lr, ALU.mult)
        P.tt(t2, nr, li, ALU.mult)
        P.tt(ci, ci, t2, ALU.subtract)
        P.tt(ci, ci, den, ALU.mult)
        nsn = P.sb([128, 16])
        P.ts(nsn, sn, -1.0, ALU.mult)
        Tc = P.sb([128, 16, 512])
        Ts = P.sb([128, 16, 512])
        P.memset(Tc[:, :, 0:1], 1.0)
        P.memset(Ts[:, :, 0:1], 0.0)
        cc, s_ = P.sb([128, 16]), P.sb([128, 16])
        P.copy(cc, cs)
        P.copy(s_, sn)
        ta, tb = P.sb([128, 256]), P.sb([128, 256])
        c2, s2 = P.sb([128, 16]), P.sb([128, 16])
        span = 1
        while span < 512:
            for j in range(16):
                P.ts(ta[:, 0:span], Ts[:, j, 0:span], s_[:, j:j + 1], ALU.mult)
                P.stt(Tc[:, j, span:2 * span], Tc[:, j, 0:span], cc[:, j:j + 1], ta[:, 0:span], ALU.mult, ALU.subtract)
                P.ts(tb[:, 0:span], Tc[:, j, 0:span], s_[:, j:j + 1], ALU.mult)
                P.stt(Ts[:, j, span:2 * span], Ts[:, j, 0:span], cc[:, j:j + 1], tb[:, 0:span], ALU.mult, ALU.add)
            P.tt(c2, cc, cc, ALU.mult)
            P.tt(s2, s_, s_, ALU.mult)
            P.tt(s_, cc, s_, ALU.mult)
            P.ts(s_, s_, 2.0, ALU.mult)
            P.tt(cc, c2, s2, ALU.subtract)
            span *= 2
        BreT = [P.sb([128, 128]) for _ in range(16)]
        BimT = [P.sb([128, 128]) for _ in range(16)]
        CrT = [P.sb([128, 128]) for _ in range(16)]
        CiT = [P.sb([128, 128]) for _ in range(16)]
        pad = [P.sb([128, 128]) for _ in range(4)]
        craw = [P.sb([128, 128]) for _ in range(2)]
        for j in range(16):
            kt = j // 4
            for which, (nm, dst) in enumerate((('even_b_re', BreT), ('even_b_im', BimT))):
                pd = pad[which]
                P.memset(pd, 0.0)
                for g2 in range(2):
                    g = 2 * j + g2
                    off = (g - 8 * kt) * 16
                    P.dma(pd[g2 * 64:(g2 + 1) * 64, off:off + 16], I[nm][i][g])
                pt = C.PS[which]
                P.tr(pt[:, 0:128], pd, C.ident)
                P.copy(dst[j], pt[:, 0:128], eng='act')
            for which, nm in enumerate(('even_c_re', 'even_c_im')):
                pd = pad[2 + which]
                P.memset(pd, 0.0)
                for g2 in range(2):
                    g = 2 * j + g2
                    off = (g - 8 * kt) * 16
                    P.dma(pd[off:off + 16, g2 * 64:(g2 + 1) * 64], I[nm][i][g])
                pt = C.PS[2 + which]
                P.tr(pt[:, 0:128], pd, C.ident)
                P.copy(craw[which], pt[:, 0:128], eng='act')
            P.ts(pad[0], craw[1], ci[:, j:j + 1], ALU.mult)
            P.stt(CrT[j], craw[0], cr[:, j:j + 1], pad[0], ALU.mult, ALU.subtract)
            P.ts(pad[1], craw[1], cr[:, j:j + 1], ALU.mult)
            P.stt(CiT[j], craw[0], ci[:, j:j + 1], pad[1], ALU.mult, ALU.add)
            P.ts(CiT[j], CiT[j], -1.0, ALU.mult)
        dsk = P.sb([128, 4])
        glb = P.sb([128, 8])
        with nc.allow_non_contiguous_dma(reason="tiny"):
            P.dma(dsk, I['even_d_skip'][i].re("(k p) -> p k", p=128))
            P.dma(glb, I['even_glu_b'][i].re("(k p) -> p k", p=128))
        nglb = P.sb([128, 8])
        P.ts(nglb, glb, -1.0, ALU.mult)
        gluw = P.sb([128, 4, 1024], BF16)
        P.dma(gluw, I['even_glu_w'][i].re("(k p) m -> p k m", p=128), eng='pool')
        ini = [P.sb([128, 2]) for _ in range(16)]
        for j in range(16):
            P.memset(ini[j], 0.0)
        uts = [P.sb([128, 4, 512]) for _ in range(2)]
        bu = [P.sb([128, 2, 512])] * 2
        zin = [P.sb([128, 2, 512])] * 2
        zz = [P.sb([128, 2, 512])] * 2
        xx = [P.sb([128, 2, 512]) for _ in range(2)]
        w1 = [P.sb([128, 512]) for _ in range(4)]
        sml = [P.sb([128, 2]) for _ in range(2)]
        yg = P.sb([128, 4, 512], BF16)
        ysb = [P.sb([128, 512]) for _ in range(2)]
        ga = [P.sb([128, 512]) for _ in range(2)]
        gb = [P.sb([128, 512]) for _ in range(2)]
        uv = C.uT.v.re("(k p) t -> p k t", p=128)
        P.dma(uts[0], uv[:, :, 0:512])
        it = 0
        for tt in range(NT):
            tsl = slice(tt * 512, (tt + 1) * 512)
            ut = uts[tt % 2]
            if tt + 1 < NT:
                P.dma(uts[(tt + 1) % 2], uv[:, :, (tt + 1) * 512:(tt + 2) * 512])
            for kt in range(4):
                py = C.PS[4 + (kt % 2)]
                for jj in range(4):
                    j = kt * 4 + jj
                    b = it % 2
                    it += 1
                    pr, pi_ = C.PS[0 + 2 * b], C.PS[1 + 2 * b]
                    P.mm(pr, BreT[j], ut[:, kt, :])
                    P.mm(pi_, BimT[j], ut[:, kt, :])
                    P.copy(bu[b][:, 0, :], pr, eng='act')
                    P.copy(bu[b][:, 1, :], pi_, eng='act')
                    P.tt(w1[0], bu[b][:, 0, :], Tc[:, j, :], ALU.mult)
                    P.tt(w1[1], bu[b][:, 1, :], Ts[:, j, :], ALU.mult, eng='pool')
                    P.tt(zin[b][:, 0, :], w1[0], w1[1], ALU.add)
                    P.tt(w1[2], bu[b][:, 1, :], Tc[:, j, :], ALU.mult, eng='pool')
                    P.tt(w1[3], bu[b][:, 0, :], Ts[:, j, :], ALU.mult)
                    P.tt(zin[b][:, 1, :], w1[2], w1[3], ALU.subtract, eng='pool')
                    P.scan(zz[b][:, 0, :], rho[:, j:j + 1].bc([128, 512]), zin[b][:, 0, :], ini[j][:, 0:1])
                    P.scan(zz[b][:, 1, :], rho[:, j:j + 1].bc([128, 512]), zin[b][:, 1, :], ini[j][:, 1:2])
                    P.tt(w1[0], zz[b][:, 0, :], Tc[:, j, :], ALU.mult)
                    P.tt(w1[1], zz[b][:, 1, :], Ts[:, j, :], ALU.mult, eng='pool')
                    P.tt(xx[b][:, 0, :], w1[0], w1[1], ALU.subtract)
                    P.tt(w1[2], zz[b][:, 1, :], Tc[:, j, :], ALU.mult, eng='pool')
                    P.tt(w1[3], zz[b][:, 0, :], Ts[:, j, :], ALU.mult)
                    P.tt(xx[b][:, 1, :], w1[2], w1[3], ALU.add, eng='pool')
                    sm_ = sml[b]
                    P.ts(sm_[:, 0:1], xx[b][:, 1, 511:512], nsn[:, j:j + 1], ALU.mult)
                    P.ts(sm_[:, 1:2], xx[b][:, 0, 511:512], sn[:, j:j + 1], ALU.mult)
                    P.stt(ini[j][:, 0:1], xx[b][:, 0, 511:512], cs[:, j:j + 1], sm_[:, 0:1], ALU.mult, ALU.add)
                    P.stt(ini[j][:, 1:2], xx[b][:, 1, 511:512], cs[:, j:j + 1], sm_[:, 1:2], ALU.mult, ALU.add)
                    P.mm(py, CrT[j], xx[b][:, 0, :], start=(jj == 0), stop=False)
                    P.mm(py, CiT[j], xx[b][:, 1, :], start=False, stop=(jj == 3))
                y = ysb[kt % 2]
                P.stt(y, ut[:, kt, :], dsk[:, kt:kt + 1], py, ALU.mult, ALU.add)
                P.act(yg[:, kt, :], y, AF.Gelu)
            for m in range(4):
                pa, pb = C.PS[6], C.PS[7]
                for k in range(4):
                    P.mm(pa, gluw[:, k, m * 128:(m + 1) * 128], yg[:, k, :], start=(k == 0), stop=(k == 3))
                for k in range(4):
                    P.mm(pb, gluw[:, k, 512 + m * 128:512 + (m + 1) * 128], yg[:, k, :], start=(k == 0), stop=(k == 3))
                a_, b_ = ga[m % 2], gb[m % 2]
                P.act(a_, pa, AF.Identity, bias=glb[:, m:m + 1])
                P.act(b_, pb, AF.Exp, bias=nglb[:, 4 + m:5 + m], scale=-1.0)
                P.ts(b_, b_, 1.0, ALU.add)
                P.recip(b_, b_)
                P.tt(a_, a_, b_, ALU.mult)
                P.dma(C.yT[512 + m * 128:512 + (m + 1) * 128, tsl], a_)
        P.barrier()
        P.flush()


def stage_ffn(P, C, L, l, xsrc, xdst, ysrc, wout, moe, G=2):
    nc = P.nc
    I = C.inp
    i = l // 2
    NT = L // 512
    G = min(G, NT)
    NG = NT // G
    mod = C.mod[l]
    with contextlib.ExitStack() as st:
        P.stack = st
        S = Ctx()
        faccs = [P.sb([128, 8, 512]) for _ in range(G)]
        S.sq = faccs[0]
        S.rstd = P.sb([128, 512])
        S.tmp = [P.sb([128, 512]) for _ in range(2)]
        hbs = [P.sb([128, 8, 512], BF16) for _ in range(G)]
        S.h32 = P.sb([128, 8, 512]) if moe else None
        S.wb = [P.sb([128, 8, 512], BF16) for _ in range(2)]
        S.wbi = 0
        w2b = [P.sb([128, 22, 128], BF16) for _ in range(2)]
        nw2 = 0
        xt = P.sb([128, 8, 512])
        ybf = P.sb([128, 8, 512], BF16)
        hids = [P.sb([128, 22, 512], BF16) for _ in range(G)]
        sa = [P.sb([128, 512]) for _ in range(3)]
        if moe:
            rw = P.sb([128, 8, 8])
            P.dma(rw, I['odd_router_w'][i].re("(k p) e -> p k e", p=128))
            lg = P.sb([8, 512])
            lt = P.sb([128, 4, 8])
            m1 = P.sb([128, 4])
            m2 = P.sb([128, 4])
            eq1 = P.sb([128, 4, 8])
            eq2 = P.sb([128, 4, 8])
            msk = P.sb([128, 4, 8])
            gg = P.sb([128, 4])
            g2_ = P.sb([128, 4])
            cmb = P.sb([128, 4, 8])
            cmbT = P.sb([8, 512])
            combs = [P.sb([128, 8, 512], BF16) for _ in range(G)]
        xv = xsrc.v.re("(k p) t -> p k t", p=128)
        yv = ysrc.v.re("(k p) t -> p k t", p=128)
        ov = xdst.v.re("(k p) t -> p k t", p=128)
        mv = C.xmid.v.re("(k p) t -> p k t", p=128)
        nsa = 0
        npp = 0
        for grp in range(NG):
            for g in range(G):
                tt = grp * G + g
                tsl = slice(tt * 512, (tt + 1) * 512)
                P.dma(xt, xv[:, :, tsl])
                P.dma(ybf, yv[:, :, tsl], eng='pool')
                for wsl, c0, w in wslab_iter(P, C, S, wout, 1024, 8):
                    for j in range(4):
                        m = c0 // 128 + j
                        pp = C.PS[1 + (m % 4)]
                        for k in range(8):
                            P.mm(pp, wsl[:, k, j * 128:(j + 1) * 128], ybf[:, k, :], start=(k == 0), stop=(k == 7))
                        P.stt(xt[:, m, :], pp, mod[:, 16 + m:17 + m], xt[:, m, :], ALU.mult, ALU.add)
                P.dma(mv[:, :, tsl], xt)
                S.hb = hbs[g]
                load_norm_h(P, C, S, xt, l, 2, want32=moe)
                if moe:
                    comb = combs[g]
                    pl = C.PS[5]
                    for k in range(8):
                        P.mm(pl[0:8, :], rw[:, k, :], S.h32[:, k, :], start=(k == 0), stop=(k == 7))
                    P.copy(lg, pl[0:8, :], eng='act')
                    pt = C.PS[6]
                    for s in range(4):
                        P.tr(pt[:, s * 8:(s + 1) * 8], lg[:, s * 128:(s + 1) * 128], C.ident[0:8, 0:8])
                    P.copy(lt, pt[:, 0:32].re("p (s e) -> p s e", e=8), eng='act')
                    P.reduce(m1, lt, ALU.max)
                    P.tt(eq1, lt, m1.v.re("p (s o) -> p s o", o=1).bc([128, 4, 8]), ALU.is_equal)
                    P.stt(msk, eq1, -1e30, lt, ALU.mult, ALU.add)
                    P.reduce(m2, msk, ALU.max)
                    P.tt(eq2, msk, m2.v.re("p (s o) -> p s o", o=1).bc([128, 4, 8]), ALU.is_equal)
                    P.tt(gg, m2, m1, ALU.subtract)
                    P.act(gg, gg, AF.Exp)
                    P.ts(gg, gg, 1.0, ALU.add)
                    P.recip(gg, gg)
                    P.ts(g2_, gg, -1.0, ALU.mult, 1.0, ALU.add)
                    P.tt(cmb, eq1, gg.v.re("p (s o) -> p s o", o=1).bc([128, 4, 8]), ALU.mult)
                    P.tt(eq2, eq2, g2_.v.re("p (s o) -> p s o", o=1).bc([128, 4, 8]), ALU.mult)
                    P.tt(cmb, cmb, eq2, ALU.add)
                    pc = C.PS[7]
                    for s in range(4):
                        P.tr(pc[0:8, s * 128:(s + 1) * 128], cmb[:, s, :], C.ident)
                    P.copy(cmbT, pc[0:8, :], eng='act')
                    for e in range(NEXP):
                        pb_ = C.PS[5 + (e % 2)]
                        P.mm(pb_, C.sel[:, e, :], cmbT)
                        P.copy(comb[:, e, :], pb_, eng='act')
            nexp = NEXP if moe else 1
            for e in range(nexp):
                if moe:
                    W13 = I['odd_expert_w13'][i][e]
                    W2 = I['odd_expert_w2'][i][e]
                else:
                    W13 = I['even_ffn_w13'][i]
                    W2 = I['even_ffn_w2'][i]
                for c0 in range(0, DFF, 256):
                    w = min(256, DFF - c0)
                    buf = S.wb[S.wbi % 2]
                    S.wbi += 1
                    P.dma(buf[:, :, 0:w], W13[:, c0:c0 + w].re("(k p) m -> p k m", p=128), eng='pool')
                    P.dma(buf[:, :, 256:256 + w], W13[:, DFF + c0:DFF + c0 + w].re("(k p) m -> p k m", p=128), eng='pool')
                    for j in range(w // 128):
                        f = c0 // 128 + j
                        for g in range(G):
                            pa, pb = C.PS[1 + 2 * (npp % 2)], C.PS[2 + 2 * (npp % 2)]
                            npp += 1
                            for k in range(8):
                                P.mm(pa, buf[:, k, j * 128:(j + 1) * 128], hbs[g][:, k, :], start=(k == 0), stop=(k == 7))
                            for k in range(8):
                                P.mm(pb, buf[:, k, 256 + j * 128:256 + (j + 1) * 128], hbs[g][:, k, :], start=(k == 0), stop=(k == 7))
                            s_ = sa[nsa % 3]
                            nsa += 1
                            P.act(s_, pa, AF.Silu)
                            if moe:
                                P.tt(s_, s_, combs[g][:, e, :], ALU.mult, eng='pool')
                            P.tt(hids[g][:, f, :], pb, s_, ALU.mult)
                for m in range(8):
                    wb2 = w2b[nw2 % 2]
                    nw2 += 1
                    P.dma(wb2, W2[:, m * 128:(m + 1) * 128].re("(f p) m -> p f m", p=128), eng='pool')
                    for g in range(G):
                        facc = faccs[g]
                        pp = C.PS[5 + ((m * G + g) % 3)]
                        for f in range(22):
                            P.mm(pp, wb2[:, f, :], hids[g][:, f, :], start=(f == 0), stop=(f == 21))
                        if e == 0:
                            P.copy(facc[:, m, :], pp, eng='act')
                        else:
                            P.tt(facc[:, m, :], pp, facc[:, m, :], ALU.add)
            for g in range(G):
                tt = grp * G + g
                tsl = slice(tt * 512, (tt + 1) * 512)
                P.dma(xt, mv[:, :, tsl])
                for m in range(8):
                    P.stt(faccs[g][:, m, :], faccs[g][:, m, :], mod[:, 40 + m:41 + m], xt[:, m, :], ALU.mult, ALU.add)
                P.dma(ov[:, :, tsl], faccs[g])
        P.barrier()
        P.flush()


def stage_attn_proj(P, C, L, l, xsrc):
    nc = P.nc
    I = C.inp
    i = l // 2
    NT = L // 512
    Wq = I['odd_w_qkv'][i]
    with contextlib.ExitStack() as st:
        P.stack = st
        S = Ctx()
        S.sq = P.sb([128, 8, 512])
        S.rstd = P.sb([128, 512])
        S.tmp = [P.sb([128, 512]) for _ in range(2)]
        S.hb = P.sb([128, 8, 512], BF16)
        S.h32 = None
        S.wb = [P.sb([128, 8, 512], BF16) for _ in range(3)]
        S.wbi = 0
        wv = P.sb([128, 8, 1024], BF16)
        P.dma(wv, Wq[:, 2048:3072].re("(k p) m -> p k m", p=128), eng='pool')
        nw = P.sb([128, 2])
        with nc.allow_non_contiguous_dma(reason="tiny"):
            for t in range(2):
                P.dma(nw[t * 64:(t + 1) * 64, 0:1], I['odd_q_norm_w'][i].re("(p o) -> p o", o=1))
                P.dma(nw[t * 64:(t + 1) * 64, 1:2], I['odd_k_norm_w'][i].re("(p o) -> p o", o=1))
        xts = [P.sb([128, 8, 512]) for _ in range(2)]
        raw = [P.sb([128, 512]) for _ in range(2)]
        sq = [P.sb([128, 512]) for _ in range(2)]
        rs = [P.sb([128, 512]) for _ in range(2)]
        ob = [P.sb([128, 512], BF16) for _ in range(3)]
        vb = [P.sb([128, 1024], BF16) for _ in range(2)]
        xv = xsrc.v.re("(k p) t -> p k t", p=128)
        P.dma(xts[0], xv[:, :, 0:512])
        n = 0
        for tt in range(NT):
            tsl = slice(tt * 512, (tt + 1) * 512)
            xt = xts[tt % 2]
            if tt + 1 < NT:
                P.dma(xts[(tt + 1) % 2], xv[:, :, (tt + 1) * 512:(tt + 2) * 512])
            load_norm_h(P, C, S, xt, l, 1)
            for wsl, c0, w in wslab_iter(P, C, S, Wq, 2048, 8):
                for j in range(4):
                    m = c0 // 128 + j
                    isq = m < 8
                    pp = C.PS[1 + (m % 4)]
                    for k in range(8):
                        P.mm(pp, wsl[:, k, j * 128:(j + 1) * 128], S.hb[:, k, :], start=(k == 0), stop=(k == 7))
                    r = raw[n % 2]
                    P.copy(r, pp, eng='act')
                    P.act(sq[n % 2], r, AF.Square)
                    pss = C.PS[5 + (n % 2)]
                    P.mm(pss, C.blk, sq[n % 2])
                    if isq:
                        rstd_from_ss(P, rs[n % 2], pss, 1.0, 64.0 * EPS)
                    else:
                        rstd_from_ss(P, rs[n % 2], pss, 1.0 / 64, EPS)
                    o = ob[n % 3]
                    P.stt(o, r, nw[:, (0 if isq else 1):(1 if isq else 2)], rs[n % 2], ALU.mult, ALU.mult)
                    P.dma(C.qkT[m * 128:(m + 1) * 128, tsl], o)
                    n += 1
            for s in range(4):
                v_ = vb[s % 2]
                for half in range(2):
                    pp = C.PS[1 + ((s * 2 + half) % 4)]
                    for k in range(8):
                        P.mm(pp, S.hb[:, k, s * 128:(s + 1) * 128], wv[:, k, half * 512:(half + 1) * 512],
                             start=(k == 0), stop=(k == 7))
                    P.copy(v_[:, half * 512:(half + 1) * 512], pp, eng=('act' if half else 'dve'))
                P.dma(C.vtok[tt * 512 + s * 128:tt * 512 + (s + 1) * 128, :], v_)
        P.barrier()
        P.flush()


def stage_attn(P, C, L, l):
    nc = P.nc
    I = C.inp
    i = l // 2
    NT = L // 512
    NK = L // 128
    lambda_init = 0.8 - 0.6 * math.exp(-0.3 * l)
    with contextlib.ExitStack() as st:
        P.stack = st
        lq = P.sb([128, 4, 64])
        for j, nm in enumerate(('odd_lambda_q1', 'odd_lambda_k1', 'odd_lambda_q2', 'odd_lambda_k2')):
            a = I[nm][i].re("(o d) -> o d", o=1)
            P.dma(lq[:, j, :], V(a.res, a.ap.to_broadcast([128, 64])))
        pr = P.sb([128, 2, 64])
        P.tt(pr[:, 0, :], lq[:, 0, :], lq[:, 1, :], ALU.mult)
        P.tt(pr[:, 1, :], lq[:, 2, :], lq[:, 3, :], ALU.mult)
        sm = P.sb([128, 2])
        P.reduce(sm, pr, ALU.add)
        P.act(sm, sm, AF.Exp)
        nlam = P.sb([128, 1])
        P.tt(nlam, sm[:, 1:2], sm[:, 0:1], ALU.subtract)
        P.ts(nlam, nlam, -lambda_init, ALU.add)
        sw = P.sb([128, 1])
        with nc.allow_non_contiguous_dma(reason="tiny"):
            P.dma(sw, I['odd_subln_w'][i].re("(p o) -> p o", o=1))
        P.ts(sw, sw, 1.0 - lambda_init, ALU.mult)
        kT = [P.sb([128, L], BF16) for _ in range(2)]
        vt = [P.sb([128, NK, 128], BF16) for _ in range(2)]
        qt_ = [P.sb([128, 512], BF16) for _ in range(2)]
        E = [P.sb([128, 512], BF16) for _ in range(4)]
        r1 = [P.sb([128, 512]) for _ in range(2)]
        r2 = [P.sb([128, 512]) for _ in range(2)]
        o1 = [P.sb([128, 512]) for _ in range(2)]
        sq = [P.sb([128, 512]) for _ in range(2)]
        ob = [P.sb([128, 512]) for _ in range(2)]
        ne = 0
        nq = 0
        for h in range(8):
            kk, vv = kT[h % 2], vt[h % 2]
            P.dma(kk, C.qkT[1024 + h * 128:1024 + (h + 1) * 128, :])
            P.dma(vv, C.vtok[:, h * 128:(h + 1) * 128].re("(n p) e -> p n e", p=128))
            for qt in range(NT):
                tsl = slice(qt * 512, (qt + 1) * 512)
                q = qt_[nq % 2]
                P.dma(q, C.qkT[h * 128:(h + 1) * 128, tsl])
                pn = [C.PS[0], C.PS[1]]
                pd = [C.PS[2], C.PS[3]]
                nkt = 4 * (qt + 1)
                its = [(kt, t) for kt in range(nkt) for t in range(2)]
                LA = 2
                ebuf = {}

                def emit_s(n):
                    nonlocal ne
                    kt, t = its[n]
                    psc = C.PS[4 + (ne % 4)]
                    P.mm(psc, kk[t * 64:(t + 1) * 64, kt * 128:(kt + 1) * 128], q[t * 64:(t + 1) * 64, :])
                    e_ = E[ne % 4]
                    ne += 1
                    P.act(e_, psc, AF.Exp, bias=C.neg8[:, 0:1])
                    r = kt - 4 * qt
                    if r >= 0:
                        P.tt(e_, e_, C.amask[:, r, :], ALU.mult, eng='pool')
                    ebuf[n] = e_

                def emit_md(n):
                    kt, t = its[n]
                    e_ = ebuf.pop(n)
                    P.mm(pn[t], vv[:, kt, :], e_, start=(kt == 0), stop=(kt == nkt - 1))
                    P.mm(pd[t], C.onesb, e_, start=(kt == 0), stop=(kt == nkt - 1))
                for n in range(len(its) + LA):
                    if n < len(its):
                        emit_s(n)
                    if n - LA >= 0:
                        emit_md(n - LA)
                b = nq % 2
                nq += 1
                P.recip(r1[b], pd[0])
                P.recip(r2[b], pd[1])
                P.tt(o1[b], pn[0], r1[b], ALU.mult)
                P.tt(r2[b], pn[1], r2[b], ALU.mult)
                P.stt(o1[b], r2[b], nlam[:, 0:1], o1[b], ALU.mult, ALU.add)
                P.act(sq[b], o1[b], AF.Square)
                pss = C.PS[2]
                P.mm(pss, C.ones, sq[b])
                rstd_from_ss(P, r1[b], pss, 1.0 / 128, EPS)
                P.stt(ob[b], o1[b], sw[:, 0:1], r1[b], ALU.mult, ALU.mult)
                P.dma(C.oT[h * 128:(h + 1) * 128, tsl], ob[b])
        P.barrier()
        P.flush()


def make_consts():
    c = {}
    c['c_ident'] = np.eye(128, dtype=np.float32)
    am = np.zeros((128, 4, 512), np.float32)
    kk = np.arange(128)[:, None]
    qq = np.arange(512)[None, :]
    for r in range(4):
        am[:, r, :] = ((r * 128 + kk) // 64 <= qq // 64)
    c['c_amask'] = am
    gm = np.zeros((64, 4, 512), np.float32)
    p = np.arange(64)[:, None]
    f = np.arange(64)[None, :]
    for cidx in range(8):
        sl = slice(cidx * 64, (cidx + 1) * 64)
        gm[:, 0, sl] = -1.0 * (p > f)
        gm[:, 1, sl] = -1.0 * (p < f)
        gm[:, 2, sl] = (p <= f)
        gm[:, 3, sl] = (p == f)
    c['c_gmask'] = gm
    sel = np.zeros((8, 8, 128), np.float32)
    for e in range(8):
        sel[e, e, :] = 1.0
    c['c_sel'] = sel
    blk = np.zeros((128, 128), np.float32)
    blk[:64, :64] = 1
    blk[64:, 64:] = 1
    c['c_blk'] = blk
    cm = np.ones((4, 512), np.float32)
    cm[:, ::64] = 0
    c['c_cmask'] = cm
    return c


INPUT_NAMES = ['ada_w', 'ada_b', 'norm_mix_w', 'norm_ffn_w',
               'even_w_in', 'even_conv_w', 'even_a_log', 'even_dt_bias', 'even_gdn_norm_w',
               'even_lam_re', 'even_lam_im', 'even_log_step', 'even_b_re', 'even_b_im', 'even_c_re', 'even_c_im',
               'even_d_skip', 'even_glu_w', 'even_glu_b', 'even_w_out', 'even_ffn_w13', 'even_ffn_w2',
               'odd_w_qkv', 'odd_q_norm_w', 'odd_k_norm_w', 'odd_lambda_q1', 'odd_lambda_k1', 'odd_lambda_q2',
               'odd_lambda_k2', 'odd_subln_w', 'odd_w_out', 'odd_router_w', 'odd_expert_w13', 'odd_expert_w2']


def build(shapes, L, depth=DEPTH, dbg=False):
    nc = bass.Bass("TRN2", target_bir_lowering=False)
    C = Ctx()
    C.depth = depth
    C.inp = {}
    consts = make_consts()
    for nm, shp in shapes.items():
        C.inp[nm] = Res(nm, nc.dram_tensor(nm, list(shp), F32, kind="ExternalInput").ap())
    for nm, arr in consts.items():
        C.inp[nm] = Res(nm, nc.dram_tensor(nm, list(arr.shape), F32, kind="ExternalInput").ap())
    C.out = Res('out', nc.dram_tensor('out', [L, D], F32, kind="ExternalOutput").ap())
    kind = "ExternalOutput" if dbg else "Internal"

    def scr(nm, shape, dt=F32):
        return Res(nm, nc.dram_tensor(nm, list(shape), dt, kind=kind).ap())
    C.xA = scr('xA', [D, L])
    C.xB = scr('xB', [D, L])
    C.qkvT = scr('qkvT', [1536, L])
    C.zT = scr('zT', [512, L])
    C.uT = scr('uT', [512, L])
    C.betaT = scr('betaT', [4, L])
    C.gT = scr('gT', [4, L])
    C.yT = scr('yT', [D, L])
    C.xmid = scr('xmid', [D, L])
    C.qkT = scr('qkT', [2048, L], BF16)
    C.vtok = scr('vtok', [L, D], BF16)
    C.oT = scr('oT', [D, L])
    with contextlib.ExitStack() as st0:
        P = Prog(nc, st0)
        C.PS = [P.ps([128, 512]) for _ in range(8)]
        C.ident = P.sb([128, 128])
        C.ones = P.sb([128, 128])
        C.onesb = P.sb([128, 128], BF16)
        C.blk = P.sb([128, 128])
        C.amask = P.sb([128, 4, 512], BF16)
        C.sel = P.sb([8, 8, 128])
        C.halfpi = P.sb([128, 1])
        C.neg8 = P.sb([128, 1])
        C.mod = [P.sb([128, 48]) for _ in range(depth)]
        C.modA = [P.sb([128, 16]) for _ in range(depth)]
        P.dma(C.ident, C.inp['c_ident'])
        P.dma(C.blk, C.inp['c_blk'])
        P.dma(C.amask, C.inp['c_amask'], eng='pool')
        P.dma(C.sel, C.inp['c_sel'])
        P.memset(C.ones, 1.0)
        P.memset(C.onesb, 1.0)
        P.memset(C.halfpi, math.pi / 2)
        P.memset(C.neg8, -8.0)
        stage_prep(P, C, L)
        cur, nxt = C.xA, C.xB
        for l in range(depth):
            i = l // 2
            if l % 2 == 0:
                stage_even_proj(P, C, L, l, cur)
                stage_gdn(P, C, L, l)
                stage_s5(P, C, L, l)
                stage_ffn(P, C, L, l, cur, nxt, C.yT, C.inp['even_w_out'][i], moe=False)
            else:
                stage_attn_proj(P, C, L, l, cur)
                stage_attn(P, C, L, l)
                stage_ffn(P, C, L, l, cur, nxt, C.oT, C.inp['odd_w_out'][i], moe=True)
            cur, nxt = nxt, cur
        stage_out(P, C, L, cur)
        P.stack = st0
        C.nins = P.nins
    return nc, consts, C


def kernel(**inputs):
    x = np.asarray(inputs['x'], dtype=np.float32)
    B, L, _ = x.shape
    shapes = {'x': (L, D), 'c': (D,)}
    for nm in INPUT_NAMES:
        shapes[nm] = tuple(np.asarray(inputs[nm]).shape)
    nc, consts, C = build(shapes, L)
    in_maps = []
    shared = {nm: np.ascontiguousarray(np.asarray(inputs[nm], dtype=np.float32)) for nm in INPUT_NAMES}
    for core in range(8):
        b = core % B
        m = dict(shared)
        m.update(consts)
        m['x'] = np.ascontiguousarray(x[b])
        m['c'] = np.ascontiguousarray(np.asarray(inputs['c'], dtype=np.float32)[b])
        in_maps.append(m)
    res = run_bass_kernel_spmd(nc, in_maps, core_ids=list(range(8)))
    out = np.stack([res.results[b]['out'] for b in range(B)], axis=0)
    return out.astype(np.float32)
```

```python
import contextlib
import math
import numpy as np
import concourse.bass as bass
import concourse.mybir as mybir
from concourse.bass_utils import run_bass_kernel_spmd

F32 = mybir.dt.float32
BF16 = mybir.dt.bfloat16
AF = mybir.ActivationFunctionType
ALU = mybir.AluOpType
AX = mybir.AxisListType

D = 1024
DEPTH = 4
EPS = 1e-6
DFF = 2816
NEXP = 8
ENGS = ['pe', 'act', 'dve', 'pool', 'sp']


class V:
    __slots__ = ('res', 'ap')

    def __init__(self, res, ap):
        self.res = res
        self.ap = ap

    def __getitem__(self, idx):
        return V(self.res, self.ap[idx])

    def bc(self, shape):
        return V(self.res, self.ap.to_broadcast(list(shape)))

    def re(self, s, **kw):
        return V(self.res, self.ap.rearrange(s, **kw))


class Res:
    __slots__ = ('name', 'w', 'r', 'ap')

    def __init__(self, name, ap=None):
        self.name = name
        self.w = {}
        self.r = {}
        self.ap = ap

    def __getitem__(self, idx):
        return V(self, self.ap[idx])

    @property
    def v(self):
        return V(self, self.ap)


def _rv(x):
    return x.v if isinstance(x, Res) else x


class Prog:
    def __init__(self, nc, stack, n_dma_sems=32):
        self.nc = nc
        self.stack = stack
        self.streams = {e: [] for e in ENGS}
        self.cnt = {e: 0 for e in ENGS}
        self.semh = {}
        for e in ['pe', 'act', 'dve', 'pool']:
            self.semh['c_' + e] = stack.enter_context(nc.semaphore('c_' + e))
        self.ndma = n_dma_sems
        self.dma_tot = [0] * n_dma_sems
        self.dma_next = 0
        for k in range(n_dma_sems):
            self.semh['d%d' % k] = stack.enter_context(nc.semaphore('d%d' % k))
        self.known = {e: {} for e in ENGS}
        self.nt = 0
        self.nins = 0

    def sb(self, shape, dtype=F32, name=None):
        self.nt += 1
        name = name or ('t%d' % self.nt)
        t = self.stack.enter_context(self.nc.sbuf_tensor(name, list(shape), dtype))
        return Res(name, t[:])

    def ps(self, shape, dtype=F32, name=None):
        self.nt += 1
        name = name or ('p%d' % self.nt)
        t = self.stack.enter_context(self.nc.psum_tensor(name, list(shape), dtype))
        return Res(name, t[:])

    def dram(self, name, shape, dtype, kind="Internal"):
        t = self.nc.dram_tensor(name, list(shape), dtype, kind=kind)
        return Res(name, t.ap())

    def _deps(self, eng, reads, writes):
        deps = {}

        def add(k, v):
            if deps.get(k, -1) < v:
                deps[k] = v
        for r in reads:
            for k, v in r.w.items():
                add(k, v)
        for w in writes:
            for k, v in w.w.items():
                add(k, v)
            for k, v in w.r.items():
                add(k, v)
        out = []
        kn = self.known[eng]
        for k, v in deps.items():
            if eng == 'pe' and k == 'c_pe':
                continue
            if kn.get(k, -1) < v:
                kn[k] = v
                out.append((k, v))
        return out

    def _record(self, ev, reads, writes, merge=False):
        k, v = ev
        for w in writes:
            if merge:
                w.w[k] = v
            else:
                w.w = {k: v}
            w.r = {}
        for r in reads:
            if r.r.get(k, -1) < v:
                r.r[k] = v

    def op(self, eng, fn, reads=(), writes=(), inc=True):
        reads = [x for x in reads if x is not None]
        waits = self._deps(eng, reads, writes)
        key = 'c_' + eng
        if inc:
            self.cnt[eng] += 1
            ev = (key, self.cnt[eng])
        else:
            ev = (key, self.cnt[eng] + 1)
        self.streams[eng].append((waits, fn, key if inc else None, 1))
        self._record(ev, reads, writes)
        self.nins += 1

    def dma(self, out, in_, eng='sp'):
        out = _rv(out)
        in_ = _rv(in_)
        k = self.dma_next
        self.dma_next = (k + 1) % self.ndma
        key = 'd%d' % k
        waits = self._deps(eng, [in_.res], [out.res])
        kn = self.known[eng]
        if kn.get(key, -1) < self.dma_tot[k]:
            kn[key] = self.dma_tot[k]
            waits.append((key, self.dma_tot[k]))
        self.dma_tot[k] += 16
        ev = (key, self.dma_tot[k])
        oa, ia = out.ap, in_.ap

        def fn(e):
            return e.dma_start(out=oa, in_=ia)
        self.streams[eng].append((waits, fn, key, 16))
        self._record(ev, [in_.res], [out.res], merge=True)
        self.nins += 1

    def collective(self, kind, out, in_, groups, eng='pool'):
        out = _rv(out)
        in_ = _rv(in_)
        k = self.dma_next
        self.dma_next = (k + 1) % self.ndma
        key = 'd%d' % k
        waits = self._deps(eng, [in_.res], [out.res])
        kn = self.known[eng]
        if kn.get(key, -1) < self.dma_tot[k]:
            kn[key] = self.dma_tot[k]
            waits.append((key, self.dma_tot[k]))
        self.dma_tot[k] += 16
        ev = (key, self.dma_tot[k])
        oa, ia = out.ap, in_.ap

        def fn(e):
            return e.collective_compute(kind, ALU.bypass, groups, [ia], [oa])
        self.streams[eng].append((waits, fn, key, 16))
        self._record(ev, [in_.res], [out.res], merge=True)
        self.nins += 1

    def barrier(self):
        allw = [('c_' + e, self.cnt[e]) for e in ['pe', 'act', 'dve', 'pool']]
        allw += [('d%d' % k, self.dma_tot[k]) for k in range(self.ndma)]
        for e in ENGS:
            kn = self.known[e]
            waits = []
            for k, v in allw:
                if e == 'pe' and k == 'c_pe':
                    continue
                if kn.get(k, -1) < v:
                    kn[k] = v
                    waits.append((k, v))
            self.streams[e].append((waits, None, None, 0))

    def flush(self):
        nc = self.nc
        engobj = {'pe': 'tensor', 'act': 'scalar', 'dve': 'vector', 'pool': 'gpsimd', 'sp': 'sync'}
        semh = self.semh
        with nc.allow_non_contiguous_dma(reason="small strided param loads"), nc.Block() as block:
            for e in ENGS:
                stream = self.streams[e]

                def body(eng, stream=stream):
                    for waits, fn, key, n in stream:
                        for k, v in waits:
                            eng.wait_ge(semh[k], v)
                        if fn is not None:
                            ins = fn(eng)
                            if key is not None:
                                ins.then_inc(semh[key], n)
                getattr(block, engobj[e])(body)
        self.streams = {e: [] for e in ENGS}

    def mm(self, out, lhsT, rhs, start=True, stop=True):
        out, lhsT, rhs = _rv(out), _rv(lhsT), _rv(rhs)
        o, l, r = out.ap, lhsT.ap, rhs.ap
        self.op('pe', lambda e: e.matmul(o, l, r, start=start, stop=stop),
                [lhsT.res, rhs.res], [out.res], inc=stop)

    def tr(self, out, in_, ident):
        out, in_, ident = _rv(out), _rv(in_), _rv(ident)
        o, i, d = out.ap, in_.ap, ident.ap
        self.op('pe', lambda e: e.transpose(o, i, d), [in_.res, ident.res], [out.res])

    def act(self, out, in_, func, bias=None, scale=None, eng='act'):
        out, in_ = _rv(out), _rv(in_)
        reads = [in_.res]
        kw = {}
        if bias is not None:
            if isinstance(bias, (V, Res)):
                bias = _rv(bias)
                reads.append(bias.res)
                kw['bias'] = bias.ap
            else:
                kw['bias'] = float(bias)
        if scale is not None:
            if isinstance(scale, (V, Res)):
                scale = _rv(scale)
                reads.append(scale.res)
                kw['scale'] = scale.ap
            else:
                kw['scale'] = float(scale)
        o, i = out.ap, in_.ap
        self.op(eng, lambda e: e.activation(o, i, func, **kw), reads, [out.res])

    def tt(self, out, a, b, op, eng='dve'):
        out, a, b = _rv(out), _rv(a), _rv(b)
        o, x, y = out.ap, a.ap, b.ap
        self.op(eng, lambda e: e.tensor_tensor(o, x, y, op), [a.res, b.res], [out.res])

    def ts(self, out, a, s1, op0, s2=None, op1=None, eng='dve'):
        out, a = _rv(out), _rv(a)
        reads = [a.res]

        def cv(s):
            if isinstance(s, (V, Res)):
                s = _rv(s)
                reads.append(s.res)
                return s.ap
            return None if s is None else float(s)
        c1, c2 = cv(s1), cv(s2)
        o, x = out.ap, a.ap
        if op1 is None:
            self.op(eng, lambda e: e.tensor_single_scalar(o, x, c1, op0), reads, [out.res])
        else:
            self.op(eng, lambda e: e.tensor_scalar(o, x, c1, c2, op0, op1), reads, [out.res])

    def stt(self, out, a, s, b, op0, op1):
        out, a, b = _rv(out), _rv(a), _rv(b)
        reads = [a.res, b.res]
        if isinstance(s, (V, Res)):
            s = _rv(s)
            reads.append(s.res)
            c = s.ap
        else:
            c = float(s)
        o, x, y = out.ap, a.ap, b.ap
        self.op('dve', lambda e: e.scalar_tensor_tensor(o, x, c, y, op0, op1), reads, [out.res])

    def copy(self, out, in_, eng='dve'):
        out, in_ = _rv(out), _rv(in_)
        o, i = out.ap, in_.ap
        if eng == 'act':
            self.op('act', lambda e: e.activation(o, i, AF.Copy), [in_.res], [out.res])
        else:
            self.op(eng, lambda e: e.tensor_copy(o, i), [in_.res], [out.res])

    def recip(self, out, in_):
        out, in_ = _rv(out), _rv(in_)
        o, i = out.ap, in_.ap
        self.op('dve', lambda e: e.reciprocal(o, i), [in_.res], [out.res])

    def memset(self, out, val, eng='dve'):
        out = _rv(out)
        o = out.ap
        self.op(eng, lambda e: e.memset(o, float(val)), [], [out.res])

    def scan(self, out, d0, d1, init, op0=ALU.mult, op1=ALU.add):
        out, d0, d1 = _rv(out), _rv(d0), _rv(d1)
        reads = [d0.res, d1.res]
        if isinstance(init, (V, Res)):
            init = _rv(init)
            reads.append(init.res)
            c = init.ap
        else:
            c = float(init)
        o, x, y = out.ap, d0.ap, d1.ap
        self.op('dve', lambda e: e.tensor_tensor_scan(o, x, y, c, op0, op1), reads, [out.res])

    def reduce(self, out, in_, op, axis=AX.X):
        out, in_ = _rv(out), _rv(in_)
        o, i = out.ap, in_.ap
        self.op('dve', lambda e: e.tensor_reduce(o, i, axis, op), [in_.res], [out.res])


class Ctx:
    pass


def rstd_from_ss(P, out_sb, ss_ps, scale, bias):
    P.act(out_sb, ss_ps, AF.Ln, bias=bias, scale=scale)
    P.act(out_sb, out_sb, AF.Exp, scale=-0.5)


def stage_prep(P, C, L):
    nc = P.nc
    I = C.inp
    with contextlib.ExitStack() as st:
        P.stack = st
        cT = P.sb([128, 8])
        with nc.allow_non_contiguous_dma(reason="tiny"):
            P.dma(cT, I['c'].v.re("(k p) -> p k", p=128))
        cond = P.sb([128, 8])
        P.act(cond, cT, AF.Silu)
        wbuf = [P.sb([128, 8, 768]) for _ in range(2)]
        pm = C.PS[0]
        for l in range(C.depth):
            bt = P.sb([128, 48])
            with nc.allow_non_contiguous_dma(reason="tiny"):
                P.dma(bt, I['ada_b'][l].re("(j p) -> p j", p=128))
            for cb in range(8):
                wb = wbuf[cb % 2]
                P.dma(wb, I['ada_w'][l][:, cb * 768:(cb + 1) * 768].re("(k p) m -> p k m", p=128))
                for j in range(6):
                    col = cb * 6 + j
                    for k in range(8):
                        P.mm(pm[:, col:col + 1], wb[:, k, j * 128:(j + 1) * 128], cond[:, k:k + 1],
                             start=(k == 0), stop=(k == 7))
            mod = C.mod[l]
            P.tt(mod, pm[:, 0:48], bt, ALU.add)
            nw = P.sb([128, 16])
            with nc.allow_non_contiguous_dma(reason="tiny"):
                P.dma(nw[:, 0:8], I['norm_mix_w'][l].re("(k p) -> p k", p=128))
                P.dma(nw[:, 8:16], I['norm_ffn_w'][l].re("(k p) -> p k", p=128))
            A = C.modA[l]
            P.stt(A[:, 0:8], mod[:, 8:16], 1.0, nw[:, 0:8], ALU.add, ALU.mult)
            P.stt(A[:, 8:16], mod[:, 32:40], 1.0, nw[:, 8:16], ALU.add, ALU.mult)
        xin = [P.sb([128, 1024]) for _ in range(2)]
        xo = [P.sb([128, 8, 512]) for _ in range(2)]
        for tt in range(L // 512):
            o = xo[tt % 2]
            for s in range(4):
                xi = xin[s % 2]
                t0 = tt * 512 + s * 128
                P.dma(xi, I['x'][t0:t0 + 128, :])
                for k in range(8):
                    pt = C.PS[1 + (k % 4)]
                    P.tr(pt[:, 0:128], xi[:, k * 128:(k + 1) * 128], C.ident)
                    P.copy(o[:, k, s * 128:(s + 1) * 128], pt[:, 0:128], eng=('act' if k % 2 else 'dve'))
            P.dma(C.xA.v.re("(k p) t -> p k t", p=128)[:, :, tt * 512:(tt + 1) * 512], o)
        P.barrier()
        P.flush()


def stage_out(P, C, L, xsrc):
    I = C.inp
    with contextlib.ExitStack() as st:
        P.stack = st
        xi = [P.sb([128, 8, 512]) for _ in range(2)]
        xo = [P.sb([128, 1024]) for _ in range(2)]
        n = 0
        for tt in range(L // 512):
            t = xi[tt % 2]
            P.dma(t, xsrc.v.re("(k p) t -> p k t", p=128)[:, :, tt * 512:(tt + 1) * 512])
            for s in range(4):
                o = xo[s % 2]
                for k in range(8):
                    pt = C.PS[1 + (k % 4)]
                    P.tr(pt[:, 0:128], t[:, k, s * 128:(s + 1) * 128], C.ident)
                    P.copy(o[:, k * 128:(k + 1) * 128], pt[:, 0:128], eng=('act' if k % 2 else 'dve'))
                t0 = tt * 512 + s * 128
                P.dma(C.out[t0:t0 + 128, :], o)
        P.barrier()
        P.flush()


def load_norm_h(P, C, S, xt, l, which, want32=False):
    A = C.modA[l]
    mod = C.mod[l]
    aoff = 0 if which == 1 else 8
    shoff = 0 if which == 1 else 24
    sq = S.sq
    P.act(sq, xt, AF.Square)
    ss = C.PS[0]
    for k in range(8):
        P.mm(ss, C.ones, sq[:, k, :], start=(k == 0), stop=(k == 7))
    rstd = S.rstd
    rstd_from_ss(P, rstd, ss, 1.0 / D, EPS)
    for k in range(8):
        tmp = S.tmp[k % 2]
        P.stt(tmp, xt[:, k, :], A[:, aoff + k:aoff + k + 1], rstd, ALU.mult, ALU.mult)
        if want32:
            P.act(S.h32[:, k, :], tmp, AF.Identity, bias=mod[:, shoff + k:shoff + k + 1])
            P.act(S.hb[:, k, :], tmp, AF.Identity, bias=mod[:, shoff + k:shoff + k + 1])
        else:
            P.act(S.hb[:, k, :], tmp, AF.Identity, bias=mod[:, shoff + k:shoff + k + 1])


def wslab_iter(P, C, S, Wv, ncols, KT, slab=512):
    c0 = 0
    i = 0
    while c0 < ncols:
        w = min(slab, ncols - c0)
        buf = S.wb[S.wbi % len(S.wb)]
        S.wbi += 1
        P.dma(buf[:, 0:KT, 0:w], Wv[:, c0:c0 + w].re("(k p) m -> p k m", p=128), eng='pool')
        yield buf, c0, w
        c0 += w
        i += 1


def stage_even_proj(P, C, L, l, xsrc):
    nc = P.nc
    I = C.inp
    i = l // 2
    Win = I['even_w_in'][i]
    with contextlib.ExitStack() as st:
        P.stack = st
        S = Ctx()
        S.sq = P.sb([128, 8, 512])
        S.rstd = P.sb([128, 512])
        S.tmp = [P.sb([128, 512]) for _ in range(2)]
        S.hb = P.sb([128, 8, 512], BF16)
        S.wb = [P.sb([128, 8, 512], BF16) for _ in range(3)]
        S.wbi = 0
        C.cmask = P.sb([4, 512])
        P.dma(C.cmask, C.inp['c_cmask'])
        xts = [P.sb([128, 8, 512]) for _ in range(2)]
        pre = [P.sb([128, 515]) for _ in range(12)]
        for m in range(12):
            P.memset(pre[m][:, 0:3], 0.0)
        cw = P.sb([128, 12, 4])
        with nc.allow_non_contiguous_dma(reason="tiny"):
            for j in range(4):
                P.dma(cw[:, :, j], I['even_conv_w'][i][j].re("(t p) -> p t", p=128))
        wba = P.sb([128, 8, 8])
        with nc.allow_non_contiguous_dma(reason="small"):
            P.dma(wba, Win[:, 2048:2056].re("(k p) m -> p k m", p=128))
        hc = P.sb([4, 2])
        with nc.allow_non_contiguous_dma(reason="tiny"):
            P.dma(hc[:, 0:1], I['even_a_log'][i].re("(h o) -> h o", o=1))
            P.dma(hc[:, 1:2], I['even_dt_bias'][i].re("(h o) -> h o", o=1))
        nA = P.sb([4, 1])
        P.act(nA, hc[:, 0:1], AF.Exp)
        P.ts(nA, nA, -1.0, ALU.mult)
        h32 = P.sb([128, 8, 512])
        S.h32 = h32
        acc = [P.sb([128, 512]) for _ in range(2)]
        ob = [P.sb([128, 512]) for _ in range(3)]
        sm = [P.sb([4, 512]) for _ in range(6)]
        NT = L // 512
        xv = xsrc.v.re("(k p) t -> p k t", p=128)
        P.dma(xts[0], xv[:, :, 0:512])
        nob = 0
        for tt in range(NT):
            xt = xts[tt % 2]
            if tt + 1 < NT:
                P.dma(xts[(tt + 1) % 2], xv[:, :, (tt + 1) * 512:(tt + 2) * 512])
            load_norm_h(P, C, S, xt, l, 1, want32=True)
            tsl = slice(tt * 512, (tt + 1) * 512)
            pb, pa = C.PS[1], C.PS[2]
            for k in range(8):
                P.mm(pb[0:4, :], wba[:, k, 0:4], h32[:, k, :], start=(k == 0), stop=(k == 7))
            for k in range(8):
                P.mm(pa[0:4, :], wba[:, k, 4:8], h32[:, k, :], start=(k == 0), stop=(k == 7))
            beta = sm[0]
            P.act(beta[:], pb[0:4, :], AF.Exp, scale=-1.0)
            P.ts(beta, beta, 1.0, ALU.add)
            P.recip(beta, beta)
            P.dma(C.betaT[:, tsl], beta)
            xa = sm[1]
            P.act(xa[:], pa[0:4, :], AF.Identity, bias=hc[:, 1:2])
            ax = sm[2]
            P.stt(ax, xa, -1.0, xa, ALU.mult, ALU.max)
            P.act(ax, ax, AF.Exp, scale=-1.0)
            P.act(ax, ax, AF.Ln, bias=1.0)
            sp = sm[3]
            P.stt(sp, xa, 0.0, ax, ALU.max, ALU.add)
            g = sm[4]
            P.ts(g, sp, nA[:, 0:1], ALU.mult)
            gc = sm[5]
            P.scan(gc, C.cmask, g, 0.0)
            P.dma(C.gT[:, tsl], gc)
            mt = 0
            for wsl, c0, w in wslab_iter(P, C, S, Win, 2048, 8):
                for j in range(w // 128):
                    m = (c0 // 128) + j
                    pp = C.PS[3 + (m % 4)]
                    for k in range(8):
                        P.mm(pp, wsl[:, k, j * 128:(j + 1) * 128], S.hb[:, k, :], start=(k == 0), stop=(k == 7))
                    if m < 12:
                        pr = pre[m]
                        P.copy(pr[:, 3:515], pp, eng='act')
                        a = acc[m % 2]
                        P.ts(a, pr[:, 0:512], cw[:, m, 0:1], ALU.mult)
                        for jj in range(1, 4):
                            P.stt(a, pr[:, jj:jj + 512], cw[:, m, jj:jj + 1], a, ALU.mult, ALU.add)
                        P.copy(pr[:, 0:3], pr[:, 512:515], eng='pool')
                        o = ob[nob % 3]
                        nob += 1
                        P.act(o, a, AF.Silu)
                        if m < 8:
                            sq = S.tmp[m % 2]
                            P.act(sq, o, AF.Square)
                            ss = C.PS[7]
                            P.mm(ss, C.ones, sq)
                            rs = S.rstd
                            if m < 4:
                                rstd_from_ss(P, rs, ss, 128.0, 128.0 * EPS)
                            else:
                                rstd_from_ss(P, rs, ss, 1.0, EPS)
                            P.tt(o, o, rs, ALU.mult)
                        P.dma(C.qkvT[m * 128:(m + 1) * 128, tsl], o)
                    else:
                        o = ob[nob % 3]
                        nob += 1
                        P.act(o, pp, AF.Silu)
                        P.dma(C.zT[(m - 12) * 128:(m - 11) * 128, tsl], o)
            for wsl, c0, w in wslab_iter(P, C, S, Win[:, 2056:2568], 512, 8):
                for j in range(4):
                    pp = C.PS[3 + (j % 4)]
                    for k in range(8):
                        P.mm(pp, wsl[:, k, j * 128:(j + 1) * 128], S.hb[:, k, :], start=(k == 0), stop=(k == 7))
                    o = ob[nob % 3]
                    nob += 1
                    P.copy(o, pp, eng='act')
                    P.dma(C.uT[j * 128:(j + 1) * 128, tsl], o)
        P.barrier()
        P.flush()


def stage_gdn(P, C, L, l):
    nc = P.nc
    I = C.inp
    i = l // 2
    NT = L // 512
    with contextlib.ExitStack() as st:
        P.stack = st
        C.gmask = P.sb([64, 4, 512])
        P.dma(C.gmask, C.inp['c_gmask'])
        Sst = [P.sb([128, 128]) for _ in range(4)]
        for h in range(4):
            P.memset(Sst[h], 0.0)
        gw = P.sb([128, 1])
        with nc.allow_non_contiguous_dma(reason="tiny"):
            P.dma(gw, I['even_gdn_norm_w'][i].re("(p o) -> p o", o=1))
        mk = lambda n, shape, dt=F32: [P.sb(shape, dt) for _ in range(n)]
        qT, kT, vT = mk(2, [128, 512]), mk(2, [128, 512]), mk(2, [128, 512])
        rows = mk(2, [1, 2, 512])
        r_eg, r_ekl, r_ng = mk(2, [1, 512]), mk(2, [1, 512]), mk(2, [1, 512])
        bcs = mk(2, [128, 3, 512])
        kb, kbe, vb, qe, kel = (mk(2, [128, 512]) for _ in range(5))
        tokm = mk(2, [64, 3, 8, 128])
        EL, EU, t1 = mk(2, [64, 512]), mk(2, [64, 512]), mk(2, [64, 512])
        Am, ATm, PTm, Rm = mk(2, [64, 512]), mk(2, [64, 512]), mk(2, [64, 512]), mk(2, [64, 512])
        WTn = mk(2, [128, 512])
        vnew = mk(3, [64, 128])
        zt = mk(2, [128, 512])
        osb = mk(2, [128, 512])
        sq = mk(2, [128, 512])
        rs = mk(2, [128, 512])
        it = 0
        for tt in range(NT):
            tsl = slice(tt * 512, (tt + 1) * 512)
            for h in range(4):
                b = it % 2
                it += 1
                P.dma(qT[b], C.qkvT[h * 128:(h + 1) * 128, tsl])
                P.dma(kT[b], C.qkvT[512 + h * 128:512 + (h + 1) * 128, tsl])
                P.dma(vT[b], C.qkvT[1024 + h * 128:1024 + (h + 1) * 128, tsl])
                P.dma(rows[b][:, 0, :], C.betaT[h:h + 1, tsl])
                P.dma(rows[b][:, 1, :], C.gT[h:h + 1, tsl])
                P.dma(zt[b], C.zT[h * 128:(h + 1) * 128, tsl])
                gc = rows[b][:, 1, :]
                P.act(r_eg[b], gc, AF.Exp)
                g3 = rows[b][:, 1, :].re("o (c j) -> o c j", j=64)
                P.tt(r_ekl[b].v.re("o (c j) -> o c j", j=64), g3[:, :, 63:64].bc([1, 8, 64]), g3, ALU.subtract)
                P.act(r_ekl[b], r_ekl[b], AF.Exp)
                P.ts(r_ng[b], gc, -1.0, ALU.mult)
                pbc = [C.PS[0], C.PS[1], C.PS[2]]
                P.mm(pbc[0], C.ones[0:1, :], rows[b][:, 0, :])
                P.mm(pbc[1], C.ones[0:1, :], r_eg[b])
                P.mm(pbc[2], C.ones[0:1, :], r_ekl[b])
                for j in range(3):
                    P.copy(bcs[b][:, j, :], pbc[j], eng='act')
                P.tt(kb[b], kT[b], bcs[b][:, 0, :], ALU.mult)
                P.tt(kbe[b], kb[b], bcs[b][:, 1, :], ALU.mult, eng='pool')
                P.tt(vb[b], vT[b], bcs[b][:, 0, :], ALU.mult)
                P.tt(qe[b], qT[b], bcs[b][:, 1, :], ALU.mult, eng='pool')
                P.tt(kel[b], kT[b], bcs[b][:, 2, :], ALU.mult)
                for c in range(8):
                    csl = slice(c * 64, (c + 1) * 64)
                    for j, src in enumerate((vb[b], kbe[b], kel[b])):
                        pt = C.PS[3 + ((c * 3 + j) % 2)]
                        P.tr(pt[0:64, 0:128], src[:, csl], C.ident)
                        P.copy(tokm[b][:, j, c, :], pt[0:64, 0:128], eng=('act' if j != 1 else 'dve'))
                pg = C.PS[5]
                for c in range(8):
                    csl = slice(c * 64, (c + 1) * 64)
                    P.mm(pg[0:64, csl], rows[b][:, 1, csl], C.ones[0:1, 0:64], start=True, stop=False)
                    P.mm(pg[0:64, csl], C.ones[0:1, 0:64], r_ng[b][:, csl], start=False, stop=True)
                P.ts(t1[b], pg[0:64, :], 0.0, ALU.min)
                P.act(EL[b], t1[b], AF.Exp)
                P.ts(t1[b], pg[0:64, :], 0.0, ALU.max)
                P.act(EU[b], t1[b], AF.Exp, scale=-1.0)
                pA, pAT, pPT = C.PS[0], C.PS[1], C.PS[2]
                for c in range(8):
                    csl = slice(c * 64, (c + 1) * 64)
                    P.mm(pA[0:64, csl], kb[b][:, csl], kT[b][:, csl])
                    P.mm(pAT[0:64, csl], kT[b][:, csl], kb[b][:, csl])
                    P.mm(pPT[0:64, csl], kT[b][:, csl], qT[b][:, csl])
                P.tt(t1[b], EL[b], C.gmask[:, 0, :], ALU.mult, eng='pool')
                P.tt(Am[b], pA[0:64, :], t1[b], ALU.mult)
                P.tt(EL[b], EU[b], C.gmask[:, 1, :], ALU.mult, eng='pool')
                P.tt(ATm[b], pAT[0:64, :], EL[b], ALU.mult)
                P.tt(EU[b], EU[b], C.gmask[:, 2, :], ALU.mult, eng='pool')
                P.tt(PTm[b], pPT[0:64, :], EU[b], ALU.mult)
                P.tt(Rm[b], ATm[b], C.gmask[:, 3, :], ALU.add)
                X, XT = Am[b], ATm[b]
                X2, XT2 = t1[b], EL[b]
                for step in range(5):
                    p1, p2, p3 = C.PS[3], C.PS[4], C.PS[5]
                    for c in range(8):
                        csl = slice(c * 64, (c + 1) * 64)
                        P.mm(p1[0:64, csl], XT[:, csl], X[:, csl])
                        P.mm(p2[0:64, csl], X[:, csl], XT[:, csl])
                    P.copy(X2, p1[0:64, :], eng='act')
                    P.copy(XT2, p2[0:64, :], eng='dve')
                    X, X2 = X2, X
                    XT, XT2 = XT2, XT
                    for c in range(8):
                        csl = slice(c * 64, (c + 1) * 64)
                        P.mm(p3[0:64, csl], X[:, csl], Rm[b][:, csl])
                    P.tt(Rm[b], Rm[b], p3[0:64, :], ALU.add)
                pW = C.PS[6]
                for c in range(8):
                    csl = slice(c * 64, (c + 1) * 64)
                    P.mm(pW[:, csl], tokm[b][:, 1, c, :], Rm[b][:, csl])
                P.act(WTn[b], pW, AF.Copy, scale=-1.0)
                po = C.PS[7]
                S = Sst[h]
                for c in range(8):
                    csl = slice(c * 64, (c + 1) * 64)
                    pv = C.PS[3 + (c % 2)]
                    P.mm(pv[0:64, 0:128], Rm[b][:, csl], tokm[b][:, 0, c, :], start=True, stop=False)
                    P.mm(pv[0:64, 0:128], WTn[b][:, csl], S, start=False, stop=True)
                    vn = vnew[c % 3]
                    P.copy(vn, pv[0:64, 0:128], eng='act')
                    P.mm(po[:, csl], S, qe[b][:, csl], start=True, stop=False)
                    P.mm(po[:, csl], vn, PTm[b][:, csl], start=False, stop=True)
                    ps_ = C.PS[5]
                    P.mm(ps_[:, 0:128], tokm[b][:, 2, c, :], vn)
                    P.stt(S, S, bcs[b][:, 1, c * 64 + 63:c * 64 + 64], ps_[:, 0:128], ALU.mult, ALU.add)
                P.copy(osb[b], po, eng='act')
                P.act(sq[b], osb[b], AF.Square)
                pss = C.PS[6]
                P.mm(pss, C.ones, sq[b])
                rstd_from_ss(P, rs[b], pss, 1.0 / 128, EPS)
                P.stt(osb[b], osb[b], gw[:, 0:1], rs[b], ALU.mult, ALU.mult)
                P.tt(osb[b], osb[b], zt[b], ALU.mult)
                P.dma(C.yT[h * 128:(h + 1) * 128, tsl], osb[b])
        P.barrier()
        P.flush()


def stage_s5(P, C, L, l):
    nc = P.nc
    I = C.inp
    i = l // 2
    NT = L // 512
    TWO_PI = 2.0 * math.pi
    with contextlib.ExitStack() as st:
        P.stack = st
        lr = P.sb([128, 16])
        li = P.sb([128, 16])
        ls = P.sb([128, 16])
        with nc.allow_non_contiguous_dma(reason="small"):
            P.dma(lr, I['even_lam_re'][i].re("(j g) n -> (g n) j", g=2))
            P.dma(li, I['even_lam_im'][i].re("(j g) n -> (g n) j", g=2))
            lsv = I['even_log_step'][i].re("(j g) -> g j", g=2)
            for g2 in range(2):
                P.dma(ls[g2 * 64:(g2 + 1) * 64, :], lsv[g2:g2 + 1, :].bc([64, 16]))
        dt = P.sb([128, 16])
        P.act(dt, ls, AF.Exp)
        P.ts(lr, lr, -1e-4, ALU.min)
        rho = P.sb([128, 16])
        P.tt(rho, lr, dt, ALU.mult)
        P.act(rho, rho, AF.Exp)
        th = P.sb([128, 16])
        P.tt(th, li, dt, ALU.mult)
        tmpa = P.sb([128, 16])
        sn = P.sb([128, 16])
        cs = P.sb([128, 16])
        P.act(sn, th, AF.Sin, scale=1.0 / 16)
        P.act(cs, th, AF.Sin, scale=1.0 / 16, bias=C.halfpi[:, 0:1])
        t_c2, t_s2 = P.sb([128, 16]), P.sb([128, 16])
        for _ in range(4):
            P.tt(t_c2, cs, cs, ALU.mult)
            P.tt(t_s2, sn, sn, ALU.mult)
            P.tt(sn, cs, sn, ALU.mult)
            P.ts(sn, sn, 2.0, ALU.mult)
            P.tt(cs, t_c2, t_s2, ALU.subtract)
        ar, ai = P.sb([128, 16]), P.sb([128, 16])
        P.tt(ar, rho, cs, ALU.mult)
        P.tt(ai, rho, sn, ALU.mult)
        nr = P.sb([128, 16])
        P.ts(nr, ar, -1.0, ALU.add)
        den = P.sb([128, 16])
        t2 = P.sb([128, 16])
        P.tt(den, lr, lr, ALU.mult)
        P.tt(t2, li, li, ALU.mult)
        P.tt(den, den, t2, ALU.add)
        P.recip(den, den)
        cr, ci = P.sb([128, 16]), P.sb([128, 16])
        P.tt(cr, nr, lr, ALU.mult)
        P.tt(t2, ai, li, ALU.mult)
        P.tt(cr, cr, t2, ALU.add)
        P.tt(cr, cr, den, ALU.mult)
        P.tt(ci, ai, lr, ALU.mult)
        P.tt(t2, nr, li, ALU.mult)
        P.tt(ci, ci, t2, ALU.subtract)
        P.tt(ci, ci, den, ALU.mult)
        nsn = P.sb([128, 16])
        P.ts(nsn, sn, -1.0, ALU.mult)
        Tc = P.sb([128, 16, 512])
        Ts = P.sb([128, 16, 512])
        P.memset(Tc[:, :, 0:1], 1.0)
        P.memset(Ts[:, :, 0:1], 0.0)
        cc, s_ = P.sb([128, 16]), P.sb([128, 16])
        P.copy(cc, cs)
        P.copy(s_, sn)
        ta, tb = P.sb([128, 256]), P.sb([128, 256])
        c2, s2 = P.sb([128, 16]), P.sb([128, 16])
        span = 1
        while span < 512:
            for j in range(16):
                P.ts(ta[:, 0:span], Ts[:, j, 0:span], s_[:, j:j + 1], ALU.mult)
                P.stt(Tc[:, j, span:2 * span], Tc[:, j, 0:span], cc[:, j:j + 1], ta[:, 0:span], ALU.mult, ALU.subtract)
                P.ts(tb[:, 0:span], Tc[:, j, 0:span], s_[:, j:j + 1], ALU.mult)
                P.stt(Ts[:, j, span:2 * span], Ts[:, j, 0:span], cc[:, j:j + 1], tb[:, 0:span], ALU.mult, ALU.add)
            P.tt(c2, cc, cc, ALU.mult)
            P.tt(s2, s_, s_, ALU.mult)
            P.tt(s_, cc, s_, ALU.mult)
            P.ts(s_, s_, 2.0, ALU.mult)
            P.tt(cc, c2, s2, ALU.subtract)
            span *= 2
        BreT = [P.sb([128, 128]) for _ in range(16)]
        BimT = [P.sb([128, 128]) for _ in range(16)]
        CrT = [P.sb([128, 128]) for _ in range(16)]
        CiT = [P.sb([128, 128]) for _ in range(16)]
        pad = [P.sb([128, 128]) for _ in range(4)]
        craw = [P.sb([128, 128]) for _ in range(2)]
        for j in range(16):
            kt = j // 4
            for which, (nm, dst) in enumerate((('even_b_re', BreT), ('even_b_im', BimT))):
                pd = pad[which]
                P.memset(pd, 0.0)
                for g2 in range(2):
                    g = 2 * j + g2
                    off = (g - 8 * kt) * 16
                    P.dma(pd[g2 * 64:(g2 + 1) * 64, off:off + 16], I[nm][i][g])
                pt = C.PS[which]
                P.tr(pt[:, 0:128], pd, C.ident)
                P.copy(dst[j], pt[:, 0:128], eng='act')
            for which, nm in enumerate(('even_c_re', 'even_c_im')):
                pd = pad[2 + which]
                P.memset(pd, 0.0)
                for g2 in range(2):
                    g = 2 * j + g2
                    off = (g - 8 * kt) * 16
                    P.dma(pd[off:off + 16, g2 * 64:(g2 + 1) * 64], I[nm][i][g])
                pt = C.PS[2 + which]
                P.tr(pt[:, 0:128], pd, C.ident)
                P.copy(craw[which], pt[:, 0:128], eng='act')
            P.ts(pad[0], craw[1], ci[:, j:j + 1], ALU.mult)
            P.stt(CrT[j], craw[0], cr[:, j:j + 1], pad[0], ALU.mult, ALU.subtract)
            P.ts(pad[1], craw[1], cr[:, j:j + 1], ALU.mult)
            P.stt(CiT[j], craw[0], ci[:, j:j + 1], pad[1], ALU.mult, ALU.add)
            P.ts(CiT[j], CiT[j], -1.0, ALU.mult)
        dsk = P.sb([128, 4])
        glb = P.sb([128, 8])
        with nc.allow_non_contiguous_dma(reason="tiny"):
            P.dma(dsk, I['even_d_skip'][i].re("(k p) -> p k", p=128))
            P.dma(glb, I['even_glu_b'][i].re("(k p) -> p k", p=128))
        nglb = P.sb([128, 8])
        P.ts(nglb, glb, -1.0, ALU.mult)
        gluw = P.sb([128, 4, 1024], BF16)
        P.dma(gluw, I['even_glu_w'][i].re("(k p) m -> p k m", p=128), eng='pool')
        ini = [P.sb([128, 2]) for _ in range(16)]
        for j in range(16):
            P.memset(ini[j], 0.0)
        uts = [P.sb([128, 4, 512]) for _ in range(2)]
        bu = [P.sb([128, 2, 512])] * 2
        zin = [P.sb([128, 2, 512])] * 2
        zz = [P.sb([128, 2, 512])] * 2
        xx = [P.sb([128, 2, 512]) for _ in range(2)]
        w1 = [P.sb([128, 512]) for _ in range(4)]
        sml = [P.sb([128, 2]) for _ in range(2)]
        yg = P.sb([128, 4, 512], BF16)
        ysb = [P.sb([128, 512]) for _ in range(2)]
        ga = [P.sb([128, 512]) for _ in range(2)]
        gb = [P.sb([128, 512]) for _ in range(2)]
        uv = C.uT.v.re("(k p) t -> p k t", p=128)
        P.dma(uts[0], uv[:, :, 0:512])
        it = 0
        for tt in range(NT):
            tsl = slice(tt * 512, (tt + 1) * 512)
            ut = uts[tt % 2]
            if tt + 1 < NT:
                P.dma(uts[(tt + 1) % 2], uv[:, :, (tt + 1) * 512:(tt + 2) * 512])
            for kt in range(4):
                py = C.PS[4 + (kt % 2)]
                for jj in range(4):
                    j = kt * 4 + jj
                    b = it % 2
                    it += 1
                    pr, pi_ = C.PS[0 + 2 * b], C.PS[1 + 2 * b]
                    P.mm(pr, BreT[j], ut[:, kt, :])
                    P.mm(pi_, BimT[j], ut[:, kt, :])
                    P.copy(bu[b][:, 0, :], pr, eng='act')
                    P.copy(bu[b][:, 1, :], pi_, eng='act')
                    P.tt(w1[0], bu[b][:, 0, :], Tc[:, j, :], ALU.mult)
                    P.tt(w1[1], bu[b][:, 1, :], Ts[:, j, :], ALU.mult, eng='pool')
                    P.tt(zin[b][:, 0, :], w1[0], w1[1], ALU.add)
                    P.tt(w1[2], bu[b][:, 1, :], Tc[:, j, :], ALU.mult, eng='pool')
                    P.tt(w1[3], bu[b][:, 0, :], Ts[:, j, :], ALU.mult)
                    P.tt(zin[b][:, 1, :], w1[2], w1[3], ALU.subtract, eng='pool')
                    P.scan(zz[b][:, 0, :], rho[:, j:j + 1].bc([128, 512]), zin[b][:, 0, :], ini[j][:, 0:1])
                    P.scan(zz[b][:, 1, :], rho[:, j:j + 1].bc([128, 512]), zin[b][:, 1, :], ini[j][:, 1:2])
                    P.tt(w1[0], zz[b][:, 0, :], Tc[:, j, :], ALU.mult)
                    P.tt(w1[1], zz[b][:, 1, :], Ts[:, j, :], ALU.mult, eng='pool')
                    P.tt(xx[b][:, 0, :], w1[0], w1[1], ALU.subtract)
                    P.tt(w1[2], zz[b][:, 1, :], Tc[:, j, :], ALU.mult, eng='pool')
                    P.tt(w1[3], zz[b][:, 0, :], Ts[:, j, :], ALU.mult)
                    P.tt(xx[b][:, 1, :], w1[2], w1[3], ALU.add, eng='pool')
                    sm_ = sml[b]
                    P.ts(sm_[:, 0:1], xx[b][:, 1, 511:512], nsn[:, j:j + 1], ALU.mult)
                    P.ts(sm_[:, 1:2], xx[b][:, 0, 511:512], sn[:, j:j + 1], ALU.mult)
                    P.stt(ini[j][:, 0:1], xx[b][:, 0, 511:512], cs[:, j:j + 1], sm_[:, 0:1], ALU.mult, ALU.add)
                    P.stt(ini[j][:, 1:2], xx[b][:, 1, 511:512], cs[:, j:j + 1], sm_[:, 1:2], ALU.mult, ALU.add)
                    P.mm(py, CrT[j], xx[b][:, 0, :], start=(jj == 0), stop=False)
                    P.mm(py, CiT[j], xx[b][:, 1, :], start=False, stop=(jj == 3))
                y = ysb[kt % 2]
                P.stt(y, ut[:, kt, :], dsk[:, kt:kt + 1], py, ALU.mult, ALU.add)
                P.act(yg[:, kt, :], y, AF.Gelu)
            for m in range(4):
                pa, pb = C.PS[6], C.PS[7]
                for k in range(4):
                    P.mm(pa, gluw[:, k, m * 128:(m + 1) * 128], yg[:, k, :], start=(k == 0), stop=(k == 3))
                for k in range(4):
                    P.mm(pb, gluw[:, k, 512 + m * 128:512 + (m + 1) * 128], yg[:, k, :], start=(k == 0), stop=(k == 3))
                a_, b_ = ga[m % 2], gb[m % 2]
                P.act(a_, pa, AF.Identity, bias=glb[:, m:m + 1])
                P.act(b_, pb, AF.Exp, bias=nglb[:, 4 + m:5 + m], scale=-1.0)
                P.ts(b_, b_, 1.0, ALU.add)
                P.recip(b_, b_)
                P.tt(a_, a_, b_, ALU.mult)
                P.dma(C.yT[512 + m * 128:512 + (m + 1) * 128, tsl], a_)
        P.barrier()
        P.flush()


def stage_ffn(P, C, L, l, xsrc, xdst, ysrc, wout, moe, G=2):
    nc = P.nc
    I = C.inp
    i = l // 2
    NT = L // 512
    G = min(G, NT)
    NG = NT // G
    mod = C.mod[l]
    with contextlib.ExitStack() as st:
        P.stack = st
        S = Ctx()
        faccs = [P.sb([128, 8, 512]) for _ in range(G)]
        S.sq = faccs[0]
        S.rstd = P.sb([128, 512])
        S.tmp = [P.sb([128, 512]) for _ in range(2)]
        hbs = [P.sb([128, 8, 512], BF16) for _ in range(G)]
        S.h32 = P.sb([128, 8, 512]) if moe else None
        S.wb = [P.sb([128, 8, 512], BF16) for _ in range(2)]
        S.wbi = 0
        w2b = [P.sb([128, 22, 128], BF16) for _ in range(2)]
        nw2 = 0
        xt = P.sb([128, 8, 512])
        ybf = P.sb([128, 8, 512], BF16)
        hids = [P.sb([128, 22, 512], BF16) for _ in range(G)]
        sa = [P.sb([128, 512]) for _ in range(3)]
        if moe:
            rw = P.sb([128, 8, 8])
            P.dma(rw, I['odd_router_w'][i].re("(k p) e -> p k e", p=128))
            lg = P.sb([8, 512])
            lt = P.sb([128, 4, 8])
            m1 = P.sb([128, 4])
            m2 = P.sb([128, 4])
            eq1 = P.sb([128, 4, 8])
            eq2 = P.sb([128, 4, 8])
            msk = P.sb([128, 4, 8])
            gg = P.sb([128, 4])
            g2_ = P.sb([128, 4])
            cmb = P.sb([128, 4, 8])
            cmbT = P.sb([8, 512])
            combs = [P.sb([128, 8, 512], BF16) for _ in range(G)]
        xv = xsrc.v.re("(k p) t -> p k t", p=128)
        yv = ysrc.v.re("(k p) t -> p k t", p=128)
        ov = xdst.v.re("(k p) t -> p k t", p=128)
        mv = C.xmid.v.re("(k p) t -> p k t", p=128)
        nsa = 0
        npp = 0
        for grp in range(NG):
            for g in range(G):
                tt = grp * G + g
                tsl = slice(tt * 512, (tt + 1) * 512)
                P.dma(xt, xv[:, :, tsl])
                P.dma(ybf, yv[:, :, tsl], eng='pool')
                for wsl, c0, w in wslab_iter(P, C, S, wout, 1024, 8):
                    for j in range(4):
                        m = c0 // 128 + j
                        pp = C.PS[1 + (m % 4)]
                        for k in range(8):
                            P.mm(pp, wsl[:, k, j * 128:(j + 1) * 128], ybf[:, k, :], start=(k == 0), stop=(k == 7))
                        P.stt(xt[:, m, :], pp, mod[:, 16 + m:17 + m], xt[:, m, :], ALU.mult, ALU.add)
                P.dma(mv[:, :, tsl], xt)
                S.hb = hbs[g]
                load_norm_h(P, C, S, xt, l, 2, want32=moe)
                if moe:
                    comb = combs[g]
                    pl = C.PS[5]
                    for k in range(8):
                        P.mm(pl[0:8, :], rw[:, k, :], S.h32[:, k, :], start=(k == 0), stop=(k == 7))
                    P.copy(lg, pl[0:8, :], eng='act')
                    pt = C.PS[6]
                    for s in range(4):
                        P.tr(pt[:, s * 8:(s + 1) * 8], lg[:, s * 128:(s + 1) * 128], C.ident[0:8, 0:8])
                    P.copy(lt, pt[:, 0:32].re("p (s e) -> p s e", e=8), eng='act')
                    P.reduce(m1, lt, ALU.max)
                    P.tt(eq1, lt, m1.v.re("p (s o) -> p s o", o=1).bc([128, 4, 8]), ALU.is_equal)
                    P.stt(msk, eq1, -1e30, lt, ALU.mult, ALU.add)
                    P.reduce(m2, msk, ALU.max)
                    P.tt(eq2, msk, m2.v.re("p (s o) -> p s o", o=1).bc([128, 4, 8]), ALU.is_equal)
                    P.tt(gg, m2, m1, ALU.subtract)
                    P.act(gg, gg, AF.Exp)
                    P.ts(gg, gg, 1.0, ALU.add)
                    P.recip(gg, gg)
                    P.ts(g2_, gg, -1.0, ALU.mult, 1.0, ALU.add)
                    P.tt(cmb, eq1, gg.v.re("p (s o) -> p s o", o=1).bc([128, 4, 8]), ALU.mult)
                    P.tt(eq2, eq2, g2_.v.re("p (s o) -> p s o", o=1).bc([128, 4, 8]), ALU.mult)
                    P.tt(cmb, cmb, eq2, ALU.add)
                    pc = C.PS[7]
                    for s in range(4):
                        P.tr(pc[0:8, s * 128:(s + 1) * 128], cmb[:, s, :], C.ident)
                    P.copy(cmbT, pc[0:8, :], eng='act')
                    for e in range(NEXP):
                        pb_ = C.PS[5 + (e % 2)]
                        P.mm(pb_, C.sel[:, e, :], cmbT)
                        P.copy(comb[:, e, :], pb_, eng='act')
            nexp = NEXP if moe else 1
            for e in range(nexp):
                if moe:
                    W13 = I['odd_expert_w13'][i][e]
                    W2 = I['odd_expert_w2'][i][e]
                else:
                    W13 = I['even_ffn_w13'][i]
                    W2 = I['even_ffn_w2'][i]
                for c0 in range(0, DFF, 256):
                    w = min(256, DFF - c0)
                    buf = S.wb[S.wbi % 2]
                    S.wbi += 1
                    P.dma(buf[:, :, 0:w], W13[:, c0:c0 + w].re("(k p) m -> p k m", p=128), eng='pool')
                    P.dma(buf[:, :, 256:256 + w], W13[:, DFF + c0:DFF + c0 + w].re("(k p) m -> p k m", p=128), eng='pool')
                    for j in range(w // 128):
                        f = c0 // 128 + j
                        for g in range(G):
                            pa, pb = C.PS[1 + 2 * (npp % 2)], C.PS[2 + 2 * (npp % 2)]
                            npp += 1
                            for k in range(8):
                                P.mm(pa, buf[:, k, j * 128:(j + 1) * 128], hbs[g][:, k, :], start=(k == 0), stop=(k == 7))
                            for k in range(8):
                                P.mm(pb, buf[:, k, 256 + j * 128:256 + (j + 1) * 128], hbs[g][:, k, :], start=(k == 0), stop=(k == 7))
                            s_ = sa[nsa % 3]
                            nsa += 1
                            P.act(s_, pa, AF.Silu)
                            if moe:
                                P.tt(s_, s_, combs[g][:, e, :], ALU.mult)
                            P.tt(hids[g][:, f, :], pb, s_, ALU.mult)
                for m in range(8):
                    wb2 = w2b[nw2 % 2]
                    nw2 += 1
                    P.dma(wb2, W2[:, m * 128:(m + 1) * 128].re("(f p) m -> p f m", p=128), eng='pool')
                    for g in range(G):
                        facc = faccs[g]
                        pp = C.PS[5 + ((m * G + g) % 3)]
                        for f in range(22):
                            P.mm(pp, wb2[:, f, :], hids[g][:, f, :], start=(f == 0), stop=(f == 21))
                        if e == 0:
                            P.copy(facc[:, m, :], pp, eng='act')
                        else:
                            P.tt(facc[:, m, :], pp, facc[:, m, :], ALU.add)
            for g in range(G):
                tt = grp * G + g
                tsl = slice(tt * 512, (tt + 1) * 512)
                P.dma(xt, mv[:, :, tsl])
                for m in range(8):
                    P.stt(faccs[g][:, m, :], faccs[g][:, m, :], mod[:, 40 + m:41 + m], xt[:, m, :], ALU.mult, ALU.add)
                P.dma(ov[:, :, tsl], faccs[g])
        P.barrier()
        P.flush()


def stage_attn_proj(P, C, L, l, xsrc):
    nc = P.nc
    I = C.inp
    i = l // 2
    NT = L // 512
    Wq = I['odd_w_qkv'][i]
    with contextlib.ExitStack() as st:
        P.stack = st
        S = Ctx()
        S.sq = P.sb([128, 8, 512])
        S.rstd = P.sb([128, 512])
        S.tmp = [P.sb([128, 512]) for _ in range(2)]
        S.hb = P.sb([128, 8, 512], BF16)
        S.h32 = None
        S.wb = [P.sb([128, 8, 512], BF16) for _ in range(3)]
        S.wbi = 0
        wv = P.sb([128, 8, 1024], BF16)
        P.dma(wv, Wq[:, 2048:3072].re("(k p) m -> p k m", p=128), eng='pool')
        nw = P.sb([128, 2])
        with nc.allow_non_contiguous_dma(reason="tiny"):
            for t in range(2):
                P.dma(nw[t * 64:(t + 1) * 64, 0:1], I['odd_q_norm_w'][i].re("(p o) -> p o", o=1))
                P.dma(nw[t * 64:(t + 1) * 64, 1:2], I['odd_k_norm_w'][i].re("(p o) -> p o", o=1))
        xts = [P.sb([128, 8, 512]) for _ in range(2)]
        raw = [P.sb([128, 512]) for _ in range(2)]
        sq = [P.sb([128, 512]) for _ in range(2)]
        rs = [P.sb([128, 512]) for _ in range(2)]
        ob = [P.sb([128, 512], BF16) for _ in range(3)]
        vb = [P.sb([128, 1024], BF16) for _ in range(2)]
        xv = xsrc.v.re("(k p) t -> p k t", p=128)
        P.dma(xts[0], xv[:, :, 0:512])
        n = 0
        for tt in range(NT):
            tsl = slice(tt * 512, (tt + 1) * 512)
            xt = xts[tt % 2]
            if tt + 1 < NT:
                P.dma(xts[(tt + 1) % 2], xv[:, :, (tt + 1) * 512:(tt + 2) * 512])
            load_norm_h(P, C, S, xt, l, 1)
            for wsl, c0, w in wslab_iter(P, C, S, Wq, 2048, 8):
                for j in range(4):
                    m = c0 // 128 + j
                    isq = m < 8
                    pp = C.PS[1 + (m % 4)]
                    for k in range(8):
                        P.mm(pp, wsl[:, k, j * 128:(j + 1) * 128], S.hb[:, k, :], start=(k == 0), stop=(k == 7))
                    r = raw[n % 2]
                    P.copy(r, pp, eng='act')
                    P.act(sq[n % 2], r, AF.Square)
                    pss = C.PS[5 + (n % 2)]
                    P.mm(pss, C.blk, sq[n % 2])
                    if isq:
                        rstd_from_ss(P, rs[n % 2], pss, 1.0, 64.0 * EPS)
                    else:
                        rstd_from_ss(P, rs[n % 2], pss, 1.0 / 64, EPS)
                    o = ob[n % 3]
                    P.stt(o, r, nw[:, (0 if isq else 1):(1 if isq else 2)], rs[n % 2], ALU.mult, ALU.mult)
                    P.dma(C.qkT[m * 128:(m + 1) * 128, tsl], o)
                    n += 1
            for s in range(4):
                v_ = vb[s % 2]
                for half in range(2):
                    pp = C.PS[1 + ((s * 2 + half) % 4)]
                    for k in range(8):
                        P.mm(pp, S.hb[:, k, s * 128:(s + 1) * 128], wv[:, k, half * 512:(half + 1) * 512],
                             start=(k == 0), stop=(k == 7))
                    P.copy(v_[:, half * 512:(half + 1) * 512], pp, eng=('act' if half else 'dve'))
                P.dma(C.vtok[tt * 512 + s * 128:tt * 512 + (s + 1) * 128, :], v_)
        P.barrier()
        P.flush()


def stage_attn(P, C, L, l):
    nc = P.nc
    I = C.inp
    i = l // 2
    NT = L // 512
    NK = L // 128
    lambda_init = 0.8 - 0.6 * math.exp(-0.3 * l)
    with contextlib.ExitStack() as st:
        P.stack = st
        lq = P.sb([128, 4, 64])
        for j, nm in enumerate(('odd_lambda_q1', 'odd_lambda_k1', 'odd_lambda_q2', 'odd_lambda_k2')):
            a = I[nm][i].re("(o d) -> o d", o=1)
            P.dma(lq[:, j, :], V(a.res, a.ap.to_broadcast([128, 64])))
        pr = P.sb([128, 2, 64])
        P.tt(pr[:, 0, :], lq[:, 0, :], lq[:, 1, :], ALU.mult)
        P.tt(pr[:, 1, :], lq[:, 2, :], lq[:, 3, :], ALU.mult)
        sm = P.sb([128, 2])
        P.reduce(sm, pr, ALU.add)
        P.act(sm, sm, AF.Exp)
        nlam = P.sb([128, 1])
        P.tt(nlam, sm[:, 1:2], sm[:, 0:1], ALU.subtract)
        P.ts(nlam, nlam, -lambda_init, ALU.add)
        sw = P.sb([128, 1])
        with nc.allow_non_contiguous_dma(reason="tiny"):
            P.dma(sw, I['odd_subln_w'][i].re("(p o) -> p o", o=1))
        P.ts(sw, sw, 1.0 - lambda_init, ALU.mult)
        kT = [P.sb([128, L], BF16) for _ in range(2)]
        vt = [P.sb([128, NK, 128], BF16) for _ in range(2)]
        qt_ = [P.sb([128, 512], BF16) for _ in range(2)]
        E = [P.sb([128, 512], BF16) for _ in range(4)]
        r1 = [P.sb([128, 512]) for _ in range(2)]
        r2 = [P.sb([128, 512]) for _ in range(2)]
        o1 = [P.sb([128, 512]) for _ in range(2)]
        sq = [P.sb([128, 512]) for _ in range(2)]
        ob = [P.sb([128, 512]) for _ in range(2)]
        ne = 0
        nq = 0
        for h in range(8):
            kk, vv = kT[h % 2], vt[h % 2]
            P.dma(kk, C.qkT[1024 + h * 128:1024 + (h + 1) * 128, :])
            P.dma(vv, C.vtok[:, h * 128:(h + 1) * 128].re("(n p) e -> p n e", p=128))
            for qt in range(NT):
                tsl = slice(qt * 512, (qt + 1) * 512)
                q = qt_[nq % 2]
                P.dma(q, C.qkT[h * 128:(h + 1) * 128, tsl])
                pn = [C.PS[0], C.PS[1]]
                pd = [C.PS[2], C.PS[3]]
                nkt = 4 * (qt + 1)
                its = [(kt, t) for kt in range(nkt) for t in range(2)]
                LA = 2
                ebuf = {}

                def emit_s(n):
                    nonlocal ne
                    kt, t = its[n]
                    psc = C.PS[4 + (ne % 4)]
                    P.mm(psc, kk[t * 64:(t + 1) * 64, kt * 128:(kt + 1) * 128], q[t * 64:(t + 1) * 64, :])
                    e_ = E[ne % 4]
                    ne += 1
                    P.act(e_, psc, AF.Exp, bias=C.neg8[:, 0:1])
                    r = kt - 4 * qt
                    if r >= 0:
                        P.tt(e_, e_, C.amask[:, r, :], ALU.mult, eng='pool')
                    ebuf[n] = e_

                def emit_md(n):
                    kt, t = its[n]
                    e_ = ebuf.pop(n)
                    P.mm(pn[t], vv[:, kt, :], e_, start=(kt == 0), stop=(kt == nkt - 1))
                    P.mm(pd[t], C.onesb, e_, start=(kt == 0), stop=(kt == nkt - 1))
                for n in range(len(its) + LA):
                    if n < len(its):
                        emit_s(n)
                    if n - LA >= 0:
                        emit_md(n - LA)
                b = nq % 2
                nq += 1
                P.recip(r1[b], pd[0])
                P.recip(r2[b], pd[1])
                P.tt(o1[b], pn[0], r1[b], ALU.mult)
                P.tt(r2[b], pn[1], r2[b], ALU.mult)
                P.stt(o1[b], r2[b], nlam[:, 0:1], o1[b], ALU.mult, ALU.add)
                P.act(sq[b], o1[b], AF.Square)
                pss = C.PS[2]
                P.mm(pss, C.ones, sq[b])
                rstd_from_ss(P, r1[b], pss, 1.0 / 128, EPS)
                P.stt(ob[b], o1[b], sw[:, 0:1], r1[b], ALU.mult, ALU.mult)
                P.dma(C.oT[h * 128:(h + 1) * 128, tsl], ob[b])
        P.barrier()
        P.flush()


def make_consts():
    c = {}
    c['c_ident'] = np.eye(128, dtype=np.float32)
    am = np.zeros((128, 4, 512), np.float32)
    kk = np.arange(128)[:, None]
    qq = np.arange(512)[None, :]
    for r in range(4):
        am[:, r, :] = ((r * 128 + kk) // 64 <= qq // 64)
    c['c_amask'] = am
    gm = np.zeros((64, 4, 512), np.float32)
    p = np.arange(64)[:, None]
    f = np.arange(64)[None, :]
    for cidx in range(8):
        sl = slice(cidx * 64, (cidx + 1) * 64)
        gm[:, 0, sl] = -1.0 * (p > f)
        gm[:, 1, sl] = -1.0 * (p < f)
        gm[:, 2, sl] = (p <= f)
        gm[:, 3, sl] = (p == f)
    c['c_gmask'] = gm
    sel = np.zeros((8, 8, 128), np.float32)
    for e in range(8):
        sel[e, e, :] = 1.0
    c['c_sel'] = sel
    blk = np.zeros((128, 128), np.float32)
    blk[:64, :64] = 1
    blk[64:, 64:] = 1
    c['c_blk'] = blk
    cm = np.ones((4, 512), np.float32)
    cm[:, ::64] = 0
    c['c_cmask'] = cm
    return c


INPUT_NAMES = ['ada_w', 'ada_b', 'norm_mix_w', 'norm_ffn_w',
               'even_w_in', 'even_conv_w', 'even_a_log', 'even_dt_bias', 'even_gdn_norm_w',
               'even_lam_re', 'even_lam_im', 'even_log_step', 'even_b_re', 'even_b_im', 'even_c_re', 'even_c_im',
               'even_d_skip', 'even_glu_w', 'even_glu_b', 'even_w_out', 'even_ffn_w13', 'even_ffn_w2',
               'odd_w_qkv', 'odd_q_norm_w', 'odd_k_norm_w', 'odd_lambda_q1', 'odd_lambda_k1', 'odd_lambda_q2',
               'odd_lambda_k2', 'odd_subln_w', 'odd_w_out', 'odd_router_w', 'odd_expert_w13', 'odd_expert_w2']


def build(shapes, L, depth=DEPTH, dbg=False):
    nc = bass.Bass("TRN2", target_bir_lowering=False)
    C = Ctx()
    C.depth = depth
    C.inp = {}
    consts = make_consts()
    for nm, shp in shapes.items():
        C.inp[nm] = Res(nm, nc.dram_tensor(nm, list(shp), F32, kind="ExternalInput").ap())
    for nm, arr in consts.items():
        C.inp[nm] = Res(nm, nc.dram_tensor(nm, list(arr.shape), F32, kind="ExternalInput").ap())
    C.out = Res('out', nc.dram_tensor('out', [L, D], F32, kind="ExternalOutput").ap())
    kind = "ExternalOutput" if dbg else "Internal"

    def scr(nm, shape, dt=F32):
        return Res(nm, nc.dram_tensor(nm, list(shape), dt, kind=kind).ap())
    C.xA = scr('xA', [D, L])
    C.xB = scr('xB', [D, L])
    C.qkvT = scr('qkvT', [1536, L])
    C.zT = scr('zT', [512, L])
    C.uT = scr('uT', [512, L])
    C.betaT = scr('betaT', [4, L])
    C.gT = scr('gT', [4, L])
    C.yT = scr('yT', [D, L])
    C.xmid = scr('xmid', [D, L])
    C.qkT = scr('qkT', [2048, L], BF16)
    C.vtok = scr('vtok', [L, D], BF16)
    C.oT = scr('oT', [D, L])
    with contextlib.ExitStack() as st0:
        P = Prog(nc, st0)
        C.PS = [P.ps([128, 512]) for _ in range(8)]
        C.ident = P.sb([128, 128])
        C.ones = P.sb([128, 128])
        C.onesb = P.sb([128, 128], BF16)
        C.blk = P.sb([128, 128])
        C.amask = P.sb([128, 4, 512], BF16)
        C.sel = P.sb([8, 8, 128])
        C.halfpi = P.sb([128, 1])
        C.neg8 = P.sb([128, 1])
        C.mod = [P.sb([128, 48]) for _ in range(depth)]
        C.modA = [P.sb([128, 16]) for _ in range(depth)]
        P.dma(C.ident, C.inp['c_ident'])
        P.dma(C.blk, C.inp['c_blk'])
        P.dma(C.amask, C.inp['c_amask'], eng='pool')
        P.dma(C.sel, C.inp['c_sel'])
        P.memset(C.ones, 1.0)
        P.memset(C.onesb, 1.0)
        P.memset(C.halfpi, math.pi / 2)
        P.memset(C.neg8, -8.0)
        stage_prep(P, C, L)
        cur, nxt = C.xA, C.xB
        for l in range(depth):
            i = l // 2
            if l % 2 == 0:
                stage_even_proj(P, C, L, l, cur)
                stage_gdn(P, C, L, l)
                stage_s5(P, C, L, l)
                stage_ffn(P, C, L, l, cur, nxt, C.yT, C.inp['even_w_out'][i], moe=False)
            else:
                stage_attn_proj(P, C, L, l, cur)
                stage_attn(P, C, L, l)
                stage_ffn(P, C, L, l, cur, nxt, C.oT, C.inp['odd_w_out'][i], moe=True)
            cur, nxt = nxt, cur
        stage_out(P, C, L, cur)
        P.stack = st0
        C.nins = P.nins
    return nc, consts, C


def kernel(**inputs):
    x = np.asarray(inputs['x'], dtype=np.float32)
    B, L, _ = x.shape
    shapes = {'x': (L, D), 'c': (D,)}
    for nm in INPUT_NAMES:
        shapes[nm] = tuple(np.asarray(inputs[nm]).shape)
    nc, consts, C = build(shapes, L)
    shared = {nm: np.ascontiguousarray(np.asarray(inputs[nm], dtype=np.float32)) for nm in INPUT_NAMES}
    zeros = {nm: np.zeros_like(v) for nm, v in shared.items()}
    zc = {nm: np.zeros_like(v) for nm, v in consts.items()}
    real = [0, 1, 4, 5][:B]
    in_maps = []
    for core in range(8):
        if core in real:
            b = real.index(core)
            m = dict(shared)
            m.update(consts)
            m['x'] = np.ascontiguousarray(x[b])
            m['c'] = np.ascontiguousarray(np.asarray(inputs['c'], dtype=np.float32)[b])
        else:
            m = dict(zeros)
            m.update(zc)
            m['x'] = np.zeros((L, D), np.float32)
            m['c'] = np.zeros((D,), np.float32)
        in_maps.append(m)
    res = run_bass_kernel_spmd(nc, in_maps, core_ids=list(range(8)))
    out = np.stack([res.results[real[b]]['out'] for b in range(B)], axis=0)
    return out.astype(np.float32)
```

```python
import contextlib
import math
import numpy as np
import concourse.bass as bass
import concourse.mybir as mybir
from concourse.bass_utils import run_bass_kernel_spmd

F32 = mybir.dt.float32
BF16 = mybir.dt.bfloat16
AF = mybir.ActivationFunctionType
ALU = mybir.AluOpType
AX = mybir.AxisListType

D = 1024
DEPTH = 4
EPS = 1e-6
DFF = 2816
NEXP = 8
ENGS = ['pe', 'act', 'dve', 'pool', 'sp']


class V:
    __slots__ = ('res', 'ap')

    def __init__(self, res, ap):
        self.res = res
        self.ap = ap

    def __getitem__(self, idx):
        return V(self.res, self.ap[idx])

    def bc(self, shape):
        return V(self.res, self.ap.to_broadcast(list(shape)))

    def re(self, s, **kw):
        return V(self.res, self.ap.rearrange(s, **kw))


class Res:
    __slots__ = ('name', 'w', 'r', 'ap')

    def __init__(self, name, ap=None):
        self.name = name
        self.w = {}
        self.r = {}
        self.ap = ap

    def __getitem__(self, idx):
        return V(self, self.ap[idx])

    @property
    def v(self):
        return V(self, self.ap)


def _rv(x):
    return x.v if isinstance(x, Res) else x


class Prog:
    def __init__(self, nc, stack, n_dma_sems=32):
        self.nc = nc
        self.stack = stack
        self.streams = {e: [] for e in ENGS}
        self.cnt = {e: 0 for e in ENGS}
        self.semh = {}
        for e in ['pe', 'act', 'dve', 'pool']:
            self.semh['c_' + e] = stack.enter_context(nc.semaphore('c_' + e))
        self.ndma = n_dma_sems
        self.dma_tot = [0] * n_dma_sems
        self.dma_next = 0
        for k in range(n_dma_sems):
            self.semh['d%d' % k] = stack.enter_context(nc.semaphore('d%d' % k))
        self.known = {e: {} for e in ENGS}
        self.nt = 0
        self.nins = 0

    def sb(self, shape, dtype=F32, name=None):
        self.nt += 1
        name = name or ('t%d' % self.nt)
        t = self.stack.enter_context(self.nc.sbuf_tensor(name, list(shape), dtype))
        return Res(name, t[:])

    def ps(self, shape, dtype=F32, name=None):
        self.nt += 1
        name = name or ('p%d' % self.nt)
        t = self.stack.enter_context(self.nc.psum_tensor(name, list(shape), dtype))
        return Res(name, t[:])

    def dram(self, name, shape, dtype, kind="Internal"):
        t = self.nc.dram_tensor(name, list(shape), dtype, kind=kind)
        return Res(name, t.ap())

    def _deps(self, eng, reads, writes):
        deps = {}

        def add(k, v):
            if deps.get(k, -1) < v:
                deps[k] = v
        for r in reads:
            for k, v in r.w.items():
                add(k, v)
        for w in writes:
            for k, v in w.w.items():
                add(k, v)
            for k, v in w.r.items():
                add(k, v)
        out = []
        kn = self.known[eng]
        for k, v in deps.items():
            if eng == 'pe' and k == 'c_pe':
                continue
            if kn.get(k, -1) < v:
                kn[k] = v
                out.append((k, v))
        return out

    def _record(self, ev, reads, writes, merge=False):
        k, v = ev
        for w in writes:
            if merge:
                w.w[k] = v
            else:
                w.w = {k: v}
            w.r = {}
        for r in reads:
            if r.r.get(k, -1) < v:
                r.r[k] = v

    def op(self, eng, fn, reads=(), writes=(), inc=True):
        reads = [x for x in reads if x is not None]
        waits = self._deps(eng, reads, writes)
        key = 'c_' + eng
        if inc:
            self.cnt[eng] += 1
            ev = (key, self.cnt[eng])
        else:
            ev = (key, self.cnt[eng] + 1)
        self.streams[eng].append((waits, fn, key if inc else None, 1))
        self._record(ev, reads, writes)
        self.nins += 1

    def dma(self, out, in_, eng='sp'):
        out = _rv(out)
        in_ = _rv(in_)
        k = self.dma_next
        self.dma_next = (k + 1) % self.ndma
        key = 'd%d' % k
        waits = self._deps(eng, [in_.res], [out.res])
        kn = self.known[eng]
        if kn.get(key, -1) < self.dma_tot[k]:
            kn[key] = self.dma_tot[k]
            waits.append((key, self.dma_tot[k]))
        self.dma_tot[k] += 16
        ev = (key, self.dma_tot[k])
        oa, ia = out.ap, in_.ap

        def fn(e):
            return e.dma_start(out=oa, in_=ia)
        self.streams[eng].append((waits, fn, key, 16))
        self._record(ev, [in_.res], [out.res], merge=True)
        self.nins += 1

    def collective(self, kind, out, in_, groups, eng='pool'):
        out = _rv(out)
        in_ = _rv(in_)
        k = self.dma_next
        self.dma_next = (k + 1) % self.ndma
        key = 'd%d' % k
        waits = self._deps(eng, [in_.res], [out.res])
        kn = self.known[eng]
        if kn.get(key, -1) < self.dma_tot[k]:
            kn[key] = self.dma_tot[k]
            waits.append((key, self.dma_tot[k]))
        self.dma_tot[k] += 16
        ev = (key, self.dma_tot[k])
        oa, ia = out.ap, in_.ap

        def fn(e):
            return e.collective_compute(kind, ALU.bypass, groups, [ia], [oa])
        self.streams[eng].append((waits, fn, key, 16))
        self._record(ev, [in_.res], [out.res], merge=True)
        self.nins += 1

    def barrier(self):
        allw = [('c_' + e, self.cnt[e]) for e in ['pe', 'act', 'dve', 'pool']]
        allw += [('d%d' % k, self.dma_tot[k]) for k in range(self.ndma)]
        for e in ENGS:
            kn = self.known[e]
            waits = []
            for k, v in allw:
                if e == 'pe' and k == 'c_pe':
                    continue
                if kn.get(k, -1) < v:
                    kn[k] = v
                    waits.append((k, v))
            self.streams[e].append((waits, None, None, 0))

    def flush(self):
        nc = self.nc
        engobj = {'pe': 'tensor', 'act': 'scalar', 'dve': 'vector', 'pool': 'gpsimd', 'sp': 'sync'}
        semh = self.semh
        with nc.allow_non_contiguous_dma(reason="small strided param loads"), nc.Block() as block:
            for e in ENGS:
                stream = self.streams[e]

                def body(eng, stream=stream):
                    for waits, fn, key, n in stream:
                        for k, v in waits:
                            eng.wait_ge(semh[k], v)
                        if fn is not None:
                            ins = fn(eng)
                            if key is not None:
                                ins.then_inc(semh[key], n)
                getattr(block, engobj[e])(body)
        self.streams = {e: [] for e in ENGS}

    def mm(self, out, lhsT, rhs, start=True, stop=True):
        out, lhsT, rhs = _rv(out), _rv(lhsT), _rv(rhs)
        o, l, r = out.ap, lhsT.ap, rhs.ap
        self.op('pe', lambda e: e.matmul(o, l, r, start=start, stop=stop),
                [lhsT.res, rhs.res], [out.res], inc=stop)

    def tr(self, out, in_, ident):
        out, in_, ident = _rv(out), _rv(in_), _rv(ident)
        o, i, d = out.ap, in_.ap, ident.ap
        self.op('pe', lambda e: e.transpose(o, i, d), [in_.res, ident.res], [out.res])

    def act(self, out, in_, func, bias=None, scale=None, eng='act'):
        out, in_ = _rv(out), _rv(in_)
        reads = [in_.res]
        kw = {}
        if bias is not None:
            if isinstance(bias, (V, Res)):
                bias = _rv(bias)
                reads.append(bias.res)
                kw['bias'] = bias.ap
            else:
                kw['bias'] = float(bias)
        if scale is not None:
            if isinstance(scale, (V, Res)):
                scale = _rv(scale)
                reads.append(scale.res)
                kw['scale'] = scale.ap
            else:
                kw['scale'] = float(scale)
        o, i = out.ap, in_.ap
        self.op(eng, lambda e: e.activation(o, i, func, **kw), reads, [out.res])

    def tt(self, out, a, b, op, eng='dve'):
        out, a, b = _rv(out), _rv(a), _rv(b)
        o, x, y = out.ap, a.ap, b.ap
        self.op(eng, lambda e: e.tensor_tensor(o, x, y, op), [a.res, b.res], [out.res])

    def ts(self, out, a, s1, op0, s2=None, op1=None, eng='dve'):
        out, a = _rv(out), _rv(a)
        reads = [a.res]

        def cv(s):
            if isinstance(s, (V, Res)):
                s = _rv(s)
                reads.append(s.res)
                return s.ap
            return None if s is None else float(s)
        c1, c2 = cv(s1), cv(s2)
        o, x = out.ap, a.ap
        if op1 is None:
            self.op(eng, lambda e: e.tensor_single_scalar(o, x, c1, op0), reads, [out.res])
        else:
            self.op(eng, lambda e: e.tensor_scalar(o, x, c1, c2, op0, op1), reads, [out.res])

    def stt(self, out, a, s, b, op0, op1):
        out, a, b = _rv(out), _rv(a), _rv(b)
        reads = [a.res, b.res]
        if isinstance(s, (V, Res)):
            s = _rv(s)
            reads.append(s.res)
            c = s.ap
        else:
            c = float(s)
        o, x, y = out.ap, a.ap, b.ap
        self.op('dve', lambda e: e.scalar_tensor_tensor(o, x, c, y, op0, op1), reads, [out.res])

    def copy(self, out, in_, eng='dve'):
        out, in_ = _rv(out), _rv(in_)
        o, i = out.ap, in_.ap
        if eng == 'act':
            self.op('act', lambda e: e.activation(o, i, AF.Copy), [in_.res], [out.res])
        else:
            self.op(eng, lambda e: e.tensor_copy(o, i), [in_.res], [out.res])

    def recip(self, out, in_):
        out, in_ = _rv(out), _rv(in_)
        o, i = out.ap, in_.ap
        self.op('dve', lambda e: e.reciprocal(o, i), [in_.res], [out.res])

    def memset(self, out, val, eng='dve'):
        out = _rv(out)
        o = out.ap
        self.op(eng, lambda e: e.memset(o, float(val)), [], [out.res])

    def scan(self, out, d0, d1, init, op0=ALU.mult, op1=ALU.add):
        out, d0, d1 = _rv(out), _rv(d0), _rv(d1)
        reads = [d0.res, d1.res]
        if isinstance(init, (V, Res)):
            init = _rv(init)
            reads.append(init.res)
            c = init.ap
        else:
            c = float(init)
        o, x, y = out.ap, d0.ap, d1.ap
        self.op('dve', lambda e: e.tensor_tensor_scan(o, x, y, c, op0, op1), reads, [out.res])

    def reduce(self, out, in_, op, axis=AX.X):
        out, in_ = _rv(out), _rv(in_)
        o, i = out.ap, in_.ap
        self.op('dve', lambda e: e.tensor_reduce(o, i, axis, op), [in_.res], [out.res])


class Ctx:
    pass


def rstd_from_ss(P, out_sb, ss_ps, scale, bias):
    P.act(out_sb, ss_ps, AF.Ln, bias=bias, scale=scale)
    P.act(out_sb, out_sb, AF.Exp, scale=-0.5)


def stage_prep(P, C, L):
    nc = P.nc
    I = C.inp
    with contextlib.ExitStack() as st:
        P.stack = st
        cT = P.sb([128, 8])
        with nc.allow_non_contiguous_dma(reason="tiny"):
            P.dma(cT, I['c'].v.re("(k p) -> p k", p=128))
        cond = P.sb([128, 8])
        P.act(cond, cT, AF.Silu)
        wbuf = [P.sb([128, 8, 768]) for _ in range(2)]
        pm = C.PS[0]
        for l in range(C.depth):
            bt = P.sb([128, 48])
            with nc.allow_non_contiguous_dma(reason="tiny"):
                P.dma(bt, I['ada_b'][l].re("(j p) -> p j", p=128))
            for cb in range(8):
                wb = wbuf[cb % 2]
                P.dma(wb, I['ada_w'][l][:, cb * 768:(cb + 1) * 768].re("(k p) m -> p k m", p=128))
                for j in range(6):
                    col = cb * 6 + j
                    for k in range(8):
                        P.mm(pm[:, col:col + 1], wb[:, k, j * 128:(j + 1) * 128], cond[:, k:k + 1],
                             start=(k == 0), stop=(k == 7))
            mod = C.mod[l]
            P.tt(mod, pm[:, 0:48], bt, ALU.add)
            nw = P.sb([128, 16])
            with nc.allow_non_contiguous_dma(reason="tiny"):
                P.dma(nw[:, 0:8], I['norm_mix_w'][l].re("(k p) -> p k", p=128))
                P.dma(nw[:, 8:16], I['norm_ffn_w'][l].re("(k p) -> p k", p=128))
            A = C.modA[l]
            P.stt(A[:, 0:8], mod[:, 8:16], 1.0, nw[:, 0:8], ALU.add, ALU.mult)
            P.stt(A[:, 8:16], mod[:, 32:40], 1.0, nw[:, 8:16], ALU.add, ALU.mult)
        xin = [P.sb([128, 1024]) for _ in range(2)]
        xo = [P.sb([128, 8, 512]) for _ in range(2)]
        for tt in range(L // 512):
            o = xo[tt % 2]
            for s in range(4):
                xi = xin[s % 2]
                t0 = tt * 512 + s * 128
                P.dma(xi, I['x'][t0:t0 + 128, :])
                for k in range(8):
                    pt = C.PS[1 + (k % 4)]
                    P.tr(pt[:, 0:128], xi[:, k * 128:(k + 1) * 128], C.ident)
                    P.copy(o[:, k, s * 128:(s + 1) * 128], pt[:, 0:128], eng=('act' if k % 2 else 'dve'))
            P.dma(C.xA.v.re("(k p) t -> p k t", p=128)[:, :, tt * 512:(tt + 1) * 512], o)
        P.barrier()
        P.flush()


def stage_out(P, C, L, xsrc):
    I = C.inp
    with contextlib.ExitStack() as st:
        P.stack = st
        xi = [P.sb([128, 8, 512]) for _ in range(2)]
        xo = [P.sb([128, 1024]) for _ in range(2)]
        n = 0
        for tt in range(L // 512):
            t = xi[tt % 2]
            P.dma(t, xsrc.v.re("(k p) t -> p k t", p=128)[:, :, tt * 512:(tt + 1) * 512])
            for s in range(4):
                o = xo[s % 2]
                for k in range(8):
                    pt = C.PS[1 + (k % 4)]
                    P.tr(pt[:, 0:128], t[:, k, s * 128:(s + 1) * 128], C.ident)
                    P.copy(o[:, k * 128:(k + 1) * 128], pt[:, 0:128], eng=('act' if k % 2 else 'dve'))
                t0 = tt * 512 + s * 128
                P.dma(C.out[t0:t0 + 128, :], o)
        P.barrier()
        P.flush()


def load_norm_h(P, C, S, xt, l, which, want32=False):
    A = C.modA[l]
    mod = C.mod[l]
    aoff = 0 if which == 1 else 8
    shoff = 0 if which == 1 else 24
    sq = S.sq
    P.act(sq, xt, AF.Square)
    ss = C.PS[0]
    for k in range(8):
        P.mm(ss, C.ones, sq[:, k, :], start=(k == 0), stop=(k == 7))
    rstd = S.rstd
    rstd_from_ss(P, rstd, ss, 1.0 / D, EPS)
    for k in range(8):
        tmp = S.tmp[k % 2]
        P.stt(tmp, xt[:, k, :], A[:, aoff + k:aoff + k + 1], rstd, ALU.mult, ALU.mult)
        if want32:
            P.act(S.h32[:, k, :], tmp, AF.Identity, bias=mod[:, shoff + k:shoff + k + 1])
            P.act(S.hb[:, k, :], tmp, AF.Identity, bias=mod[:, shoff + k:shoff + k + 1])
        else:
            P.act(S.hb[:, k, :], tmp, AF.Identity, bias=mod[:, shoff + k:shoff + k + 1])


def wslab_iter(P, C, S, Wv, ncols, KT, slab=512):
    c0 = 0
    i = 0
    while c0 < ncols:
        w = min(slab, ncols - c0)
        buf = S.wb[S.wbi % len(S.wb)]
        S.wbi += 1
        P.dma(buf[:, 0:KT, 0:w], Wv[:, c0:c0 + w].re("(k p) m -> p k m", p=128), eng='pool')
        yield buf, c0, w
        c0 += w
        i += 1


def stage_even_proj(P, C, L, l, xsrc):
    nc = P.nc
    I = C.inp
    i = l // 2
    Win = I['even_w_in'][i]
    with contextlib.ExitStack() as st:
        P.stack = st
        S = Ctx()
        S.sq = P.sb([128, 8, 512])
        S.rstd = P.sb([128, 512])
        S.tmp = [P.sb([128, 512]) for _ in range(2)]
        S.hb = P.sb([128, 8, 512], BF16)
        S.wb = [P.sb([128, 8, 512], BF16) for _ in range(3)]
        S.wbi = 0
        C.cmask = P.sb([4, 512])
        P.dma(C.cmask, C.inp['c_cmask'])
        xts = [P.sb([128, 8, 512]) for _ in range(2)]
        pre = [P.sb([128, 515]) for _ in range(12)]
        for m in range(12):
            P.memset(pre[m][:, 0:3], 0.0)
        cw = P.sb([128, 12, 4])
        with nc.allow_non_contiguous_dma(reason="tiny"):
            for j in range(4):
                P.dma(cw[:, :, j], I['even_conv_w'][i][j].re("(t p) -> p t", p=128))
        wba = P.sb([128, 8, 8])
        with nc.allow_non_contiguous_dma(reason="small"):
            P.dma(wba, Win[:, 2048:2056].re("(k p) m -> p k m", p=128))
        hc = P.sb([4, 2])
        with nc.allow_non_contiguous_dma(reason="tiny"):
            P.dma(hc[:, 0:1], I['even_a_log'][i].re("(h o) -> h o", o=1))
            P.dma(hc[:, 1:2], I['even_dt_bias'][i].re("(h o) -> h o", o=1))
        nA = P.sb([4, 1])
        P.act(nA, hc[:, 0:1], AF.Exp)
        P.ts(nA, nA, -1.0, ALU.mult)
        h32 = P.sb([128, 8, 512])
        S.h32 = h32
        acc = [P.sb([128, 512]) for _ in range(2)]
        ob = [P.sb([128, 512]) for _ in range(3)]
        sm = [P.sb([4, 512]) for _ in range(6)]
        NT = L // 512
        xv = xsrc.v.re("(k p) t -> p k t", p=128)
        P.dma(xts[0], xv[:, :, 0:512])
        nob = 0
        for tt in range(NT):
            xt = xts[tt % 2]
            if tt + 1 < NT:
                P.dma(xts[(tt + 1) % 2], xv[:, :, (tt + 1) * 512:(tt + 2) * 512])
            load_norm_h(P, C, S, xt, l, 1, want32=True)
            tsl = slice(tt * 512, (tt + 1) * 512)
            pb, pa = C.PS[1], C.PS[2]
            for k in range(8):
                P.mm(pb[0:4, :], wba[:, k, 0:4], h32[:, k, :], start=(k == 0), stop=(k == 7))
            for k in range(8):
                P.mm(pa[0:4, :], wba[:, k, 4:8], h32[:, k, :], start=(k == 0), stop=(k == 7))
            beta = sm[0]
            P.act(beta[:], pb[0:4, :], AF.Exp, scale=-1.0)
            P.ts(beta, beta, 1.0, ALU.add)
            P.recip(beta, beta)
            P.dma(C.betaT[:, tsl], beta)
            xa = sm[1]
            P.act(xa[:], pa[0:4, :], AF.Identity, bias=hc[:, 1:2])
            ax = sm[2]
            P.stt(ax, xa, -1.0, xa, ALU.mult, ALU.max)
            P.act(ax, ax, AF.Exp, scale=-1.0)
            P.act(ax, ax, AF.Ln, bias=1.0)
            sp = sm[3]
            P.stt(sp, xa, 0.0, ax, ALU.max, ALU.add)
            g = sm[4]
            P.ts(g, sp, nA[:, 0:1], ALU.mult)
            gc = sm[5]
            P.scan(gc, C.cmask, g, 0.0)
            P.dma(C.gT[:, tsl], gc)
            mt = 0
            for wsl, c0, w in wslab_iter(P, C, S, Win, 2048, 8):
                for j in range(w // 128):
                    m = (c0 // 128) + j
                    pp = C.PS[3 + (m % 4)]
                    for k in range(8):
                        P.mm(pp, wsl[:, k, j * 128:(j + 1) * 128], S.hb[:, k, :], start=(k == 0), stop=(k == 7))
                    if m < 12:
                        pr = pre[m]
                        P.copy(pr[:, 3:515], pp, eng='act')
                        a = acc[m % 2]
                        P.ts(a, pr[:, 0:512], cw[:, m, 0:1], ALU.mult)
                        for jj in range(1, 4):
                            P.stt(a, pr[:, jj:jj + 512], cw[:, m, jj:jj + 1], a, ALU.mult, ALU.add)
                        P.copy(pr[:, 0:3], pr[:, 512:515], eng='pool')
                        o = ob[nob % 3]
                        nob += 1
                        P.act(o, a, AF.Silu)
                        if m < 8:
                            sq = S.tmp[m % 2]
                            P.act(sq, o, AF.Square)
                            ss = C.PS[7]
                            P.mm(ss, C.ones, sq)
                            rs = S.rstd
                            if m < 4:
                                rstd_from_ss(P, rs, ss, 128.0, 128.0 * EPS)
                            else:
                                rstd_from_ss(P, rs, ss, 1.0, EPS)
                            P.tt(o, o, rs, ALU.mult)
                        P.dma(C.qkvT[m * 128:(m + 1) * 128, tsl], o)
                    else:
                        o = ob[nob % 3]
                        nob += 1
                        P.act(o, pp, AF.Silu)
                        P.dma(C.zT[(m - 12) * 128:(m - 11) * 128, tsl], o)
            for wsl, c0, w in wslab_iter(P, C, S, Win[:, 2056:2568], 512, 8):
                for j in range(4):
                    pp = C.PS[3 + (j % 4)]
                    for k in range(8):
                        P.mm(pp, wsl[:, k, j * 128:(j + 1) * 128], S.hb[:, k, :], start=(k == 0), stop=(k == 7))
                    o = ob[nob % 3]
                    nob += 1
                    P.copy(o, pp, eng='act')
                    P.dma(C.uT[j * 128:(j + 1) * 128, tsl], o)
        P.barrier()
        P.flush()


def stage_gdn(P, C, L, l):
    nc = P.nc
    I = C.inp
    i = l // 2
    NT = L // 512
    with contextlib.ExitStack() as st:
        P.stack = st
        C.gmask = P.sb([64, 4, 512])
        P.dma(C.gmask, C.inp['c_gmask'])
        Sst = [P.sb([128, 128]) for _ in range(4)]
        for h in range(4):
            P.memset(Sst[h], 0.0)
        gw = P.sb([128, 1])
        with nc.allow_non_contiguous_dma(reason="tiny"):
            P.dma(gw, I['even_gdn_norm_w'][i].re("(p o) -> p o", o=1))
        mk = lambda n, shape, dt=F32: [P.sb(shape, dt) for _ in range(n)]
        qT, kT, vT = mk(2, [128, 512]), mk(2, [128, 512]), mk(2, [128, 512])
        rows = mk(2, [1, 2, 512])
        r_eg, r_ekl, r_ng = mk(2, [1, 512]), mk(2, [1, 512]), mk(2, [1, 512])
        bcs = mk(2, [128, 3, 512])
        kb, kbe, vb, qe, kel = (mk(2, [128, 512]) for _ in range(5))
        tokm = mk(2, [64, 3, 8, 128])
        EL, EU, t1 = mk(2, [64, 512]), mk(2, [64, 512]), mk(2, [64, 512])
        Am, ATm, PTm, Rm = mk(2, [64, 512]), mk(2, [64, 512]), mk(2, [64, 512]), mk(2, [64, 512])
        WTn = mk(2, [128, 512])
        vnew = [mk(2, [64, 128]) for _ in range(2)]
        zt = mk(2, [128, 512])
        osb = mk(2, [128, 512])
        sq = mk(2, [128, 512])
        rs = mk(2, [128, 512])
        def chain(tt, h, b, BK):
            tsl = slice(tt * 512, (tt + 1) * 512)
            P.dma(qT[b], C.qkvT[h * 128:(h + 1) * 128, tsl])
            P.dma(kT[b], C.qkvT[512 + h * 128:512 + (h + 1) * 128, tsl])
            P.dma(vT[b], C.qkvT[1024 + h * 128:1024 + (h + 1) * 128, tsl])
            P.dma(rows[b][:, 0, :], C.betaT[h:h + 1, tsl])
            P.dma(rows[b][:, 1, :], C.gT[h:h + 1, tsl])
            P.dma(zt[b], C.zT[h * 128:(h + 1) * 128, tsl])
            gc = rows[b][:, 1, :]
            P.act(r_eg[b], gc, AF.Exp)
            g3 = rows[b][:, 1, :].re("o (c j) -> o c j", j=64)
            P.tt(r_ekl[b].v.re("o (c j) -> o c j", j=64), g3[:, :, 63:64].bc([1, 8, 64]), g3, ALU.subtract)
            P.act(r_ekl[b], r_ekl[b], AF.Exp)
            P.ts(r_ng[b], gc, -1.0, ALU.mult)
            yield
            pbc = [BK[0], BK[1], BK[2]]
            P.mm(pbc[0], C.ones[0:1, :], rows[b][:, 0, :])
            P.mm(pbc[1], C.ones[0:1, :], r_eg[b])
            P.mm(pbc[2], C.ones[0:1, :], r_ekl[b])
            for j in range(3):
                P.copy(bcs[b][:, j, :], pbc[j], eng='act')
            P.tt(kb[b], kT[b], bcs[b][:, 0, :], ALU.mult)
            P.tt(kbe[b], kb[b], bcs[b][:, 1, :], ALU.mult, eng='pool')
            P.tt(vb[b], vT[b], bcs[b][:, 0, :], ALU.mult)
            P.tt(qe[b], qT[b], bcs[b][:, 1, :], ALU.mult, eng='pool')
            P.tt(kel[b], kT[b], bcs[b][:, 2, :], ALU.mult)
            yield
            for c in range(8):
                csl = slice(c * 64, (c + 1) * 64)
                for j, src in enumerate((vb[b], kbe[b], kel[b])):
                    pt = BK[3 - ((c * 3 + j) % 2)]
                    P.tr(pt[0:64, 0:128], src[:, csl], C.ident)
                    P.copy(tokm[b][:, j, c, :], pt[0:64, 0:128], eng=('act' if j != 1 else 'dve'))
                yield
            pg = BK[2]
            for c in range(8):
                csl = slice(c * 64, (c + 1) * 64)
                P.mm(pg[0:64, csl], rows[b][:, 1, csl], C.ones[0:1, 0:64], start=True, stop=False)
                P.mm(pg[0:64, csl], C.ones[0:1, 0:64], r_ng[b][:, csl], start=False, stop=True)
            P.ts(t1[b], pg[0:64, :], 0.0, ALU.min)
            P.act(EL[b], t1[b], AF.Exp)
            P.ts(t1[b], pg[0:64, :], 0.0, ALU.max)
            P.act(EU[b], t1[b], AF.Exp, scale=-1.0)
            yield
            pA, pAT, pPT = BK[0], BK[1], BK[2]
            for c in range(8):
                csl = slice(c * 64, (c + 1) * 64)
                P.mm(pA[0:64, csl], kb[b][:, csl], kT[b][:, csl])
                P.mm(pAT[0:64, csl], kT[b][:, csl], kb[b][:, csl])
                P.mm(pPT[0:64, csl], kT[b][:, csl], qT[b][:, csl])
            P.tt(t1[b], EL[b], C.gmask[:, 0, :], ALU.mult, eng='pool')
            P.tt(Am[b], pA[0:64, :], t1[b], ALU.mult)
            P.tt(EL[b], EU[b], C.gmask[:, 1, :], ALU.mult, eng='pool')
            P.tt(ATm[b], pAT[0:64, :], EL[b], ALU.mult)
            P.tt(EU[b], EU[b], C.gmask[:, 2, :], ALU.mult, eng='pool')
            P.tt(PTm[b], pPT[0:64, :], EU[b], ALU.mult)
            yield
            P.tt(Rm[b], ATm[b], C.gmask[:, 3, :], ALU.add)
            X, XT = Am[b], ATm[b]
            X2, XT2 = t1[b], EL[b]
            for step in range(5):
                p1, p2, p3 = BK[0], BK[1], BK[2]
                for c in range(8):
                    csl = slice(c * 64, (c + 1) * 64)
                    P.mm(p1[0:64, csl], XT[:, csl], X[:, csl])
                    P.mm(p2[0:64, csl], X[:, csl], XT[:, csl])
                P.copy(X2, p1[0:64, :], eng='act')
                P.copy(XT2, p2[0:64, :], eng='dve')
                yield
                X, X2 = X2, X
                XT, XT2 = XT2, XT
                for c in range(8):
                    csl = slice(c * 64, (c + 1) * 64)
                    P.mm(p3[0:64, csl], X[:, csl], Rm[b][:, csl])
                P.tt(Rm[b], Rm[b], p3[0:64, :], ALU.add)
                yield
            pW = BK[0]
            for c in range(8):
                csl = slice(c * 64, (c + 1) * 64)
                P.mm(pW[:, csl], tokm[b][:, 1, c, :], Rm[b][:, csl])
            P.act(WTn[b], pW, AF.Copy, scale=-1.0)
            yield
            po = BK[3]
            S = Sst[h]
            for c in range(8):
                csl = slice(c * 64, (c + 1) * 64)
                pv = BK[1]
                P.mm(pv[0:64, 0:128], Rm[b][:, csl], tokm[b][:, 0, c, :], start=True, stop=False)
                P.mm(pv[0:64, 0:128], WTn[b][:, csl], S, start=False, stop=True)
                vn = vnew[b][c % 2]
                P.copy(vn, pv[0:64, 0:128], eng='act')
                P.mm(po[:, csl], S, qe[b][:, csl], start=True, stop=False)
                P.mm(po[:, csl], vn, PTm[b][:, csl], start=False, stop=True)
                ps_ = BK[2]
                P.mm(ps_[:, 0:128], tokm[b][:, 2, c, :], vn)
                P.stt(S, S, bcs[b][:, 1, c * 64 + 63:c * 64 + 64], ps_[:, 0:128], ALU.mult, ALU.add)
                yield
            P.copy(osb[b], po, eng='act')
            P.act(sq[b], osb[b], AF.Square)
            pss = BK[0]
            P.mm(pss, C.ones, sq[b])
            rstd_from_ss(P, rs[b], pss, 1.0 / 128, EPS)
            P.stt(osb[b], osb[b], gw[:, 0:1], rs[b], ALU.mult, ALU.mult)
            P.tt(osb[b], osb[b], zt[b], ALU.mult)
            P.dma(C.yT[h * 128:(h + 1) * 128, tsl], osb[b])
        for tt in range(NT):
            for hp in range(2):
                gens = [chain(tt, 2 * hp, 0, C.PS[0:4]), chain(tt, 2 * hp + 1, 1, C.PS[4:8])]
                while gens:
                    for g_ in list(gens):
                        try:
                            next(g_)
                        except StopIteration:
                            gens.remove(g_)
        P.barrier()
        P.flush()


def stage_s5(P, C, L, l):
    nc = P.nc
    I = C.inp
    i = l // 2
    NT = L // 512
    TWO_PI = 2.0 * math.pi
    with contextlib.ExitStack() as st:
        P.stack = st
        lr = P.sb([128, 16])
        li = P.sb([128, 16])
        ls = P.sb([128, 16])
        with nc.allow_non_contiguous_dma(reason="small"):
            P.dma(lr, I['even_lam_re'][i].re("(j g) n -> (g n) j", g=2))
            P.dma(li, I['even_lam_im'][i].re("(j g) n -> (g n) j", g=2))
            lsv = I['even_log_step'][i].re("(j g) -> g j", g=2)
            for g2 in range(2):
                P.dma(ls[g2 * 64:(g2 + 1) * 64, :], lsv[g2:g2 + 1, :].bc([64, 16]))
        dt = P.sb([128, 16])
        P.act(dt, ls, AF.Exp)
        P.ts(lr, lr, -1e-4, ALU.min)
        rho = P.sb([128, 16])
        P.tt(rho, lr, dt, ALU.mult)
        P.act(rho, rho, AF.Exp)
        th = P.sb([128, 16])
        P.tt(th, li, dt, ALU.mult)
        tmpa = P.sb([128, 16])
        sn = P.sb([128, 16])
        cs = P.sb([128, 16])
        P.act(sn, th, AF.Sin, scale=1.0 / 16)
        P.act(cs, th, AF.Sin, scale=1.0 / 16, bias=C.halfpi[:, 0:1])
        t_c2, t_s2 = P.sb([128, 16]), P.sb([128, 16])
        for _ in range(4):
            P.tt(t_c2, cs, cs, ALU.mult)
            P.tt(t_s2, sn, sn, ALU.mult)
            P.tt(sn, cs, sn, ALU.mult)
            P.ts(sn, sn, 2.0, ALU.mult)
            P.tt(cs, t_c2, t_s2, ALU.subtract)
        ar, ai = P.sb([128, 16]), P.sb([128, 16])
        P.tt(ar, rho, cs, ALU.mult)
        P.tt(ai, rho, sn, ALU.mult)
        nr = P.sb([128, 16])
        P.ts(nr, ar, -1.0, ALU.add)
        den = P.sb([128, 16])
        t2 = P.sb([128, 16])
        P.tt(den, lr, lr, ALU.mult)
        P.tt(t2, li, li, ALU.mult)
        P.tt(den, den, t2, ALU.add)
        P.recip(den, den)
        cr, ci = P.sb([128, 16]), P.sb([128, 16])
        P.tt(cr, nr, lr, ALU.mult)
        P.tt(t2, ai, li, ALU.mult)
        P.tt(cr, cr, t2, ALU.add)
        P.tt(cr, cr, den, ALU.mult)
        P.tt(ci, ai, lr, ALU.mult)
        P.tt(t2, nr, li, ALU.mult)
        P.tt(ci, ci, t2, ALU.subtract)
        P.tt(ci, ci, den, ALU.mult)
        nsn = P.sb([128, 16])
        P.ts(nsn, sn, -1.0, ALU.mult)
        Tc = P.sb([128, 16, 512])
        Ts = P.sb([128, 16, 512])
        P.memset(Tc[:, :, 0:1], 1.0)
        P.memset(Ts[:, :, 0:1], 0.0)
        cc, s_ = P.sb([128, 16]), P.sb([128, 16])
        P.copy(cc, cs)
        P.copy(s_, sn)
        ta, tb = P.sb([128, 256]), P.sb([128, 256])
        c2, s2 = P.sb([128, 16]), P.sb([128, 16])
        span = 1
        while span < 512:
            for j in range(16):
                P.ts(ta[:, 0:span], Ts[:, j, 0:span], s_[:, j:j + 1], ALU.mult)
                P.stt(Tc[:, j, span:2 * span], Tc[:, j, 0:span], cc[:, j:j + 1], ta[:, 0:span], ALU.mult, ALU.subtract)
                P.ts(tb[:, 0:span], Tc[:, j, 0:span], s_[:, j:j + 1], ALU.mult)
                P.stt(Ts[:, j, span:2 * span], Ts[:, j, 0:span], cc[:, j:j + 1], tb[:, 0:span], ALU.mult, ALU.add)
            P.tt(c2, cc, cc, ALU.mult)
            P.tt(s2, s_, s_, ALU.mult)
            P.tt(s_, cc, s_, ALU.mult)
            P.ts(s_, s_, 2.0, ALU.mult)
            P.tt(cc, c2, s2, ALU.subtract)
            span *= 2
        BreT = [P.sb([128, 128]) for _ in range(16)]
        BimT = [P.sb([128, 128]) for _ in range(16)]
        CrT = [P.sb([128, 128]) for _ in range(16)]
        CiT = [P.sb([128, 128]) for _ in range(16)]
        pad = [P.sb([128, 128]) for _ in range(4)]
        craw = [P.sb([128, 128]) for _ in range(2)]
        for j in range(16):
            kt = j // 4
            for which, (nm, dst) in enumerate((('even_b_re', BreT), ('even_b_im', BimT))):
                pd = pad[which]
                P.memset(pd, 0.0)
                for g2 in range(2):
                    g = 2 * j + g2
                    off = (g - 8 * kt) * 16
                    P.dma(pd[g2 * 64:(g2 + 1) * 64, off:off + 16], I[nm][i][g])
                pt = C.PS[which]
                P.tr(pt[:, 0:128], pd, C.ident)
                P.copy(dst[j], pt[:, 0:128], eng='act')
            for which, nm in enumerate(('even_c_re', 'even_c_im')):
                pd = pad[2 + which]
                P.memset(pd, 0.0)
                for g2 in range(2):
                    g = 2 * j + g2
                    off = (g - 8 * kt) * 16
                    P.dma(pd[off:off + 16, g2 * 64:(g2 + 1) * 64], I[nm][i][g])
                pt = C.PS[2 + which]
                P.tr(pt[:, 0:128], pd, C.ident)
                P.copy(craw[which], pt[:, 0:128], eng='act')
            P.ts(pad[0], craw[1], ci[:, j:j + 1], ALU.mult)
            P.stt(CrT[j], craw[0], cr[:, j:j + 1], pad[0], ALU.mult, ALU.subtract)
            P.ts(pad[1], craw[1], cr[:, j:j + 1], ALU.mult)
            P.stt(CiT[j], craw[0], ci[:, j:j + 1], pad[1], ALU.mult, ALU.add)
            P.ts(CiT[j], CiT[j], -1.0, ALU.mult)
        dsk = P.sb([128, 4])
        glb = P.sb([128, 8])
        with nc.allow_non_contiguous_dma(reason="tiny"):
            P.dma(dsk, I['even_d_skip'][i].re("(k p) -> p k", p=128))
            P.dma(glb, I['even_glu_b'][i].re("(k p) -> p k", p=128))
        nglb = P.sb([128, 8])
        P.ts(nglb, glb, -1.0, ALU.mult)
        gluw = P.sb([128, 4, 1024], BF16)
        P.dma(gluw, I['even_glu_w'][i].re("(k p) m -> p k m", p=128), eng='pool')
        ini = [P.sb([128, 2]) for _ in range(16)]
        for j in range(16):
            P.memset(ini[j], 0.0)
        uts = [P.sb([128, 4, 512]) for _ in range(2)]
        bu = [P.sb([128, 2, 512])] * 2
        zin = [P.sb([128, 2, 512])] * 2
        zz = [P.sb([128, 2, 512])] * 2
        xx = [P.sb([128, 2, 512]) for _ in range(2)]
        w1 = [P.sb([128, 512]) for _ in range(4)]
        sml = [P.sb([128, 2]) for _ in range(2)]
        yg = P.sb([128, 4, 512], BF16)
        ysb = [P.sb([128, 512]) for _ in range(2)]
        ga = [P.sb([128, 512]) for _ in range(2)]
        gb = [P.sb([128, 512]) for _ in range(2)]
        uv = C.uT.v.re("(k p) t -> p k t", p=128)
        P.dma(uts[0], uv[:, :, 0:512])
        it = 0
        for tt in range(NT):
            tsl = slice(tt * 512, (tt + 1) * 512)
            ut = uts[tt % 2]
            if tt + 1 < NT:
                P.dma(uts[(tt + 1) % 2], uv[:, :, (tt + 1) * 512:(tt + 2) * 512])
            for kt in range(4):
                py = C.PS[4 + (kt % 2)]
                for jj in range(4):
                    j = kt * 4 + jj
                    b = it % 2
                    it += 1
                    pr, pi_ = C.PS[0 + 2 * b], C.PS[1 + 2 * b]
                    P.mm(pr, BreT[j], ut[:, kt, :])
                    P.mm(pi_, BimT[j], ut[:, kt, :])
                    P.copy(bu[b][:, 0, :], pr, eng='act')
                    P.copy(bu[b][:, 1, :], pi_, eng='act')
                    P.tt(w1[0], bu[b][:, 0, :], Tc[:, j, :], ALU.mult)
                    P.tt(w1[1], bu[b][:, 1, :], Ts[:, j, :], ALU.mult, eng='pool')
                    P.tt(zin[b][:, 0, :], w1[0], w1[1], ALU.add)
                    P.tt(w1[2], bu[b][:, 1, :], Tc[:, j, :], ALU.mult, eng='pool')
                    P.tt(w1[3], bu[b][:, 0, :], Ts[:, j, :], ALU.mult)
                    P.tt(zin[b][:, 1, :], w1[2], w1[3], ALU.subtract, eng='pool')
                    P.scan(zz[b][:, 0, :], rho[:, j:j + 1].bc([128, 512]), zin[b][:, 0, :], ini[j][:, 0:1])
                    P.scan(zz[b][:, 1, :], rho[:, j:j + 1].bc([128, 512]), zin[b][:, 1, :], ini[j][:, 1:2])
                    P.tt(w1[0], zz[b][:, 0, :], Tc[:, j, :], ALU.mult)
                    P.tt(w1[1], zz[b][:, 1, :], Ts[:, j, :], ALU.mult, eng='pool')
                    P.tt(xx[b][:, 0, :], w1[0], w1[1], ALU.subtract)
                    P.tt(w1[2], zz[b][:, 1, :], Tc[:, j, :], ALU.mult, eng='pool')
                    P.tt(w1[3], zz[b][:, 0, :], Ts[:, j, :], ALU.mult)
                    P.tt(xx[b][:, 1, :], w1[2], w1[3], ALU.add, eng='pool')
                    sm_ = sml[b]
                    P.ts(sm_[:, 0:1], xx[b][:, 1, 511:512], nsn[:, j:j + 1], ALU.mult)
                    P.ts(sm_[:, 1:2], xx[b][:, 0, 511:512], sn[:, j:j + 1], ALU.mult)
                    P.stt(ini[j][:, 0:1], xx[b][:, 0, 511:512], cs[:, j:j + 1], sm_[:, 0:1], ALU.mult, ALU.add)
                    P.stt(ini[j][:, 1:2], xx[b][:, 1, 511:512], cs[:, j:j + 1], sm_[:, 1:2], ALU.mult, ALU.add)
                    P.mm(py, CrT[j], xx[b][:, 0, :], start=(jj == 0), stop=False)
                    P.mm(py, CiT[j], xx[b][:, 1, :], start=False, stop=(jj == 3))
                y = ysb[kt % 2]
                P.stt(y, ut[:, kt, :], dsk[:, kt:kt + 1], py, ALU.mult, ALU.add)
                P.act(yg[:, kt, :], y, AF.Gelu)
            for m in range(4):
                pa, pb = C.PS[6], C.PS[7]
                for k in range(4):
                    P.mm(pa, gluw[:, k, m * 128:(m + 1) * 128], yg[:, k, :], start=(k == 0), stop=(k == 3))
                for k in range(4):
                    P.mm(pb, gluw[:, k, 512 + m * 128:512 + (m + 1) * 128], yg[:, k, :], start=(k == 0), stop=(k == 3))
                a_, b_ = ga[m % 2], gb[m % 2]
                P.act(a_, pa, AF.Identity, bias=glb[:, m:m + 1])
                P.act(b_, pb, AF.Exp, bias=nglb[:, 4 + m:5 + m], scale=-1.0)
                P.ts(b_, b_, 1.0, ALU.add)
                P.recip(b_, b_)
                P.tt(a_, a_, b_, ALU.mult)
                P.dma(C.yT[512 + m * 128:512 + (m + 1) * 128, tsl], a_)
        P.barrier()
        P.flush()


def stage_ffn(P, C, L, l, xsrc, xdst, ysrc, wout, moe, G=2):
    nc = P.nc
    I = C.inp
    i = l // 2
    NT = L // 512
    G = min(G, NT)
    NG = NT // G
    mod = C.mod[l]
    with contextlib.ExitStack() as st:
        P.stack = st
        S = Ctx()
        faccs = [P.sb([128, 8, 512]) for _ in range(G)]
        S.sq = faccs[0]
        S.rstd = P.sb([128, 512])
        S.tmp = [P.sb([128, 512]) for _ in range(2)]
        hbs = [P.sb([128, 8, 512], BF16) for _ in range(G)]
        S.h32 = P.sb([128, 8, 512]) if moe else None
        S.wb = [P.sb([128, 8, 512], BF16) for _ in range(2)]
        S.wbi = 0
        w2b = [P.sb([128, 22, 128], BF16) for _ in range(2)]
        nw2 = 0
        xt = P.sb([128, 8, 512])
        ybf = P.sb([128, 8, 512], BF16)
        hids = [P.sb([128, 22, 512], BF16) for _ in range(G)]
        sa = [P.sb([128, 512]) for _ in range(3)]
        if moe:
            rw = P.sb([128, 8, 8])
            P.dma(rw, I['odd_router_w'][i].re("(k p) e -> p k e", p=128))
            lg = P.sb([8, 512])
            lt = P.sb([128, 4, 8])
            m1 = P.sb([128, 4])
            m2 = P.sb([128, 4])
            eq1 = P.sb([128, 4, 8])
            eq2 = P.sb([128, 4, 8])
            msk = P.sb([128, 4, 8])
            gg = P.sb([128, 4])
            g2_ = P.sb([128, 4])
            cmb = P.sb([128, 4, 8])
            cmbT = P.sb([8, 512])
            combs = [P.sb([128, 8, 512], BF16) for _ in range(G)]
        xv = xsrc.v.re("(k p) t -> p k t", p=128)
        yv = ysrc.v.re("(k p) t -> p k t", p=128)
        ov = xdst.v.re("(k p) t -> p k t", p=128)
        mv = C.xmid.v.re("(k p) t -> p k t", p=128)
        nsa = 0
        npp = 0
        for grp in range(NG):
            for g in range(G):
                tt = grp * G + g
                tsl = slice(tt * 512, (tt + 1) * 512)
                P.dma(xt, xv[:, :, tsl])
                P.dma(ybf, yv[:, :, tsl], eng='pool')
                for wsl, c0, w in wslab_iter(P, C, S, wout, 1024, 8):
                    for j in range(4):
                        m = c0 // 128 + j
                        pp = C.PS[1 + (m % 4)]
                        for k in range(8):
                            P.mm(pp, wsl[:, k, j * 128:(j + 1) * 128], ybf[:, k, :], start=(k == 0), stop=(k == 7))
                        P.stt(xt[:, m, :], pp, mod[:, 16 + m:17 + m], xt[:, m, :], ALU.mult, ALU.add)
                P.dma(mv[:, :, tsl], xt)
                S.hb = hbs[g]
                load_norm_h(P, C, S, xt, l, 2, want32=moe)
                if moe:
                    comb = combs[g]
                    pl = C.PS[5]
                    for k in range(8):
                        P.mm(pl[0:8, :], rw[:, k, :], S.h32[:, k, :], start=(k == 0), stop=(k == 7))
                    P.copy(lg, pl[0:8, :], eng='act')
                    pt = C.PS[6]
                    for s in range(4):
                        P.tr(pt[:, s * 8:(s + 1) * 8], lg[:, s * 128:(s + 1) * 128], C.ident[0:8, 0:8])
                    P.copy(lt, pt[:, 0:32].re("p (s e) -> p s e", e=8), eng='act')
                    P.reduce(m1, lt, ALU.max)
                    P.tt(eq1, lt, m1.v.re("p (s o) -> p s o", o=1).bc([128, 4, 8]), ALU.is_equal)
                    P.stt(msk, eq1, -1e30, lt, ALU.mult, ALU.add)
                    P.reduce(m2, msk, ALU.max)
                    P.tt(eq2, msk, m2.v.re("p (s o) -> p s o", o=1).bc([128, 4, 8]), ALU.is_equal)
                    P.tt(gg, m2, m1, ALU.subtract)
                    P.act(gg, gg, AF.Exp)
                    P.ts(gg, gg, 1.0, ALU.add)
                    P.recip(gg, gg)
                    P.ts(g2_, gg, -1.0, ALU.mult, 1.0, ALU.add)
                    P.tt(cmb, eq1, gg.v.re("p (s o) -> p s o", o=1).bc([128, 4, 8]), ALU.mult)
                    P.tt(eq2, eq2, g2_.v.re("p (s o) -> p s o", o=1).bc([128, 4, 8]), ALU.mult)
                    P.tt(cmb, cmb, eq2, ALU.add)
                    pc = C.PS[7]
                    for s in range(4):
                        P.tr(pc[0:8, s * 128:(s + 1) * 128], cmb[:, s, :], C.ident)
                    P.copy(cmbT, pc[0:8, :], eng='act')
                    for e in range(NEXP):
                        pb_ = C.PS[5 + (e % 2)]
                        P.mm(pb_, C.sel[:, e, :], cmbT)
                        P.copy(comb[:, e, :], pb_, eng='act')
            nexp = NEXP if moe else 1
            for e in range(nexp):
                if moe:
                    W13 = I['odd_expert_w13'][i][e]
                    W2 = I['odd_expert_w2'][i][e]
                else:
                    W13 = I['even_ffn_w13'][i]
                    W2 = I['even_ffn_w2'][i]
                for c0 in range(0, DFF, 256):
                    w = min(256, DFF - c0)
                    buf = S.wb[S.wbi % 2]
                    S.wbi += 1
                    P.dma(buf[:, :, 0:w], W13[:, c0:c0 + w].re("(k p) m -> p k m", p=128), eng='pool')
                    P.dma(buf[:, :, 256:256 + w], W13[:, DFF + c0:DFF + c0 + w].re("(k p) m -> p k m", p=128), eng='pool')
                    for j in range(w // 128):
                        f = c0 // 128 + j
                        for g in range(G):
                            pa, pb = C.PS[1 + 2 * (npp % 2)], C.PS[2 + 2 * (npp % 2)]
                            npp += 1
                            for k in range(8):
                                P.mm(pa, buf[:, k, j * 128:(j + 1) * 128], hbs[g][:, k, :], start=(k == 0), stop=(k == 7))
                            for k in range(8):
                                P.mm(pb, buf[:, k, 256 + j * 128:256 + (j + 1) * 128], hbs[g][:, k, :], start=(k == 0), stop=(k == 7))
                            s_ = sa[nsa % 3]
                            nsa += 1
                            P.act(s_, pa, AF.Silu)
                            if moe:
                                P.tt(s_, s_, combs[g][:, e, :], ALU.mult)
                            P.tt(hids[g][:, f, :], pb, s_, ALU.mult)
                for m in range(8):
                    wb2 = w2b[nw2 % 2]
                    nw2 += 1
                    P.dma(wb2, W2[:, m * 128:(m + 1) * 128].re("(f p) m -> p f m", p=128), eng='pool')
                    for g in range(G):
                        facc = faccs[g]
                        pp = C.PS[5 + ((m * G + g) % 3)]
                        for f in range(22):
                            P.mm(pp, wb2[:, f, :], hids[g][:, f, :], start=(f == 0), stop=(f == 21))
                        if e == 0:
                            P.copy(facc[:, m, :], pp, eng='act')
                        else:
                            P.tt(facc[:, m, :], pp, facc[:, m, :], ALU.add)
            for g in range(G):
                tt = grp * G + g
                tsl = slice(tt * 512, (tt + 1) * 512)
                P.dma(xt, mv[:, :, tsl])
                for m in range(8):
                    P.stt(faccs[g][:, m, :], faccs[g][:, m, :], mod[:, 40 + m:41 + m], xt[:, m, :], ALU.mult, ALU.add)
                P.dma(ov[:, :, tsl], faccs[g])
        P.barrier()
        P.flush()


def stage_attn_proj(P, C, L, l, xsrc):
    nc = P.nc
    I = C.inp
    i = l // 2
    NT = L // 512
    Wq = I['odd_w_qkv'][i]
    with contextlib.ExitStack() as st:
        P.stack = st
        S = Ctx()
        S.sq = P.sb([128, 8, 512])
        S.rstd = P.sb([128, 512])
        S.tmp = [P.sb([128, 512]) for _ in range(2)]
        S.hb = P.sb([128, 8, 512], BF16)
        S.h32 = None
        S.wb = [P.sb([128, 8, 512], BF16) for _ in range(3)]
        S.wbi = 0
        wv = P.sb([128, 8, 1024], BF16)
        P.dma(wv, Wq[:, 2048:3072].re("(k p) m -> p k m", p=128), eng='pool')
        nw = P.sb([128, 2])
        with nc.allow_non_contiguous_dma(reason="tiny"):
            for t in range(2):
                P.dma(nw[t * 64:(t + 1) * 64, 0:1], I['odd_q_norm_w'][i].re("(p o) -> p o", o=1))
                P.dma(nw[t * 64:(t + 1) * 64, 1:2], I['odd_k_norm_w'][i].re("(p o) -> p o", o=1))
        xts = [P.sb([128, 8, 512]) for _ in range(2)]
        raw = [P.sb([128, 512]) for _ in range(2)]
        sq = [P.sb([128, 512]) for _ in range(2)]
        rs = [P.sb([128, 512]) for _ in range(2)]
        ob = [P.sb([128, 512], BF16) for _ in range(3)]
        vb = [P.sb([128, 1024], BF16) for _ in range(2)]
        xv = xsrc.v.re("(k p) t -> p k t", p=128)
        P.dma(xts[0], xv[:, :, 0:512])
        n = 0
        for tt in range(NT):
            tsl = slice(tt * 512, (tt + 1) * 512)
            xt = xts[tt % 2]
            if tt + 1 < NT:
                P.dma(xts[(tt + 1) % 2], xv[:, :, (tt + 1) * 512:(tt + 2) * 512])
            load_norm_h(P, C, S, xt, l, 1)
            for wsl, c0, w in wslab_iter(P, C, S, Wq, 2048, 8):
                for j in range(4):
                    m = c0 // 128 + j
                    isq = m < 8
                    pp = C.PS[1 + (m % 4)]
                    for k in range(8):
                        P.mm(pp, wsl[:, k, j * 128:(j + 1) * 128], S.hb[:, k, :], start=(k == 0), stop=(k == 7))
                    r = raw[n % 2]
                    P.copy(r, pp, eng='act')
                    P.act(sq[n % 2], r, AF.Square)
                    pss = C.PS[5 + (n % 2)]
                    P.mm(pss, C.blk, sq[n % 2])
                    if isq:
                        rstd_from_ss(P, rs[n % 2], pss, 1.0, 64.0 * EPS)
                    else:
                        rstd_from_ss(P, rs[n % 2], pss, 1.0 / 64, EPS)
                    o = ob[n % 3]
                    P.stt(o, r, nw[:, (0 if isq else 1):(1 if isq else 2)], rs[n % 2], ALU.mult, ALU.mult)
                    P.dma(C.qkT[m * 128:(m + 1) * 128, tsl], o)
                    n += 1
            for s in range(4):
                v_ = vb[s % 2]
                for half in range(2):
                    pp = C.PS[1 + ((s * 2 + half) % 4)]
                    for k in range(8):
                        P.mm(pp, S.hb[:, k, s * 128:(s + 1) * 128], wv[:, k, half * 512:(half + 1) * 512],
                             start=(k == 0), stop=(k == 7))
                    P.copy(v_[:, half * 512:(half + 1) * 512], pp, eng=('act' if half else 'dve'))
                P.dma(C.vtok[tt * 512 + s * 128:tt * 512 + (s + 1) * 128, :], v_)
        P.barrier()
        P.flush()


def stage_attn(P, C, L, l):
    nc = P.nc
    I = C.inp
    i = l // 2
    NT = L // 512
    NK = L // 128
    lambda_init = 0.8 - 0.6 * math.exp(-0.3 * l)
    with contextlib.ExitStack() as st:
        P.stack = st
        lq = P.sb([128, 4, 64])
        for j, nm in enumerate(('odd_lambda_q1', 'odd_lambda_k1', 'odd_lambda_q2', 'odd_lambda_k2')):
            a = I[nm][i].re("(o d) -> o d", o=1)
            P.dma(lq[:, j, :], V(a.res, a.ap.to_broadcast([128, 64])))
        pr = P.sb([128, 2, 64])
        P.tt(pr[:, 0, :], lq[:, 0, :], lq[:, 1, :], ALU.mult)
        P.tt(pr[:, 1, :], lq[:, 2, :], lq[:, 3, :], ALU.mult)
        sm = P.sb([128, 2])
        P.reduce(sm, pr, ALU.add)
        P.act(sm, sm, AF.Exp)
        nlam = P.sb([128, 1])
        P.tt(nlam, sm[:, 1:2], sm[:, 0:1], ALU.subtract)
        P.ts(nlam, nlam, -lambda_init, ALU.add)
        sw = P.sb([128, 1])
        with nc.allow_non_contiguous_dma(reason="tiny"):
            P.dma(sw, I['odd_subln_w'][i].re("(p o) -> p o", o=1))
        P.ts(sw, sw, 1.0 - lambda_init, ALU.mult)
        kT = [P.sb([128, L], BF16) for _ in range(2)]
        vt = [P.sb([128, NK, 128], BF16) for _ in range(2)]
        qt_ = [P.sb([128, 512], BF16) for _ in range(2)]
        E = [P.sb([128, 1024], BF16) for _ in range(4)]
        r1 = [P.sb([128, 512]) for _ in range(2)]
        r2 = [P.sb([128, 512]) for _ in range(2)]
        o1 = [P.sb([128, 512]) for _ in range(2)]
        sq = [P.sb([128, 512]) for _ in range(2)]
        ob = [P.sb([128, 512]) for _ in range(2)]
        ne = 0
        nq = 0
        for h in range(8):
            kk, vv = kT[h % 2], vt[h % 2]
            P.dma(kk, C.qkT[1024 + h * 128:1024 + (h + 1) * 128, :])
            P.dma(vv, C.vtok[:, h * 128:(h + 1) * 128].re("(n p) e -> p n e", p=128))
            for qt in range(NT):
                tsl = slice(qt * 512, (qt + 1) * 512)
                q = qt_[nq % 2]
                P.dma(q, C.qkT[h * 128:(h + 1) * 128, tsl])
                pn = [C.PS[0], C.PS[1]]
                pd = [C.PS[2], C.PS[3]]
                nkt = 4 * (qt + 1)
                LA = 1
                ebuf = {}

                def emit_s(kt):
                    nonlocal ne
                    half = ne % 2
                    psc = C.PSS[:, half * 1024:(half + 1) * 1024]
                    for t in range(2):
                        P.mm(psc[:, t * 512:(t + 1) * 512], kk[t * 64:(t + 1) * 64, kt * 128:(kt + 1) * 128],
                             q[t * 64:(t + 1) * 64, :])
                    e_ = E[ne % 4]
                    ne += 1
                    P.act(e_, psc, AF.Exp, bias=C.neg8[:, 0:1])
                    r = kt - 4 * qt
                    if r >= 0:
                        P.tt(e_.v.re("p (t q) -> p t q", t=2), e_.v.re("p (t q) -> p t q", t=2),
                             C.amask[:, r:r + 1, :].bc([128, 2, 512]), ALU.mult, eng='pool')
                    ebuf[kt] = e_

                def emit_md(kt):
                    e_ = ebuf.pop(kt)
                    for t in range(2):
                        P.mm(pn[t], vv[:, kt, :], e_[:, t * 512:(t + 1) * 512], start=(kt == 0), stop=(kt == nkt - 1))
                        P.mm(pd[t], C.onesb, e_[:, t * 512:(t + 1) * 512], start=(kt == 0), stop=(kt == nkt - 1))
                for n in range(nkt + LA):
                    if n < nkt:
                        emit_s(n)
                    if n - LA >= 0:
                        emit_md(n - LA)
                b = nq % 2
                nq += 1
                P.recip(r1[b], pd[0])
                P.recip(r2[b], pd[1])
                P.tt(o1[b], pn[0], r1[b], ALU.mult)
                P.tt(r2[b], pn[1], r2[b], ALU.mult)
                P.stt(o1[b], r2[b], nlam[:, 0:1], o1[b], ALU.mult, ALU.add)
                P.act(sq[b], o1[b], AF.Square)
                pss = C.PS[2]
                P.mm(pss, C.ones, sq[b])
                rstd_from_ss(P, r1[b], pss, 1.0 / 128, EPS)
                P.stt(ob[b], o1[b], sw[:, 0:1], r1[b], ALU.mult, ALU.mult)
                P.dma(C.oT[h * 128:(h + 1) * 128, tsl], ob[b])
        P.barrier()
        P.flush()


def make_consts():
    c = {}
    c['c_ident'] = np.eye(128, dtype=np.float32)
    am = np.zeros((128, 4, 512), np.float32)
    kk = np.arange(128)[:, None]
    qq = np.arange(512)[None, :]
    for r in range(4):
        am[:, r, :] = ((r * 128 + kk) // 64 <= qq // 64)
    c['c_amask'] = am
    gm = np.zeros((64, 4, 512), np.float32)
    p = np.arange(64)[:, None]
    f = np.arange(64)[None, :]
    for cidx in range(8):
        sl = slice(cidx * 64, (cidx + 1) * 64)
        gm[:, 0, sl] = -1.0 * (p > f)
        gm[:, 1, sl] = -1.0 * (p < f)
        gm[:, 2, sl] = (p <= f)
        gm[:, 3, sl] = (p == f)
    c['c_gmask'] = gm
    sel = np.zeros((8, 8, 128), np.float32)
    for e in range(8):
        sel[e, e, :] = 1.0
    c['c_sel'] = sel
    blk = np.zeros((128, 128), np.float32)
    blk[:64, :64] = 1
    blk[64:, 64:] = 1
    c['c_blk'] = blk
    cm = np.ones((4, 512), np.float32)
    cm[:, ::64] = 0
    c['c_cmask'] = cm
    return c


INPUT_NAMES = ['ada_w', 'ada_b', 'norm_mix_w', 'norm_ffn_w',
               'even_w_in', 'even_conv_w', 'even_a_log', 'even_dt_bias', 'even_gdn_norm_w',
               'even_lam_re', 'even_lam_im', 'even_log_step', 'even_b_re', 'even_b_im', 'even_c_re', 'even_c_im',
               'even_d_skip', 'even_glu_w', 'even_glu_b', 'even_w_out', 'even_ffn_w13', 'even_ffn_w2',
               'odd_w_qkv', 'odd_q_norm_w', 'odd_k_norm_w', 'odd_lambda_q1', 'odd_lambda_k1', 'odd_lambda_q2',
               'odd_lambda_k2', 'odd_subln_w', 'odd_w_out', 'odd_router_w', 'odd_expert_w13', 'odd_expert_w2']


def build(shapes, L, depth=DEPTH, dbg=False):
    nc = bass.Bass("TRN2", target_bir_lowering=False)
    C = Ctx()
    C.depth = depth
    C.inp = {}
    consts = make_consts()
    for nm, shp in shapes.items():
        C.inp[nm] = Res(nm, nc.dram_tensor(nm, list(shp), F32, kind="ExternalInput").ap())
    for nm, arr in consts.items():
        C.inp[nm] = Res(nm, nc.dram_tensor(nm, list(arr.shape), F32, kind="ExternalInput").ap())
    C.out = Res('out', nc.dram_tensor('out', [L, D], F32, kind="ExternalOutput").ap())
    kind = "ExternalOutput" if dbg else "Internal"

    def scr(nm, shape, dt=F32):
        return Res(nm, nc.dram_tensor(nm, list(shape), dt, kind=kind).ap())
    C.xA = scr('xA', [D, L])
    C.xB = scr('xB', [D, L])
    C.qkvT = scr('qkvT', [1536, L])
    C.zT = scr('zT', [512, L])
    C.uT = scr('uT', [512, L])
    C.betaT = scr('betaT', [4, L])
    C.gT = scr('gT', [4, L])
    C.yT = scr('yT', [D, L])
    C.xmid = scr('xmid', [D, L])
    C.qkT = scr('qkT', [2048, L], BF16)
    C.vtok = scr('vtok', [L, D], BF16)
    C.oT = scr('oT', [D, L])
    with contextlib.ExitStack() as st0:
        P = Prog(nc, st0)
        C.PS = [P.ps([128, 512]) for _ in range(4)]
        C.PSS = P.ps([128, 2048])
        C.PS += [Res('pss%d' % j, C.PSS.ap[:, j * 512:(j + 1) * 512]) for j in range(4)]
        C.ident = P.sb([128, 128])
        C.ones = P.sb([128, 128])
        C.onesb = P.sb([128, 128], BF16)
        C.blk = P.sb([128, 128])
        C.amask = P.sb([128, 4, 512], BF16)
        C.sel = P.sb([8, 8, 128])
        C.halfpi = P.sb([128, 1])
        C.neg8 = P.sb([128, 1])
        C.mod = [P.sb([128, 48]) for _ in range(depth)]
        C.modA = [P.sb([128, 16]) for _ in range(depth)]
        P.dma(C.ident, C.inp['c_ident'])
        P.dma(C.blk, C.inp['c_blk'])
        P.dma(C.amask, C.inp['c_amask'], eng='pool')
        P.dma(C.sel, C.inp['c_sel'])
        P.memset(C.ones, 1.0)
        P.memset(C.onesb, 1.0)
        P.memset(C.halfpi, math.pi / 2)
        P.memset(C.neg8, -8.0)
        stage_prep(P, C, L)
        cur, nxt = C.xA, C.xB
        for l in range(depth):
            i = l // 2
            if l % 2 == 0:
                stage_even_proj(P, C, L, l, cur)
                stage_gdn(P, C, L, l)
                stage_s5(P, C, L, l)
                stage_ffn(P, C, L, l, cur, nxt, C.yT, C.inp['even_w_out'][i], moe=False)
            else:
                stage_attn_proj(P, C, L, l, cur)
                stage_attn(P, C, L, l)
                stage_ffn(P, C, L, l, cur, nxt, C.oT, C.inp['odd_w_out'][i], moe=True)
            cur, nxt = nxt, cur
        stage_out(P, C, L, cur)
        P.stack = st0
        C.nins = P.nins
    return nc, consts, C


def kernel(**inputs):
    x = np.asarray(inputs['x'], dtype=np.float32)
    B, L, _ = x.shape
    shapes = {'x': (L, D), 'c': (D,)}
    for nm in INPUT_NAMES:
        shapes[nm] = tuple(np.asarray(inputs[nm]).shape)
    nc, consts, C = build(shapes, L)
    shared = {nm: np.ascontiguousarray(np.asarray(inputs[nm], dtype=np.float32)) for nm in INPUT_NAMES}
    zeros = {nm: np.zeros_like(v) for nm, v in shared.items()}
    zc = {nm: np.zeros_like(v) for nm, v in consts.items()}
    real = [0, 1, 4, 5][:B]
    in_maps = []
    for core in range(8):
        if core in real:
            b = real.index(core)
            m = dict(shared)
            m.update(consts)
            m['x'] = np.ascontiguousarray(x[b])
            m['c'] = np.ascontiguousarray(np.asarray(inputs['c'], dtype=np.float32)[b])
        else:
            m = dict(zeros)
            m.update(zc)
            m['x'] = np.zeros((L, D), np.float32)
            m['c'] = np.zeros((D,), np.float32)
        in_maps.append(m)
    res = run_bass_kernel_spmd(nc, in_maps, core_ids=list(range(8)))
    out = np.stack([res.results[real[b]]['out'] for b in range(B)], axis=0)
    return out.astype(np.float32)
```

```python
import contextlib
import math
import numpy as np
import concourse.bass as bass
import concourse.mybir as mybir
from concourse.bass_utils import run_bass_kernel_spmd

F32 = mybir.dt.float32
BF16 = mybir.dt.bfloat16
AF = mybir.ActivationFunctionType
ALU = mybir.AluOpType
AX = mybir.AxisListType

D = 1024
DEPTH = 4
EPS = 1e-6
DFF = 2816
NEXP = 8
ENGS = ['pe', 'act', 'dve', 'pool', 'sp']


class V:
    __slots__ = ('res', 'ap')

    def __init__(self, res, ap):
        self.res = res
        self.ap = ap

    def __getitem__(self, idx):
        return V(self.res, self.ap[idx])

    def bc(self, shape):
        return V(self.res, self.ap.to_broadcast(list(shape)))

    def re(self, s, **kw):
        return V(self.res, self.ap.rearrange(s, **kw))


class Res:
    __slots__ = ('name', 'w', 'r', 'ap')

    def __init__(self, name, ap=None):
        self.name = name
        self.w = {}
        self.r = {}
        self.ap = ap

    def __getitem__(self, idx):
        return V(self, self.ap[idx])

    @property
    def v(self):
        return V(self, self.ap)


def _rv(x):
    return x.v if isinstance(x, Res) else x


class Prog:
    def __init__(self, nc, stack, n_dma_sems=32):
        self.nc = nc
        self.stack = stack
        self.streams = {e: [] for e in ENGS}
        self.cnt = {e: 0 for e in ENGS}
        self.semh = {}
        for e in ['pe', 'act', 'dve', 'pool']:
            self.semh['c_' + e] = stack.enter_context(nc.semaphore('c_' + e))
        self.ndma = n_dma_sems
        self.dma_tot = [0] * n_dma_sems
        self.dma_next = 0
        for k in range(n_dma_sems):
            self.semh['d%d' % k] = stack.enter_context(nc.semaphore('d%d' % k))
        self.known = {e: {} for e in ENGS}
        self.nt = 0
        self.nins = 0

    def sb(self, shape, dtype=F32, name=None):
        self.nt += 1
        name = name or ('t%d' % self.nt)
        t = self.stack.enter_context(self.nc.sbuf_tensor(name, list(shape), dtype))
        return Res(name, t[:])

    def ps(self, shape, dtype=F32, name=None):
        self.nt += 1
        name = name or ('p%d' % self.nt)
        t = self.stack.enter_context(self.nc.psum_tensor(name, list(shape), dtype))
        return Res(name, t[:])

    def dram(self, name, shape, dtype, kind="Internal"):
        t = self.nc.dram_tensor(name, list(shape), dtype, kind=kind)
        return Res(name, t.ap())

    def _deps(self, eng, reads, writes):
        deps = {}

        def add(k, v):
            if deps.get(k, -1) < v:
                deps[k] = v
        for r in reads:
            for k, v in r.w.items():
                add(k, v)
        for w in writes:
            for k, v in w.w.items():
                add(k, v)
            for k, v in w.r.items():
                add(k, v)
        out = []
        kn = self.known[eng]
        for k, v in deps.items():
            if eng == 'pe' and k == 'c_pe':
                continue
            if kn.get(k, -1) < v:
                kn[k] = v
                out.append((k, v))
        return out

    def _record(self, ev, reads, writes, merge=False):
        k, v = ev
        for w in writes:
            if merge:
                w.w[k] = v
            else:
                w.w = {k: v}
            w.r = {}
        for r in reads:
            if r.r.get(k, -1) < v:
                r.r[k] = v

    def op(self, eng, fn, reads=(), writes=(), inc=True):
        reads = [x for x in reads if x is not None]
        waits = self._deps(eng, reads, writes)
        key = 'c_' + eng
        if inc:
            self.cnt[eng] += 1
            ev = (key, self.cnt[eng])
        else:
            ev = (key, self.cnt[eng] + 1)
        self.streams[eng].append((waits, fn, key if inc else None, 1))
        self._record(ev, reads, writes)
        self.nins += 1

    def dma(self, out, in_, eng='sp'):
        out = _rv(out)
        in_ = _rv(in_)
        k = self.dma_next
        self.dma_next = (k + 1) % self.ndma
        key = 'd%d' % k
        waits = self._deps(eng, [in_.res], [out.res])
        kn = self.known[eng]
        if kn.get(key, -1) < self.dma_tot[k]:
            kn[key] = self.dma_tot[k]
            waits.append((key, self.dma_tot[k]))
        self.dma_tot[k] += 16
        ev = (key, self.dma_tot[k])
        oa, ia = out.ap, in_.ap

        def fn(e):
            return e.dma_start(out=oa, in_=ia)
        self.streams[eng].append((waits, fn, key, 16))
        self._record(ev, [in_.res], [out.res], merge=True)
        self.nins += 1

    def collective(self, kind, out, in_, groups, eng='pool'):
        out = _rv(out)
        in_ = _rv(in_)
        k = self.dma_next
        self.dma_next = (k + 1) % self.ndma
        key = 'd%d' % k
        waits = self._deps(eng, [in_.res], [out.res])
        kn = self.known[eng]
        if kn.get(key, -1) < self.dma_tot[k]:
            kn[key] = self.dma_tot[k]
            waits.append((key, self.dma_tot[k]))
        self.dma_tot[k] += 16
        ev = (key, self.dma_tot[k])
        oa, ia = out.ap, in_.ap

        def fn(e):
            return e.collective_compute(kind, ALU.bypass, groups, [ia], [oa])
        self.streams[eng].append((waits, fn, key, 16))
        self._record(ev, [in_.res], [out.res], merge=True)
        self.nins += 1

    def barrier(self):
        allw = [('c_' + e, self.cnt[e]) for e in ['pe', 'act', 'dve', 'pool']]
        allw += [('d%d' % k, self.dma_tot[k]) for k in range(self.ndma)]
        for e in ENGS:
            kn = self.known[e]
            waits = []
            for k, v in allw:
                if e == 'pe' and k == 'c_pe':
                    continue
                if kn.get(k, -1) < v:
                    kn[k] = v
                    waits.append((k, v))
            self.streams[e].append((waits, None, None, 0))

    def flush(self):
        nc = self.nc
        engobj = {'pe': 'tensor', 'act': 'scalar', 'dve': 'vector', 'pool': 'gpsimd', 'sp': 'sync'}
        semh = self.semh
        with nc.allow_non_contiguous_dma(reason="small strided param loads"), nc.Block() as block:
            for e in ENGS:
                stream = self.streams[e]

                def body(eng, stream=stream):
                    for waits, fn, key, n in stream:
                        for k, v in waits:
                            eng.wait_ge(semh[k], v)
                        if fn is not None:
                            ins = fn(eng)
                            if key is not None:
                                ins.then_inc(semh[key], n)
                getattr(block, engobj[e])(body)
        self.streams = {e: [] for e in ENGS}

    def mm(self, out, lhsT, rhs, start=True, stop=True):
        out, lhsT, rhs = _rv(out), _rv(lhsT), _rv(rhs)
        o, l, r = out.ap, lhsT.ap, rhs.ap
        self.op('pe', lambda e: e.matmul(o, l, r, start=start, stop=stop),
                [lhsT.res, rhs.res], [out.res], inc=stop)

    def tr(self, out, in_, ident):
        out, in_, ident = _rv(out), _rv(in_), _rv(ident)
        o, i, d = out.ap, in_.ap, ident.ap
        self.op('pe', lambda e: e.transpose(o, i, d), [in_.res, ident.res], [out.res])

    def act(self, out, in_, func, bias=None, scale=None, eng='act'):
        out, in_ = _rv(out), _rv(in_)
        reads = [in_.res]
        kw = {}
        if bias is not None:
            if isinstance(bias, (V, Res)):
                bias = _rv(bias)
                reads.append(bias.res)
                kw['bias'] = bias.ap
            else:
                kw['bias'] = float(bias)
        if scale is not None:
            if isinstance(scale, (V, Res)):
                scale = _rv(scale)
                reads.append(scale.res)
                kw['scale'] = scale.ap
            else:
                kw['scale'] = float(scale)
        o, i = out.ap, in_.ap
        self.op(eng, lambda e: e.activation(o, i, func, **kw), reads, [out.res])

    def tt(self, out, a, b, op, eng='dve'):
        out, a, b = _rv(out), _rv(a), _rv(b)
        o, x, y = out.ap, a.ap, b.ap
        self.op(eng, lambda e: e.tensor_tensor(o, x, y, op), [a.res, b.res], [out.res])

    def ts(self, out, a, s1, op0, s2=None, op1=None, eng='dve'):
        out, a = _rv(out), _rv(a)
        reads = [a.res]

        def cv(s):
            if isinstance(s, (V, Res)):
                s = _rv(s)
                reads.append(s.res)
                return s.ap
            return None if s is None else float(s)
        c1, c2 = cv(s1), cv(s2)
        o, x = out.ap, a.ap
        if op1 is None:
            self.op(eng, lambda e: e.tensor_single_scalar(o, x, c1, op0), reads, [out.res])
        else:
            self.op(eng, lambda e: e.tensor_scalar(o, x, c1, c2, op0, op1), reads, [out.res])

    def stt(self, out, a, s, b, op0, op1):
        out, a, b = _rv(out), _rv(a), _rv(b)
        reads = [a.res, b.res]
        if isinstance(s, (V, Res)):
            s = _rv(s)
            reads.append(s.res)
            c = s.ap
        else:
            c = float(s)
        o, x, y = out.ap, a.ap, b.ap
        self.op('dve', lambda e: e.scalar_tensor_tensor(o, x, c, y, op0, op1), reads, [out.res])

    def copy(self, out, in_, eng='dve'):
        out, in_ = _rv(out), _rv(in_)
        o, i = out.ap, in_.ap
        if eng == 'act':
            self.op('act', lambda e: e.activation(o, i, AF.Copy), [in_.res], [out.res])
        else:
            self.op(eng, lambda e: e.tensor_copy(o, i), [in_.res], [out.res])

    def recip(self, out, in_):
        out, in_ = _rv(out), _rv(in_)
        o, i = out.ap, in_.ap
        self.op('dve', lambda e: e.reciprocal(o, i), [in_.res], [out.res])

    def memset(self, out, val, eng='dve'):
        out = _rv(out)
        o = out.ap
        self.op(eng, lambda e: e.memset(o, float(val)), [], [out.res])

    def scan(self, out, d0, d1, init, op0=ALU.mult, op1=ALU.add):
        out, d0, d1 = _rv(out), _rv(d0), _rv(d1)
        reads = [d0.res, d1.res]
        if isinstance(init, (V, Res)):
            init = _rv(init)
            reads.append(init.res)
            c = init.ap
        else:
            c = float(init)
        o, x, y = out.ap, d0.ap, d1.ap
        self.op('dve', lambda e: e.tensor_tensor_scan(o, x, y, c, op0, op1), reads, [out.res])

    def reduce(self, out, in_, op, axis=AX.X):
        out, in_ = _rv(out), _rv(in_)
        o, i = out.ap, in_.ap
        self.op('dve', lambda e: e.tensor_reduce(o, i, axis, op), [in_.res], [out.res])


class Ctx:
    pass


def rstd_from_ss(P, out_sb, ss_ps, scale, bias):
    P.act(out_sb, ss_ps, AF.Ln, bias=bias, scale=scale)
    P.act(out_sb, out_sb, AF.Exp, scale=-0.5)


def stage_prep(P, C, L):
    nc = P.nc
    I = C.inp
    with contextlib.ExitStack() as st:
        P.stack = st
        cT = P.sb([128, 8])
        with nc.allow_non_contiguous_dma(reason="tiny"):
            P.dma(cT, I['c'].v.re("(k p) -> p k", p=128))
        cond = P.sb([128, 8])
        P.act(cond, cT, AF.Silu)
        wbuf = [P.sb([128, 8, 768]) for _ in range(2)]
        pm = C.PS[0]
        for l in range(C.depth):
            bt = P.sb([128, 48])
            with nc.allow_non_contiguous_dma(reason="tiny"):
                P.dma(bt, I['ada_b'][l].re("(j p) -> p j", p=128))
            for cb in range(8):
                wb = wbuf[cb % 2]
                P.dma(wb, I['ada_w'][l][:, cb * 768:(cb + 1) * 768].re("(k p) m -> p k m", p=128))
                for j in range(6):
                    col = cb * 6 + j
                    for k in range(8):
                        P.mm(pm[:, col:col + 1], wb[:, k, j * 128:(j + 1) * 128], cond[:, k:k + 1],
                             start=(k == 0), stop=(k == 7))
            mod = C.mod[l]
            P.tt(mod, pm[:, 0:48], bt, ALU.add)
            nw = P.sb([128, 16])
            with nc.allow_non_contiguous_dma(reason="tiny"):
                P.dma(nw[:, 0:8], I['norm_mix_w'][l].re("(k p) -> p k", p=128))
                P.dma(nw[:, 8:16], I['norm_ffn_w'][l].re("(k p) -> p k", p=128))
            A = C.modA[l]
            P.stt(A[:, 0:8], mod[:, 8:16], 1.0, nw[:, 0:8], ALU.add, ALU.mult)
            P.stt(A[:, 8:16], mod[:, 32:40], 1.0, nw[:, 8:16], ALU.add, ALU.mult)
        xin = [P.sb([128, 1024]) for _ in range(2)]
        xo = [P.sb([128, 8, 512]) for _ in range(2)]
        for tt in range(L // 512):
            o = xo[tt % 2]
            for s in range(4):
                xi = xin[s % 2]
                t0 = tt * 512 + s * 128
                P.dma(xi, I['x'][t0:t0 + 128, :])
                for k in range(8):
                    pt = C.PS[1 + (k % 4)]
                    P.tr(pt[:, 0:128], xi[:, k * 128:(k + 1) * 128], C.ident)
                    P.copy(o[:, k, s * 128:(s + 1) * 128], pt[:, 0:128], eng=('act' if k % 2 else 'dve'))
            P.dma(C.xA.v.re("(k p) t -> p k t", p=128)[:, :, tt * 512:(tt + 1) * 512], o)
        P.barrier()
        P.flush()


def stage_out(P, C, L, xsrc):
    I = C.inp
    with contextlib.ExitStack() as st:
        P.stack = st
        xi = [P.sb([128, 8, 512]) for _ in range(2)]
        xo = [P.sb([128, 1024]) for _ in range(2)]
        n = 0
        for tt in range(L // 512):
            t = xi[tt % 2]
            P.dma(t, xsrc.v.re("(k p) t -> p k t", p=128)[:, :, tt * 512:(tt + 1) * 512])
            for s in range(4):
                o = xo[s % 2]
                for k in range(8):
                    pt = C.PS[1 + (k % 4)]
                    P.tr(pt[:, 0:128], t[:, k, s * 128:(s + 1) * 128], C.ident)
                    P.copy(o[:, k * 128:(k + 1) * 128], pt[:, 0:128], eng=('act' if k % 2 else 'dve'))
                t0 = tt * 512 + s * 128
                P.dma(C.out[t0:t0 + 128, :], o)
        P.barrier()
        P.flush()


def load_norm_h(P, C, S, xt, l, which, want32=False):
    A = C.modA[l]
    mod = C.mod[l]
    aoff = 0 if which == 1 else 8
    shoff = 0 if which == 1 else 24
    sq = S.sq
    P.act(sq, xt, AF.Square)
    ss = C.PS[0]
    for k in range(8):
        P.mm(ss, C.ones, sq[:, k, :], start=(k == 0), stop=(k == 7))
    rstd = S.rstd
    rstd_from_ss(P, rstd, ss, 1.0 / D, EPS)
    for k in range(8):
        tmp = S.tmp[k % 2]
        P.stt(tmp, xt[:, k, :], A[:, aoff + k:aoff + k + 1], rstd, ALU.mult, ALU.mult)
        if want32:
            P.act(S.h32[:, k, :], tmp, AF.Identity, bias=mod[:, shoff + k:shoff + k + 1])
            P.act(S.hb[:, k, :], tmp, AF.Identity, bias=mod[:, shoff + k:shoff + k + 1])
        else:
            P.act(S.hb[:, k, :], tmp, AF.Identity, bias=mod[:, shoff + k:shoff + k + 1])


def wslab_iter(P, C, S, Wv, ncols, KT, slab=512):
    c0 = 0
    i = 0
    while c0 < ncols:
        w = min(slab, ncols - c0)
        buf = S.wb[S.wbi % len(S.wb)]
        S.wbi += 1
        P.dma(buf[:, 0:KT, 0:w], Wv[:, c0:c0 + w].re("(k p) m -> p k m", p=128), eng='pool')
        yield buf, c0, w
        c0 += w
        i += 1


def stage_even_proj(P, C, L, l, xsrc):
    nc = P.nc
    I = C.inp
    i = l // 2
    Win = I['even_w_in'][i]
    with contextlib.ExitStack() as st:
        P.stack = st
        S = Ctx()
        S.sq = P.sb([128, 8, 512])
        S.rstd = P.sb([128, 512])
        S.tmp = [P.sb([128, 512]) for _ in range(2)]
        S.hb = P.sb([128, 8, 512], BF16)
        S.wb = [P.sb([128, 8, 512], BF16) for _ in range(3)]
        S.wbi = 0
        C.cmask = P.sb([4, 512])
        P.dma(C.cmask, C.inp['c_cmask'])
        xts = [P.sb([128, 8, 512]) for _ in range(2)]
        pre = [P.sb([128, 515]) for _ in range(12)]
        for m in range(12):
            P.memset(pre[m][:, 0:3], 0.0)
        cw = P.sb([128, 12, 4])
        with nc.allow_non_contiguous_dma(reason="tiny"):
            for j in range(4):
                P.dma(cw[:, :, j], I['even_conv_w'][i][j].re("(t p) -> p t", p=128))
        wba = P.sb([128, 8, 8])
        with nc.allow_non_contiguous_dma(reason="small"):
            P.dma(wba, Win[:, 2048:2056].re("(k p) m -> p k m", p=128))
        hc = P.sb([4, 2])
        with nc.allow_non_contiguous_dma(reason="tiny"):
            P.dma(hc[:, 0:1], I['even_a_log'][i].re("(h o) -> h o", o=1))
            P.dma(hc[:, 1:2], I['even_dt_bias'][i].re("(h o) -> h o", o=1))
        nA = P.sb([4, 1])
        P.act(nA, hc[:, 0:1], AF.Exp)
        P.ts(nA, nA, -1.0, ALU.mult)
        h32 = P.sb([128, 8, 512])
        S.h32 = h32
        acc = [P.sb([128, 512]) for _ in range(2)]
        ob = [P.sb([128, 512]) for _ in range(3)]
        sm = [P.sb([4, 512]) for _ in range(6)]
        NT = L // 512
        xv = xsrc.v.re("(k p) t -> p k t", p=128)
        P.dma(xts[0], xv[:, :, 0:512])
        nob = 0
        for tt in range(NT):
            xt = xts[tt % 2]
            if tt + 1 < NT:
                P.dma(xts[(tt + 1) % 2], xv[:, :, (tt + 1) * 512:(tt + 2) * 512])
            load_norm_h(P, C, S, xt, l, 1, want32=True)
            tsl = slice(tt * 512, (tt + 1) * 512)
            pb, pa = C.PS[1], C.PS[2]
            for k in range(8):
                P.mm(pb[0:4, :], wba[:, k, 0:4], h32[:, k, :], start=(k == 0), stop=(k == 7))
            for k in range(8):
                P.mm(pa[0:4, :], wba[:, k, 4:8], h32[:, k, :], start=(k == 0), stop=(k == 7))
            beta = sm[0]
            P.act(beta[:], pb[0:4, :], AF.Exp, scale=-1.0)
            P.ts(beta, beta, 1.0, ALU.add)
            P.recip(beta, beta)
            P.dma(C.betaT[:, tsl], beta)
            xa = sm[1]
            P.act(xa[:], pa[0:4, :], AF.Identity, bias=hc[:, 1:2])
            ax = sm[2]
            P.stt(ax, xa, -1.0, xa, ALU.mult, ALU.max)
            P.act(ax, ax, AF.Exp, scale=-1.0)
            P.act(ax, ax, AF.Ln, bias=1.0)
            sp = sm[3]
            P.stt(sp, xa, 0.0, ax, ALU.max, ALU.add)
            g = sm[4]
            P.ts(g, sp, nA[:, 0:1], ALU.mult)
            gc = sm[5]
            P.scan(gc, C.cmask, g, 0.0)
            P.dma(C.gT[:, tsl], gc)
            mt = 0
            for wsl, c0, w in wslab_iter(P, C, S, Win, 2048, 8):
                for j in range(w // 128):
                    m = (c0 // 128) + j
                    pp = C.PS[3 + (m % 4)]
                    for k in range(8):
                        P.mm(pp, wsl[:, k, j * 128:(j + 1) * 128], S.hb[:, k, :], start=(k == 0), stop=(k == 7))
                    if m < 12:
                        pr = pre[m]
                        P.copy(pr[:, 3:515], pp, eng='act')
                        a = acc[m % 2]
                        P.ts(a, pr[:, 0:512], cw[:, m, 0:1], ALU.mult)
                        for jj in range(1, 4):
                            P.stt(a, pr[:, jj:jj + 512], cw[:, m, jj:jj + 1], a, ALU.mult, ALU.add)
                        P.copy(pr[:, 0:3], pr[:, 512:515], eng='pool')
                        o = ob[nob % 3]
                        nob += 1
                        P.act(o, a, AF.Silu)
                        if m < 8:
                            sq = S.tmp[m % 2]
                            P.act(sq, o, AF.Square)
                            ss = C.PS[7]
                            P.mm(ss, C.ones, sq)
                            rs = S.rstd
                            if m < 4:
                                rstd_from_ss(P, rs, ss, 128.0, 128.0 * EPS)
                            else:
                                rstd_from_ss(P, rs, ss, 1.0, EPS)
                            P.tt(o, o, rs, ALU.mult)
                        P.dma(C.qkvT[m * 128:(m + 1) * 128, tsl], o)
                    else:
                        o = ob[nob % 3]
                        nob += 1
                        P.act(o, pp, AF.Silu)
                        P.dma(C.zT[(m - 12) * 128:(m - 11) * 128, tsl], o)
            for wsl, c0, w in wslab_iter(P, C, S, Win[:, 2056:2568], 512, 8):
                for j in range(4):
                    pp = C.PS[3 + (j % 4)]
                    for k in range(8):
                        P.mm(pp, wsl[:, k, j * 128:(j + 1) * 128], S.hb[:, k, :], start=(k == 0), stop=(k == 7))
                    o = ob[nob % 3]
                    nob += 1
                    P.copy(o, pp, eng='act')
                    P.dma(C.uT[j * 128:(j + 1) * 128, tsl], o)
        P.barrier()
        P.flush()


def stage_gdn(P, C, L, l):
    nc = P.nc
    I = C.inp
    i = l // 2
    NT = L // 512
    with contextlib.ExitStack() as st:
        P.stack = st
        C.gmask = P.sb([64, 4, 512])
        P.dma(C.gmask, C.inp['c_gmask'])
        Sst = [P.sb([128, 128]) for _ in range(4)]
        for h in range(4):
            P.memset(Sst[h], 0.0)
        gw = P.sb([128, 1])
        with nc.allow_non_contiguous_dma(reason="tiny"):
            P.dma(gw, I['even_gdn_norm_w'][i].re("(p o) -> p o", o=1))
        mk = lambda n, shape, dt=F32: [P.sb(shape, dt) for _ in range(n)]
        qT, kT, vT = mk(2, [128, 512]), mk(2, [128, 512]), mk(2, [128, 512])
        rows = mk(2, [1, 2, 512])
        r_eg, r_ekl, r_ng = mk(2, [1, 512]), mk(2, [1, 512]), mk(2, [1, 512])
        bcs = mk(2, [128, 3, 512])
        kb, kbe, vb, qe, kel = (mk(2, [128, 512]) for _ in range(5))
        tokm = mk(2, [64, 3, 8, 128])
        EL, EU, t1 = mk(2, [64, 512]), mk(2, [64, 512]), mk(2, [64, 512])
        Am, ATm, PTm, Rm = mk(2, [64, 512]), mk(2, [64, 512]), mk(2, [64, 512]), mk(2, [64, 512])
        WTn = mk(2, [128, 512])
        vnew = [mk(2, [64, 128]) for _ in range(2)]
        zt = mk(2, [128, 512])
        osb = mk(2, [128, 512])
        sq = mk(2, [128, 512])
        rs = mk(2, [128, 512])
        def chain(tt, h, b, BK):
            tsl = slice(tt * 512, (tt + 1) * 512)
            P.dma(qT[b], C.qkvT[h * 128:(h + 1) * 128, tsl])
            P.dma(kT[b], C.qkvT[512 + h * 128:512 + (h + 1) * 128, tsl])
            P.dma(vT[b], C.qkvT[1024 + h * 128:1024 + (h + 1) * 128, tsl])
            P.dma(rows[b][:, 0, :], C.betaT[h:h + 1, tsl])
            P.dma(rows[b][:, 1, :], C.gT[h:h + 1, tsl])
            P.dma(zt[b], C.zT[h * 128:(h + 1) * 128, tsl])
            gc = rows[b][:, 1, :]
            P.act(r_eg[b], gc, AF.Exp)
            g3 = rows[b][:, 1, :].re("o (c j) -> o c j", j=64)
            P.tt(r_ekl[b].v.re("o (c j) -> o c j", j=64), g3[:, :, 63:64].bc([1, 8, 64]), g3, ALU.subtract)
            P.act(r_ekl[b], r_ekl[b], AF.Exp)
            P.ts(r_ng[b], gc, -1.0, ALU.mult)
            yield
            pbc = [BK[0], BK[1], BK[2]]
            P.mm(pbc[0], C.ones[0:1, :], rows[b][:, 0, :])
            P.mm(pbc[1], C.ones[0:1, :], r_eg[b])
            P.mm(pbc[2], C.ones[0:1, :], r_ekl[b])
            for j in range(3):
                P.copy(bcs[b][:, j, :], pbc[j], eng='act')
            P.tt(kb[b], kT[b], bcs[b][:, 0, :], ALU.mult)
            P.tt(kbe[b], kb[b], bcs[b][:, 1, :], ALU.mult, eng='pool')
            P.tt(vb[b], vT[b], bcs[b][:, 0, :], ALU.mult)
            P.tt(qe[b], qT[b], bcs[b][:, 1, :], ALU.mult, eng='pool')
            P.tt(kel[b], kT[b], bcs[b][:, 2, :], ALU.mult)
            yield
            for c in range(8):
                csl = slice(c * 64, (c + 1) * 64)
                for j, src in enumerate((vb[b], kbe[b], kel[b])):
                    pt = BK[3 - ((c * 3 + j) % 2)]
                    P.tr(pt[0:64, 0:128], src[:, csl], C.ident)
                    P.copy(tokm[b][:, j, c, :], pt[0:64, 0:128], eng=('act' if j != 1 else 'dve'))
                yield
            pg = BK[2]
            for c in range(8):
                csl = slice(c * 64, (c + 1) * 64)
                P.mm(pg[0:64, csl], rows[b][:, 1, csl], C.ones[0:1, 0:64], start=True, stop=False)
                P.mm(pg[0:64, csl], C.ones[0:1, 0:64], r_ng[b][:, csl], start=False, stop=True)
            P.ts(t1[b], pg[0:64, :], 0.0, ALU.min)
            P.act(EL[b], t1[b], AF.Exp)
            P.ts(t1[b], pg[0:64, :], 0.0, ALU.max)
            P.act(EU[b], t1[b], AF.Exp, scale=-1.0)
            yield
            pA, pAT, pPT = BK[0], BK[1], BK[2]
            for c in range(8):
                csl = slice(c * 64, (c + 1) * 64)
                P.mm(pA[0:64, csl], kb[b][:, csl], kT[b][:, csl])
                P.mm(pAT[0:64, csl], kT[b][:, csl], kb[b][:, csl])
                P.mm(pPT[0:64, csl], kT[b][:, csl], qT[b][:, csl])
            P.tt(t1[b], EL[b], C.gmask[:, 0, :], ALU.mult, eng='pool')
            P.tt(Am[b], pA[0:64, :], t1[b], ALU.mult)
            P.tt(EL[b], EU[b], C.gmask[:, 1, :], ALU.mult, eng='pool')
            P.tt(ATm[b], pAT[0:64, :], EL[b], ALU.mult)
            P.tt(EU[b], EU[b], C.gmask[:, 2, :], ALU.mult, eng='pool')
            P.tt(PTm[b], pPT[0:64, :], EU[b], ALU.mult)
            yield
            P.tt(Rm[b], ATm[b], C.gmask[:, 3, :], ALU.add)
            X, XT = Am[b], ATm[b]
            X2, XT2 = t1[b], EL[b]
            for step in range(5):
                p1, p2, p3 = BK[0], BK[1], BK[2]
                for c in range(8):
                    csl = slice(c * 64, (c + 1) * 64)
                    P.mm(p1[0:64, csl], XT[:, csl], X[:, csl])
                    P.mm(p2[0:64, csl], X[:, csl], XT[:, csl])
                P.copy(X2, p1[0:64, :], eng='act')
                P.copy(XT2, p2[0:64, :], eng='dve')
                yield
                X, X2 = X2, X
                XT, XT2 = XT2, XT
                for c in range(8):
                    csl = slice(c * 64, (c + 1) * 64)
                    P.mm(p3[0:64, csl], X[:, csl], Rm[b][:, csl])
                P.tt(Rm[b], Rm[b], p3[0:64, :], ALU.add)
                yield
            pW = BK[0]
            for c in range(8):
                csl = slice(c * 64, (c + 1) * 64)
                P.mm(pW[:, csl], tokm[b][:, 1, c, :], Rm[b][:, csl])
            P.act(WTn[b], pW, AF.Copy, scale=-1.0)
            yield
            po = BK[3]
            S = Sst[h]
            for c in range(8):
                csl = slice(c * 64, (c + 1) * 64)
                pv = BK[1]
                P.mm(pv[0:64, 0:128], Rm[b][:, csl], tokm[b][:, 0, c, :], start=True, stop=False)
                P.mm(pv[0:64, 0:128], WTn[b][:, csl], S, start=False, stop=True)
                vn = vnew[b][c % 2]
                P.copy(vn, pv[0:64, 0:128], eng='act')
                P.mm(po[:, csl], S, qe[b][:, csl], start=True, stop=False)
                P.mm(po[:, csl], vn, PTm[b][:, csl], start=False, stop=True)
                ps_ = BK[2]
                P.mm(ps_[:, 0:128], tokm[b][:, 2, c, :], vn)
                P.stt(S, S, bcs[b][:, 1, c * 64 + 63:c * 64 + 64], ps_[:, 0:128], ALU.mult, ALU.add)
                yield
            P.copy(osb[b], po, eng='act')
            P.act(sq[b], osb[b], AF.Square)
            pss = BK[0]
            P.mm(pss, C.ones, sq[b])
            rstd_from_ss(P, rs[b], pss, 1.0 / 128, EPS)
            P.stt(osb[b], osb[b], gw[:, 0:1], rs[b], ALU.mult, ALU.mult)
            P.tt(osb[b], osb[b], zt[b], ALU.mult)
            P.dma(C.yT[h * 128:(h + 1) * 128, tsl], osb[b])
        for tt in range(NT):
            for hp in range(2):
                gens = [chain(tt, 2 * hp, 0, C.PS[0:4]), chain(tt, 2 * hp + 1, 1, C.PS[4:8])]
                while gens:
                    for g_ in list(gens):
                        try:
                            next(g_)
                        except StopIteration:
                            gens.remove(g_)
        P.barrier()
        P.flush()


def stage_s5(P, C, L, l):
    nc = P.nc
    I = C.inp
    i = l // 2
    NT = L // 512
    TWO_PI = 2.0 * math.pi
    with contextlib.ExitStack() as st:
        P.stack = st
        lr = P.sb([128, 16])
        li = P.sb([128, 16])
        ls = P.sb([128, 16])
        with nc.allow_non_contiguous_dma(reason="small"):
            P.dma(lr, I['even_lam_re'][i].re("(j g) n -> (g n) j", g=2))
            P.dma(li, I['even_lam_im'][i].re("(j g) n -> (g n) j", g=2))
            lsv = I['even_log_step'][i].re("(j g) -> g j", g=2)
            for g2 in range(2):
                P.dma(ls[g2 * 64:(g2 + 1) * 64, :], lsv[g2:g2 + 1, :].bc([64, 16]))
        dt = P.sb([128, 16])
        P.act(dt, ls, AF.Exp)
        P.ts(lr, lr, -1e-4, ALU.min)
        rho = P.sb([128, 16])
        P.tt(rho, lr, dt, ALU.mult)
        P.act(rho, rho, AF.Exp)
        th = P.sb([128, 16])
        P.tt(th, li, dt, ALU.mult)
        tmpa = P.sb([128, 16])
        sn = P.sb([128, 16])
        cs = P.sb([128, 16])
        P.act(sn, th, AF.Sin, scale=1.0 / 16)
        P.act(cs, th, AF.Sin, scale=1.0 / 16, bias=C.halfpi[:, 0:1])
        t_c2, t_s2 = P.sb([128, 16]), P.sb([128, 16])
        for _ in range(4):
            P.tt(t_c2, cs, cs, ALU.mult)
            P.tt(t_s2, sn, sn, ALU.mult)
            P.tt(sn, cs, sn, ALU.mult)
            P.ts(sn, sn, 2.0, ALU.mult)
            P.tt(cs, t_c2, t_s2, ALU.subtract)
        ar, ai = P.sb([128, 16]), P.sb([128, 16])
        P.tt(ar, rho, cs, ALU.mult)
        P.tt(ai, rho, sn, ALU.mult)
        nr = P.sb([128, 16])
        P.ts(nr, ar, -1.0, ALU.add)
        den = P.sb([128, 16])
        t2 = P.sb([128, 16])
        P.tt(den, lr, lr, ALU.mult)
        P.tt(t2, li, li, ALU.mult)
        P.tt(den, den, t2, ALU.add)
        P.recip(den, den)
        cr, ci = P.sb([128, 16]), P.sb([128, 16])
        P.tt(cr, nr, lr, ALU.mult)
        P.tt(t2, ai, li, ALU.mult)
        P.tt(cr, cr, t2, ALU.add)
        P.tt(cr, cr, den, ALU.mult)
        P.tt(ci, ai, lr, ALU.mult)
        P.tt(t2, nr, li, ALU.mult)
        P.tt(ci, ci, t2, ALU.subtract)
        P.tt(ci, ci, den, ALU.mult)
        nsn = P.sb([128, 16])
        P.ts(nsn, sn, -1.0, ALU.mult)
        Tc = P.sb([128, 16, 512])
        Ts = P.sb([128, 16, 512])
        P.memset(Tc[:, :, 0:1], 1.0)
        P.memset(Ts[:, :, 0:1], 0.0)
        cc, s_ = P.sb([128, 16]), P.sb([128, 16])
        P.copy(cc, cs)
        P.copy(s_, sn)
        ta, tb = P.sb([128, 256]), P.sb([128, 256])
        c2, s2 = P.sb([128, 16]), P.sb([128, 16])
        span = 1
        while span < 512:
            for j in range(16):
                P.ts(ta[:, 0:span], Ts[:, j, 0:span], s_[:, j:j + 1], ALU.mult)
                P.stt(Tc[:, j, span:2 * span], Tc[:, j, 0:span], cc[:, j:j + 1], ta[:, 0:span], ALU.mult, ALU.subtract)
                P.ts(tb[:, 0:span], Tc[:, j, 0:span], s_[:, j:j + 1], ALU.mult)
                P.stt(Ts[:, j, span:2 * span], Ts[:, j, 0:span], cc[:, j:j + 1], tb[:, 0:span], ALU.mult, ALU.add)
            P.tt(c2, cc, cc, ALU.mult)
            P.tt(s2, s_, s_, ALU.mult)
            P.tt(s_, cc, s_, ALU.mult)
            P.ts(s_, s_, 2.0, ALU.mult)
            P.tt(cc, c2, s2, ALU.subtract)
            span *= 2
        BreT = [P.sb([128, 128]) for _ in range(16)]
        BimT = [P.sb([128, 128]) for _ in range(16)]
        CrT = [P.sb([128, 128]) for _ in range(16)]
        CiT = [P.sb([128, 128]) for _ in range(16)]
        pad = [P.sb([128, 128]) for _ in range(4)]
        craw = [P.sb([128, 128]) for _ in range(2)]
        for j in range(16):
            kt = j // 4
            for which, (nm, dst) in enumerate((('even_b_re', BreT), ('even_b_im', BimT))):
                pd = pad[which]
                P.memset(pd, 0.0)
                for g2 in range(2):
                    g = 2 * j + g2
                    off = (g - 8 * kt) * 16
                    P.dma(pd[g2 * 64:(g2 + 1) * 64, off:off + 16], I[nm][i][g])
                pt = C.PS[which]
                P.tr(pt[:, 0:128], pd, C.ident)
                P.copy(dst[j], pt[:, 0:128], eng='act')
            for which, nm in enumerate(('even_c_re', 'even_c_im')):
                pd = pad[2 + which]
                P.memset(pd, 0.0)
                for g2 in range(2):
                    g = 2 * j + g2
                    off = (g - 8 * kt) * 16
                    P.dma(pd[off:off + 16, g2 * 64:(g2 + 1) * 64], I[nm][i][g])
                pt = C.PS[2 + which]
                P.tr(pt[:, 0:128], pd, C.ident)
                P.copy(craw[which], pt[:, 0:128], eng='act')
            P.ts(pad[0], craw[1], ci[:, j:j + 1], ALU.mult)
            P.stt(CrT[j], craw[0], cr[:, j:j + 1], pad[0], ALU.mult, ALU.subtract)
            P.ts(pad[1], craw[1], cr[:, j:j + 1], ALU.mult)
            P.stt(CiT[j], craw[0], ci[:, j:j + 1], pad[1], ALU.mult, ALU.add)
            P.ts(CiT[j], CiT[j], -1.0, ALU.mult)
        dsk = P.sb([128, 4])
        glb = P.sb([128, 8])
        with nc.allow_non_contiguous_dma(reason="tiny"):
            P.dma(dsk, I['even_d_skip'][i].re("(k p) -> p k", p=128))
            P.dma(glb, I['even_glu_b'][i].re("(k p) -> p k", p=128))
        nglb = P.sb([128, 8])
        P.ts(nglb, glb, -1.0, ALU.mult)
        gluw = P.sb([128, 4, 1024], BF16)
        P.dma(gluw, I['even_glu_w'][i].re("(k p) m -> p k m", p=128), eng='pool')
        ini = [P.sb([128, 2]) for _ in range(16)]
        for j in range(16):
            P.memset(ini[j], 0.0)
        uts = [P.sb([128, 4, 512]) for _ in range(2)]
        bu = [P.sb([128, 2, 512]) for _ in range(2)]
        zin = [P.sb([128, 2, 512]) for _ in range(2)]
        zz = [P.sb([128, 2, 512]) for _ in range(2)]
        w1s = [[P.sb([128, 512]) for _ in range(4)] for _ in range(2)]
        xx = [P.sb([128, 2, 512]) for _ in range(2)]
        sml = [P.sb([128, 2]) for _ in range(2)]
        yg = P.sb([128, 4, 512], BF16)
        ysb = [P.sb([128, 512]) for _ in range(2)]
        ga = [P.sb([128, 512]) for _ in range(2)]
        gb = [P.sb([128, 512]) for _ in range(2)]
        uv = C.uT.v.re("(k p) t -> p k t", p=128)
        P.dma(uts[0], uv[:, :, 0:512])
        it = 0
        for tt in range(NT):
            tsl = slice(tt * 512, (tt + 1) * 512)
            ut = uts[tt % 2]
            if tt + 1 < NT:
                P.dma(uts[(tt + 1) % 2], uv[:, :, (tt + 1) * 512:(tt + 2) * 512])
            def chain(j, b, kt):
                pr, pi_ = C.PS[0 + 2 * b], C.PS[1 + 2 * b]
                W = w1s[b]
                P.mm(pr, BreT[j], ut[:, kt, :])
                P.mm(pi_, BimT[j], ut[:, kt, :])
                P.copy(bu[b][:, 0, :], pr, eng='act')
                P.copy(bu[b][:, 1, :], pi_, eng='act')
                yield
                P.tt(W[0], bu[b][:, 0, :], Tc[:, j, :], ALU.mult)
                P.tt(W[1], bu[b][:, 1, :], Ts[:, j, :], ALU.mult, eng='pool')
                yield
                P.tt(zin[b][:, 0, :], W[0], W[1], ALU.add)
                P.tt(W[2], bu[b][:, 1, :], Tc[:, j, :], ALU.mult, eng='pool')
                yield
                P.tt(W[3], bu[b][:, 0, :], Ts[:, j, :], ALU.mult)
                yield
                P.tt(zin[b][:, 1, :], W[2], W[3], ALU.subtract, eng='pool')
                P.scan(zz[b][:, 0, :], rho[:, j:j + 1].bc([128, 512]), zin[b][:, 0, :], ini[j][:, 0:1])
                yield
                P.scan(zz[b][:, 1, :], rho[:, j:j + 1].bc([128, 512]), zin[b][:, 1, :], ini[j][:, 1:2])
                yield
                P.tt(W[0], zz[b][:, 0, :], Tc[:, j, :], ALU.mult)
                P.tt(W[1], zz[b][:, 1, :], Ts[:, j, :], ALU.mult, eng='pool')
                yield
                P.tt(xx[b][:, 0, :], W[0], W[1], ALU.subtract)
                P.tt(W[2], zz[b][:, 1, :], Tc[:, j, :], ALU.mult, eng='pool')
                yield
                P.tt(W[3], zz[b][:, 0, :], Ts[:, j, :], ALU.mult)
                yield
                P.tt(xx[b][:, 1, :], W[2], W[3], ALU.add, eng='pool')
                yield
                sm_ = sml[b]
                P.ts(sm_[:, 0:1], xx[b][:, 1, 511:512], nsn[:, j:j + 1], ALU.mult)
                P.ts(sm_[:, 1:2], xx[b][:, 0, 511:512], sn[:, j:j + 1], ALU.mult)
                P.stt(ini[j][:, 0:1], xx[b][:, 0, 511:512], cs[:, j:j + 1], sm_[:, 0:1], ALU.mult, ALU.add)
                P.stt(ini[j][:, 1:2], xx[b][:, 1, 511:512], cs[:, j:j + 1], sm_[:, 1:2], ALU.mult, ALU.add)

            for kt in range(4):
                py = C.PS[4 + (kt % 2)]
                for jp in range(2):
                    js = [kt * 4 + jp * 2, kt * 4 + jp * 2 + 1]
                    gens = [chain(js[0], 0, kt), chain(js[1], 1, kt)]
                    while gens:
                        for g_ in list(gens):
                            try:
                                next(g_)
                            except StopIteration:
                                gens.remove(g_)
                    for b, j in enumerate(js):
                        first = (jp == 0 and b == 0)
                        last = (jp == 1 and b == 1)
                        P.mm(py, CrT[j], xx[b][:, 0, :], start=first, stop=False)
                        P.mm(py, CiT[j], xx[b][:, 1, :], start=False, stop=last)
                y = ysb[kt % 2]
                P.stt(y, ut[:, kt, :], dsk[:, kt:kt + 1], py, ALU.mult, ALU.add)
                P.act(yg[:, kt, :], y, AF.Gelu)
            for m in range(4):
                pa, pb = C.PS[6], C.PS[7]
                for k in range(4):
                    P.mm(pa, gluw[:, k, m * 128:(m + 1) * 128], yg[:, k, :], start=(k == 0), stop=(k == 3))
                for k in range(4):
                    P.mm(pb, gluw[:, k, 512 + m * 128:512 + (m + 1) * 128], yg[:, k, :], start=(k == 0), stop=(k == 3))
                a_, b_ = ga[m % 2], gb[m % 2]
                P.act(a_, pa, AF.Identity, bias=glb[:, m:m + 1])
                P.act(b_, pb, AF.Exp, bias=nglb[:, 4 + m:5 + m], scale=-1.0)
                P.ts(b_, b_, 1.0, ALU.add)
                P.recip(b_, b_)
                P.tt(a_, a_, b_, ALU.mult)
                P.dma(C.yT[512 + m * 128:512 + (m + 1) * 128, tsl], a_)
        P.barrier()
        P.flush()


def stage_ffn(P, C, L, l, xsrc, xdst, ysrc, wout, moe, G=2):
    nc = P.nc
    I = C.inp
    i = l // 2
    NT = L // 512
    G = min(G, NT)
    NG = NT // G
    mod = C.mod[l]
    with contextlib.ExitStack() as st:
        P.stack = st
        S = Ctx()
        faccs = [P.sb([128, 8, 512]) for _ in range(G)]
        S.sq = faccs[0]
        S.rstd = P.sb([128, 512])
        S.tmp = [P.sb([128, 512]) for _ in range(2)]
        hbs = [P.sb([128, 8, 512], BF16) for _ in range(G)]
        S.h32 = P.sb([128, 8, 512]) if moe else None
        S.wb = [P.sb([128, 8, 512], BF16) for _ in range(2)]
        S.wbi = 0
        w2b = [P.sb([128, 22, 128], BF16) for _ in range(2)]
        nw2 = 0
        xt = P.sb([128, 8, 512])
        ybf = P.sb([128, 8, 512], BF16)
        hids = [P.sb([128, 22, 512], BF16) for _ in range(G)]
        sa = [P.sb([128, 512]) for _ in range(3)]
        if moe:
            rw = P.sb([128, 8, 8])
            P.dma(rw, I['odd_router_w'][i].re("(k p) e -> p k e", p=128))
            lg = P.sb([8, 512])
            lt = P.sb([128, 4, 8])
            m1 = P.sb([128, 4])
            m2 = P.sb([128, 4])
            eq1 = P.sb([128, 4, 8])
            eq2 = P.sb([128, 4, 8])
            msk = P.sb([128, 4, 8])
            gg = P.sb([128, 4])
            g2_ = P.sb([128, 4])
            cmb = P.sb([128, 4, 8])
            cmbT = P.sb([8, 512])
            combs = [P.sb([128, 8, 512], BF16) for _ in range(G)]
        xv = xsrc.v.re("(k p) t -> p k t", p=128)
        yv = ysrc.v.re("(k p) t -> p k t", p=128)
        ov = xdst.v.re("(k p) t -> p k t", p=128)
        mv = C.xmid.v.re("(k p) t -> p k t", p=128)
        nsa = 0
        npp = 0
        for grp in range(NG):
            for g in range(G):
                tt = grp * G + g
                tsl = slice(tt * 512, (tt + 1) * 512)
                P.dma(xt, xv[:, :, tsl])
                P.dma(ybf, yv[:, :, tsl], eng='pool')
                for wsl, c0, w in wslab_iter(P, C, S, wout, 1024, 8):
                    for j in range(4):
                        m = c0 // 128 + j
                        pp = C.PS[1 + (m % 4)]
                        for k in range(8):
                            P.mm(pp, wsl[:, k, j * 128:(j + 1) * 128], ybf[:, k, :], start=(k == 0), stop=(k == 7))
                        P.stt(xt[:, m, :], pp, mod[:, 16 + m:17 + m], xt[:, m, :], ALU.mult, ALU.add)
                P.dma(mv[:, :, tsl], xt)
                S.hb = hbs[g]
                load_norm_h(P, C, S, xt, l, 2, want32=moe)
                if moe:
                    comb = combs[g]
                    pl = C.PS[5]
                    for k in range(8):
                        P.mm(pl[0:8, :], rw[:, k, :], S.h32[:, k, :], start=(k == 0), stop=(k == 7))
                    P.copy(lg, pl[0:8, :], eng='act')
                    pt = C.PS[6]
                    for s in range(4):
                        P.tr(pt[:, s * 8:(s + 1) * 8], lg[:, s * 128:(s + 1) * 128], C.ident[0:8, 0:8])
                    P.copy(lt, pt[:, 0:32].re("p (s e) -> p s e", e=8), eng='act')
                    P.reduce(m1, lt, ALU.max)
                    P.tt(eq1, lt, m1.v.re("p (s o) -> p s o", o=1).bc([128, 4, 8]), ALU.is_equal)
                    P.stt(msk, eq1, -1e30, lt, ALU.mult, ALU.add)
                    P.reduce(m2, msk, ALU.max)
                    P.tt(eq2, msk, m2.v.re("p (s o) -> p s o", o=1).bc([128, 4, 8]), ALU.is_equal)
                    P.tt(gg, m2, m1, ALU.subtract)
                    P.act(gg, gg, AF.Exp)
                    P.ts(gg, gg, 1.0, ALU.add)
                    P.recip(gg, gg)
                    P.ts(g2_, gg, -1.0, ALU.mult, 1.0, ALU.add)
                    P.tt(cmb, eq1, gg.v.re("p (s o) -> p s o", o=1).bc([128, 4, 8]), ALU.mult)
                    P.tt(eq2, eq2, g2_.v.re("p (s o) -> p s o", o=1).bc([128, 4, 8]), ALU.mult)
                    P.tt(cmb, cmb, eq2, ALU.add)
                    pc = C.PS[7]
                    for s in range(4):
                        P.tr(pc[0:8, s * 128:(s + 1) * 128], cmb[:, s, :], C.ident)
                    P.copy(cmbT, pc[0:8, :], eng='act')
                    for e in range(NEXP):
                        pb_ = C.PS[5 + (e % 2)]
                        P.mm(pb_, C.sel[:, e, :], cmbT)
                        P.copy(comb[:, e, :], pb_, eng='act')
            nexp = NEXP if moe else 1
            for e in range(nexp):
                if moe:
                    W13 = I['odd_expert_w13'][i][e]
                    W2 = I['odd_expert_w2'][i][e]
                else:
                    W13 = I['even_ffn_w13'][i]
                    W2 = I['even_ffn_w2'][i]
                for c0 in range(0, DFF, 256):
                    w = min(256, DFF - c0)
                    buf = S.wb[S.wbi % 2]
                    S.wbi += 1
                    P.dma(buf[:, :, 0:w], W13[:, c0:c0 + w].re("(k p) m -> p k m", p=128), eng='pool')
                    P.dma(buf[:, :, 256:256 + w], W13[:, DFF + c0:DFF + c0 + w].re("(k p) m -> p k m", p=128), eng='pool')
                    for j in range(w // 128):
                        f = c0 // 128 + j
                        for g in range(G):
                            pa, pb = C.PS[1 + 2 * (npp % 2)], C.PS[2 + 2 * (npp % 2)]
                            npp += 1
                            for k in range(8):
                                P.mm(pa, buf[:, k, j * 128:(j + 1) * 128], hbs[g][:, k, :], start=(k == 0), stop=(k == 7))
                            for k in range(8):
                                P.mm(pb, buf[:, k, 256 + j * 128:256 + (j + 1) * 128], hbs[g][:, k, :], start=(k == 0), stop=(k == 7))
                            s_ = sa[nsa % 3]
                            nsa += 1
                            P.act(s_, pa, AF.Silu)
                            if moe:
                                P.tt(s_, s_, combs[g][:, e, :], ALU.mult)
                            P.tt(hids[g][:, f, :], pb, s_, ALU.mult)
                for m in range(8):
                    wb2 = w2b[nw2 % 2]
                    nw2 += 1
                    P.dma(wb2, W2[:, m * 128:(m + 1) * 128].re("(f p) m -> p f m", p=128), eng='pool')
                    for g in range(G):
                        facc = faccs[g]
                        pp = C.PS[5 + ((m * G + g) % 3)]
                        for f in range(22):
                            P.mm(pp, wb2[:, f, :], hids[g][:, f, :], start=(f == 0), stop=(f == 21))
                        if e == 0:
                            P.copy(facc[:, m, :], pp, eng='act')
                        else:
                            P.tt(facc[:, m, :], pp, facc[:, m, :], ALU.add)
            for g in range(G):
                tt = grp * G + g
                tsl = slice(tt * 512, (tt + 1) * 512)
                P.dma(xt, mv[:, :, tsl])
                for m in range(8):
                    P.stt(faccs[g][:, m, :], faccs[g][:, m, :], mod[:, 40 + m:41 + m], xt[:, m, :], ALU.mult, ALU.add)
                P.dma(ov[:, :, tsl], faccs[g])
        P.barrier()
        P.flush()


def stage_attn_proj(P, C, L, l, xsrc):
    nc = P.nc
    I = C.inp
    i = l // 2
    NT = L // 512
    Wq = I['odd_w_qkv'][i]
    with contextlib.ExitStack() as st:
        P.stack = st
        S = Ctx()
        S.sq = P.sb([128, 8, 512])
        S.rstd = P.sb([128, 512])
        S.tmp = [P.sb([128, 512]) for _ in range(2)]
        S.hb = P.sb([128, 8, 512], BF16)
        S.h32 = None
        S.wb = [P.sb([128, 8, 512], BF16) for _ in range(3)]
        S.wbi = 0
        wv = P.sb([128, 8, 1024], BF16)
        P.dma(wv, Wq[:, 2048:3072].re("(k p) m -> p k m", p=128), eng='pool')
        nw = P.sb([128, 2])
        with nc.allow_non_contiguous_dma(reason="tiny"):
            for t in range(2):
                P.dma(nw[t * 64:(t + 1) * 64, 0:1], I['odd_q_norm_w'][i].re("(p o) -> p o", o=1))
                P.dma(nw[t * 64:(t + 1) * 64, 1:2], I['odd_k_norm_w'][i].re("(p o) -> p o", o=1))
        xts = [P.sb([128, 8, 512]) for _ in range(2)]
        raw = [P.sb([128, 512]) for _ in range(2)]
        sq = [P.sb([128, 512]) for _ in range(2)]
        rs = [P.sb([128, 512]) for _ in range(2)]
        ob = [P.sb([128, 512], BF16) for _ in range(3)]
        vb = [P.sb([128, 1024], BF16) for _ in range(2)]
        xv = xsrc.v.re("(k p) t -> p k t", p=128)
        P.dma(xts[0], xv[:, :, 0:512])
        n = 0
        for tt in range(NT):
            tsl = slice(tt * 512, (tt + 1) * 512)
            xt = xts[tt % 2]
            if tt + 1 < NT:
                P.dma(xts[(tt + 1) % 2], xv[:, :, (tt + 1) * 512:(tt + 2) * 512])
            load_norm_h(P, C, S, xt, l, 1)
            for wsl, c0, w in wslab_iter(P, C, S, Wq, 2048, 8):
                for j in range(4):
                    m = c0 // 128 + j
                    isq = m < 8
                    pp = C.PS[1 + (m % 4)]
                    for k in range(8):
                        P.mm(pp, wsl[:, k, j * 128:(j + 1) * 128], S.hb[:, k, :], start=(k == 0), stop=(k == 7))
                    r = raw[n % 2]
                    P.copy(r, pp, eng='act')
                    P.act(sq[n % 2], r, AF.Square)
                    pss = C.PS[5 + (n % 2)]
                    P.mm(pss, C.blk, sq[n % 2])
                    if isq:
                        rstd_from_ss(P, rs[n % 2], pss, 1.0, 64.0 * EPS)
                    else:
                        rstd_from_ss(P, rs[n % 2], pss, 1.0 / 64, EPS)
                    o = ob[n % 3]
                    P.stt(o, r, nw[:, (0 if isq else 1):(1 if isq else 2)], rs[n % 2], ALU.mult, ALU.mult)
                    P.dma(C.qkT[m * 128:(m + 1) * 128, tsl], o)
                    n += 1
            for s in range(4):
                v_ = vb[s % 2]
                for half in range(2):
                    pp = C.PS[1 + ((s * 2 + half) % 4)]
                    for k in range(8):
                        P.mm(pp, S.hb[:, k, s * 128:(s + 1) * 128], wv[:, k, half * 512:(half + 1) * 512],
                             start=(k == 0), stop=(k == 7))
                    P.copy(v_[:, half * 512:(half + 1) * 512], pp, eng=('act' if half else 'dve'))
                P.dma(C.vtok[tt * 512 + s * 128:tt * 512 + (s + 1) * 128, :], v_)
        P.barrier()
        P.flush()


def stage_attn(P, C, L, l):
    nc = P.nc
    I = C.inp
    i = l // 2
    NT = L // 512
    NK = L // 128
    lambda_init = 0.8 - 0.6 * math.exp(-0.3 * l)
    with contextlib.ExitStack() as st:
        P.stack = st
        lq = P.sb([128, 4, 64])
        for j, nm in enumerate(('odd_lambda_q1', 'odd_lambda_k1', 'odd_lambda_q2', 'odd_lambda_k2')):
            a = I[nm][i].re("(o d) -> o d", o=1)
            P.dma(lq[:, j, :], V(a.res, a.ap.to_broadcast([128, 64])))
        pr = P.sb([128, 2, 64])
        P.tt(pr[:, 0, :], lq[:, 0, :], lq[:, 1, :], ALU.mult)
        P.tt(pr[:, 1, :], lq[:, 2, :], lq[:, 3, :], ALU.mult)
        sm = P.sb([128, 2])
        P.reduce(sm, pr, ALU.add)
        P.act(sm, sm, AF.Exp)
        nlam = P.sb([128, 1])
        P.tt(nlam, sm[:, 1:2], sm[:, 0:1], ALU.subtract)
        P.ts(nlam, nlam, -lambda_init, ALU.add)
        sw = P.sb([128, 1])
        with nc.allow_non_contiguous_dma(reason="tiny"):
            P.dma(sw, I['odd_subln_w'][i].re("(p o) -> p o", o=1))
        P.ts(sw, sw, 1.0 - lambda_init, ALU.mult)
        kT = [P.sb([128, L], BF16) for _ in range(2)]
        vt = [P.sb([128, NK, 128], BF16) for _ in range(2)]
        qt_ = [P.sb([128, 512], BF16) for _ in range(2)]
        E = [P.sb([128, 1024], BF16) for _ in range(4)]
        r1 = [P.sb([128, 512]) for _ in range(2)]
        r2 = [P.sb([128, 512]) for _ in range(2)]
        o1 = [P.sb([128, 512]) for _ in range(2)]
        sq = [P.sb([128, 512]) for _ in range(2)]
        ob = [P.sb([128, 512]) for _ in range(2)]
        ne = 0
        nq = 0
        for h in range(8):
            kk, vv = kT[h % 2], vt[h % 2]
            P.dma(kk, C.qkT[1024 + h * 128:1024 + (h + 1) * 128, :])
            P.dma(vv, C.vtok[:, h * 128:(h + 1) * 128].re("(n p) e -> p n e", p=128))
            for qt in range(NT):
                tsl = slice(qt * 512, (qt + 1) * 512)
                q = qt_[nq % 2]
                P.dma(q, C.qkT[h * 128:(h + 1) * 128, tsl])
                pn = [C.PS[0], C.PS[1]]
                pd = [C.PS[2], C.PS[3]]
                nkt = 4 * (qt + 1)
                LA = 1
                ebuf = {}

                def emit_s(kt):
                    nonlocal ne
                    half = ne % 2
                    psc = C.PSS[:, half * 1024:(half + 1) * 1024]
                    for t in range(2):
                        P.mm(psc[:, t * 512:(t + 1) * 512], kk[t * 64:(t + 1) * 64, kt * 128:(kt + 1) * 128],
                             q[t * 64:(t + 1) * 64, :])
                    e_ = E[ne % 4]
                    ne += 1
                    P.act(e_, psc, AF.Exp, bias=C.neg8[:, 0:1])
                    r = kt - 4 * qt
                    if r >= 0:
                        P.tt(e_.v.re("p (t q) -> p t q", t=2), e_.v.re("p (t q) -> p t q", t=2),
                             C.amask[:, r:r + 1, :].bc([128, 2, 512]), ALU.mult, eng='pool')
                    ebuf[kt] = e_

                def emit_md(kt):
                    e_ = ebuf.pop(kt)
                    for t in range(2):
                        P.mm(pn[t], vv[:, kt, :], e_[:, t * 512:(t + 1) * 512], start=(kt == 0), stop=(kt == nkt - 1))
                        P.mm(pd[t], C.onesb, e_[:, t * 512:(t + 1) * 512], start=(kt == 0), stop=(kt == nkt - 1))
                for n in range(nkt + LA):
                    if n < nkt:
                        emit_s(n)
                    if n - LA >= 0:
                        emit_md(n - LA)
                b = nq % 2
                nq += 1
                P.recip(r1[b], pd[0])
                P.recip(r2[b], pd[1])
                P.tt(o1[b], pn[0], r1[b], ALU.mult)
                P.tt(r2[b], pn[1], r2[b], ALU.mult)
                P.stt(o1[b], r2[b], nlam[:, 0:1], o1[b], ALU.mult, ALU.add)
                P.act(sq[b], o1[b], AF.Square)
                pss = C.PS[2]
                P.mm(pss, C.ones, sq[b])
                rstd_from_ss(P, r1[b], pss, 1.0 / 128, EPS)
                P.stt(ob[b], o1[b], sw[:, 0:1], r1[b], ALU.mult, ALU.mult)
                P.dma(C.oT[h * 128:(h + 1) * 128, tsl], ob[b])
        P.barrier()
        P.flush()


def make_consts():
    c = {}
    c['c_ident'] = np.eye(128, dtype=np.float32)
    am = np.zeros((128, 4, 512), np.float32)
    kk = np.arange(128)[:, None]
    qq = np.arange(512)[None, :]
    for r in range(4):
        am[:, r, :] = ((r * 128 + kk) // 64 <= qq // 64)
    c['c_amask'] = am
    gm = np.zeros((64, 4, 512), np.float32)
    p = np.arange(64)[:, None]
    f = np.arange(64)[None, :]
    for cidx in range(8):
        sl = slice(cidx * 64, (cidx + 1) * 64)
        gm[:, 0, sl] = -1.0 * (p > f)
        gm[:, 1, sl] = -1.0 * (p < f)
        gm[:, 2, sl] = (p <= f)
        gm[:, 3, sl] = (p == f)
    c['c_gmask'] = gm
    sel = np.zeros((8, 8, 128), np.float32)
    for e in range(8):
        sel[e, e, :] = 1.0
    c['c_sel'] = sel
    blk = np.zeros((128, 128), np.float32)
    blk[:64, :64] = 1
    blk[64:, 64:] = 1
    c['c_blk'] = blk
    cm = np.ones((4, 512), np.float32)
    cm[:, ::64] = 0
    c['c_cmask'] = cm
    return c


INPUT_NAMES = ['ada_w', 'ada_b', 'norm_mix_w', 'norm_ffn_w',
               'even_w_in', 'even_conv_w', 'even_a_log', 'even_dt_bias', 'even_gdn_norm_w',
               'even_lam_re', 'even_lam_im', 'even_log_step', 'even_b_re', 'even_b_im', 'even_c_re', 'even_c_im',
               'even_d_skip', 'even_glu_w', 'even_glu_b', 'even_w_out', 'even_ffn_w13', 'even_ffn_w2',
               'odd_w_qkv', 'odd_q_norm_w', 'odd_k_norm_w', 'odd_lambda_q1', 'odd_lambda_k1', 'odd_lambda_q2',
               'odd_lambda_k2', 'odd_subln_w', 'odd_w_out', 'odd_router_w', 'odd_expert_w13', 'odd_expert_w2']


def build(shapes, L, depth=DEPTH, dbg=False):
    nc = bass.Bass("TRN2", target_bir_lowering=False)
    C = Ctx()
    C.depth = depth
    C.inp = {}
    consts = make_consts()
    for nm, shp in shapes.items():
        C.inp[nm] = Res(nm, nc.dram_tensor(nm, list(shp), F32, kind="ExternalInput").ap())
    for nm, arr in consts.items():
        C.inp[nm] = Res(nm, nc.dram_tensor(nm, list(arr.shape), F32, kind="ExternalInput").ap())
    C.out = Res('out', nc.dram_tensor('out', [L, D], F32, kind="ExternalOutput").ap())
    kind = "ExternalOutput" if dbg else "Internal"

    def scr(nm, shape, dt=F32):
        return Res(nm, nc.dram_tensor(nm, list(shape), dt, kind=kind).ap())
    C.xA = scr('xA', [D, L])
    C.xB = scr('xB', [D, L])
    C.qkvT = scr('qkvT', [1536, L])
    C.zT = scr('zT', [512, L])
    C.uT = scr('uT', [512, L])
    C.betaT = scr('betaT', [4, L])
    C.gT = scr('gT', [4, L])
    C.yT = scr('yT', [D, L])
    C.xmid = scr('xmid', [D, L])
    C.qkT = scr('qkT', [2048, L], BF16)
    C.vtok = scr('vtok', [L, D], BF16)
    C.oT = scr('oT', [D, L])
    with contextlib.ExitStack() as st0:
        P = Prog(nc, st0)
        C.PS = [P.ps([128, 512]) for _ in range(4)]
        C.PSS = P.ps([128, 2048])
        C.PS += [Res('pss%d' % j, C.PSS.ap[:, j * 512:(j + 1) * 512]) for j in range(4)]
        C.ident = P.sb([128, 128])
        C.ones = P.sb([128, 128])
        C.onesb = P.sb([128, 128], BF16)
        C.blk = P.sb([128, 128])
        C.amask = P.sb([128, 4, 512], BF16)
        C.sel = P.sb([8, 8, 128])
        C.halfpi = P.sb([128, 1])
        C.neg8 = P.sb([128, 1])
        C.mod = [P.sb([128, 48]) for _ in range(depth)]
        C.modA = [P.sb([128, 16]) for _ in range(depth)]
        P.dma(C.ident, C.inp['c_ident'])
        P.dma(C.blk, C.inp['c_blk'])
        P.dma(C.amask, C.inp['c_amask'], eng='pool')
        P.dma(C.sel, C.inp['c_sel'])
        P.memset(C.ones, 1.0)
        P.memset(C.onesb, 1.0)
        P.memset(C.halfpi, math.pi / 2)
        P.memset(C.neg8, -8.0)
        stage_prep(P, C, L)
        cur, nxt = C.xA, C.xB
        for l in range(depth):
            i = l // 2
            if l % 2 == 0:
                stage_even_proj(P, C, L, l, cur)
                stage_gdn(P, C, L, l)
                stage_s5(P, C, L, l)
                stage_ffn(P, C, L, l, cur, nxt, C.yT, C.inp['even_w_out'][i], moe=False)
            else:
                stage_attn_proj(P, C, L, l, cur)
                stage_attn(P, C, L, l)
                stage_ffn(P, C, L, l, cur, nxt, C.oT, C.inp['odd_w_out'][i], moe=True)
            cur, nxt = nxt, cur
        stage_out(P, C, L, cur)
        P.stack = st0
        C.nins = P.nins
    return nc, consts, C


def kernel(**inputs):
    x = np.asarray(inputs['x'], dtype=np.float32)
    B, L, _ = x.shape
    shapes = {'x': (L, D), 'c': (D,)}
    for nm in INPUT_NAMES:
        shapes[nm] = tuple(np.asarray(inputs[nm]).shape)
    nc, consts, C = build(shapes, L)
    shared = {nm: np.ascontiguousarray(np.asarray(inputs[nm], dtype=np.float32)) for nm in INPUT_NAMES}
    zeros = {nm: np.zeros_like(v) for nm, v in shared.items()}
    zc = {nm: np.zeros_like(v) for nm, v in consts.items()}
    real = [0, 1, 4, 5][:B]
    in_maps = []
    for core in range(8):
        if core in real:
            b = real.index(core)
            m = dict(shared)
            m.update(consts)
            m['x'] = np.ascontiguousarray(x[b])
            m['c'] = np.ascontiguousarray(np.asarray(inputs['c'], dtype=np.float32)[b])
        else:
            m = dict(zeros)
            m.update(zc)
            m['x'] = np.zeros((L, D), np.float32)
            m['c'] = np.zeros((D,), np.float32)
        in_maps.append(m)
    res = run_bass_kernel_spmd(nc, in_maps, core_ids=list(range(8)))
    out = np.stack([res.results[real[b]]['out'] for b in range(B)], axis=0)
    return out.astype(np.float32)
```

```python
import contextlib
import math
import numpy as np
import concourse.bass as bass
import concourse.mybir as mybir
from concourse.bass_utils import run_bass_kernel_spmd

F32 = mybir.dt.float32
BF16 = mybir.dt.bfloat16
AF = mybir.ActivationFunctionType
ALU = mybir.AluOpType
AX = mybir.AxisListType

D = 1024
DEPTH = 4
EPS = 1e-6
DFF = 2816
NEXP = 8
ENGS = ['pe', 'act', 'dve', 'pool', 'sp']


class V:
    __slots__ = ('res', 'ap')

    def __init__(self, res, ap):
        self.res = res
        self.ap = ap

    def __getitem__(self, idx):
        return V(self.res, self.ap[idx])

    def bc(self, shape):
        return V(self.res, self.ap.to_broadcast(list(shape)))

    def re(self, s, **kw):
        return V(self.res, self.ap.rearrange(s, **kw))


class Res:
    __slots__ = ('name', 'w', 'r', 'ap')

    def __init__(self, name, ap=None):
        self.name = name
        self.w = {}
        self.r = {}
        self.ap = ap

    def __getitem__(self, idx):
        return V(self, self.ap[idx])

    @property
    def v(self):
        return V(self, self.ap)


def _rv(x):
    return x.v if isinstance(x, Res) else x


class Prog:
    def __init__(self, nc, stack, n_dma_sems=32):
        self.nc = nc
        self.stack = stack
        self.streams = {e: [] for e in ENGS}
        self.cnt = {e: 0 for e in ENGS}
        self.semh = {}
        for e in ['pe', 'act', 'dve', 'pool']:
            self.semh['c_' + e] = stack.enter_context(nc.semaphore('c_' + e))
        self.ndma = n_dma_sems
        self.dma_tot = [0] * n_dma_sems
        self.dma_next = {'sp': 0, 'pool': 0, 'act': 0}
        self.dma_range = {'sp': (0, n_dma_sems // 2), 'pool': (n_dma_sems // 2, n_dma_sems), 'act': (0, n_dma_sems // 2)}
        for k in range(n_dma_sems):
            self.semh['d%d' % k] = stack.enter_context(nc.semaphore('d%d' % k))
        self.known = {e: {} for e in ENGS}
        self.nt = 0
        self.nins = 0

    def sb(self, shape, dtype=F32, name=None):
        self.nt += 1
        name = name or ('t%d' % self.nt)
        t = self.stack.enter_context(self.nc.sbuf_tensor(name, list(shape), dtype))
        return Res(name, t[:])

    def ps(self, shape, dtype=F32, name=None):
        self.nt += 1
        name = name or ('p%d' % self.nt)
        t = self.stack.enter_context(self.nc.psum_tensor(name, list(shape), dtype))
        return Res(name, t[:])

    def dram(self, name, shape, dtype, kind="Internal"):
        t = self.nc.dram_tensor(name, list(shape), dtype, kind=kind)
        return Res(name, t.ap())

    def _deps(self, eng, reads, writes):
        deps = {}

        def add(k, v):
            if deps.get(k, -1) < v:
                deps[k] = v
        for r in reads:
            for k, v in r.w.items():
                add(k, v)
        for w in writes:
            for k, v in w.w.items():
                add(k, v)
            for k, v in w.r.items():
                add(k, v)
        out = []
        kn = self.known[eng]
        for k, v in deps.items():
            if eng == 'pe' and k == 'c_pe':
                continue
            if kn.get(k, -1) < v:
                kn[k] = v
                out.append((k, v))
        return out

    def _record(self, ev, reads, writes, merge=False):
        k, v = ev
        for w in writes:
            if merge:
                w.w[k] = v
            else:
                w.w = {k: v}
            w.r = {}
        for r in reads:
            if r.r.get(k, -1) < v:
                r.r[k] = v

    def op(self, eng, fn, reads=(), writes=(), inc=True):
        reads = [x for x in reads if x is not None]
        waits = self._deps(eng, reads, writes)
        key = 'c_' + eng
        if inc:
            self.cnt[eng] += 1
            ev = (key, self.cnt[eng])
        else:
            ev = (key, self.cnt[eng] + 1)
        self.streams[eng].append((waits, fn, key if inc else None, 1))
        self._record(ev, reads, writes)
        self.nins += 1

    def dma(self, out, in_, eng='sp'):
        out = _rv(out)
        in_ = _rv(in_)
        lo, hi = self.dma_range[eng]
        k = lo + self.dma_next[eng]
        self.dma_next[eng] = (self.dma_next[eng] + 1) % (hi - lo)
        key = 'd%d' % k
        waits = self._deps(eng, [in_.res], [out.res])
        kn = self.known[eng]
        if kn.get(key, -1) < self.dma_tot[k]:
            kn[key] = self.dma_tot[k]
            waits.append((key, self.dma_tot[k]))
        self.dma_tot[k] += 16
        ev = (key, self.dma_tot[k])
        oa, ia = out.ap, in_.ap

        def fn(e):
            return e.dma_start(out=oa, in_=ia)
        self.streams[eng].append((waits, fn, key, 16))
        self._record(ev, [in_.res], [out.res], merge=True)
        self.nins += 1

    def collective(self, kind, out, in_, groups, eng='pool'):
        out = _rv(out)
        in_ = _rv(in_)
        lo, hi = self.dma_range[eng]
        k = lo + self.dma_next[eng]
        self.dma_next[eng] = (self.dma_next[eng] + 1) % (hi - lo)
        key = 'd%d' % k
        waits = self._deps(eng, [in_.res], [out.res])
        kn = self.known[eng]
        if kn.get(key, -1) < self.dma_tot[k]:
            kn[key] = self.dma_tot[k]
            waits.append((key, self.dma_tot[k]))
        self.dma_tot[k] += 16
        ev = (key, self.dma_tot[k])
        oa, ia = out.ap, in_.ap

        def fn(e):
            return e.collective_compute(kind, ALU.bypass, groups, [ia], [oa])
        self.streams[eng].append((waits, fn, key, 16))
        self._record(ev, [in_.res], [out.res], merge=True)
        self.nins += 1

    def barrier(self):
        allw = [('c_' + e, self.cnt[e]) for e in ['pe', 'act', 'dve', 'pool']]
        allw += [('d%d' % k, self.dma_tot[k]) for k in range(self.ndma)]
        for e in ENGS:
            kn = self.known[e]
            waits = []
            for k, v in allw:
                if e == 'pe' and k == 'c_pe':
                    continue
                if kn.get(k, -1) < v:
                    kn[k] = v
                    waits.append((k, v))
            self.streams[e].append((waits, None, None, 0))

    def flush(self):
        nc = self.nc
        engobj = {'pe': 'tensor', 'act': 'scalar', 'dve': 'vector', 'pool': 'gpsimd', 'sp': 'sync'}
        semh = self.semh
        with nc.allow_non_contiguous_dma(reason="small strided param loads"), nc.Block() as block:
            for e in ENGS:
                stream = self.streams[e]

                def body(eng, stream=stream):
                    for waits, fn, key, n in stream:
                        for k, v in waits:
                            eng.wait_ge(semh[k], v)
                        if fn is not None:
                            ins = fn(eng)
                            if key is not None:
                                ins.then_inc(semh[key], n)
                getattr(block, engobj[e])(body)
        self.streams = {e: [] for e in ENGS}

    def mm(self, out, lhsT, rhs, start=True, stop=True):
        out, lhsT, rhs = _rv(out), _rv(lhsT), _rv(rhs)
        o, l, r = out.ap, lhsT.ap, rhs.ap
        self.op('pe', lambda e: e.matmul(o, l, r, start=start, stop=stop),
                [lhsT.res, rhs.res], [out.res], inc=stop)

    def tr(self, out, in_, ident):
        out, in_, ident = _rv(out), _rv(in_), _rv(ident)
        o, i, d = out.ap, in_.ap, ident.ap
        self.op('pe', lambda e: e.transpose(o, i, d), [in_.res, ident.res], [out.res])

    def act(self, out, in_, func, bias=None, scale=None, eng='act'):
        out, in_ = _rv(out), _rv(in_)
        reads = [in_.res]
        kw = {}
        if bias is not None:
            if isinstance(bias, (V, Res)):
                bias = _rv(bias)
                reads.append(bias.res)
                kw['bias'] = bias.ap
            else:
                kw['bias'] = float(bias)
        if scale is not None:
            if isinstance(scale, (V, Res)):
                scale = _rv(scale)
                reads.append(scale.res)
                kw['scale'] = scale.ap
            else:
                kw['scale'] = float(scale)
        o, i = out.ap, in_.ap
        self.op(eng, lambda e: e.activation(o, i, func, **kw), reads, [out.res])

    def tt(self, out, a, b, op, eng='dve'):
        out, a, b = _rv(out), _rv(a), _rv(b)
        o, x, y = out.ap, a.ap, b.ap
        self.op(eng, lambda e: e.tensor_tensor(o, x, y, op), [a.res, b.res], [out.res])

    def ts(self, out, a, s1, op0, s2=None, op1=None, eng='dve'):
        out, a = _rv(out), _rv(a)
        reads = [a.res]

        def cv(s):
            if isinstance(s, (V, Res)):
                s = _rv(s)
                reads.append(s.res)
                return s.ap
            return None if s is None else float(s)
        c1, c2 = cv(s1), cv(s2)
        o, x = out.ap, a.ap
        if op1 is None:
            self.op(eng, lambda e: e.tensor_single_scalar(o, x, c1, op0), reads, [out.res])
        else:
            self.op(eng, lambda e: e.tensor_scalar(o, x, c1, c2, op0, op1), reads, [out.res])

    def stt(self, out, a, s, b, op0, op1):
        out, a, b = _rv(out), _rv(a), _rv(b)
        reads = [a.res, b.res]
        if isinstance(s, (V, Res)):
            s = _rv(s)
            reads.append(s.res)
            c = s.ap
        else:
            c = float(s)
        o, x, y = out.ap, a.ap, b.ap
        self.op('dve', lambda e: e.scalar_tensor_tensor(o, x, c, y, op0, op1), reads, [out.res])

    def copy(self, out, in_, eng='dve'):
        out, in_ = _rv(out), _rv(in_)
        o, i = out.ap, in_.ap
        if eng == 'act':
            self.op('act', lambda e: e.activation(o, i, AF.Copy), [in_.res], [out.res])
        else:
            self.op(eng, lambda e: e.tensor_copy(o, i), [in_.res], [out.res])

    def recip(self, out, in_):
        out, in_ = _rv(out), _rv(in_)
        o, i = out.ap, in_.ap
        self.op('dve', lambda e: e.reciprocal(o, i), [in_.res], [out.res])

    def memset(self, out, val, eng='dve'):
        out = _rv(out)
        o = out.ap
        self.op(eng, lambda e: e.memset(o, float(val)), [], [out.res])

    def scan(self, out, d0, d1, init, op0=ALU.mult, op1=ALU.add):
        out, d0, d1 = _rv(out), _rv(d0), _rv(d1)
        reads = [d0.res, d1.res]
        if isinstance(init, (V, Res)):
            init = _rv(init)
            reads.append(init.res)
            c = init.ap
        else:
            c = float(init)
        o, x, y = out.ap, d0.ap, d1.ap
        self.op('dve', lambda e: e.tensor_tensor_scan(o, x, y, c, op0, op1), reads, [out.res])

    def reduce(self, out, in_, op, axis=AX.X):
        out, in_ = _rv(out), _rv(in_)
        o, i = out.ap, in_.ap
        self.op('dve', lambda e: e.tensor_reduce(o, i, axis, op), [in_.res], [out.res])


class Ctx:
    pass


def rstd_from_ss(P, out_sb, ss_ps, scale, bias):
    P.act(out_sb, ss_ps, AF.Ln, bias=bias, scale=scale)
    P.act(out_sb, out_sb, AF.Exp, scale=-0.5)


def stage_prep(P, C, L):
    nc = P.nc
    I = C.inp
    with contextlib.ExitStack() as st:
        P.stack = st
        cT = P.sb([128, 8])
        with nc.allow_non_contiguous_dma(reason="tiny"):
            P.dma(cT, I['c'].v.re("(k p) -> p k", p=128))
        cond = P.sb([128, 8])
        P.act(cond, cT, AF.Silu)
        wbuf = [P.sb([128, 8, 768]) for _ in range(2)]
        pm = C.PS[0]
        for l in range(C.depth):
            bt = P.sb([128, 48])
            with nc.allow_non_contiguous_dma(reason="tiny"):
                P.dma(bt, I['ada_b'][l].re("(j p) -> p j", p=128))
            for cb in range(8):
                wb = wbuf[cb % 2]
                P.dma(wb, I['ada_w'][l][:, cb * 768:(cb + 1) * 768].re("(k p) m -> p k m", p=128))
                for j in range(6):
                    col = cb * 6 + j
                    for k in range(8):
                        P.mm(pm[:, col:col + 1], wb[:, k, j * 128:(j + 1) * 128], cond[:, k:k + 1],
                             start=(k == 0), stop=(k == 7))
            mod = C.mod[l]
            P.tt(mod, pm[:, 0:48], bt, ALU.add)
            nw = P.sb([128, 16])
            with nc.allow_non_contiguous_dma(reason="tiny"):
                P.dma(nw[:, 0:8], I['norm_mix_w'][l].re("(k p) -> p k", p=128))
                P.dma(nw[:, 8:16], I['norm_ffn_w'][l].re("(k p) -> p k", p=128))
            A = C.modA[l]
            P.stt(A[:, 0:8], mod[:, 8:16], 1.0, nw[:, 0:8], ALU.add, ALU.mult)
            P.stt(A[:, 8:16], mod[:, 32:40], 1.0, nw[:, 8:16], ALU.add, ALU.mult)
        xin = [P.sb([128, 1024]) for _ in range(2)]
        xo = [P.sb([128, 8, 512]) for _ in range(2)]
        for tt in range(L // 512):
            o = xo[tt % 2]
            for s in range(4):
                xi = xin[s % 2]
                t0 = tt * 512 + s * 128
                P.dma(xi, I['x'][t0:t0 + 128, :])
                for k in range(8):
                    pt = C.PS[1 + (k % 4)]
                    P.tr(pt[:, 0:128], xi[:, k * 128:(k + 1) * 128], C.ident)
                    P.copy(o[:, k, s * 128:(s + 1) * 128], pt[:, 0:128], eng=('act' if k % 2 else 'dve'))
            P.dma(C.xA.v.re("(k p) t -> p k t", p=128)[:, :, tt * 512:(tt + 1) * 512], o)
        P.barrier()
        P.flush()


def stage_out(P, C, L, xsrc):
    I = C.inp
    with contextlib.ExitStack() as st:
        P.stack = st
        xi = [P.sb([128, 8, 512]) for _ in range(2)]
        xo = [P.sb([128, 1024]) for _ in range(2)]
        n = 0
        for tt in range(L // 512):
            t = xi[tt % 2]
            P.dma(t, xsrc.v.re("(k p) t -> p k t", p=128)[:, :, tt * 512:(tt + 1) * 512])
            for s in range(4):
                o = xo[s % 2]
                for k in range(8):
                    pt = C.PS[1 + (k % 4)]
                    P.tr(pt[:, 0:128], t[:, k, s * 128:(s + 1) * 128], C.ident)
                    P.copy(o[:, k * 128:(k + 1) * 128], pt[:, 0:128], eng=('act' if k % 2 else 'dve'))
                t0 = tt * 512 + s * 128
                P.dma(C.out[t0:t0 + 128, :], o)
        P.barrier()
        P.flush()


def load_norm_h(P, C, S, xt, l, which, want32=False):
    A = C.modA[l]
    mod = C.mod[l]
    aoff = 0 if which == 1 else 8
    shoff = 0 if which == 1 else 24
    sq = S.sq
    P.act(sq, xt, AF.Square)
    ss = C.PS[0]
    for k in range(8):
        P.mm(ss, C.ones, sq[:, k, :], start=(k == 0), stop=(k == 7))
    rstd = S.rstd
    rstd_from_ss(P, rstd, ss, 1.0 / D, EPS)
    for k in range(8):
        tmp = S.tmp[k % 2]
        P.stt(tmp, xt[:, k, :], A[:, aoff + k:aoff + k + 1], rstd, ALU.mult, ALU.mult)
        if want32:
            P.act(S.h32[:, k, :], tmp, AF.Identity, bias=mod[:, shoff + k:shoff + k + 1])
            P.act(S.hb[:, k, :], tmp, AF.Identity, bias=mod[:, shoff + k:shoff + k + 1])
        else:
            P.act(S.hb[:, k, :], tmp, AF.Identity, bias=mod[:, shoff + k:shoff + k + 1])


def wslab_iter(P, C, S, Wv, ncols, KT, slab=512):
    c0 = 0
    i = 0
    while c0 < ncols:
        w = min(slab, ncols - c0)
        buf = S.wb[S.wbi % len(S.wb)]
        S.wbi += 1
        P.dma(buf[:, 0:KT, 0:w], Wv[:, c0:c0 + w].re("(k p) m -> p k m", p=128), eng='pool')
        yield buf, c0, w
        c0 += w
        i += 1


def stage_even_proj(P, C, L, l, xsrc):
    nc = P.nc
    I = C.inp
    i = l // 2
    Win = I['even_w_in'][i]
    with contextlib.ExitStack() as st:
        P.stack = st
        S = Ctx()
        S.sq = P.sb([128, 8, 512])
        S.rstd = P.sb([128, 512])
        S.tmp = [P.sb([128, 512]) for _ in range(2)]
        S.hb = P.sb([128, 8, 512], BF16)
        S.wb = [P.sb([128, 8, 512], BF16) for _ in range(3)]
        S.wbi = 0
        C.cmask = P.sb([4, 512])
        P.dma(C.cmask, C.inp['c_cmask'])
        xts = [P.sb([128, 8, 512]) for _ in range(2)]
        pre = [P.sb([128, 515]) for _ in range(12)]
        for m in range(12):
            P.memset(pre[m][:, 0:3], 0.0)
        cw = P.sb([128, 12, 4])
        with nc.allow_non_contiguous_dma(reason="tiny"):
            for j in range(4):
                P.dma(cw[:, :, j], I['even_conv_w'][i][j].re("(t p) -> p t", p=128))
        wba = P.sb([128, 8, 8])
        with nc.allow_non_contiguous_dma(reason="small"):
            P.dma(wba, Win[:, 2048:2056].re("(k p) m -> p k m", p=128))
        hc = P.sb([4, 2])
        with nc.allow_non_contiguous_dma(reason="tiny"):
            P.dma(hc[:, 0:1], I['even_a_log'][i].re("(h o) -> h o", o=1))
            P.dma(hc[:, 1:2], I['even_dt_bias'][i].re("(h o) -> h o", o=1))
        nA = P.sb([4, 1])
        P.act(nA, hc[:, 0:1], AF.Exp)
        P.ts(nA, nA, -1.0, ALU.mult)
        h32 = P.sb([128, 8, 512])
        S.h32 = h32
        acc = [P.sb([128, 512]) for _ in range(2)]
        ob = [P.sb([128, 512]) for _ in range(3)]
        sm = [P.sb([4, 512]) for _ in range(6)]
        NT = L // 512
        xv = xsrc.v.re("(k p) t -> p k t", p=128)
        P.dma(xts[0], xv[:, :, 0:512])
        nob = 0
        for tt in range(NT):
            xt = xts[tt % 2]
            if tt + 1 < NT:
                P.dma(xts[(tt + 1) % 2], xv[:, :, (tt + 1) * 512:(tt + 2) * 512])
            load_norm_h(P, C, S, xt, l, 1, want32=True)
            tsl = slice(tt * 512, (tt + 1) * 512)
            pb, pa = C.PS[1], C.PS[2]
            for k in range(8):
                P.mm(pb[0:4, :], wba[:, k, 0:4], h32[:, k, :], start=(k == 0), stop=(k == 7))
            for k in range(8):
                P.mm(pa[0:4, :], wba[:, k, 4:8], h32[:, k, :], start=(k == 0), stop=(k == 7))
            beta = sm[0]
            P.act(beta[:], pb[0:4, :], AF.Exp, scale=-1.0)
            P.ts(beta, beta, 1.0, ALU.add)
            P.recip(beta, beta)
            P.dma(C.betaT[:, tsl], beta)
            xa = sm[1]
            P.act(xa[:], pa[0:4, :], AF.Identity, bias=hc[:, 1:2])
            ax = sm[2]
            P.stt(ax, xa, -1.0, xa, ALU.mult, ALU.max)
            P.act(ax, ax, AF.Exp, scale=-1.0)
            P.act(ax, ax, AF.Ln, bias=1.0)
            sp = sm[3]
            P.stt(sp, xa, 0.0, ax, ALU.max, ALU.add)
            g = sm[4]
            P.ts(g, sp, nA[:, 0:1], ALU.mult)
            gc = sm[5]
            P.scan(gc, C.cmask, g, 0.0)
            P.dma(C.gT[:, tsl], gc)
            mt = 0
            for wsl, c0, w in wslab_iter(P, C, S, Win, 2048, 8):
                for j in range(w // 128):
                    m = (c0 // 128) + j
                    pp = C.PS[3 + (m % 4)]
                    for k in range(8):
                        P.mm(pp, wsl[:, k, j * 128:(j + 1) * 128], S.hb[:, k, :], start=(k == 0), stop=(k == 7))
                    if m < 12:
                        pr = pre[m]
                        P.copy(pr[:, 3:515], pp, eng='act')
                        a = acc[m % 2]
                        P.ts(a, pr[:, 0:512], cw[:, m, 0:1], ALU.mult)
                        for jj in range(1, 4):
                            P.stt(a, pr[:, jj:jj + 512], cw[:, m, jj:jj + 1], a, ALU.mult, ALU.add)
                        P.copy(pr[:, 0:3], pr[:, 512:515], eng='pool')
                        o = ob[nob % 3]
                        nob += 1
                        P.act(o, a, AF.Silu)
                        if m < 8:
                            sq = S.tmp[m % 2]
                            P.act(sq, o, AF.Square)
                            ss = C.PS[7]
                            P.mm(ss, C.ones, sq)
                            rs = S.rstd
                            if m < 4:
                                rstd_from_ss(P, rs, ss, 128.0, 128.0 * EPS)
                            else:
                                rstd_from_ss(P, rs, ss, 1.0, EPS)
                            P.tt(o, o, rs, ALU.mult)
                        P.dma(C.qkvT[m * 128:(m + 1) * 128, tsl], o)
                    else:
                        o = ob[nob % 3]
                        nob += 1
                        P.act(o, pp, AF.Silu)
                        P.dma(C.zT[(m - 12) * 128:(m - 11) * 128, tsl], o)
            for wsl, c0, w in wslab_iter(P, C, S, Win[:, 2056:2568], 512, 8):
                for j in range(4):
                    pp = C.PS[3 + (j % 4)]
                    for k in range(8):
                        P.mm(pp, wsl[:, k, j * 128:(j + 1) * 128], S.hb[:, k, :], start=(k == 0), stop=(k == 7))
                    o = ob[nob % 3]
                    nob += 1
                    P.copy(o, pp, eng='act')
                    P.dma(C.uT[j * 128:(j + 1) * 128, tsl], o)
        P.barrier()
        P.flush()


def stage_gdn(P, C, L, l):
    nc = P.nc
    I = C.inp
    i = l // 2
    NT = L // 512
    with contextlib.ExitStack() as st:
        P.stack = st
        C.gmask = P.sb([64, 4, 512])
        P.dma(C.gmask, C.inp['c_gmask'])
        Sst = [P.sb([128, 128]) for _ in range(4)]
        for h in range(4):
            P.memset(Sst[h], 0.0)
        gw = P.sb([128, 1])
        with nc.allow_non_contiguous_dma(reason="tiny"):
            P.dma(gw, I['even_gdn_norm_w'][i].re("(p o) -> p o", o=1))
        mk = lambda n, shape, dt=F32: [P.sb(shape, dt) for _ in range(n)]
        qT, kT, vT = mk(2, [128, 512]), mk(2, [128, 512]), mk(2, [128, 512])
        rows = mk(2, [1, 2, 512])
        r_eg, r_ekl, r_ng = mk(2, [1, 512]), mk(2, [1, 512]), mk(2, [1, 512])
        bcs = mk(2, [128, 3, 512])
        kb, kbe, vb, qe, kel = (mk(2, [128, 512]) for _ in range(5))
        tokm = mk(2, [64, 3, 8, 128])
        EL, EU, t1 = mk(2, [64, 512]), mk(2, [64, 512]), mk(2, [64, 512])
        Am, ATm, PTm, Rm = mk(2, [64, 512]), mk(2, [64, 512]), mk(2, [64, 512]), mk(2, [64, 512])
        WTn = mk(2, [128, 512])
        vnew = [mk(2, [64, 128]) for _ in range(2)]
        zt = mk(2, [128, 512])
        osb = mk(2, [128, 512])
        sq = mk(2, [128, 512])
        rs = mk(2, [128, 512])
        def chain(tt, h, b, BK):
            tsl = slice(tt * 512, (tt + 1) * 512)
            P.dma(qT[b], C.qkvT[h * 128:(h + 1) * 128, tsl])
            P.dma(kT[b], C.qkvT[512 + h * 128:512 + (h + 1) * 128, tsl])
            P.dma(vT[b], C.qkvT[1024 + h * 128:1024 + (h + 1) * 128, tsl])
            P.dma(rows[b][:, 0, :], C.betaT[h:h + 1, tsl])
            P.dma(rows[b][:, 1, :], C.gT[h:h + 1, tsl])
            P.dma(zt[b], C.zT[h * 128:(h + 1) * 128, tsl])
            gc = rows[b][:, 1, :]
            P.act(r_eg[b], gc, AF.Exp)
            g3 = rows[b][:, 1, :].re("o (c j) -> o c j", j=64)
            P.tt(r_ekl[b].v.re("o (c j) -> o c j", j=64), g3[:, :, 63:64].bc([1, 8, 64]), g3, ALU.subtract)
            P.act(r_ekl[b], r_ekl[b], AF.Exp)
            P.ts(r_ng[b], gc, -1.0, ALU.mult)
            yield
            pbc = [BK[0], BK[1], BK[2]]
            P.mm(pbc[0], C.ones[0:1, :], rows[b][:, 0, :])
            P.mm(pbc[1], C.ones[0:1, :], r_eg[b])
            P.mm(pbc[2], C.ones[0:1, :], r_ekl[b])
            for j in range(3):
                P.copy(bcs[b][:, j, :], pbc[j], eng='act')
            P.tt(kb[b], kT[b], bcs[b][:, 0, :], ALU.mult)
            P.tt(kbe[b], kb[b], bcs[b][:, 1, :], ALU.mult, eng='pool')
            P.tt(vb[b], vT[b], bcs[b][:, 0, :], ALU.mult)
            P.tt(qe[b], qT[b], bcs[b][:, 1, :], ALU.mult, eng='pool')
            P.tt(kel[b], kT[b], bcs[b][:, 2, :], ALU.mult)
            yield
            for c in range(8):
                csl = slice(c * 64, (c + 1) * 64)
                for j, src in enumerate((vb[b], kbe[b], kel[b])):
                    pt = BK[3 - ((c * 3 + j) % 2)]
                    P.tr(pt[0:64, 0:128], src[:, csl], C.ident)
                    P.copy(tokm[b][:, j, c, :], pt[0:64, 0:128], eng=('act' if j != 1 else 'dve'))
                yield
            pg = BK[2]
            for c in range(8):
                csl = slice(c * 64, (c + 1) * 64)
                P.mm(pg[0:64, csl], rows[b][:, 1, csl], C.ones[0:1, 0:64], start=True, stop=False)
                P.mm(pg[0:64, csl], C.ones[0:1, 0:64], r_ng[b][:, csl], start=False, stop=True)
            P.ts(t1[b], pg[0:64, :], 0.0, ALU.min)
            P.act(EL[b], t1[b], AF.Exp)
            P.ts(t1[b], pg[0:64, :], 0.0, ALU.max)
            P.act(EU[b], t1[b], AF.Exp, scale=-1.0)
            yield
            pA, pAT, pPT = BK[0], BK[1], BK[2]
            for c in range(8):
                csl = slice(c * 64, (c + 1) * 64)
                P.mm(pA[0:64, csl], kb[b][:, csl], kT[b][:, csl])
                P.mm(pAT[0:64, csl], kT[b][:, csl], kb[b][:, csl])
                P.mm(pPT[0:64, csl], kT[b][:, csl], qT[b][:, csl])
            P.tt(t1[b], EL[b], C.gmask[:, 0, :], ALU.mult, eng='pool')
            P.tt(Am[b], pA[0:64, :], t1[b], ALU.mult)
            P.tt(EL[b], EU[b], C.gmask[:, 1, :], ALU.mult, eng='pool')
            P.tt(ATm[b], pAT[0:64, :], EL[b], ALU.mult)
            P.tt(EU[b], EU[b], C.gmask[:, 2, :], ALU.mult, eng='pool')
            P.tt(PTm[b], pPT[0:64, :], EU[b], ALU.mult)
            yield
            P.tt(Rm[b], ATm[b], C.gmask[:, 3, :], ALU.add)
            X, XT = Am[b], ATm[b]
            X2, XT2 = t1[b], EL[b]
            for step in range(5):
                p1, p2, p3 = BK[0], BK[1], BK[2]
                for c in range(8):
                    csl = slice(c * 64, (c + 1) * 64)
                    P.mm(p1[0:64, csl], XT[:, csl], X[:, csl])
                    P.mm(p2[0:64, csl], X[:, csl], XT[:, csl])
                P.copy(X2, p1[0:64, :], eng='act')
                P.copy(XT2, p2[0:64, :], eng='dve')
                yield
                X, X2 = X2, X
                XT, XT2 = XT2, XT
                for c in range(8):
                    csl = slice(c * 64, (c + 1) * 64)
                    P.mm(p3[0:64, csl], X[:, csl], Rm[b][:, csl])
                P.tt(Rm[b], Rm[b], p3[0:64, :], ALU.add)
                yield
            pW = BK[0]
            for c in range(8):
                csl = slice(c * 64, (c + 1) * 64)
                P.mm(pW[:, csl], tokm[b][:, 1, c, :], Rm[b][:, csl])
            P.act(WTn[b], pW, AF.Copy, scale=-1.0)
            yield
            po = BK[3]
            S = Sst[h]
            for c in range(8):
                csl = slice(c * 64, (c + 1) * 64)
                pv = BK[1]
                P.mm(pv[0:64, 0:128], Rm[b][:, csl], tokm[b][:, 0, c, :], start=True, stop=False)
                P.mm(pv[0:64, 0:128], WTn[b][:, csl], S, start=False, stop=True)
                vn = vnew[b][c % 2]
                P.copy(vn, pv[0:64, 0:128], eng='act')
                P.mm(po[:, csl], S, qe[b][:, csl], start=True, stop=False)
                P.mm(po[:, csl], vn, PTm[b][:, csl], start=False, stop=True)
                ps_ = BK[2]
                P.mm(ps_[:, 0:128], tokm[b][:, 2, c, :], vn)
                P.stt(S, S, bcs[b][:, 1, c * 64 + 63:c * 64 + 64], ps_[:, 0:128], ALU.mult, ALU.add)
                yield
            P.copy(osb[b], po, eng='act')
            P.act(sq[b], osb[b], AF.Square)
            pss = BK[0]
            P.mm(pss, C.ones, sq[b])
            rstd_from_ss(P, rs[b], pss, 1.0 / 128, EPS)
            P.stt(osb[b], osb[b], gw[:, 0:1], rs[b], ALU.mult, ALU.mult)
            P.tt(osb[b], osb[b], zt[b], ALU.mult)
            P.dma(C.yT[h * 128:(h + 1) * 128, tsl], osb[b])
        for tt in range(NT):
            for hp in range(2):
                gens = [chain(tt, 2 * hp, 0, C.PS[0:4]), chain(tt, 2 * hp + 1, 1, C.PS[4:8])]
                while gens:
                    for g_ in list(gens):
                        try:
                            next(g_)
                        except StopIteration:
                            gens.remove(g_)
        P.barrier()
        P.flush()


def stage_s5(P, C, L, l):
    nc = P.nc
    I = C.inp
    i = l // 2
    NT = L // 512
    TWO_PI = 2.0 * math.pi
    with contextlib.ExitStack() as st:
        P.stack = st
        lr = P.sb([128, 16])
        li = P.sb([128, 16])
        ls = P.sb([128, 16])
        with nc.allow_non_contiguous_dma(reason="small"):
            P.dma(lr, I['even_lam_re'][i].re("(j g) n -> (g n) j", g=2))
            P.dma(li, I['even_lam_im'][i].re("(j g) n -> (g n) j", g=2))
            lsv = I['even_log_step'][i].re("(j g) -> g j", g=2)
            for g2 in range(2):
                P.dma(ls[g2 * 64:(g2 + 1) * 64, :], lsv[g2:g2 + 1, :].bc([64, 16]))
        dt = P.sb([128, 16])
        P.act(dt, ls, AF.Exp)
        P.ts(lr, lr, -1e-4, ALU.min)
        rho = P.sb([128, 16])
        P.tt(rho, lr, dt, ALU.mult)
        P.act(rho, rho, AF.Exp)
        th = P.sb([128, 16])
        P.tt(th, li, dt, ALU.mult)
        tmpa = P.sb([128, 16])
        sn = P.sb([128, 16])
        cs = P.sb([128, 16])
        P.act(sn, th, AF.Sin, scale=1.0 / 16)
        P.act(cs, th, AF.Sin, scale=1.0 / 16, bias=C.halfpi[:, 0:1])
        t_c2, t_s2 = P.sb([128, 16]), P.sb([128, 16])
        for _ in range(4):
            P.tt(t_c2, cs, cs, ALU.mult)
            P.tt(t_s2, sn, sn, ALU.mult)
            P.tt(sn, cs, sn, ALU.mult)
            P.ts(sn, sn, 2.0, ALU.mult)
            P.tt(cs, t_c2, t_s2, ALU.subtract)
        ar, ai = P.sb([128, 16]), P.sb([128, 16])
        P.tt(ar, rho, cs, ALU.mult)
        P.tt(ai, rho, sn, ALU.mult)
        nr = P.sb([128, 16])
        P.ts(nr, ar, -1.0, ALU.add)
        den = P.sb([128, 16])
        t2 = P.sb([128, 16])
        P.tt(den, lr, lr, ALU.mult)
        P.tt(t2, li, li, ALU.mult)
        P.tt(den, den, t2, ALU.add)
        P.recip(den, den)
        cr, ci = P.sb([128, 16]), P.sb([128, 16])
        P.tt(cr, nr, lr, ALU.mult)
        P.tt(t2, ai, li, ALU.mult)
        P.tt(cr, cr, t2, ALU.add)
        P.tt(cr, cr, den, ALU.mult)
        P.tt(ci, ai, lr, ALU.mult)
        P.tt(t2, nr, li, ALU.mult)
        P.tt(ci, ci, t2, ALU.subtract)
        P.tt(ci, ci, den, ALU.mult)
        nsn = P.sb([128, 16])
        P.ts(nsn, sn, -1.0, ALU.mult)
        Tc = P.sb([128, 16, 512])
        Ts = P.sb([128, 16, 512])
        P.memset(Tc[:, :, 0:1], 1.0)
        P.memset(Ts[:, :, 0:1], 0.0)
        cc, s_ = P.sb([128, 16]), P.sb([128, 16])
        P.copy(cc, cs)
        P.copy(s_, sn)
        ta, tb = P.sb([128, 256]), P.sb([128, 256])
        c2, s2 = P.sb([128, 16]), P.sb([128, 16])
        span = 1
        while span < 512:
            for j in range(16):
                P.ts(ta[:, 0:span], Ts[:, j, 0:span], s_[:, j:j + 1], ALU.mult)
                P.stt(Tc[:, j, span:2 * span], Tc[:, j, 0:span], cc[:, j:j + 1], ta[:, 0:span], ALU.mult, ALU.subtract)
                P.ts(tb[:, 0:span], Tc[:, j, 0:span], s_[:, j:j + 1], ALU.mult)
                P.stt(Ts[:, j, span:2 * span], Ts[:, j, 0:span], cc[:, j:j + 1], tb[:, 0:span], ALU.mult, ALU.add)
            P.tt(c2, cc, cc, ALU.mult)
            P.tt(s2, s_, s_, ALU.mult)
            P.tt(s_, cc, s_, ALU.mult)
            P.ts(s_, s_, 2.0, ALU.mult)
            P.tt(cc, c2, s2, ALU.subtract)
            span *= 2
        BreT = [P.sb([128, 128]) for _ in range(16)]
        BimT = [P.sb([128, 128]) for _ in range(16)]
        CrT = [P.sb([128, 128]) for _ in range(16)]
        CiT = [P.sb([128, 128]) for _ in range(16)]
        pad = [P.sb([128, 128]) for _ in range(4)]
        craw = [P.sb([128, 128]) for _ in range(2)]
        for j in range(16):
            kt = j // 4
            for which, (nm, dst) in enumerate((('even_b_re', BreT), ('even_b_im', BimT))):
                pd = pad[which]
                P.memset(pd, 0.0)
                for g2 in range(2):
                    g = 2 * j + g2
                    off = (g - 8 * kt) * 16
                    P.dma(pd[g2 * 64:(g2 + 1) * 64, off:off + 16], I[nm][i][g])
                pt = C.PS[which]
                P.tr(pt[:, 0:128], pd, C.ident)
                P.copy(dst[j], pt[:, 0:128], eng='act')
            for which, nm in enumerate(('even_c_re', 'even_c_im')):
                pd = pad[2 + which]
                P.memset(pd, 0.0)
                for g2 in range(2):
                    g = 2 * j + g2
                    off = (g - 8 * kt) * 16
                    P.dma(pd[off:off + 16, g2 * 64:(g2 + 1) * 64], I[nm][i][g])
                pt = C.PS[2 + which]
                P.tr(pt[:, 0:128], pd, C.ident)
                P.copy(craw[which], pt[:, 0:128], eng='act')
            P.ts(pad[0], craw[1], ci[:, j:j + 1], ALU.mult)
            P.stt(CrT[j], craw[0], cr[:, j:j + 1], pad[0], ALU.mult, ALU.subtract)
            P.ts(pad[1], craw[1], cr[:, j:j + 1], ALU.mult)
            P.stt(CiT[j], craw[0], ci[:, j:j + 1], pad[1], ALU.mult, ALU.add)
            P.ts(CiT[j], CiT[j], -1.0, ALU.mult)
        dsk = P.sb([128, 4])
        glb = P.sb([128, 8])
        with nc.allow_non_contiguous_dma(reason="tiny"):
            P.dma(dsk, I['even_d_skip'][i].re("(k p) -> p k", p=128))
            P.dma(glb, I['even_glu_b'][i].re("(k p) -> p k", p=128))
        nglb = P.sb([128, 8])
        P.ts(nglb, glb, -1.0, ALU.mult)
        gluw = P.sb([128, 4, 1024], BF16)
        P.dma(gluw, I['even_glu_w'][i].re("(k p) m -> p k m", p=128), eng='pool')
        ini = [P.sb([128, 2]) for _ in range(16)]
        for j in range(16):
            P.memset(ini[j], 0.0)
        uts = [P.sb([128, 4, 512]) for _ in range(2)]
        bu = [[P.sb([128, 512]) for _ in range(2)] for _ in range(2)]
        zin = [[P.sb([128, 512]) for _ in range(2)] for _ in range(2)]
        zz = [[P.sb([128, 512]) for _ in range(2)] for _ in range(2)]
        w1s = [[P.sb([128, 512]) for _ in range(4)] for _ in range(2)]
        xx = [[P.sb([128, 512]) for _ in range(2)] for _ in range(2)]
        sml = [P.sb([128, 2]) for _ in range(2)]
        yg = P.sb([128, 4, 512], BF16)
        ysb = [P.sb([128, 512]) for _ in range(2)]
        ga = [P.sb([128, 512]) for _ in range(2)]
        gb = [P.sb([128, 512]) for _ in range(2)]
        uv = C.uT.v.re("(k p) t -> p k t", p=128)
        P.dma(uts[0], uv[:, :, 0:512])
        it = 0
        for tt in range(NT):
            tsl = slice(tt * 512, (tt + 1) * 512)
            ut = uts[tt % 2]
            if tt + 1 < NT:
                P.dma(uts[(tt + 1) % 2], uv[:, :, (tt + 1) * 512:(tt + 2) * 512])
            def chain(j, b, kt):
                pr, pi_ = C.PS[0 + 2 * b], C.PS[1 + 2 * b]
                W = w1s[b]
                P.mm(pr, BreT[j], ut[:, kt, :])
                P.mm(pi_, BimT[j], ut[:, kt, :])
                P.copy(bu[b][0].v, pr, eng='act')
                P.copy(bu[b][1].v, pi_, eng='act')
                yield
                P.tt(W[0], bu[b][0].v, Tc[:, j, :], ALU.mult)
                P.tt(W[1], bu[b][1].v, Ts[:, j, :], ALU.mult, eng='pool')
                yield
                P.tt(zin[b][0].v, W[0], W[1], ALU.add)
                P.tt(W[2], bu[b][1].v, Tc[:, j, :], ALU.mult, eng='pool')
                yield
                P.tt(W[3], bu[b][0].v, Ts[:, j, :], ALU.mult)
                yield
                P.tt(zin[b][1].v, W[2], W[3], ALU.subtract, eng='pool')
                P.scan(zz[b][0].v, rho[:, j:j + 1].bc([128, 512]), zin[b][0].v, ini[j][:, 0:1])
                yield
                P.scan(zz[b][1].v, rho[:, j:j + 1].bc([128, 512]), zin[b][1].v, ini[j][:, 1:2])
                yield
                P.tt(W[0], zz[b][0].v, Tc[:, j, :], ALU.mult)
                P.tt(W[1], zz[b][1].v, Ts[:, j, :], ALU.mult, eng='pool')
                yield
                P.tt(xx[b][0].v, W[0], W[1], ALU.subtract)
                P.tt(W[2], zz[b][1].v, Tc[:, j, :], ALU.mult, eng='pool')
                yield
                P.tt(W[3], zz[b][0].v, Ts[:, j, :], ALU.mult)
                yield
                P.tt(xx[b][1].v, W[2], W[3], ALU.add, eng='pool')
                yield
                sm_ = sml[b]
                P.ts(sm_[:, 0:1], xx[b][1][:, 511:512], nsn[:, j:j + 1], ALU.mult)
                P.ts(sm_[:, 1:2], xx[b][0][:, 511:512], sn[:, j:j + 1], ALU.mult)
                P.stt(ini[j][:, 0:1], xx[b][0][:, 511:512], cs[:, j:j + 1], sm_[:, 0:1], ALU.mult, ALU.add)
                P.stt(ini[j][:, 1:2], xx[b][1][:, 511:512], cs[:, j:j + 1], sm_[:, 1:2], ALU.mult, ALU.add)

            for kt in range(4):
                py = C.PS[4 + (kt % 2)]
                for jp in range(2):
                    js = [kt * 4 + jp * 2, kt * 4 + jp * 2 + 1]
                    gens = [chain(js[0], 0, kt), chain(js[1], 1, kt)]
                    while gens:
                        for g_ in list(gens):
                            try:
                                next(g_)
                            except StopIteration:
                                gens.remove(g_)
                    for b, j in enumerate(js):
                        first = (jp == 0 and b == 0)
                        last = (jp == 1 and b == 1)
                        P.mm(py, CrT[j], xx[b][0].v, start=first, stop=False)
                        P.mm(py, CiT[j], xx[b][1].v, start=False, stop=last)
                y = ysb[kt % 2]
                P.stt(y, ut[:, kt, :], dsk[:, kt:kt + 1], py, ALU.mult, ALU.add)
                P.act(yg[:, kt, :], y, AF.Gelu)
            for m in range(4):
                pa, pb = C.PS[6], C.PS[7]
                for k in range(4):
                    P.mm(pa, gluw[:, k, m * 128:(m + 1) * 128], yg[:, k, :], start=(k == 0), stop=(k == 3))
                for k in range(4):
                    P.mm(pb, gluw[:, k, 512 + m * 128:512 + (m + 1) * 128], yg[:, k, :], start=(k == 0), stop=(k == 3))
                a_, b_ = ga[m % 2], gb[m % 2]
                P.act(a_, pa, AF.Identity, bias=glb[:, m:m + 1])
                P.act(b_, pb, AF.Exp, bias=nglb[:, 4 + m:5 + m], scale=-1.0)
                P.ts(b_, b_, 1.0, ALU.add)
                P.recip(b_, b_)
                P.tt(a_, a_, b_, ALU.mult)
                P.dma(C.yT[512 + m * 128:512 + (m + 1) * 128, tsl], a_)
        P.barrier()
        P.flush()


def stage_ffn(P, C, L, l, xsrc, xdst, ysrc, wout, moe, G=2):
    nc = P.nc
    I = C.inp
    i = l // 2
    NT = L // 512
    G = min(G, NT)
    NG = NT // G
    mod = C.mod[l]
    with contextlib.ExitStack() as st:
        P.stack = st
        S = Ctx()
        faccs = [P.sb([128, 8, 512]) for _ in range(G)]
        S.sq = faccs[0]
        S.rstd = P.sb([128, 512])
        S.tmp = [P.sb([128, 512]) for _ in range(2)]
        hbs = [P.sb([128, 8, 512], BF16) for _ in range(G)]
        S.h32 = P.sb([128, 8, 512]) if moe else None
        S.wb = [P.sb([128, 8, 512], BF16) for _ in range(2)]
        S.wbi = 0
        w2b = [P.sb([128, 22, 128], BF16) for _ in range(2)]
        nw2 = 0
        xt = P.sb([128, 8, 512])
        ybf = P.sb([128, 8, 512], BF16)
        hids = [P.sb([128, 22, 512], BF16) for _ in range(G)]
        sa = [P.sb([128, 512]) for _ in range(3)]
        if moe:
            rw = P.sb([128, 8, 8])
            P.dma(rw, I['odd_router_w'][i].re("(k p) e -> p k e", p=128))
            lg = P.sb([8, 512])
            lt = P.sb([128, 4, 8])
            m1 = P.sb([128, 4])
            m2 = P.sb([128, 4])
            eq1 = P.sb([128, 4, 8])
            eq2 = P.sb([128, 4, 8])
            msk = P.sb([128, 4, 8])
            gg = P.sb([128, 4])
            g2_ = P.sb([128, 4])
            cmb = P.sb([128, 4, 8])
            cmbT = P.sb([8, 512])
            combs = [P.sb([128, 8, 512], BF16) for _ in range(G)]
        xv = xsrc.v.re("(k p) t -> p k t", p=128)
        yv = ysrc.v.re("(k p) t -> p k t", p=128)
        ov = xdst.v.re("(k p) t -> p k t", p=128)
        mv = C.xmid.v.re("(k p) t -> p k t", p=128)
        nsa = 0
        npp = 0
        for grp in range(NG):
            for g in range(G):
                tt = grp * G + g
                tsl = slice(tt * 512, (tt + 1) * 512)
                P.dma(xt, xv[:, :, tsl])
                P.dma(ybf, yv[:, :, tsl], eng='pool')
                for wsl, c0, w in wslab_iter(P, C, S, wout, 1024, 8):
                    for j in range(4):
                        m = c0 // 128 + j
                        pp = C.PS[1 + (m % 4)]
                        for k in range(8):
                            P.mm(pp, wsl[:, k, j * 128:(j + 1) * 128], ybf[:, k, :], start=(k == 0), stop=(k == 7))
                        P.stt(xt[:, m, :], pp, mod[:, 16 + m:17 + m], xt[:, m, :], ALU.mult, ALU.add)
                P.dma(mv[:, :, tsl], xt)
                S.hb = hbs[g]
                load_norm_h(P, C, S, xt, l, 2, want32=moe)
                if moe:
                    comb = combs[g]
                    pl = C.PS[5]
                    for k in range(8):
                        P.mm(pl[0:8, :], rw[:, k, :], S.h32[:, k, :], start=(k == 0), stop=(k == 7))
                    P.copy(lg, pl[0:8, :], eng='act')
                    pt = C.PS[6]
                    for s in range(4):
                        P.tr(pt[:, s * 8:(s + 1) * 8], lg[:, s * 128:(s + 1) * 128], C.ident[0:8, 0:8])
                    P.copy(lt, pt[:, 0:32].re("p (s e) -> p s e", e=8), eng='act')
                    P.reduce(m1, lt, ALU.max)
                    P.tt(eq1, lt, m1.v.re("p (s o) -> p s o", o=1).bc([128, 4, 8]), ALU.is_equal)
                    P.stt(msk, eq1, -1e30, lt, ALU.mult, ALU.add)
                    P.reduce(m2, msk, ALU.max)
                    P.tt(eq2, msk, m2.v.re("p (s o) -> p s o", o=1).bc([128, 4, 8]), ALU.is_equal)
                    P.tt(gg, m2, m1, ALU.subtract)
                    P.act(gg, gg, AF.Exp)
                    P.ts(gg, gg, 1.0, ALU.add)
                    P.recip(gg, gg)
                    P.ts(g2_, gg, -1.0, ALU.mult, 1.0, ALU.add)
                    P.tt(cmb, eq1, gg.v.re("p (s o) -> p s o", o=1).bc([128, 4, 8]), ALU.mult)
                    P.tt(eq2, eq2, g2_.v.re("p (s o) -> p s o", o=1).bc([128, 4, 8]), ALU.mult)
                    P.tt(cmb, cmb, eq2, ALU.add)
                    pc = C.PS[7]
                    for s in range(4):
                        P.tr(pc[0:8, s * 128:(s + 1) * 128], cmb[:, s, :], C.ident)
                    P.copy(cmbT, pc[0:8, :], eng='act')
                    for e in range(NEXP):
                        pb_ = C.PS[5 + (e % 2)]
                        P.mm(pb_, C.sel[:, e, :], cmbT)
                        P.copy(comb[:, e, :], pb_, eng='act')
            nexp = NEXP if moe else 1
            for e in range(nexp):
                if moe:
                    W13 = I['odd_expert_w13'][i][e]
                    W2 = I['odd_expert_w2'][i][e]
                else:
                    W13 = I['even_ffn_w13'][i]
                    W2 = I['even_ffn_w2'][i]
                for c0 in range(0, DFF, 256):
                    w = min(256, DFF - c0)
                    buf = S.wb[S.wbi % 2]
                    S.wbi += 1
                    P.dma(buf[:, :, 0:w], W13[:, c0:c0 + w].re("(k p) m -> p k m", p=128), eng='pool')
                    P.dma(buf[:, :, 256:256 + w], W13[:, DFF + c0:DFF + c0 + w].re("(k p) m -> p k m", p=128), eng='pool')
                    for j in range(w // 128):
                        f = c0 // 128 + j
                        for g in range(G):
                            pa, pb = C.PS[1 + 2 * (npp % 2)], C.PS[2 + 2 * (npp % 2)]
                            npp += 1
                            for k in range(8):
                                P.mm(pa, buf[:, k, j * 128:(j + 1) * 128], hbs[g][:, k, :], start=(k == 0), stop=(k == 7))
                            for k in range(8):
                                P.mm(pb, buf[:, k, 256 + j * 128:256 + (j + 1) * 128], hbs[g][:, k, :], start=(k == 0), stop=(k == 7))
                            s_ = sa[nsa % 3]
                            nsa += 1
                            P.act(s_, pa, AF.Silu)
                            if moe:
                                P.tt(s_, s_, combs[g][:, e, :], ALU.mult)
                            P.tt(hids[g][:, f, :], pb, s_, ALU.mult)
                for m in range(8):
                    wb2 = w2b[nw2 % 2]
                    nw2 += 1
                    P.dma(wb2, W2[:, m * 128:(m + 1) * 128].re("(f p) m -> p f m", p=128), eng='pool')
                    for g in range(G):
                        facc = faccs[g]
                        pp = C.PS[5 + ((m * G + g) % 3)]
                        for f in range(22):
                            P.mm(pp, wb2[:, f, :], hids[g][:, f, :], start=(f == 0), stop=(f == 21))
                        if e == 0:
                            P.copy(facc[:, m, :], pp, eng='act')
                        else:
                            P.tt(facc[:, m, :], pp, facc[:, m, :], ALU.add)
            for g in range(G):
                tt = grp * G + g
                tsl = slice(tt * 512, (tt + 1) * 512)
                P.dma(xt, mv[:, :, tsl])
                for m in range(8):
                    P.stt(faccs[g][:, m, :], faccs[g][:, m, :], mod[:, 40 + m:41 + m], xt[:, m, :], ALU.mult, ALU.add)
                P.dma(ov[:, :, tsl], faccs[g])
        P.barrier()
        P.flush()


def stage_attn_proj(P, C, L, l, xsrc):
    nc = P.nc
    I = C.inp
    i = l // 2
    NT = L // 512
    Wq = I['odd_w_qkv'][i]
    with contextlib.ExitStack() as st:
        P.stack = st
        S = Ctx()
        S.sq = P.sb([128, 8, 512])
        S.rstd = P.sb([128, 512])
        S.tmp = [P.sb([128, 512]) for _ in range(2)]
        S.hb = P.sb([128, 8, 512], BF16)
        S.h32 = None
        S.wb = [P.sb([128, 8, 512], BF16) for _ in range(3)]
        S.wbi = 0
        wv = P.sb([128, 8, 1024], BF16)
        P.dma(wv, Wq[:, 2048:3072].re("(k p) m -> p k m", p=128), eng='pool')
        nw = P.sb([128, 2])
        with nc.allow_non_contiguous_dma(reason="tiny"):
            for t in range(2):
                P.dma(nw[t * 64:(t + 1) * 64, 0:1], I['odd_q_norm_w'][i].re("(p o) -> p o", o=1))
                P.dma(nw[t * 64:(t + 1) * 64, 1:2], I['odd_k_norm_w'][i].re("(p o) -> p o", o=1))
        xts = [P.sb([128, 8, 512]) for _ in range(2)]
        raw = [P.sb([128, 512]) for _ in range(2)]
        sq = [P.sb([128, 512]) for _ in range(2)]
        rs = [P.sb([128, 512]) for _ in range(2)]
        ob = [P.sb([128, 512], BF16) for _ in range(3)]
        vb = [P.sb([128, 1024], BF16) for _ in range(2)]
        xv = xsrc.v.re("(k p) t -> p k t", p=128)
        P.dma(xts[0], xv[:, :, 0:512])
        n = 0
        for tt in range(NT):
            tsl = slice(tt * 512, (tt + 1) * 512)
            xt = xts[tt % 2]
            if tt + 1 < NT:
                P.dma(xts[(tt + 1) % 2], xv[:, :, (tt + 1) * 512:(tt + 2) * 512])
            load_norm_h(P, C, S, xt, l, 1)
            for wsl, c0, w in wslab_iter(P, C, S, Wq, 2048, 8):
                for j in range(4):
                    m = c0 // 128 + j
                    isq = m < 8
                    pp = C.PS[1 + (m % 4)]
                    for k in range(8):
                        P.mm(pp, wsl[:, k, j * 128:(j + 1) * 128], S.hb[:, k, :], start=(k == 0), stop=(k == 7))
                    r = raw[n % 2]
                    P.copy(r, pp, eng='act')
                    P.act(sq[n % 2], r, AF.Square)
                    pss = C.PS[5 + (n % 2)]
                    P.mm(pss, C.blk, sq[n % 2])
                    if isq:
                        rstd_from_ss(P, rs[n % 2], pss, 1.0, 64.0 * EPS)
                    else:
                        rstd_from_ss(P, rs[n % 2], pss, 1.0 / 64, EPS)
                    o = ob[n % 3]
                    P.stt(o, r, nw[:, (0 if isq else 1):(1 if isq else 2)], rs[n % 2], ALU.mult, ALU.mult)
                    P.dma(C.qkT[m * 128:(m + 1) * 128, tsl], o)
                    n += 1
            for s in range(4):
                v_ = vb[s % 2]
                for half in range(2):
                    pp = C.PS[1 + ((s * 2 + half) % 4)]
                    for k in range(8):
                        P.mm(pp, S.hb[:, k, s * 128:(s + 1) * 128], wv[:, k, half * 512:(half + 1) * 512],
                             start=(k == 0), stop=(k == 7))
                    P.copy(v_[:, half * 512:(half + 1) * 512], pp, eng=('act' if half else 'dve'))
                P.dma(C.vtok[tt * 512 + s * 128:tt * 512 + (s + 1) * 128, :], v_)
        P.barrier()
        P.flush()


def stage_attn(P, C, L, l):
    nc = P.nc
    I = C.inp
    i = l // 2
    NT = L // 512
    NK = L // 128
    lambda_init = 0.8 - 0.6 * math.exp(-0.3 * l)
    with contextlib.ExitStack() as st:
        P.stack = st
        lq = P.sb([128, 4, 64])
        for j, nm in enumerate(('odd_lambda_q1', 'odd_lambda_k1', 'odd_lambda_q2', 'odd_lambda_k2')):
            a = I[nm][i].re("(o d) -> o d", o=1)
            P.dma(lq[:, j, :], V(a.res, a.ap.to_broadcast([128, 64])))
        pr = P.sb([128, 2, 64])
        P.tt(pr[:, 0, :], lq[:, 0, :], lq[:, 1, :], ALU.mult)
        P.tt(pr[:, 1, :], lq[:, 2, :], lq[:, 3, :], ALU.mult)
        sm = P.sb([128, 2])
        P.reduce(sm, pr, ALU.add)
        P.act(sm, sm, AF.Exp)
        nlam = P.sb([128, 1])
        P.tt(nlam, sm[:, 1:2], sm[:, 0:1], ALU.subtract)
        P.ts(nlam, nlam, -lambda_init, ALU.add)
        sw = P.sb([128, 1])
        with nc.allow_non_contiguous_dma(reason="tiny"):
            P.dma(sw, I['odd_subln_w'][i].re("(p o) -> p o", o=1))
        P.ts(sw, sw, 1.0 - lambda_init, ALU.mult)
        kT = [P.sb([128, L], BF16) for _ in range(2)]
        vt = [P.sb([128, NK, 128], BF16) for _ in range(2)]
        qt_ = [P.sb([128, 512], BF16) for _ in range(2)]
        E = [P.sb([128, 1024], BF16) for _ in range(4)]
        r1 = [P.sb([128, 512]) for _ in range(2)]
        r2 = [P.sb([128, 512]) for _ in range(2)]
        o1 = [P.sb([128, 512]) for _ in range(2)]
        sq = [P.sb([128, 512]) for _ in range(2)]
        ob = [P.sb([128, 512]) for _ in range(2)]
        ne = 0
        nq = 0
        for h in range(8):
            kk, vv = kT[h % 2], vt[h % 2]
            P.dma(kk, C.qkT[1024 + h * 128:1024 + (h + 1) * 128, :])
            P.dma(vv, C.vtok[:, h * 128:(h + 1) * 128].re("(n p) e -> p n e", p=128))
            for qt in range(NT):
                tsl = slice(qt * 512, (qt + 1) * 512)
                q = qt_[nq % 2]
                P.dma(q, C.qkT[h * 128:(h + 1) * 128, tsl])
                pn = [C.PS[0], C.PS[1]]
                pd = [C.PS[2], C.PS[3]]
                nkt = 4 * (qt + 1)
                LA = 1
                ebuf = {}

                def emit_s(kt):
                    nonlocal ne
                    half = ne % 2
                    psc = C.PSH[half].v
                    for t in range(2):
                        P.mm(psc[:, t * 512:(t + 1) * 512], kk[t * 64:(t + 1) * 64, kt * 128:(kt + 1) * 128],
                             q[t * 64:(t + 1) * 64, :])
                    e_ = E[ne % 4]
                    ne += 1
                    P.act(e_, psc, AF.Exp, bias=C.neg8[:, 0:1])
                    r = kt - 4 * qt
                    if r >= 0:
                        P.tt(e_.v.re("p (t q) -> p t q", t=2), e_.v.re("p (t q) -> p t q", t=2),
                             C.amask[:, r:r + 1, :].bc([128, 2, 512]), ALU.mult, eng='pool')
                    ebuf[kt] = e_

                def emit_md(kt):
                    e_ = ebuf.pop(kt)
                    for t in range(2):
                        P.mm(pn[t], vv[:, kt, :], e_[:, t * 512:(t + 1) * 512], start=(kt == 0), stop=(kt == nkt - 1))
                        P.mm(pd[t], C.onesb, e_[:, t * 512:(t + 1) * 512], start=(kt == 0), stop=(kt == nkt - 1))
                for n in range(nkt + LA):
                    if n < nkt:
                        emit_s(n)
                    if n - LA >= 0:
                        emit_md(n - LA)
                b = nq % 2
                nq += 1
                P.recip(r1[b], pd[0])
                P.recip(r2[b], pd[1])
                P.tt(o1[b], pn[0], r1[b], ALU.mult)
                P.tt(r2[b], pn[1], r2[b], ALU.mult)
                P.stt(o1[b], r2[b], nlam[:, 0:1], o1[b], ALU.mult, ALU.add)
                P.act(sq[b], o1[b], AF.Square)
                pss = C.PS[2]
                P.mm(pss, C.ones, sq[b])
                rstd_from_ss(P, r1[b], pss, 1.0 / 128, EPS)
                P.stt(ob[b], o1[b], sw[:, 0:1], r1[b], ALU.mult, ALU.mult)
                P.dma(C.oT[h * 128:(h + 1) * 128, tsl], ob[b])
        P.barrier()
        P.flush()


def make_consts():
    c = {}
    c['c_ident'] = np.eye(128, dtype=np.float32)
    am = np.zeros((128, 4, 512), np.float32)
    kk = np.arange(128)[:, None]
    qq = np.arange(512)[None, :]
    for r in range(4):
        am[:, r, :] = ((r * 128 + kk) // 64 <= qq // 64)
    c['c_amask'] = am
    gm = np.zeros((64, 4, 512), np.float32)
    p = np.arange(64)[:, None]
    f = np.arange(64)[None, :]
    for cidx in range(8):
        sl = slice(cidx * 64, (cidx + 1) * 64)
        gm[:, 0, sl] = -1.0 * (p > f)
        gm[:, 1, sl] = -1.0 * (p < f)
        gm[:, 2, sl] = (p <= f)
        gm[:, 3, sl] = (p == f)
    c['c_gmask'] = gm
    sel = np.zeros((8, 8, 128), np.float32)
    for e in range(8):
        sel[e, e, :] = 1.0
    c['c_sel'] = sel
    blk = np.zeros((128, 128), np.float32)
    blk[:64, :64] = 1
    blk[64:, 64:] = 1
    c['c_blk'] = blk
    cm = np.ones((4, 512), np.float32)
    cm[:, ::64] = 0
    c['c_cmask'] = cm
    return c


INPUT_NAMES = ['ada_w', 'ada_b', 'norm_mix_w', 'norm_ffn_w',
               'even_w_in', 'even_conv_w', 'even_a_log', 'even_dt_bias', 'even_gdn_norm_w',
               'even_lam_re', 'even_lam_im', 'even_log_step', 'even_b_re', 'even_b_im', 'even_c_re', 'even_c_im',
               'even_d_skip', 'even_glu_w', 'even_glu_b', 'even_w_out', 'even_ffn_w13', 'even_ffn_w2',
               'odd_w_qkv', 'odd_q_norm_w', 'odd_k_norm_w', 'odd_lambda_q1', 'odd_lambda_k1', 'odd_lambda_q2',
               'odd_lambda_k2', 'odd_subln_w', 'odd_w_out', 'odd_router_w', 'odd_expert_w13', 'odd_expert_w2']


def build(shapes, L, depth=DEPTH, dbg=False):
    nc = bass.Bass("TRN2", target_bir_lowering=False)
    C = Ctx()
    C.depth = depth
    C.inp = {}
    consts = make_consts()
    for nm, shp in shapes.items():
        C.inp[nm] = Res(nm, nc.dram_tensor(nm, list(shp), F32, kind="ExternalInput").ap())
    for nm, arr in consts.items():
        C.inp[nm] = Res(nm, nc.dram_tensor(nm, list(arr.shape), F32, kind="ExternalInput").ap())
    C.out = Res('out', nc.dram_tensor('out', [L, D], F32, kind="ExternalOutput").ap())
    kind = "ExternalOutput" if dbg else "Internal"

    def scr(nm, shape, dt=F32):
        return Res(nm, nc.dram_tensor(nm, list(shape), dt, kind=kind).ap())
    C.xA = scr('xA', [D, L])
    C.xB = scr('xB', [D, L])
    C.qkvT = scr('qkvT', [1536, L])
    C.zT = scr('zT', [512, L])
    C.uT = scr('uT', [512, L])
    C.betaT = scr('betaT', [4, L])
    C.gT = scr('gT', [4, L])
    C.yT = scr('yT', [D, L])
    C.xmid = scr('xmid', [D, L])
    C.qkT = scr('qkT', [2048, L], BF16)
    C.vtok = scr('vtok', [L, D], BF16)
    C.oT = scr('oT', [D, L])
    with contextlib.ExitStack() as st0:
        P = Prog(nc, st0)
        C.PS = [P.ps([128, 512]) for _ in range(4)]
        C.PSS = P.ps([128, 2048])
        C.PS += [Res('pss%d' % j, C.PSS.ap[:, j * 512:(j + 1) * 512]) for j in range(4)]
        C.PSH = [Res('psh%d' % j, C.PSS.ap[:, j * 1024:(j + 1) * 1024]) for j in range(2)]
        C.ident = P.sb([128, 128])
        C.ones = P.sb([128, 128])
        C.onesb = P.sb([128, 128], BF16)
        C.blk = P.sb([128, 128])
        C.amask = P.sb([128, 4, 512], BF16)
        C.sel = P.sb([8, 8, 128])
        C.halfpi = P.sb([128, 1])
        C.neg8 = P.sb([128, 1])
        C.mod = [P.sb([128, 48]) for _ in range(depth)]
        C.modA = [P.sb([128, 16]) for _ in range(depth)]
        P.dma(C.ident, C.inp['c_ident'])
        P.dma(C.blk, C.inp['c_blk'])
        P.dma(C.amask, C.inp['c_amask'], eng='pool')
        P.dma(C.sel, C.inp['c_sel'])
        P.memset(C.ones, 1.0)
        P.memset(C.onesb, 1.0)
        P.memset(C.halfpi, math.pi / 2)
        P.memset(C.neg8, -8.0)
        stage_prep(P, C, L)
        cur, nxt = C.xA, C.xB
        for l in range(depth):
            i = l // 2
            if l % 2 == 0:
                stage_even_proj(P, C, L, l, cur)
                stage_gdn(P, C, L, l)
                stage_s5(P, C, L, l)
                stage_ffn(P, C, L, l, cur, nxt, C.yT, C.inp['even_w_out'][i], moe=False)
            else:
                stage_attn_proj(P, C, L, l, cur)
                stage_attn(P, C, L, l)
                stage_ffn(P, C, L, l, cur, nxt, C.oT, C.inp['odd_w_out'][i], moe=True)
            cur, nxt = nxt, cur
        stage_out(P, C, L, cur)
        P.stack = st0
        C.nins = P.nins
    return nc, consts, C


def kernel(**inputs):
    x = np.asarray(inputs['x'], dtype=np.float32)
    B, L, _ = x.shape
    shapes = {'x': (L, D), 'c': (D,)}
    for nm in INPUT_NAMES:
        shapes[nm] = tuple(np.asarray(inputs[nm]).shape)
    nc, consts, C = build(shapes, L)
    shared = {nm: np.ascontiguousarray(np.asarray(inputs[nm], dtype=np.float32)) for nm in INPUT_NAMES}
    zeros = {nm: np.zeros_like(v) for nm, v in shared.items()}
    zc = {nm: np.zeros_like(v) for nm, v in consts.items()}
    real = [0, 1, 4, 5][:B]
    in_maps = []
    for core in range(8):
        if core in real:
            b = real.index(core)
            m = dict(shared)
            m.update(consts)
            m['x'] = np.ascontiguousarray(x[b])
            m['c'] = np.ascontiguousarray(np.asarray(inputs['c'], dtype=np.float32)[b])
        else:
            m = dict(zeros)
            m.update(zc)
            m['x'] = np.zeros((L, D), np.float32)
            m['c'] = np.zeros((D,), np.float32)
        in_maps.append(m)
    res = run_bass_kernel_spmd(nc, in_maps, core_ids=list(range(8)))
    out = np.stack([res.results[real[b]]['out'] for b in range(B)], axis=0)
    return out.astype(np.float32)
```

```python
import contextlib
import math
import numpy as np
import concourse.bass as bass
import concourse.mybir as mybir
from concourse.bass_utils import run_bass_kernel_spmd

F32 = mybir.dt.float32
BF16 = mybir.dt.bfloat16
AF = mybir.ActivationFunctionType
ALU = mybir.AluOpType
AX = mybir.AxisListType

D = 1024
DEPTH = 4
EPS = 1e-6
DFF = 2816
NEXP = 8
ENGS = ['pe', 'act', 'dve', 'pool', 'sp']


class V:
    __slots__ = ('res', 'ap')

    def __init__(self, res, ap):
        self.res = res
        self.ap = ap

    def __getitem__(self, idx):
        return V(self.res, self.ap[idx])

    def bc(self, shape):
        return V(self.res, self.ap.to_broadcast(list(shape)))

    def re(self, s, **kw):
        return V(self.res, self.ap.rearrange(s, **kw))


class Res:
    __slots__ = ('name', 'w', 'r', 'ap')

    def __init__(self, name, ap=None):
        self.name = name
        self.w = {}
        self.r = {}
        self.ap = ap

    def __getitem__(self, idx):
        return V(self, self.ap[idx])

    @property
    def v(self):
        return V(self, self.ap)


def _rv(x):
    return x.v if isinstance(x, Res) else x


class Prog:
    def __init__(self, nc, stack, n_dma_sems=32):
        self.nc = nc
        self.stack = stack
        self.streams = {e: [] for e in ENGS}
        self.cnt = {e: 0 for e in ENGS}
        self.semh = {}
        for e in ['pe', 'act', 'dve', 'pool']:
            self.semh['c_' + e] = stack.enter_context(nc.semaphore('c_' + e))
        self.ndma = n_dma_sems
        self.dma_tot = [0] * n_dma_sems
        self.dma_next = {'sp': 0, 'pool': 0, 'act': 0}
        self.dma_range = {'sp': (0, n_dma_sems // 2), 'pool': (n_dma_sems // 2, n_dma_sems), 'act': (0, n_dma_sems // 2)}
        for k in range(n_dma_sems):
            self.semh['d%d' % k] = stack.enter_context(nc.semaphore('d%d' % k))
        self.known = {e: {} for e in ENGS}
        self.nt = 0
        self.nins = 0

    def sb(self, shape, dtype=F32, name=None):
        self.nt += 1
        name = name or ('t%d' % self.nt)
        t = self.stack.enter_context(self.nc.sbuf_tensor(name, list(shape), dtype))
        return Res(name, t[:])

    def ps(self, shape, dtype=F32, name=None):
        self.nt += 1
        name = name or ('p%d' % self.nt)
        t = self.stack.enter_context(self.nc.psum_tensor(name, list(shape), dtype))
        return Res(name, t[:])

    def dram(self, name, shape, dtype, kind="Internal"):
        t = self.nc.dram_tensor(name, list(shape), dtype, kind=kind)
        return Res(name, t.ap())

    def _deps(self, eng, reads, writes):
        deps = {}

        def add(k, v):
            if deps.get(k, -1) < v:
                deps[k] = v
        for r in reads:
            for k, v in r.w.items():
                add(k, v)
        for w in writes:
            for k, v in w.w.items():
                add(k, v)
            for k, v in w.r.items():
                add(k, v)
        out = []
        kn = self.known[eng]
        for k, v in deps.items():
            if eng == 'pe' and k == 'c_pe':
                continue
            if kn.get(k, -1) < v:
                kn[k] = v
                out.append((k, v))
        return out

    def _record(self, ev, reads, writes, merge=False):
        k, v = ev
        for w in writes:
            if merge:
                w.w[k] = v
            else:
                w.w = {k: v}
            w.r = {}
        for r in reads:
            if r.r.get(k, -1) < v:
                r.r[k] = v

    def op(self, eng, fn, reads=(), writes=(), inc=True):
        reads = [x for x in reads if x is not None]
        waits = self._deps(eng, reads, writes)
        key = 'c_' + eng
        if inc:
            self.cnt[eng] += 1
            ev = (key, self.cnt[eng])
        else:
            ev = (key, self.cnt[eng] + 1)
        self.streams[eng].append((waits, fn, key if inc else None, 1))
        self._record(ev, reads, writes)
        self.nins += 1

    def dma(self, out, in_, eng='sp'):
        out = _rv(out)
        in_ = _rv(in_)
        lo, hi = self.dma_range[eng]
        k = lo + self.dma_next[eng]
        self.dma_next[eng] = (self.dma_next[eng] + 1) % (hi - lo)
        key = 'd%d' % k
        waits = self._deps(eng, [in_.res], [out.res])
        kn = self.known[eng]
        if kn.get(key, -1) < self.dma_tot[k]:
            kn[key] = self.dma_tot[k]
            waits.append((key, self.dma_tot[k]))
        self.dma_tot[k] += 16
        ev = (key, self.dma_tot[k])
        oa, ia = out.ap, in_.ap

        def fn(e):
            return e.dma_start(out=oa, in_=ia)
        self.streams[eng].append((waits, fn, key, 16))
        self._record(ev, [in_.res], [out.res], merge=True)
        self.nins += 1

    def collective(self, kind, out, in_, groups, eng='pool'):
        out = _rv(out)
        in_ = _rv(in_)
        lo, hi = self.dma_range[eng]
        k = lo + self.dma_next[eng]
        self.dma_next[eng] = (self.dma_next[eng] + 1) % (hi - lo)
        key = 'd%d' % k
        waits = self._deps(eng, [in_.res], [out.res])
        kn = self.known[eng]
        if kn.get(key, -1) < self.dma_tot[k]:
            kn[key] = self.dma_tot[k]
            waits.append((key, self.dma_tot[k]))
        self.dma_tot[k] += 16
        ev = (key, self.dma_tot[k])
        oa, ia = out.ap, in_.ap

        def fn(e):
            return e.collective_compute(kind, ALU.bypass, groups, [ia], [oa])
        self.streams[eng].append((waits, fn, key, 16))
        self._record(ev, [in_.res], [out.res], merge=True)
        self.nins += 1

    def barrier(self):
        allw = [('c_' + e, self.cnt[e]) for e in ['pe', 'act', 'dve', 'pool']]
        allw += [('d%d' % k, self.dma_tot[k]) for k in range(self.ndma)]
        for e in ENGS:
            kn = self.known[e]
            waits = []
            for k, v in allw:
                if e == 'pe' and k == 'c_pe':
                    continue
                if kn.get(k, -1) < v:
                    kn[k] = v
                    waits.append((k, v))
            self.streams[e].append((waits, None, None, 0))

    def flush(self):
        nc = self.nc
        engobj = {'pe': 'tensor', 'act': 'scalar', 'dve': 'vector', 'pool': 'gpsimd', 'sp': 'sync'}
        semh = self.semh
        with nc.allow_non_contiguous_dma(reason="small strided param loads"), nc.Block() as block:
            for e in ENGS:
                stream = self.streams[e]

                def body(eng, stream=stream):
                    for waits, fn, key, n in stream:
                        for k, v in waits:
                            eng.wait_ge(semh[k], v)
                        if fn is not None:
                            ins = fn(eng)
                            if key is not None:
                                ins.then_inc(semh[key], n)
                getattr(block, engobj[e])(body)
        self.streams = {e: [] for e in ENGS}

    def mm(self, out, lhsT, rhs, start=True, stop=True):
        out, lhsT, rhs = _rv(out), _rv(lhsT), _rv(rhs)
        o, l, r = out.ap, lhsT.ap, rhs.ap
        self.op('pe', lambda e: e.matmul(o, l, r, start=start, stop=stop),
                [lhsT.res, rhs.res], [out.res], inc=stop)

    def tr(self, out, in_, ident):
        out, in_, ident = _rv(out), _rv(in_), _rv(ident)
        o, i, d = out.ap, in_.ap, ident.ap
        self.op('pe', lambda e: e.transpose(o, i, d), [in_.res, ident.res], [out.res])

    def act(self, out, in_, func, bias=None, scale=None, eng='act'):
        out, in_ = _rv(out), _rv(in_)
        reads = [in_.res]
        kw = {}
        if bias is not None:
            if isinstance(bias, (V, Res)):
                bias = _rv(bias)
                reads.append(bias.res)
                kw['bias'] = bias.ap
            else:
                kw['bias'] = float(bias)
        if scale is not None:
            if isinstance(scale, (V, Res)):
                scale = _rv(scale)
                reads.append(scale.res)
                kw['scale'] = scale.ap
            else:
                kw['scale'] = float(scale)
        o, i = out.ap, in_.ap
        self.op(eng, lambda e: e.activation(o, i, func, **kw), reads, [out.res])

    def tt(self, out, a, b, op, eng='dve'):
        out, a, b = _rv(out), _rv(a), _rv(b)
        o, x, y = out.ap, a.ap, b.ap
        self.op(eng, lambda e: e.tensor_tensor(o, x, y, op), [a.res, b.res], [out.res])

    def ts(self, out, a, s1, op0, s2=None, op1=None, eng='dve'):
        out, a = _rv(out), _rv(a)
        reads = [a.res]

        def cv(s):
            if isinstance(s, (V, Res)):
                s = _rv(s)
                reads.append(s.res)
                return s.ap
            return None if s is None else float(s)
        c1, c2 = cv(s1), cv(s2)
        o, x = out.ap, a.ap
        if op1 is None:
            self.op(eng, lambda e: e.tensor_single_scalar(o, x, c1, op0), reads, [out.res])
        else:
            self.op(eng, lambda e: e.tensor_scalar(o, x, c1, c2, op0, op1), reads, [out.res])

    def stt(self, out, a, s, b, op0, op1):
        out, a, b = _rv(out), _rv(a), _rv(b)
        reads = [a.res, b.res]
        if isinstance(s, (V, Res)):
            s = _rv(s)
            reads.append(s.res)
            c = s.ap
        else:
            c = float(s)
        o, x, y = out.ap, a.ap, b.ap
        self.op('dve', lambda e: e.scalar_tensor_tensor(o, x, c, y, op0, op1), reads, [out.res])

    def copy(self, out, in_, eng='dve'):
        out, in_ = _rv(out), _rv(in_)
        o, i = out.ap, in_.ap
        if eng == 'act':
            self.op('act', lambda e: e.activation(o, i, AF.Copy), [in_.res], [out.res])
        else:
            self.op(eng, lambda e: e.tensor_copy(o, i), [in_.res], [out.res])

    def recip(self, out, in_):
        out, in_ = _rv(out), _rv(in_)
        o, i = out.ap, in_.ap
        self.op('dve', lambda e: e.reciprocal(o, i), [in_.res], [out.res])

    def memset(self, out, val, eng='dve'):
        out = _rv(out)
        o = out.ap
        self.op(eng, lambda e: e.memset(o, float(val)), [], [out.res])

    def scan(self, out, d0, d1, init, op0=ALU.mult, op1=ALU.add):
        out, d0, d1 = _rv(out), _rv(d0), _rv(d1)
        reads = [d0.res, d1.res]
        if isinstance(init, (V, Res)):
            init = _rv(init)
            reads.append(init.res)
            c = init.ap
        else:
            c = float(init)
        o, x, y = out.ap, d0.ap, d1.ap
        self.op('dve', lambda e: e.tensor_tensor_scan(o, x, y, c, op0, op1), reads, [out.res])

    def reduce(self, out, in_, op, axis=AX.X):
        out, in_ = _rv(out), _rv(in_)
        o, i = out.ap, in_.ap
        self.op('dve', lambda e: e.tensor_reduce(o, i, axis, op), [in_.res], [out.res])


class Ctx:
    pass


def rstd_from_ss(P, out_sb, ss_ps, scale, bias):
    P.act(out_sb, ss_ps, AF.Ln, bias=bias, scale=scale)
    P.act(out_sb, out_sb, AF.Exp, scale=-0.5)


def stage_prep(P, C, L):
    nc = P.nc
    I = C.inp
    with contextlib.ExitStack() as st:
        P.stack = st
        cT = P.sb([128, 8])
        with nc.allow_non_contiguous_dma(reason="tiny"):
            P.dma(cT, I['c'].v.re("(k p) -> p k", p=128))
        cond = P.sb([128, 8])
        P.act(cond, cT, AF.Silu)
        wbuf = [P.sb([128, 8, 768]) for _ in range(2)]
        pm = C.PS[0]
        for l in range(C.depth):
            bt = P.sb([128, 48])
            with nc.allow_non_contiguous_dma(reason="tiny"):
                P.dma(bt, I['ada_b'][l].re("(j p) -> p j", p=128))
            for cb in range(8):
                wb = wbuf[cb % 2]
                P.dma(wb, I['ada_w'][l][:, cb * 768:(cb + 1) * 768].re("(k p) m -> p k m", p=128))
                for j in range(6):
                    col = cb * 6 + j
                    for k in range(8):
                        P.mm(pm[:, col:col + 1], wb[:, k, j * 128:(j + 1) * 128], cond[:, k:k + 1],
                             start=(k == 0), stop=(k == 7))
            mod = C.mod[l]
            P.tt(mod, pm[:, 0:48], bt, ALU.add)
            nw = P.sb([128, 16])
            with nc.allow_non_contiguous_dma(reason="tiny"):
                P.dma(nw[:, 0:8], I['norm_mix_w'][l].re("(k p) -> p k", p=128))
                P.dma(nw[:, 8:16], I['norm_ffn_w'][l].re("(k p) -> p k", p=128))
            A = C.modA[l]
            P.stt(A[:, 0:8], mod[:, 8:16], 1.0, nw[:, 0:8], ALU.add, ALU.mult)
            P.stt(A[:, 8:16], mod[:, 32:40], 1.0, nw[:, 8:16], ALU.add, ALU.mult)
        xin = [P.sb([128, 1024]) for _ in range(2)]
        xo = [P.sb([128, 8, 512]) for _ in range(2)]
        for tt in range(L // 512):
            o = xo[tt % 2]
            for s in range(4):
                xi = xin[s % 2]
                t0 = tt * 512 + s * 128
                P.dma(xi, I['x'][t0:t0 + 128, :])
                for k in range(8):
                    pt = C.PS[1 + (k % 4)]
                    P.tr(pt[:, 0:128], xi[:, k * 128:(k + 1) * 128], C.ident)
                    P.copy(o[:, k, s * 128:(s + 1) * 128], pt[:, 0:128], eng=('act' if k % 2 else 'dve'))
            P.dma(C.xA.v.re("(k p) t -> p k t", p=128)[:, :, tt * 512:(tt + 1) * 512], o)
        P.barrier()
        P.flush()


def stage_out(P, C, L, xsrc):
    I = C.inp
    with contextlib.ExitStack() as st:
        P.stack = st
        xi = [P.sb([128, 8, 512]) for _ in range(2)]
        xo = [P.sb([128, 1024]) for _ in range(2)]
        n = 0
        for tt in range(L // 512):
            t = xi[tt % 2]
            P.dma(t, xsrc.v.re("(k p) t -> p k t", p=128)[:, :, tt * 512:(tt + 1) * 512])
            for s in range(4):
                o = xo[s % 2]
                for k in range(8):
                    pt = C.PS[1 + (k % 4)]
                    P.tr(pt[:, 0:128], t[:, k, s * 128:(s + 1) * 128], C.ident)
                    P.copy(o[:, k * 128:(k + 1) * 128], pt[:, 0:128], eng=('act' if k % 2 else 'dve'))
                t0 = tt * 512 + s * 128
                P.dma(C.out[t0:t0 + 128, :], o)
        P.barrier()
        P.flush()


def load_norm_h(P, C, S, xt, l, which, want32=False):
    A = C.modA[l]
    mod = C.mod[l]
    aoff = 0 if which == 1 else 8
    shoff = 0 if which == 1 else 24
    sq = S.sq
    P.act(sq, xt, AF.Square)
    ss = C.PS[0]
    for k in range(8):
        P.mm(ss, C.ones, sq[:, k, :], start=(k == 0), stop=(k == 7))
    rstd = S.rstd
    rstd_from_ss(P, rstd, ss, 1.0 / D, EPS)
    for k in range(8):
        tmp = S.tmp[k % 2]
        P.stt(tmp, xt[:, k, :], A[:, aoff + k:aoff + k + 1], rstd, ALU.mult, ALU.mult)
        if want32:
            P.act(S.h32[:, k, :], tmp, AF.Identity, bias=mod[:, shoff + k:shoff + k + 1])
            P.act(S.hb[:, k, :], tmp, AF.Identity, bias=mod[:, shoff + k:shoff + k + 1])
        else:
            P.act(S.hb[:, k, :], tmp, AF.Identity, bias=mod[:, shoff + k:shoff + k + 1])


def wslab_iter(P, C, S, Wv, ncols, KT, slab=512):
    c0 = 0
    i = 0
    while c0 < ncols:
        w = min(slab, ncols - c0)
        buf = S.wb[S.wbi % len(S.wb)]
        S.wbi += 1
        P.dma(buf[:, 0:KT, 0:w], Wv[:, c0:c0 + w].re("(k p) m -> p k m", p=128), eng='pool')
        yield buf, c0, w
        c0 += w
        i += 1


def stage_even_proj(P, C, L, l, xsrc):
    nc = P.nc
    I = C.inp
    i = l // 2
    Win = I['even_w_in'][i]
    with contextlib.ExitStack() as st:
        P.stack = st
        S = Ctx()
        S.sq = P.sb([128, 8, 512])
        S.rstd = P.sb([128, 512])
        S.tmp = [P.sb([128, 512]) for _ in range(2)]
        S.hb = P.sb([128, 8, 512], BF16)
        S.wb = [P.sb([128, 8, 512], BF16) for _ in range(3)]
        S.wbi = 0
        C.cmask = P.sb([4, 512])
        P.dma(C.cmask, C.inp['c_cmask'])
        xts = [P.sb([128, 8, 512]) for _ in range(2)]
        pre = [P.sb([128, 515]) for _ in range(12)]
        for m in range(12):
            P.memset(pre[m][:, 0:3], 0.0)
        cw = P.sb([128, 12, 4])
        with nc.allow_non_contiguous_dma(reason="tiny"):
            for j in range(4):
                P.dma(cw[:, :, j], I['even_conv_w'][i][j].re("(t p) -> p t", p=128))
        wba = P.sb([128, 8, 8])
        with nc.allow_non_contiguous_dma(reason="small"):
            P.dma(wba, Win[:, 2048:2056].re("(k p) m -> p k m", p=128))
        hc = P.sb([4, 2])
        with nc.allow_non_contiguous_dma(reason="tiny"):
            P.dma(hc[:, 0:1], I['even_a_log'][i].re("(h o) -> h o", o=1))
            P.dma(hc[:, 1:2], I['even_dt_bias'][i].re("(h o) -> h o", o=1))
        nA = P.sb([4, 1])
        P.act(nA, hc[:, 0:1], AF.Exp)
        P.ts(nA, nA, -1.0, ALU.mult)
        h32 = P.sb([128, 8, 512])
        S.h32 = h32
        acc = [P.sb([128, 512]) for _ in range(2)]
        ob = [P.sb([128, 512]) for _ in range(3)]
        sm = [P.sb([4, 512]) for _ in range(6)]
        NT = L // 512
        xv = xsrc.v.re("(k p) t -> p k t", p=128)
        P.dma(xts[0], xv[:, :, 0:512])
        nob = 0
        for tt in range(NT):
            xt = xts[tt % 2]
            if tt + 1 < NT:
                P.dma(xts[(tt + 1) % 2], xv[:, :, (tt + 1) * 512:(tt + 2) * 512])
            load_norm_h(P, C, S, xt, l, 1, want32=True)
            tsl = slice(tt * 512, (tt + 1) * 512)
            pb, pa = C.PS[1], C.PS[2]
            for k in range(8):
                P.mm(pb[0:4, :], wba[:, k, 0:4], h32[:, k, :], start=(k == 0), stop=(k == 7))
            for k in range(8):
                P.mm(pa[0:4, :], wba[:, k, 4:8], h32[:, k, :], start=(k == 0), stop=(k == 7))
            beta = sm[0]
            P.act(beta[:], pb[0:4, :], AF.Exp, scale=-1.0)
            P.ts(beta, beta, 1.0, ALU.add)
            P.recip(beta, beta)
            P.dma(C.betaT[:, tsl], beta)
            xa = sm[1]
            P.act(xa[:], pa[0:4, :], AF.Identity, bias=hc[:, 1:2])
            ax = sm[2]
            P.stt(ax, xa, -1.0, xa, ALU.mult, ALU.max)
            P.act(ax, ax, AF.Exp, scale=-1.0)
            P.act(ax, ax, AF.Ln, bias=1.0)
            sp = sm[3]
            P.stt(sp, xa, 0.0, ax, ALU.max, ALU.add)
            g = sm[4]
            P.ts(g, sp, nA[:, 0:1], ALU.mult)
            gc = sm[5]
            P.scan(gc, C.cmask, g, 0.0)
            P.dma(C.gT[:, tsl], gc)
            mt = 0
            for wsl, c0, w in wslab_iter(P, C, S, Win, 2048, 8):
                for j in range(w // 128):
                    m = (c0 // 128) + j
                    pp = C.PS[3 + (m % 4)]
                    for k in range(8):
                        P.mm(pp, wsl[:, k, j * 128:(j + 1) * 128], S.hb[:, k, :], start=(k == 0), stop=(k == 7))
                    if m < 12:
                        pr = pre[m]
                        P.copy(pr[:, 3:515], pp, eng='act')
                        a = acc[m % 2]
                        P.ts(a, pr[:, 0:512], cw[:, m, 0:1], ALU.mult)
                        for jj in range(1, 4):
                            P.stt(a, pr[:, jj:jj + 512], cw[:, m, jj:jj + 1], a, ALU.mult, ALU.add)
                        P.copy(pr[:, 0:3], pr[:, 512:515], eng='pool')
                        o = ob[nob % 3]
                        nob += 1
                        P.act(o, a, AF.Silu)
                        if m < 8:
                            sq = S.tmp[m % 2]
                            P.act(sq, o, AF.Square)
                            ss = C.PS[7]
                            P.mm(ss, C.ones, sq)
                            rs = S.rstd
                            if m < 4:
                                rstd_from_ss(P, rs, ss, 128.0, 128.0 * EPS)
                            else:
                                rstd_from_ss(P, rs, ss, 1.0, EPS)
                            P.tt(o, o, rs, ALU.mult)
                        P.dma(C.qkvT[m * 128:(m + 1) * 128, tsl], o)
                    else:
                        o = ob[nob % 3]
                        nob += 1
                        P.act(o, pp, AF.Silu)
                        P.dma(C.zT[(m - 12) * 128:(m - 11) * 128, tsl], o)
            for wsl, c0, w in wslab_iter(P, C, S, Win[:, 2056:2568], 512, 8):
                for j in range(4):
                    pp = C.PS[3 + (j % 4)]
                    for k in range(8):
                        P.mm(pp, wsl[:, k, j * 128:(j + 1) * 128], S.hb[:, k, :], start=(k == 0), stop=(k == 7))
                    o = ob[nob % 3]
                    nob += 1
                    P.copy(o, pp, eng='act')
                    P.dma(C.uT[j * 128:(j + 1) * 128, tsl], o)
        P.barrier()
        P.flush()


def stage_gdn(P, C, L, l):
    nc = P.nc
    I = C.inp
    i = l // 2
    NT = L // 512
    with contextlib.ExitStack() as st:
        P.stack = st
        C.gmask = P.sb([64, 4, 512])
        P.dma(C.gmask, C.inp['c_gmask'])
        Sst = [P.sb([128, 128]) for _ in range(4)]
        for h in range(4):
            P.memset(Sst[h], 0.0)
        gw = P.sb([128, 1])
        with nc.allow_non_contiguous_dma(reason="tiny"):
            P.dma(gw, I['even_gdn_norm_w'][i].re("(p o) -> p o", o=1))
        mk = lambda n, shape, dt=F32: [P.sb(shape, dt) for _ in range(n)]
        qT, kT, vT = mk(2, [128, 512]), mk(2, [128, 512]), mk(2, [128, 512])
        rows = mk(2, [1, 2, 512])
        r_eg, r_ekl, r_ng = mk(2, [1, 512]), mk(2, [1, 512]), mk(2, [1, 512])
        bcs = mk(2, [128, 3, 512])
        kb, kbe, vb, qe, kel = (mk(2, [128, 512]) for _ in range(5))
        tokm = mk(2, [64, 3, 8, 128])
        EL, EU, t1 = mk(2, [64, 512]), mk(2, [64, 512]), mk(2, [64, 512])
        Am, ATm, PTm, Rm = mk(2, [64, 512]), mk(2, [64, 512]), mk(2, [64, 512]), mk(2, [64, 512])
        WTn = mk(2, [128, 512])
        vnew = [mk(2, [64, 128]) for _ in range(2)]
        zt = mk(2, [128, 512])
        osb = mk(2, [128, 512])
        sq = mk(2, [128, 512])
        rs = mk(2, [128, 512])
        def chain(tt, h, b, BK):
            tsl = slice(tt * 512, (tt + 1) * 512)
            P.dma(qT[b], C.qkvT[h * 128:(h + 1) * 128, tsl])
            P.dma(kT[b], C.qkvT[512 + h * 128:512 + (h + 1) * 128, tsl])
            P.dma(vT[b], C.qkvT[1024 + h * 128:1024 + (h + 1) * 128, tsl])
            P.dma(rows[b][:, 0, :], C.betaT[h:h + 1, tsl])
            P.dma(rows[b][:, 1, :], C.gT[h:h + 1, tsl])
            P.dma(zt[b], C.zT[h * 128:(h + 1) * 128, tsl])
            gc = rows[b][:, 1, :]
            P.act(r_eg[b], gc, AF.Exp)
            g3 = rows[b][:, 1, :].re("o (c j) -> o c j", j=64)
            P.tt(r_ekl[b].v.re("o (c j) -> o c j", j=64), g3[:, :, 63:64].bc([1, 8, 64]), g3, ALU.subtract)
            P.act(r_ekl[b], r_ekl[b], AF.Exp)
            P.ts(r_ng[b], gc, -1.0, ALU.mult)
            yield
            pbc = [BK[0], BK[1], BK[2]]
            P.mm(pbc[0], C.ones[0:1, :], rows[b][:, 0, :])
            P.mm(pbc[1], C.ones[0:1, :], r_eg[b])
            P.mm(pbc[2], C.ones[0:1, :], r_ekl[b])
            for j in range(3):
                P.copy(bcs[b][:, j, :], pbc[j], eng='act')
            P.tt(kb[b], kT[b], bcs[b][:, 0, :], ALU.mult)
            P.tt(kbe[b], kb[b], bcs[b][:, 1, :], ALU.mult, eng='pool')
            P.tt(vb[b], vT[b], bcs[b][:, 0, :], ALU.mult)
            P.tt(qe[b], qT[b], bcs[b][:, 1, :], ALU.mult, eng='pool')
            P.tt(kel[b], kT[b], bcs[b][:, 2, :], ALU.mult)
            yield
            for c in range(8):
                csl = slice(c * 64, (c + 1) * 64)
                for j, src in enumerate((vb[b], kbe[b], kel[b])):
                    pt = BK[3 - ((c * 3 + j) % 2)]
                    P.tr(pt[0:64, 0:128], src[:, csl], C.ident)
                    P.copy(tokm[b][:, j, c, :], pt[0:64, 0:128], eng=('act' if j != 1 else 'dve'))
                yield
            pg = BK[2]
            for c in range(8):
                csl = slice(c * 64, (c + 1) * 64)
                P.mm(pg[0:64, csl], rows[b][:, 1, csl], C.ones[0:1, 0:64], start=True, stop=False)
                P.mm(pg[0:64, csl], C.ones[0:1, 0:64], r_ng[b][:, csl], start=False, stop=True)
            P.ts(t1[b], pg[0:64, :], 0.0, ALU.min)
            P.act(EL[b], t1[b], AF.Exp)
            P.ts(t1[b], pg[0:64, :], 0.0, ALU.max)
            P.act(EU[b], t1[b], AF.Exp, scale=-1.0)
            yield
            pA, pAT, pPT = BK[0], BK[1], BK[2]
            for c in range(8):
                csl = slice(c * 64, (c + 1) * 64)
                P.mm(pA[0:64, csl], kb[b][:, csl], kT[b][:, csl])
                P.mm(pAT[0:64, csl], kT[b][:, csl], kb[b][:, csl])
                P.mm(pPT[0:64, csl], kT[b][:, csl], qT[b][:, csl])
            P.tt(t1[b], EL[b], C.gmask[:, 0, :], ALU.mult, eng='pool')
            P.tt(Am[b], pA[0:64, :], t1[b], ALU.mult)
            P.tt(EL[b], EU[b], C.gmask[:, 1, :], ALU.mult, eng='pool')
            P.tt(ATm[b], pAT[0:64, :], EL[b], ALU.mult)
            P.tt(EU[b], EU[b], C.gmask[:, 2, :], ALU.mult, eng='pool')
            P.tt(PTm[b], pPT[0:64, :], EU[b], ALU.mult)
            yield
            P.tt(Rm[b], ATm[b], C.gmask[:, 3, :], ALU.add)
            X, XT = Am[b], ATm[b]
            X2, XT2 = t1[b], EL[b]
            for step in range(5):
                p1, p2, p3 = BK[0], BK[1], BK[2]
                for c in range(8):
                    csl = slice(c * 64, (c + 1) * 64)
                    P.mm(p1[0:64, csl], XT[:, csl], X[:, csl])
                    P.mm(p2[0:64, csl], X[:, csl], XT[:, csl])
                P.copy(X2, p1[0:64, :], eng='act')
                P.copy(XT2, p2[0:64, :], eng='dve')
                yield
                X, X2 = X2, X
                XT, XT2 = XT2, XT
                for c in range(8):
                    csl = slice(c * 64, (c + 1) * 64)
                    P.mm(p3[0:64, csl], X[:, csl], Rm[b][:, csl])
                P.tt(Rm[b], Rm[b], p3[0:64, :], ALU.add)
                yield
            pW = BK[0]
            for c in range(8):
                csl = slice(c * 64, (c + 1) * 64)
                P.mm(pW[:, csl], tokm[b][:, 1, c, :], Rm[b][:, csl])
            P.act(WTn[b], pW, AF.Copy, scale=-1.0)
            yield
            po = BK[3]
            S = Sst[h]
            for c in range(8):
                csl = slice(c * 64, (c + 1) * 64)
                pv = BK[1]
                P.mm(pv[0:64, 0:128], Rm[b][:, csl], tokm[b][:, 0, c, :], start=True, stop=False)
                P.mm(pv[0:64, 0:128], WTn[b][:, csl], S, start=False, stop=True)
                vn = vnew[b][c % 2]
                P.copy(vn, pv[0:64, 0:128], eng='act')
                P.mm(po[:, csl], S, qe[b][:, csl], start=True, stop=False)
                P.mm(po[:, csl], vn, PTm[b][:, csl], start=False, stop=True)
                ps_ = BK[2]
                P.mm(ps_[:, 0:128], tokm[b][:, 2, c, :], vn)
                P.stt(S, S, bcs[b][:, 1, c * 64 + 63:c * 64 + 64], ps_[:, 0:128], ALU.mult, ALU.add)
                yield
            P.copy(osb[b], po, eng='act')
            P.act(sq[b], osb[b], AF.Square)
            pss = BK[0]
            P.mm(pss, C.ones, sq[b])
            rstd_from_ss(P, rs[b], pss, 1.0 / 128, EPS)
            P.stt(osb[b], osb[b], gw[:, 0:1], rs[b], ALU.mult, ALU.mult)
            P.tt(osb[b], osb[b], zt[b], ALU.mult)
            P.dma(C.yT[h * 128:(h + 1) * 128, tsl], osb[b])
        for tt in range(NT):
            for hp in range(2):
                gens = [chain(tt, 2 * hp, 0, C.PS[0:4]), chain(tt, 2 * hp + 1, 1, C.PS[4:8])]
                while gens:
                    for g_ in list(gens):
                        try:
                            next(g_)
                        except StopIteration:
                            gens.remove(g_)
        P.barrier()
        P.flush()


def stage_s5(P, C, L, l):
    nc = P.nc
    I = C.inp
    i = l // 2
    NT = L // 512
    TWO_PI = 2.0 * math.pi
    with contextlib.ExitStack() as st:
        P.stack = st
        lr = P.sb([128, 16])
        li = P.sb([128, 16])
        ls = P.sb([128, 16])
        with nc.allow_non_contiguous_dma(reason="small"):
            P.dma(lr, I['even_lam_re'][i].re("(j g) n -> (g n) j", g=2))
            P.dma(li, I['even_lam_im'][i].re("(j g) n -> (g n) j", g=2))
            lsv = I['even_log_step'][i].re("(j g) -> g j", g=2)
            for g2 in range(2):
                P.dma(ls[g2 * 64:(g2 + 1) * 64, :], lsv[g2:g2 + 1, :].bc([64, 16]))
        dt = P.sb([128, 16])
        P.act(dt, ls, AF.Exp)
        P.ts(lr, lr, -1e-4, ALU.min)
        rho = P.sb([128, 16])
        P.tt(rho, lr, dt, ALU.mult)
        P.act(rho, rho, AF.Exp)
        th = P.sb([128, 16])
        P.tt(th, li, dt, ALU.mult)
        tmpa = P.sb([128, 16])
        sn = P.sb([128, 16])
        cs = P.sb([128, 16])
        P.act(sn, th, AF.Sin, scale=1.0 / 16)
        P.act(cs, th, AF.Sin, scale=1.0 / 16, bias=C.halfpi[:, 0:1])
        t_c2, t_s2 = P.sb([128, 16]), P.sb([128, 16])
        for _ in range(4):
            P.tt(t_c2, cs, cs, ALU.mult)
            P.tt(t_s2, sn, sn, ALU.mult)
            P.tt(sn, cs, sn, ALU.mult)
            P.ts(sn, sn, 2.0, ALU.mult)
            P.tt(cs, t_c2, t_s2, ALU.subtract)
        ar, ai = P.sb([128, 16]), P.sb([128, 16])
        P.tt(ar, rho, cs, ALU.mult)
        P.tt(ai, rho, sn, ALU.mult)
        nr = P.sb([128, 16])
        P.ts(nr, ar, -1.0, ALU.add)
        den = P.sb([128, 16])
        t2 = P.sb([128, 16])
        P.tt(den, lr, lr, ALU.mult)
        P.tt(t2, li, li, ALU.mult)
        P.tt(den, den, t2, ALU.add)
        P.recip(den, den)
        cr, ci = P.sb([128, 16]), P.sb([128, 16])
        P.tt(cr, nr, lr, ALU.mult)
        P.tt(t2, ai, li, ALU.mult)
        P.tt(cr, cr, t2, ALU.add)
        P.tt(cr, cr, den, ALU.mult)
        P.tt(ci, ai, lr, ALU.mult)
        P.tt(t2, nr, li, ALU.mult)
        P.tt(ci, ci, t2, ALU.subtract)
        P.tt(ci, ci, den, ALU.mult)
        nsn = P.sb([128, 16])
        P.ts(nsn, sn, -1.0, ALU.mult)
        Tc = P.sb([128, 16, 512])
        Ts = P.sb([128, 16, 512])
        P.memset(Tc[:, :, 0:1], 1.0)
        P.memset(Ts[:, :, 0:1], 0.0)
        cc, s_ = P.sb([128, 16]), P.sb([128, 16])
        P.copy(cc, cs)
        P.copy(s_, sn)
        ta, tb = P.sb([128, 256]), P.sb([128, 256])
        c2, s2 = P.sb([128, 16]), P.sb([128, 16])
        span = 1
        while span < 512:
            for j in range(16):
                P.ts(ta[:, 0:span], Ts[:, j, 0:span], s_[:, j:j + 1], ALU.mult)
                P.stt(Tc[:, j, span:2 * span], Tc[:, j, 0:span], cc[:, j:j + 1], ta[:, 0:span], ALU.mult, ALU.subtract)
                P.ts(tb[:, 0:span], Tc[:, j, 0:span], s_[:, j:j + 1], ALU.mult)
                P.stt(Ts[:, j, span:2 * span], Ts[:, j, 0:span], cc[:, j:j + 1], tb[:, 0:span], ALU.mult, ALU.add)
            P.tt(c2, cc, cc, ALU.mult)
            P.tt(s2, s_, s_, ALU.mult)
            P.tt(s_, cc, s_, ALU.mult)
            P.ts(s_, s_, 2.0, ALU.mult)
            P.tt(cc, c2, s2, ALU.subtract)
            span *= 2
        BreT = [P.sb([128, 128]) for _ in range(16)]
        BimT = [P.sb([128, 128]) for _ in range(16)]
        CrT = [P.sb([128, 128]) for _ in range(16)]
        CiT = [P.sb([128, 128]) for _ in range(16)]
        pad = [P.sb([128, 128]) for _ in range(4)]
        craw = [P.sb([128, 128]) for _ in range(2)]
        for j in range(16):
            kt = j // 4
            for which, (nm, dst) in enumerate((('even_b_re', BreT), ('even_b_im', BimT))):
                pd = pad[which]
                P.memset(pd, 0.0)
                for g2 in range(2):
                    g = 2 * j + g2
                    off = (g - 8 * kt) * 16
                    P.dma(pd[g2 * 64:(g2 + 1) * 64, off:off + 16], I[nm][i][g])
                pt = C.PS[which]
                P.tr(pt[:, 0:128], pd, C.ident)
                P.copy(dst[j], pt[:, 0:128], eng='act')
            for which, nm in enumerate(('even_c_re', 'even_c_im')):
                pd = pad[2 + which]
                P.memset(pd, 0.0)
                for g2 in range(2):
                    g = 2 * j + g2
                    off = (g - 8 * kt) * 16
                    P.dma(pd[off:off + 16, g2 * 64:(g2 + 1) * 64], I[nm][i][g])
                pt = C.PS[2 + which]
                P.tr(pt[:, 0:128], pd, C.ident)
                P.copy(craw[which], pt[:, 0:128], eng='act')
            P.ts(pad[0], craw[1], ci[:, j:j + 1], ALU.mult)
            P.stt(CrT[j], craw[0], cr[:, j:j + 1], pad[0], ALU.mult, ALU.subtract)
            P.ts(pad[1], craw[1], cr[:, j:j + 1], ALU.mult)
            P.stt(CiT[j], craw[0], ci[:, j:j + 1], pad[1], ALU.mult, ALU.add)
            P.ts(CiT[j], CiT[j], -1.0, ALU.mult)
        dsk = P.sb([128, 4])
        glb = P.sb([128, 8])
        with nc.allow_non_contiguous_dma(reason="tiny"):
            P.dma(dsk, I['even_d_skip'][i].re("(k p) -> p k", p=128))
            P.dma(glb, I['even_glu_b'][i].re("(k p) -> p k", p=128))
        nglb = P.sb([128, 8])
        P.ts(nglb, glb, -1.0, ALU.mult)
        gluw = P.sb([128, 4, 1024], BF16)
        P.dma(gluw, I['even_glu_w'][i].re("(k p) m -> p k m", p=128), eng='pool')
        ini = [P.sb([128, 2]) for _ in range(16)]
        for j in range(16):
            P.memset(ini[j], 0.0)
        uts = [P.sb([128, 4, 512]) for _ in range(2)]
        bu = [[P.sb([128, 512]) for _ in range(2)] for _ in range(2)]
        zin = [[P.sb([128, 512]) for _ in range(2)] for _ in range(2)]
        zz = [[P.sb([128, 512]) for _ in range(2)] for _ in range(2)]
        w1s = [[P.sb([128, 512]) for _ in range(4)] for _ in range(2)]
        xx = [[P.sb([128, 512]) for _ in range(2)] for _ in range(2)]
        sml = [P.sb([128, 2]) for _ in range(2)]
        yg = P.sb([128, 4, 512], BF16)
        ysb = [P.sb([128, 512]) for _ in range(2)]
        ga = [P.sb([128, 512]) for _ in range(2)]
        gb = [P.sb([128, 512]) for _ in range(2)]
        uv = C.uT.v.re("(k p) t -> p k t", p=128)
        P.dma(uts[0], uv[:, :, 0:512])
        it = 0
        for tt in range(NT):
            tsl = slice(tt * 512, (tt + 1) * 512)
            ut = uts[tt % 2]
            if tt + 1 < NT:
                P.dma(uts[(tt + 1) % 2], uv[:, :, (tt + 1) * 512:(tt + 2) * 512])
            def chain(j, b, kt):
                pr, pi_ = C.PS[0 + 2 * b], C.PS[1 + 2 * b]
                W = w1s[b]
                P.mm(pr, BreT[j], ut[:, kt, :])
                P.mm(pi_, BimT[j], ut[:, kt, :])
                P.copy(bu[b][0].v, pr, eng='act')
                P.copy(bu[b][1].v, pi_, eng='act')
                yield
                P.tt(W[0], bu[b][0].v, Tc[:, j, :], ALU.mult)
                P.tt(W[1], bu[b][1].v, Ts[:, j, :], ALU.mult, eng='pool')
                yield
                P.tt(zin[b][0].v, W[0], W[1], ALU.add)
                P.tt(W[2], bu[b][1].v, Tc[:, j, :], ALU.mult, eng='pool')
                yield
                P.tt(W[3], bu[b][0].v, Ts[:, j, :], ALU.mult)
                yield
                P.tt(zin[b][1].v, W[2], W[3], ALU.subtract, eng='pool')
                P.scan(zz[b][0].v, rho[:, j:j + 1].bc([128, 512]), zin[b][0].v, ini[j][:, 0:1])
                yield
                P.scan(zz[b][1].v, rho[:, j:j + 1].bc([128, 512]), zin[b][1].v, ini[j][:, 1:2])
                yield
                P.tt(W[0], zz[b][0].v, Tc[:, j, :], ALU.mult)
                P.tt(W[1], zz[b][1].v, Ts[:, j, :], ALU.mult, eng='pool')
                yield
                P.tt(xx[b][0].v, W[0], W[1], ALU.subtract)
                P.tt(W[2], zz[b][1].v, Tc[:, j, :], ALU.mult, eng='pool')
                yield
                P.tt(W[3], zz[b][0].v, Ts[:, j, :], ALU.mult)
                yield
                P.tt(xx[b][1].v, W[2], W[3], ALU.add, eng='pool')
                yield
                sm_ = sml[b]
                P.ts(sm_[:, 0:1], xx[b][1][:, 511:512], nsn[:, j:j + 1], ALU.mult)
                P.ts(sm_[:, 1:2], xx[b][0][:, 511:512], sn[:, j:j + 1], ALU.mult)
                P.stt(ini[j][:, 0:1], xx[b][0][:, 511:512], cs[:, j:j + 1], sm_[:, 0:1], ALU.mult, ALU.add)
                P.stt(ini[j][:, 1:2], xx[b][1][:, 511:512], cs[:, j:j + 1], sm_[:, 1:2], ALU.mult, ALU.add)

            for kt in range(4):
                py = C.PS[4 + (kt % 2)]
                for jp in range(2):
                    js = [kt * 4 + jp * 2, kt * 4 + jp * 2 + 1]
                    gens = [chain(js[0], 0, kt), chain(js[1], 1, kt)]
                    while gens:
                        for g_ in list(gens):
                            try:
                                next(g_)
                            except StopIteration:
                                gens.remove(g_)
                    for b, j in enumerate(js):
                        first = (jp == 0 and b == 0)
                        last = (jp == 1 and b == 1)
                        P.mm(py, CrT[j], xx[b][0].v, start=first, stop=False)
                        P.mm(py, CiT[j], xx[b][1].v, start=False, stop=last)
                y = ysb[kt % 2]
                P.stt(y, ut[:, kt, :], dsk[:, kt:kt + 1], py, ALU.mult, ALU.add)
                P.act(yg[:, kt, :], y, AF.Gelu)
            for m in range(4):
                pa, pb = C.PS[6], C.PS[7]
                for k in range(4):
                    P.mm(pa, gluw[:, k, m * 128:(m + 1) * 128], yg[:, k, :], start=(k == 0), stop=(k == 3))
                for k in range(4):
                    P.mm(pb, gluw[:, k, 512 + m * 128:512 + (m + 1) * 128], yg[:, k, :], start=(k == 0), stop=(k == 3))
                a_, b_ = ga[m % 2], gb[m % 2]
                P.act(a_, pa, AF.Identity, bias=glb[:, m:m + 1])
                P.act(b_, pb, AF.Exp, bias=nglb[:, 4 + m:5 + m], scale=-1.0)
                P.ts(b_, b_, 1.0, ALU.add)
                P.recip(b_, b_)
                P.tt(a_, a_, b_, ALU.mult)
                P.dma(C.yT[512 + m * 128:512 + (m + 1) * 128, tsl], a_)
        P.barrier()
        P.flush()


def stage_ffn(P, C, L, l, xsrc, xdst, ysrc, wout, moe, G=2):
    nc = P.nc
    I = C.inp
    i = l // 2
    NT = L // 512
    G = min(G, NT)
    NG = NT // G
    mod = C.mod[l]
    with contextlib.ExitStack() as st:
        P.stack = st
        S = Ctx()
        faccs = [P.sb([128, 8, 512]) for _ in range(G)]
        S.sq = faccs[0]
        S.rstd = P.sb([128, 512])
        S.tmp = [P.sb([128, 512]) for _ in range(2)]
        hbs = [P.sb([128, 8, 512], BF16) for _ in range(G)]
        S.h32 = P.sb([128, 8, 512]) if moe else None
        S.wb = [P.sb([128, 8, 512], BF16) for _ in range(2)]
        S.wbi = 0
        w2b = [P.sb([128, 22, 128], BF16) for _ in range(2)]
        nw2 = 0
        xt = P.sb([128, 8, 512])
        ybf = P.sb([128, 8, 512], BF16)
        hids = [P.sb([128, 22, 512], BF16) for _ in range(G)]
        sa = [P.sb([128, 512]) for _ in range(3)]
        if moe:
            rw = P.sb([128, 8, 8])
            P.dma(rw, I['odd_router_w'][i].re("(k p) e -> p k e", p=128))
            lg = P.sb([8, 512])
            lt = P.sb([128, 4, 8])
            m1 = P.sb([128, 4])
            m2 = P.sb([128, 4])
            eq1 = P.sb([128, 4, 8])
            eq2 = P.sb([128, 4, 8])
            msk = P.sb([128, 4, 8])
            gg = P.sb([128, 4])
            g2_ = P.sb([128, 4])
            cmb = P.sb([128, 4, 8])
            cmbT = P.sb([8, 512])
            combs = [P.sb([128, 8, 512], BF16) for _ in range(G)]
        xv = xsrc.v.re("(k p) t -> p k t", p=128)
        yv = ysrc.v.re("(k p) t -> p k t", p=128)
        ov = xdst.v.re("(k p) t -> p k t", p=128)
        mv = C.xmid.v.re("(k p) t -> p k t", p=128)
        nsa = 0
        npp = 0
        for grp in range(NG):
            for g in range(G):
                tt = grp * G + g
                tsl = slice(tt * 512, (tt + 1) * 512)
                P.dma(xt, xv[:, :, tsl])
                P.dma(ybf, yv[:, :, tsl], eng='pool')
                for wsl, c0, w in wslab_iter(P, C, S, wout, 1024, 8):
                    for j in range(4):
                        m = c0 // 128 + j
                        pp = C.PS[1 + (m % 4)]
                        for k in range(8):
                            P.mm(pp, wsl[:, k, j * 128:(j + 1) * 128], ybf[:, k, :], start=(k == 0), stop=(k == 7))
                        P.stt(xt[:, m, :], pp, mod[:, 16 + m:17 + m], xt[:, m, :], ALU.mult, ALU.add)
                P.dma(mv[:, :, tsl], xt)
                S.hb = hbs[g]
                load_norm_h(P, C, S, xt, l, 2, want32=moe)
                if moe:
                    comb = combs[g]
                    pl = C.PS[5]
                    for k in range(8):
                        P.mm(pl[0:8, :], rw[:, k, :], S.h32[:, k, :], start=(k == 0), stop=(k == 7))
                    P.copy(lg, pl[0:8, :], eng='act')
                    pt = C.PS[6]
                    for s in range(4):
                        P.tr(pt[:, s * 8:(s + 1) * 8], lg[:, s * 128:(s + 1) * 128], C.ident[0:8, 0:8])
                    P.copy(lt, pt[:, 0:32].re("p (s e) -> p s e", e=8), eng='act')
                    P.reduce(m1, lt, ALU.max)
                    P.tt(eq1, lt, m1.v.re("p (s o) -> p s o", o=1).bc([128, 4, 8]), ALU.is_equal)
                    P.stt(msk, eq1, -1e30, lt, ALU.mult, ALU.add)
                    P.reduce(m2, msk, ALU.max)
                    P.tt(eq2, msk, m2.v.re("p (s o) -> p s o", o=1).bc([128, 4, 8]), ALU.is_equal)
                    P.tt(gg, m2, m1, ALU.subtract)
                    P.act(gg, gg, AF.Exp)
                    P.ts(gg, gg, 1.0, ALU.add)
                    P.recip(gg, gg)
                    P.ts(g2_, gg, -1.0, ALU.mult, 1.0, ALU.add)
                    P.tt(cmb, eq1, gg.v.re("p (s o) -> p s o", o=1).bc([128, 4, 8]), ALU.mult)
                    P.tt(eq2, eq2, g2_.v.re("p (s o) -> p s o", o=1).bc([128, 4, 8]), ALU.mult)
                    P.tt(cmb, cmb, eq2, ALU.add)
                    pc = C.PS[7]
                    for s in range(4):
                        P.tr(pc[0:8, s * 128:(s + 1) * 128], cmb[:, s, :], C.ident)
                    P.copy(cmbT, pc[0:8, :], eng='act')
                    for e in range(NEXP):
                        pb_ = C.PS[5 + (e % 2)]
                        P.mm(pb_, C.sel[:, e, :], cmbT)
                        P.copy(comb[:, e, :], pb_, eng='act')
            nexp = NEXP if moe else 1
            for e in range(nexp):
                if moe:
                    W13 = I['odd_expert_w13'][i][e]
                    W2 = I['odd_expert_w2'][i][e]
                else:
                    W13 = I['even_ffn_w13'][i]
                    W2 = I['even_ffn_w2'][i]
                for c0 in range(0, DFF, 256):
                    w = min(256, DFF - c0)
                    buf = S.wb[S.wbi % 2]
                    S.wbi += 1
                    P.dma(buf[:, :, 0:w], W13[:, c0:c0 + w].re("(k p) m -> p k m", p=128), eng='pool')
                    P.dma(buf[:, :, 256:256 + w], W13[:, DFF + c0:DFF + c0 + w].re("(k p) m -> p k m", p=128), eng='pool')
                    for j in range(w // 128):
                        f = c0 // 128 + j
                        for g in range(G):
                            pa, pb = C.PS[1 + 2 * (npp % 2)], C.PS[2 + 2 * (npp % 2)]
                            npp += 1
                            for k in range(8):
                                P.mm(pa, buf[:, k, j * 128:(j + 1) * 128], hbs[g][:, k, :], start=(k == 0), stop=(k == 7))
                            for k in range(8):
                                P.mm(pb, buf[:, k, 256 + j * 128:256 + (j + 1) * 128], hbs[g][:, k, :], start=(k == 0), stop=(k == 7))
                            s_ = sa[nsa % 3]
                            nsa += 1
                            P.act(s_, pa, AF.Silu)
                            if moe:
                                P.tt(s_, s_, combs[g][:, e, :], ALU.mult)
                            P.tt(hids[g][:, f, :], pb, s_, ALU.mult)
                for m in range(8):
                    wb2 = w2b[nw2 % 2]
                    nw2 += 1
                    P.dma(wb2, W2[:, m * 128:(m + 1) * 128].re("(f p) m -> p f m", p=128), eng='pool')
                    for g in range(G):
                        facc = faccs[g]
                        pp = C.PS[5 + ((m * G + g) % 3)]
                        for f in range(22):
                            P.mm(pp, wb2[:, f, :], hids[g][:, f, :], start=(f == 0), stop=(f == 21))
                        if e == 0:
                            P.copy(facc[:, m, :], pp, eng='act')
                        else:
                            P.tt(facc[:, m, :], pp, facc[:, m, :], ALU.add)
            for g in range(G):
                tt = grp * G + g
                tsl = slice(tt * 512, (tt + 1) * 512)
                P.dma(xt, mv[:, :, tsl])
                for m in range(8):
                    P.stt(faccs[g][:, m, :], faccs[g][:, m, :], mod[:, 40 + m:41 + m], xt[:, m, :], ALU.mult, ALU.add)
                P.dma(ov[:, :, tsl], faccs[g])
        P.barrier()
        P.flush()


def stage_attn_proj(P, C, L, l, xsrc):
    nc = P.nc
    I = C.inp
    i = l // 2
    NT = L // 512
    Wq = I['odd_w_qkv'][i]
    with contextlib.ExitStack() as st:
        P.stack = st
        S = Ctx()
        S.sq = P.sb([128, 8, 512])
        S.rstd = P.sb([128, 512])
        S.tmp = [P.sb([128, 512]) for _ in range(2)]
        S.hb = P.sb([128, 8, 512], BF16)
        S.h32 = None
        S.wb = [P.sb([128, 8, 512], BF16) for _ in range(3)]
        S.wbi = 0
        wv = P.sb([128, 8, 1024], BF16)
        P.dma(wv, Wq[:, 2048:3072].re("(k p) m -> p k m", p=128), eng='pool')
        nw = P.sb([128, 2])
        with nc.allow_non_contiguous_dma(reason="tiny"):
            for t in range(2):
                P.dma(nw[t * 64:(t + 1) * 64, 0:1], I['odd_q_norm_w'][i].re("(p o) -> p o", o=1))
                P.dma(nw[t * 64:(t + 1) * 64, 1:2], I['odd_k_norm_w'][i].re("(p o) -> p o", o=1))
        xts = [P.sb([128, 8, 512]) for _ in range(2)]
        raw = [P.sb([128, 512]) for _ in range(2)]
        sq = [P.sb([128, 512]) for _ in range(2)]
        rs = [P.sb([128, 512]) for _ in range(2)]
        ob = [P.sb([128, 512], BF16) for _ in range(3)]
        vb = [P.sb([128, 1024], BF16) for _ in range(2)]
        xv = xsrc.v.re("(k p) t -> p k t", p=128)
        P.dma(xts[0], xv[:, :, 0:512])
        n = 0
        for tt in range(NT):
            tsl = slice(tt * 512, (tt + 1) * 512)
            xt = xts[tt % 2]
            if tt + 1 < NT:
                P.dma(xts[(tt + 1) % 2], xv[:, :, (tt + 1) * 512:(tt + 2) * 512])
            load_norm_h(P, C, S, xt, l, 1)
            for wsl, c0, w in wslab_iter(P, C, S, Wq, 2048, 8):
                for j in range(4):
                    m = c0 // 128 + j
                    isq = m < 8
                    pp = C.PS[1 + (m % 4)]
                    for k in range(8):
                        P.mm(pp, wsl[:, k, j * 128:(j + 1) * 128], S.hb[:, k, :], start=(k == 0), stop=(k == 7))
                    r = raw[n % 2]
                    P.copy(r, pp, eng='act')
                    P.act(sq[n % 2], r, AF.Square)
                    pss = C.PS[5 + (n % 2)]
                    P.mm(pss, C.blk, sq[n % 2])
                    if isq:
                        rstd_from_ss(P, rs[n % 2], pss, 1.0, 64.0 * EPS)
                    else:
                        rstd_from_ss(P, rs[n % 2], pss, 1.0 / 64, EPS)
                    o = ob[n % 3]
                    P.stt(o, r, nw[:, (0 if isq else 1):(1 if isq else 2)], rs[n % 2], ALU.mult, ALU.mult)
                    P.dma(C.qkT[m * 128:(m + 1) * 128, tsl], o)
                    n += 1
            for s in range(4):
                v_ = vb[s % 2]
                for half in range(2):
                    pp = C.PS[1 + ((s * 2 + half) % 4)]
                    for k in range(8):
                        P.mm(pp, S.hb[:, k, s * 128:(s + 1) * 128], wv[:, k, half * 512:(half + 1) * 512],
                             start=(k == 0), stop=(k == 7))
                    P.copy(v_[:, half * 512:(half + 1) * 512], pp, eng=('act' if half else 'dve'))
                P.dma(C.vtok[tt * 512 + s * 128:tt * 512 + (s + 1) * 128, :], v_)
        P.barrier()
        P.flush()


def stage_attn(P, C, L, l):
    nc = P.nc
    I = C.inp
    i = l // 2
    NT = L // 512
    NK = L // 128
    lambda_init = 0.8 - 0.6 * math.exp(-0.3 * l)
    with contextlib.ExitStack() as st:
        P.stack = st
        lq = P.sb([128, 4, 64])
        for j, nm in enumerate(('odd_lambda_q1', 'odd_lambda_k1', 'odd_lambda_q2', 'odd_lambda_k2')):
            a = I[nm][i].re("(o d) -> o d", o=1)
            P.dma(lq[:, j, :], V(a.res, a.ap.to_broadcast([128, 64])))
        pr = P.sb([128, 2, 64])
        P.tt(pr[:, 0, :], lq[:, 0, :], lq[:, 1, :], ALU.mult)
        P.tt(pr[:, 1, :], lq[:, 2, :], lq[:, 3, :], ALU.mult)
        sm = P.sb([128, 2])
        P.reduce(sm, pr, ALU.add)
        P.act(sm, sm, AF.Exp)
        nlam = P.sb([128, 1])
        P.tt(nlam, sm[:, 1:2], sm[:, 0:1], ALU.subtract)
        P.ts(nlam, nlam, -lambda_init, ALU.add)
        sw = P.sb([128, 1])
        with nc.allow_non_contiguous_dma(reason="tiny"):
            P.dma(sw, I['odd_subln_w'][i].re("(p o) -> p o", o=1))
        P.ts(sw, sw, 1.0 - lambda_init, ALU.mult)
        kT = [P.sb([128, L], BF16) for _ in range(2)]
        vt = [P.sb([128, NK, 128], BF16) for _ in range(2)]
        qt_ = [P.sb([128, 512], BF16) for _ in range(2)]
        E = [P.sb([128, 1024], BF16) for _ in range(4)]
        r1 = [P.sb([128, 512]) for _ in range(2)]
        r2 = [P.sb([128, 512]) for _ in range(2)]
        o1 = [P.sb([128, 512]) for _ in range(2)]
        sq = [P.sb([128, 512]) for _ in range(2)]
        ob = [P.sb([128, 512]) for _ in range(2)]
        ne = 0
        nq = 0
        for h in range(8):
            kk, vv = kT[h % 2], vt[h % 2]
            P.dma(kk, C.qkT[1024 + h * 128:1024 + (h + 1) * 128, :])
            P.dma(vv, C.vtok[:, h * 128:(h + 1) * 128].re("(n p) e -> p n e", p=128))
            for qt in range(NT):
                tsl = slice(qt * 512, (qt + 1) * 512)
                q = qt_[nq % 2]
                P.dma(q, C.qkT[h * 128:(h + 1) * 128, tsl])
                pn = [C.PS[0], C.PS[1]]
                pd = [C.PS[2], C.PS[3]]
                nkt = 4 * (qt + 1)
                LA = 1
                ebuf = {}

                def emit_s(kt):
                    nonlocal ne
                    half = ne % 2
                    psc = C.PSH[half].v
                    for t in range(2):
                        P.mm(psc[:, t * 512:(t + 1) * 512], kk[t * 64:(t + 1) * 64, kt * 128:(kt + 1) * 128],
                             q[t * 64:(t + 1) * 64, :])
                    e_ = E[ne % 4]
                    ne += 1
                    P.act(e_, psc, AF.Exp, bias=C.neg8[:, 0:1])
                    r = kt - 4 * qt
                    if r >= 0:
                        P.tt(e_.v.re("p (t q) -> p t q", t=2), e_.v.re("p (t q) -> p t q", t=2),
                             C.amask[:, r:r + 1, :].bc([128, 2, 512]), ALU.mult, eng='pool')
                    ebuf[kt] = e_

                def emit_md(kt):
                    e_ = ebuf.pop(kt)
                    for t in range(2):
                        P.mm(pn[t], vv[:, kt, :], e_[:, t * 512:(t + 1) * 512], start=(kt == 0), stop=(kt == nkt - 1))
                        P.mm(pd[t], C.onesb, e_[:, t * 512:(t + 1) * 512], start=(kt == 0), stop=(kt == nkt - 1))
                for n in range(nkt + LA):
                    if n < nkt:
                        emit_s(n)
                    if n - LA >= 0:
                        emit_md(n - LA)
                b = nq % 2
                nq += 1
                P.recip(r1[b], pd[0])
                P.recip(r2[b], pd[1])
                P.tt(o1[b], pn[0], r1[b], ALU.mult)
                P.tt(r2[b], pn[1], r2[b], ALU.mult)
                P.stt(o1[b], r2[b], nlam[:, 0:1], o1[b], ALU.mult, ALU.add)
                P.act(sq[b], o1[b], AF.Square)
                pss = C.PS[2]
                P.mm(pss, C.ones, sq[b])
                rstd_from_ss(P, r1[b], pss, 1.0 / 128, EPS)
                P.stt(ob[b], o1[b], sw[:, 0:1], r1[b], ALU.mult, ALU.mult)
                P.dma(C.oT[h * 128:(h + 1) * 128, tsl], ob[b])
        P.barrier()
        P.flush()


def make_consts():
    c = {}
    c['c_ident'] = np.eye(128, dtype=np.float32)
    am = np.zeros((128, 4, 512), np.float32)
    kk = np.arange(128)[:, None]
    qq = np.arange(512)[None, :]
    for r in range(4):
        am[:, r, :] = ((r * 128 + kk) // 64 <= qq // 64)
    c['c_amask'] = am
    gm = np.zeros((64, 4, 512), np.float32)
    p = np.arange(64)[:, None]
    f = np.arange(64)[None, :]
    for cidx in range(8):
        sl = slice(cidx * 64, (cidx + 1) * 64)
        gm[:, 0, sl] = -1.0 * (p > f)
        gm[:, 1, sl] = -1.0 * (p < f)
        gm[:, 2, sl] = (p <= f)
        gm[:, 3, sl] = (p == f)
    c['c_gmask'] = gm
    sel = np.zeros((8, 8, 128), np.float32)
    for e in range(8):
        sel[e, e, :] = 1.0
    c['c_sel'] = sel
    blk = np.zeros((128, 128), np.float32)
    blk[:64, :64] = 1
    blk[64:, 64:] = 1
    c['c_blk'] = blk
    cm = np.ones((4, 512), np.float32)
    cm[:, ::64] = 0
    c['c_cmask'] = cm
    return c


INPUT_NAMES = ['ada_w', 'ada_b', 'norm_mix_w', 'norm_ffn_w',
               'even_w_in', 'even_conv_w', 'even_a_log', 'even_dt_bias', 'even_gdn_norm_w',
               'even_lam_re', 'even_lam_im', 'even_log_step', 'even_b_re', 'even_b_im', 'even_c_re', 'even_c_im',
               'even_d_skip', 'even_glu_w', 'even_glu_b', 'even_w_out', 'even_ffn_w13', 'even_ffn_w2',
               'odd_w_qkv', 'odd_q_norm_w', 'odd_k_norm_w', 'odd_lambda_q1', 'odd_lambda_k1', 'odd_lambda_q2',
               'odd_lambda_k2', 'odd_subln_w', 'odd_w_out', 'odd_router_w', 'odd_expert_w13', 'odd_expert_w2']


def build(shapes, L, depth=DEPTH, dbg=False):
    nc = bass.Bass("TRN2", target_bir_lowering=False)
    C = Ctx()
    C.depth = depth
    C.inp = {}
    consts = make_consts()
    for nm, shp in shapes.items():
        C.inp[nm] = Res(nm, nc.dram_tensor(nm, list(shp), F32, kind="ExternalInput").ap())
    for nm, arr in consts.items():
        C.inp[nm] = Res(nm, nc.dram_tensor(nm, list(arr.shape), F32, kind="ExternalInput").ap())
    C.out = Res('out', nc.dram_tensor('out', [L, D], F32, kind="ExternalOutput").ap())
    kind = "ExternalOutput" if dbg else "Internal"

    def scr(nm, shape, dt=F32):
        return Res(nm, nc.dram_tensor(nm, list(shape), dt, kind=kind).ap())
    C.xA = scr('xA', [D, L])
    C.xB = scr('xB', [D, L])
    C.qkvT = scr('qkvT', [1536, L])
    C.zT = scr('zT', [512, L])
    C.uT = scr('uT', [512, L])
    C.betaT = scr('betaT', [4, L])
    C.gT = scr('gT', [4, L])
    C.yT = scr('yT', [D, L])
    C.xmid = scr('xmid', [D, L])
    C.qkT = scr('qkT', [2048, L], BF16)
    C.vtok = scr('vtok', [L, D], BF16)
    C.oT = scr('oT', [D, L])
    with contextlib.ExitStack() as st0:
        P = Prog(nc, st0)
        C.PS = [P.ps([128, 512]) for _ in range(4)]
        C.PSS = P.ps([128, 2048])
        C.PS += [Res('pss%d' % j, C.PSS.ap[:, j * 512:(j + 1) * 512]) for j in range(4)]
        C.PSH = [Res('psh%d' % j, C.PSS.ap[:, j * 1024:(j + 1) * 1024]) for j in range(2)]
        C.ident = P.sb([128, 128])
        C.ones = P.sb([128, 128])
        C.onesb = P.sb([128, 128], BF16)
        C.blk = P.sb([128, 128])
        C.amask = P.sb([128, 4, 512], BF16)
        C.sel = P.sb([8, 8, 128])
        C.halfpi = P.sb([128, 1])
        C.neg8 = P.sb([128, 1])
        C.mod = [P.sb([128, 48]) for _ in range(depth)]
        C.modA = [P.sb([128, 16]) for _ in range(depth)]
        P.dma(C.ident, C.inp['c_ident'])
        P.dma(C.blk, C.inp['c_blk'])
        P.dma(C.amask, C.inp['c_amask'], eng='pool')
        P.dma(C.sel, C.inp['c_sel'])
        P.memset(C.ones, 1.0)
        P.memset(C.onesb, 1.0)
        P.memset(C.halfpi, math.pi / 2)
        P.memset(C.neg8, -8.0)
        stage_prep(P, C, L)
        cur, nxt = C.xA, C.xB
        for l in range(depth):
            i = l // 2
            if l % 2 == 0:
                stage_even_proj(P, C, L, l, cur)
                stage_gdn(P, C, L, l)
                stage_s5(P, C, L, l)
                stage_ffn(P, C, L, l, cur, nxt, C.yT, C.inp['even_w_out'][i], moe=False)
            else:
                stage_attn_proj(P, C, L, l, cur)
                stage_attn(P, C, L, l)
                stage_ffn(P, C, L, l, cur, nxt, C.oT, C.inp['odd_w_out'][i], moe=True)
            cur, nxt = nxt, cur
        stage_out(P, C, L, cur)
        P.stack = st0
        C.nins = P.nins
    return nc, consts, C


def kernel(**inputs):
    x = np.asarray(inputs['x'], dtype=np.float32)
    B, L, _ = x.shape
    shapes = {'x': (L, D), 'c': (D,)}
    for nm in INPUT_NAMES:
        shapes[nm] = tuple(np.asarray(inputs[nm]).shape)
    nc, consts, C = build(shapes, L)
    shared = {nm: np.ascontiguousarray(np.asarray(inputs[nm], dtype=np.float32)) for nm in INPUT_NAMES}
    zeros = {nm: np.zeros_like(v) for nm, v in shared.items()}
    real = [0, 1, 4, 5][:B]
    in_maps = []
    for core in range(8):
        if core in real:
            b = real.index(core)
            m = dict(shared)
            m.update(consts)
            m['x'] = np.ascontiguousarray(x[b])
            m['c'] = np.ascontiguousarray(np.asarray(inputs['c'], dtype=np.float32)[b])
        else:
            m = dict(zeros)
            m.update(consts)
            m['x'] = np.zeros((L, D), np.float32)
            m['c'] = np.zeros((D,), np.float32)
        in_maps.append(m)
    res = run_bass_kernel_spmd(nc, in_maps, core_ids=list(range(8)))
    out = np.stack([res.results[real[b]]['out'] for b in range(B)], axis=0)
    return out.astype(np.float32)
```
